# Optimizing a Trainium2 kernel written in Bass

```python
import jax
import jax.numpy as jnp
from jax import lax
import numpy as np


D_MODEL = 1024
BATCH = 32
SEQ = 2048
DEPTH = 2

CTX_LEN = 256
GRID_W = 64
N_MIXERS = 2
ADA_CHUNKS = 6
NORM_EPS = 1e-6

HG_HEADS = 8
HG_DK = D_MODEL // HG_HEADS
HG_DV = D_MODEL // HG_HEADS
HG_CHUNK = 32

MLA_HEADS = 16
MLA_Q_LORA = 256
MLA_KV_LORA = 128
MLA_NOPE = 64
MLA_ROPE = 32
MLA_V = 64
MLA_QK = MLA_NOPE + MLA_ROPE
ROPE_THETA = 10000.0
Q_BLOCK = 128

MOE_GROUPS = 4
MOE_EXPERTS_PER_GROUP = 8
MOE_EXPERTS = MOE_GROUPS * MOE_EXPERTS_PER_GROUP
MOE_TOP_K = 2
MOE_FF = 512
MOE_BLOCK = 256

kernel_name = 'hybrid_hgrn2_mla_hmoe_dit'


def rms_norm(x, g):
    xf = x.astype(jnp.float32)
    y = xf * lax.rsqrt(jnp.mean(xf * xf, axis=-1, keepdims=True) + NORM_EPS)
    return (y * g.astype(jnp.float32)).astype(x.dtype)


def modulate(x, g, shift, scale):
    return rms_norm(x, g) * (1 + scale) + shift


def gla_chunkwise(q, k, v, log_f, s0):
    b_, h_, L, _ = q.shape
    n_chunks = L // HG_CHUNK

    def chunks(a):
        return jnp.moveaxis(a.reshape(b_, h_, n_chunks, HG_CHUNK, a.shape[-1]), 2, 0)

    lower_tri = jnp.tril(jnp.ones((HG_CHUNK, HG_CHUNK), dtype=bool))

    def step(s, blk):
        qc, kc, vc, gc = blk
        b = jnp.cumsum(gc, axis=-2)
        b_end = b[:, :, -1:, :]
        q_dec = qc * jnp.exp(b)
        scores = jnp.einsum('bhtc,bhsc->bhts', q_dec, kc * jnp.exp(-b))
        scores = jnp.where(lower_tri, scores, 0.0)
        o = jnp.einsum('bhts,bhsv->bhtv', scores, vc) + jnp.einsum('bhtc,bhcv->bhtv', q_dec, s)
        s_new = jnp.exp(b_end[:, :, 0, :])[..., None] * s + jnp.einsum('bhsc,bhsv->bhcv', kc * jnp.exp(b_end - b), vc)
        return s_new, o

    s_fin, o = lax.scan(step, s0, (chunks(q), chunks(k), chunks(v), chunks(log_f)))
    return jnp.moveaxis(o, 0, 2).reshape(b_, h_, L, v.shape[-1]), s_fin


def hgrn2_mixer(h_lat, h_ctx, w_in, lower_bound, out_norm_g, w_out, with_ctx_out):
    lb = lower_bound.astype(jnp.float32).reshape(2, HG_HEADS, 1, HG_DK)

    def prepare(h):
        b_, L, _ = h.shape
        p = jnp.einsum('bld,de->ble', h, w_in)
        q, z_fw, z_bw, v, g = jnp.split(p, 5, axis=-1)

        def heads(a):
            return a.reshape(b_, L, HG_HEADS, -1).transpose(0, 2, 1, 3).astype(jnp.float32)

        f_fw = lb[0] + (1 - lb[0]) * jax.nn.sigmoid(heads(z_fw))
        f_bw = lb[1] + (1 - lb[1]) * jax.nn.sigmoid(heads(z_bw))
        return jax.nn.silu(heads(q)), heads(v), (1 - f_fw, jnp.log(f_fw)), (1 - f_bw, jnp.log(f_bw)), g

    def flip(a):
        return jnp.flip(a, axis=2)

    def readout(o, g):
        b_, _, L, _ = o.shape
        o = rms_norm(o, out_norm_g).transpose(0, 2, 1, 3).reshape(b_, L, D_MODEL)
        return jnp.einsum('ble,ed->bld', o.astype(g.dtype) * jax.nn.silu(g), w_out)

    qc, vc, (kc_f, gc_f), (kc_b, gc_b), g_c = prepare(h_ctx)
    s0 = jnp.zeros(qc.shape[:2] + (HG_DK, HG_DV), jnp.float32)
    o_c_f, s_f = gla_chunkwise(qc, kc_f, vc, gc_f, s0)
    o_c_b, s_b = gla_chunkwise(flip(qc), flip(kc_b), flip(vc), flip(gc_b), s0)
    ql, vl, (kl_f, gl_f), (kl_b, gl_b), g_l = prepare(h_lat)
    o_l_f, _ = gla_chunkwise(ql, kl_f, vl, gl_f, s_f)
    o_l_b, _ = gla_chunkwise(flip(ql), flip(kl_b), flip(vl), flip(gl_b), s_b)
    y_lat = readout(o_l_f + flip(o_l_b), g_l)
    y_ctx = readout(o_c_f + flip(o_c_b), g_c) if with_ctx_out else None
    return y_lat, y_ctx


def axial_rope_tables(L):
    rows = L // GRID_W
    row = jnp.repeat(jnp.arange(rows), GRID_W)
    col = jnp.tile(jnp.arange(GRID_W), rows)
    half = MLA_ROPE // 2
    inv_freq = ROPE_THETA ** (-jnp.arange(0, half, 2, dtype=jnp.float32) / half)
    ang = jnp.stack([row, col], axis=-1).astype(jnp.float32)[..., None] * inv_freq
    ang = jnp.concatenate([ang, ang], axis=-1)
    return jnp.cos(ang), jnp.sin(ang)


def apply_axial_rope(x, cos, sin):
    b_, L, h_, _ = x.shape
    x_nope = x[..., :MLA_NOPE]
    x_rope = x[..., MLA_NOPE:].reshape(b_, L, h_, 2, MLA_ROPE // 2)
    x1, x2 = jnp.split(x_rope, 2, axis=-1)
    rot = jnp.concatenate([-x2, x1], axis=-1)
    cos = cos[:, None].astype(x.dtype)
    sin = sin[:, None].astype(x.dtype)
    x_rope = (x_rope * cos + rot * sin).reshape(b_, L, h_, MLA_ROPE)
    return jnp.concatenate([x_nope, x_rope], axis=-1)


def mla_queries(c_q, q_norm_g, w_qb, q_qk_g):
    b_, L, _ = c_q.shape
    q = jnp.einsum('blr,re->ble', rms_norm(c_q, q_norm_g), w_qb).reshape(b_, L, MLA_HEADS, MLA_QK)
    return rms_norm(q, q_qk_g)


def mla_keys_values(c_kv, k_rope, kv_norm_g, w_kvb, k_qk_g):
    b_, L, _ = c_kv.shape
    kv = jnp.einsum('blr,re->ble', rms_norm(c_kv, kv_norm_g), w_kvb).reshape(b_, L, MLA_HEADS, MLA_NOPE + MLA_V)
    k_nope, v = jnp.split(kv, [MLA_NOPE], axis=-1)
    k = jnp.concatenate([k_nope, jnp.broadcast_to(k_rope[:, :, None, :], (b_, L, MLA_HEADS, MLA_ROPE))], axis=-1)
    return rms_norm(k, k_qk_g), v


def softmax_attend(q, k, v):
    s = jnp.einsum('bqhd,bkhd->bhqk', q, k).astype(jnp.float32) * (MLA_QK ** -0.5)
    p = jax.nn.softmax(s, axis=-1).astype(v.dtype)
    return jnp.einsum('bhqk,bkhd->bqhd', p, v)


def mla_mixer(h_lat, h_ctx, w_in, q_norm_g, kv_norm_g, w_qb, w_kvb, q_qk_g, k_qk_g, w_out, with_ctx_out):
    b_, L, _ = h_lat.shape
    ctx_len = h_ctx.shape[1]
    cos, sin = axial_rope_tables(L)
    c_q, c_kv, k_rope = jnp.split(jnp.einsum('bld,de->ble', h_lat, w_in), [MLA_Q_LORA, MLA_Q_LORA + MLA_KV_LORA], axis=-1)
    q_lat = apply_axial_rope(mla_queries(c_q, q_norm_g, w_qb, q_qk_g), cos, sin)
    k_lat, v_lat = mla_keys_values(c_kv, k_rope, kv_norm_g, w_kvb, k_qk_g)
    k_lat = apply_axial_rope(k_lat, cos, sin)
    c_kv_c, k_rope_c = jnp.split(jnp.einsum('bld,de->ble', h_ctx, w_in[:, MLA_Q_LORA:]), [MLA_KV_LORA], axis=-1)
    k_ctx, v_ctx = mla_keys_values(c_kv_c, k_rope_c, kv_norm_g, w_kvb, k_qk_g)
    k_all = jnp.concatenate([k_ctx, k_lat], axis=1)
    v_all = jnp.concatenate([v_ctx, v_lat], axis=1)
    n_blk = L // Q_BLOCK
    q_blocks = q_lat.reshape(b_, n_blk, Q_BLOCK, MLA_HEADS, MLA_QK).transpose(1, 0, 2, 3, 4)
    o_lat = lax.map(lambda qb: softmax_attend(qb, k_all, v_all), q_blocks)
    o_lat = o_lat.transpose(1, 0, 2, 3, 4).reshape(b_, L, MLA_HEADS * MLA_V)
    y_lat = jnp.einsum('ble,ed->bld', o_lat, w_out)
    y_ctx = None
    if with_ctx_out:
        q_ctx = mla_queries(jnp.einsum('bld,de->ble', h_ctx, w_in[:, :MLA_Q_LORA]), q_norm_g, w_qb, q_qk_g)
        o_ctx = softmax_attend(q_ctx, k_ctx, v_ctx).reshape(b_, ctx_len, MLA_HEADS * MLA_V)
        y_ctx = jnp.einsum('ble,ed->bld', o_ctx, w_out)
    return y_lat, y_ctx


def hier_moe(h, w_group, w_expert, w_gate, w_up, w_down):
    n_tok, d = h.shape
    hf = h.astype(jnp.float32)
    g_prob = jax.nn.softmax(hf @ w_group.astype(jnp.float32), axis=-1)
    g_sel = jnp.argmax(g_prob, axis=-1)
    p_group = jnp.take_along_axis(g_prob, g_sel[:, None], axis=-1)
    e_logits = (hf @ w_expert.astype(jnp.float32)).reshape(n_tok, MOE_GROUPS, MOE_EXPERTS_PER_GROUP)
    e_logits = jnp.take_along_axis(e_logits, g_sel[:, None, None], axis=1)[:, 0]
    top_p, top_i = lax.top_k(jax.nn.softmax(e_logits, axis=-1), MOE_TOP_K)
    gate_w = p_group * top_p / jnp.sum(top_p, axis=-1, keepdims=True)
    expert_id = (g_sel[:, None] * MOE_EXPERTS_PER_GROUP + top_i).astype(jnp.int32)

    n_slot = n_tok * MOE_TOP_K
    e_flat = expert_id.reshape(n_slot)
    order = jnp.argsort(e_flat)
    e_sorted = e_flat[order]
    tok_sorted = order // MOE_TOP_K
    counts = jnp.zeros((MOE_EXPERTS,), jnp.int32).at[e_flat].add(1)
    starts = jnp.cumsum(counts) - counts
    padded = (counts + MOE_BLOCK - 1) // MOE_BLOCK * MOE_BLOCK
    pad_ends = jnp.cumsum(padded)
    pad_starts = pad_ends - padded
    dest = pad_starts[e_sorted] + jnp.arange(n_slot, dtype=jnp.int32) - starts[e_sorted]
    n_blocks = (n_slot + MOE_BLOCK - 1) // MOE_BLOCK + MOE_EXPERTS
    buf = jnp.zeros((n_blocks * MOE_BLOCK, d), h.dtype).at[dest].set(h[tok_sorted])
    block_start = jnp.arange(n_blocks, dtype=jnp.int32) * MOE_BLOCK
    block_expert = jnp.minimum(jnp.searchsorted(pad_ends, block_start, side='right'), MOE_EXPERTS - 1)

    def expert_block(args):
        xb, e = args
        return (jax.nn.silu(xb @ w_gate[e]) * (xb @ w_up[e])) @ w_down[e]

    y_buf = lax.map(expert_block, (buf.reshape(n_blocks, MOE_BLOCK, d), block_expert)).reshape(n_blocks * MOE_BLOCK, d)
    y_slot = jnp.zeros((n_slot, d), h.dtype).at[order].set(y_buf[dest])
    return jnp.einsum('nkd,nk->nd', y_slot.reshape(n_tok, MOE_TOP_K, d), gate_w.astype(h.dtype))


def setup_inputs(seed: int = 0) -> dict:
    key = jax.random.key(seed)
    ks = jax.random.split(key, 25)
    n_hg = (DEPTH + 1) // 2
    n_mla = DEPTH // 2
    D = D_MODEL

    def nrm(k, shape, scale):
        return scale * jax.random.normal(k, shape, jnp.float32)

    return {
        'x': nrm(ks[0], (BATCH, SEQ, D), 1.0),
        'c': nrm(ks[1], (BATCH, D), 1.0),
        'ctx': nrm(ks[2], (BATCH, CTX_LEN, D), 1.0),
        'c_ctx': nrm(ks[3], (D,), 1.0),
        'ada_w': nrm(ks[4], (DEPTH, D, ADA_CHUNKS * D), 0.5 * D ** -0.5),
        'ada_b': nrm(ks[5], (DEPTH, ADA_CHUNKS * D), 0.02),
        'norm_mix_g': 1.0 + nrm(ks[6], (DEPTH, D), 0.02),
        'norm_ffn_g': 1.0 + nrm(ks[7], (DEPTH, D), 0.02),
        'hg_w_in': nrm(ks[8], (n_hg, D, 5 * D), D ** -0.5),
        'hg_lower_bounds': nrm(ks[9], (2, DEPTH + 1, D), 0.1),
        'hg_out_norm_g': 1.0 + nrm(ks[10], (n_hg, HG_DV), 0.02),
        'hg_w_out': nrm(ks[11], (n_hg, D, D), D ** -0.5),
        'mla_w_in': nrm(ks[12], (n_mla, D, MLA_Q_LORA + MLA_KV_LORA + MLA_ROPE), D ** -0.5),
        'mla_q_norm_g': 1.0 + nrm(ks[13], (n_mla, MLA_Q_LORA), 0.02),
        'mla_kv_norm_g': 1.0 + nrm(ks[14], (n_mla, MLA_KV_LORA), 0.02),
        'mla_w_qb': nrm(ks[15], (n_mla, MLA_Q_LORA, MLA_HEADS * MLA_QK), MLA_Q_LORA ** -0.5),
        'mla_w_kvb': nrm(ks[16], (n_mla, MLA_KV_LORA, MLA_HEADS * (MLA_NOPE + MLA_V)), MLA_KV_LORA ** -0.5),
        'mla_q_qknorm_g': 1.0 + nrm(ks[17], (n_mla, MLA_QK), 0.02),
        'mla_k_qknorm_g': 1.0 + nrm(ks[18], (n_mla, MLA_QK), 0.02),
        'mla_w_out': nrm(ks[19], (n_mla, MLA_HEADS * MLA_V, D), (MLA_HEADS * MLA_V) ** -0.5),
        'moe_w_group': nrm(ks[20], (DEPTH, D, MOE_GROUPS), D ** -0.5),
        'moe_w_expert': nrm(ks[21], (DEPTH, D, MOE_EXPERTS), D ** -0.5),
        'moe_w_gate': nrm(ks[22], (DEPTH, MOE_EXPERTS, D, MOE_FF), D ** -0.5),
        'moe_w_up': nrm(ks[23], (DEPTH, MOE_EXPERTS, D, MOE_FF), D ** -0.5),
        'moe_w_down': nrm(ks[24], (DEPTH, MOE_EXPERTS, MOE_FF, D), MOE_FF ** -0.5),
    }


def reference(x, c, ctx, c_ctx, ada_w, ada_b, norm_mix_g, norm_ffn_g, hg_w_in, hg_lower_bounds, hg_out_norm_g, hg_w_out, mla_w_in, mla_q_norm_g, mla_kv_norm_g, mla_w_qb, mla_w_kvb, mla_q_qknorm_g, mla_k_qknorm_g, mla_w_out, moe_w_group, moe_w_expert, moe_w_gate, moe_w_up, moe_w_down):
    b_, seq, d = x.shape
    ctx_len = ctx.shape[1]
    lower_bounds = jnp.cumsum(jax.nn.softmax(hg_lower_bounds.astype(jnp.float32), axis=1), axis=1)
    x_lat, x_ctx = x, ctx
    for i in range(DEPTH):
        last = i == DEPTH - 1
        j = i // N_MIXERS
        mod_lat = (jnp.einsum('bd,de->be', jax.nn.silu(c), ada_w[i]) + ada_b[i])[:, None, :]
        mod_ctx = jax.nn.silu(c_ctx) @ ada_w[i] + ada_b[i]
        sh_m, sc_m, gt_m, sh_f, sc_f, gt_f = jnp.split(mod_lat, ADA_CHUNKS, axis=-1)
        csh_m, csc_m, cgt_m, csh_f, csc_f, cgt_f = jnp.split(mod_ctx, ADA_CHUNKS, axis=-1)
        h_lat = modulate(x_lat, norm_mix_g[i], sh_m, sc_m)
        h_ctx = modulate(x_ctx, norm_mix_g[i], csh_m, csc_m)
        if i % N_MIXERS == 0:
            y_lat, y_ctx = hgrn2_mixer(h_lat, h_ctx, hg_w_in[j], lower_bounds[:, i], hg_out_norm_g[j], hg_w_out[j], not last)
        else:
            y_lat, y_ctx = mla_mixer(h_lat, h_ctx, mla_w_in[j], mla_q_norm_g[j], mla_kv_norm_g[j], mla_w_qb[j], mla_w_kvb[j], mla_q_qknorm_g[j], mla_k_qknorm_g[j], mla_w_out[j], not last)
        x_lat = x_lat + gt_m * y_lat
        h2_lat = modulate(x_lat, norm_ffn_g[i], sh_f, sc_f).reshape(b_ * seq, d)
        if last:
            f_lat = hier_moe(h2_lat, moe_w_group[i], moe_w_expert[i], moe_w_gate[i], moe_w_up[i], moe_w_down[i])
        else:
            x_ctx = x_ctx + cgt_m * y_ctx
            h2_ctx = modulate(x_ctx, norm_ffn_g[i], csh_f, csc_f).reshape(b_ * ctx_len, d)
            f_all = hier_moe(jnp.concatenate([h2_ctx, h2_lat], axis=0), moe_w_group[i], moe_w_expert[i], moe_w_gate[i], moe_w_up[i], moe_w_down[i])
            x_ctx = x_ctx + cgt_f * f_all[: b_ * ctx_len].reshape(b_, ctx_len, d)
            f_lat = f_all[b_ * ctx_len:]
        x_lat = x_lat + gt_f * f_lat.reshape(b_, seq, d)
    return x_lat
```

```python
import numpy as np
import concourse.bass as bass
import concourse.mybir as mybir
from concourse.bass_utils import run_bass_kernel_spmd
from contextlib import ExitStack

F32 = mybir.dt.float32
BF16 = mybir.dt.bfloat16
I32 = mybir.dt.int32
AF = mybir.ActivationFunctionType
ALU = mybir.AluOpType
AX = mybir.AxisListType

ENGS = ("pe", "act", "dve", "pool", "sp")

D = 1024
KC = 8
CTX = 256
SEQ = 2048
T = CTX + SEQ
NT = T // 128
EPS = 1e-6
NEXP = 32
FF = 512
BIG = 1.0e30


class Key:
    __slots__ = ("w", "r", "dsem", "dcnt")

    def __init__(self):
        self.w = None
        self.r = []
        self.dsem = None
        self.dcnt = 0


class Tl:
    __slots__ = ("t", "k")

    def __init__(self, t, k):
        self.t = t
        self.k = k

    def __getitem__(self, idx):
        return self.t[idx]


def _k(x):
    return x.k if isinstance(x, Tl) else x


class Rot:
    def __init__(self, items):
        self.items = items
        self.i = 0

    def next(self):
        it = self.items[self.i % len(self.items)]
        self.i += 1
        return it


class Sched:
    def __init__(self, nc, n_dma_sems=80):
        self.nc = nc
        self.stack = ExitStack()
        self.esem = {e: self.stack.enter_context(nc.semaphore("es_" + e)) for e in ENGS}
        self.ecnt = {e: 0 for e in ENGS}
        self.dpool = [[self.stack.enter_context(nc.semaphore("ds%d" % i)), 0] for i in range(n_dma_sems)]
        self.dfree = list(range(n_dma_sems))
        self.all_keys = []
        self._reset_stage()

    def _reset_stage(self):
        self.ops = {e: [] for e in ENGS}
        self.seen = {e: {} for e in ENGS}
        self.stage_dsems = {}

    def key(self):
        k = Key()
        self.all_keys.append(k)
        return k

    def keys(self, n):
        return [self.key() for _ in range(n)]

    def _deps(self, eng, reads, writes):
        deps = []
        for t in reads:
            if t.w is not None:
                deps.append(t.w)
        for t in writes:
            if t.w is not None:
                deps.append(t.w)
            deps.extend(t.r)
        waits = {}
        seen = self.seen[eng]
        for d in deps:
            if d[0] == "e":
                if d[1] == eng and eng in ("pe", "sp"):
                    continue
                k = ("e", d[1])
                v = d[2]
            else:
                k = ("d", d[1])
                v = self.dpool[d[1]][1]
            if seen.get(k, -1) >= v:
                continue
            if waits.get(k, -1) < v:
                waits[k] = v
        for k, v in waits.items():
            seen[k] = v
        return waits

    def op(self, eng, fn, reads=(), writes=()):
        reads = [_k(x) for x in reads]
        writes = [_k(x) for x in writes]
        waits = self._deps(eng, reads, writes)
        seq = len(self.ops[eng])
        self.ops[eng].append({"fn": fn, "waits": waits, "inc": False, "dma": None})
        me = ("e", eng, seq)
        for t in reads:
            t.r.append(me)
        for t in writes:
            t.w = me
            t.r = []

    def dma(self, eng, fn, reads=(), writes=(), semkey=None):
        reads = [_k(x) for x in reads]
        writes = [_k(x) for x in writes]
        waits = self._deps(eng, reads, writes)
        sk = _k(semkey) if semkey is not None else (writes[0] if writes else reads[0])
        if sk.dsem is None:
            idx = self.dfree.pop()
            sk.dsem = idx
            sk.dcnt = self.dpool[idx][1]
        sk.dcnt += 16
        self.dpool[sk.dsem][1] = sk.dcnt
        self.stage_dsems[sk.dsem] = sk.dcnt
        self.ops[eng].append({"fn": fn, "waits": waits, "inc": False, "dma": sk.dsem})
        me = ("d", sk.dsem, sk.dcnt)
        for t in reads:
            t.r.append(me)
        for t in writes:
            t.w = me
            t.r = []

    def flush(self):
        nc = self.nc
        fin = {}
        for idx, cnt in self.stage_dsems.items():
            if self.seen["sp"].get(("d", idx), -1) < cnt:
                fin[idx] = cnt
        for e in ENGS:
            for rec in self.ops[e]:
                for k, v in rec["waits"].items():
                    if k[0] == "e":
                        self.ops[k[1]][v]["inc"] = True
        val = {}
        for e in ENGS:
            c = self.ecnt[e]
            vs = []
            for rec in self.ops[e]:
                if rec["inc"]:
                    c += 1
                vs.append(c)
            self.ecnt[e] = c
            val[e] = vs
        engobj = {"pe": "tensor", "act": "scalar", "dve": "vector", "pool": "gpsimd", "sp": "sync"}
        esem, dpool, ops = self.esem, self.dpool, self.ops

        def mk(e):
            def body(eng):
                for rec in ops[e]:
                    for k, v in rec["waits"].items():
                        if k[0] == "e":
                            eng.wait_ge(esem[k[1]], val[k[1]][v])
                        else:
                            eng.wait_ge(dpool[k[1]][0], v)
                    ins = rec["fn"](eng)
                    if rec["dma"] is not None:
                        ins.then_inc(dpool[rec["dma"]][0], 16)
                    elif rec["inc"]:
                        ins.then_inc(esem[e], 1)
                if e == "sp":
                    for idx, v in fin.items():
                        eng.wait_ge(dpool[idx][0], v)
            return body

        with nc.Block() as block:
            for e in ENGS:
                if ops[e] or (e == "sp" and fin):
                    getattr(block, engobj[e])(mk(e))
        for k in self.all_keys:
            if k.dsem is not None:
                self.dfree.append(k.dsem)
                k.dsem = None
            k.w = None
            k.r = []
        n = {e: len(ops[e]) for e in ENGS}
        self._reset_stage()
        return n

    def close(self):
        self.stack.close()


class KB:
    def __init__(self, NB=4, stages=("mod", "hgrn", "moe0", "mla", "moe1"), dbg=()):
        self.NB = NB
        self.stages = stages
        self.dbg = dbg
        nc = bass.Bass("TRN2", target_bir_lowering=False)
        self.nc = nc
        self.S = Sched(nc)
        self.uid = 0
        shapes = {
            "x": (NB, SEQ, D), "c": (NB, D), "ctx": (NB, CTX, D), "c_ctx": (D,),
            "ada_w": (2, D, 6 * D), "ada_b": (2, 6 * D), "norm_mix_g": (2, D), "norm_ffn_g": (2, D),
            "hg_w_in": (D, 5 * D), "hg_lb": (2, 3, D), "hg_out_norm_g": (128,), "hg_w_out": (D, D),
            "mla_w_in": (D, 416), "mla_q_norm_g": (256,), "mla_kv_norm_g": (128,), "mla_w_qb": (256, 1536),
            "mla_w_kvb": (128, 2048), "mla_q_qknorm_g": (96,), "mla_k_qknorm_g": (96,), "mla_w_out": (D, D),
            "moe_w_group": (2, D, 4), "moe_w_expert": (2, D, 32), "moe_w_gate": (2, NEXP, D, FF),
            "moe_w_up": (2, NEXP, D, FF), "moe_w_down": (2, NEXP, FF, D),
            "k_maskf": (128, 128), "k_maskb": (128, 128), "k_bm": (128, 4, 128), "k_cos": (SEQ, 16), "k_sin": (SEQ, 16),
        }

        class LazyIn(dict):
            def __missing__(d_, name):
                ap = nc.dram_tensor(name, list(shapes[name]), F32, kind="ExternalInput").ap()
                d_[name] = ap
                return ap

        I = LazyIn()
        self.I = I
        self.out = nc.dram_tensor("out", [NB, SEQ, D], F32, kind="ExternalOutput").ap()
        self.xres = nc.dram_tensor("xres", [NB, T, D], F32).ap()
        self.mod = nc.dram_tensor("modv", [2, 5, 6 * D], F32).ap()
        self.cT_d = nc.dram_tensor("cT_d", [128, 3, T], BF16).ap()
        self.krr_d = nc.dram_tensor("krr_d", [128, NT, 32], F32).ap()
        self.sskr_d = nc.dram_tensor("sskr_d", [128, NT], F32).ap()
        NTLmax = NB * NT
        self.NBLKmax = 2 * NTLmax + NEXP
        self.h2d = nc.dram_tensor("h2d", [NTLmax * 128, D], BF16).ap()
        self.xs = nc.dram_tensor("xs", [self.NBLKmax * 128, D], BF16).ap()
        self.ys = nc.dram_tensor("ys", [self.NBLKmax * 128, D], F32).ap()
        self.wgb = nc.dram_tensor("wgb", [NEXP * 128, 4096], BF16).ap()
        self.wub = nc.dram_tensor("wub", [NEXP * 128, 4096], BF16).ap()
        self.wdb = nc.dram_tensor("wdb", [NEXP * 128, 4096], BF16).ap()
        self.kx = self.S.keys(NB)
        self.kmod = self.S.key()
        self.kscr = self.S.key()
        self.D_ = {}
        for name, shape in dbg:
            self.D_[name] = nc.dram_tensor("dbg_" + name, list(shape), F32, kind="ExternalOutput").ap()

    def sb(self, st, shape, dt, nm="t"):
        self.uid += 1
        t = st.enter_context(self.nc.sbuf_tensor("%s_%d" % (nm, self.uid), list(shape), dt))
        return Tl(t, self.S.key())

    def sbr(self, st, n, shape, dt, nm="r"):
        return Rot([self.sb(st, shape, dt, nm) for _ in range(n)])

    def psb(self, st, nm="ps"):
        self.uid += 1
        t = st.enter_context(self.nc.psum_tensor("%s_%d" % (nm, self.uid), [128, 512], F32))
        return Tl(t, self.S.key())

    def mm(self, out, lhsT, rhs, start, stop, R, W):
        self.S.op("pe", lambda e: e.matmul(out, lhsT=lhsT, rhs=rhs, start=start, stop=stop), R, W)

    def tr(self, out, in_, ident, R, W):
        self.S.op("pe", lambda e: e.transpose(out=out, in_=in_, identity=ident), R, W)

    def act(self, out, in_, func, R, W, bias=None, scale=None, accum_out=None):
        kw = {}
        if bias is not None:
            kw["bias"] = bias
        if scale is not None:
            kw["scale"] = scale
        if accum_out is not None:
            kw["accum_out"] = accum_out
        self.S.op("act", lambda e: e.activation(out=out, in_=in_, func=func, **kw), R, W)

    def ts(self, eng, out, in0, s1, s2, op0, op1, R, W):
        if op1 is None:
            self.S.op(eng, lambda e: e.tensor_scalar(out=out, in0=in0, scalar1=s1, scalar2=None, op0=op0), R, W)
        else:
            self.S.op(eng, lambda e: e.tensor_scalar(out=out, in0=in0, scalar1=s1, scalar2=s2, op0=op0, op1=op1), R, W)

    def tt(self, eng, out, in0, in1, op, R, W):
        self.S.op(eng, lambda e: e.tensor_tensor(out=out, in0=in0, in1=in1, op=op), R, W)

    def stt(self, out, in0, scalar, in1, op0, op1, R, W):
        self.S.op("dve", lambda e: e.scalar_tensor_tensor(out=out, in0=in0, scalar=scalar, in1=in1, op0=op0, op1=op1), R, W)

    def cp(self, eng, out, in_, R, W):
        if eng == "act":
            self.S.op("act", lambda e: e.activation(out=out, in_=in_, func=AF.Copy), R, W)
        else:
            self.S.op(eng, lambda e: e.tensor_copy(out=out, in_=in_), R, W)

    def red(self, out, in_, op, R, W, negate=None):
        self.S.op("dve", lambda e: e.tensor_reduce(out=out, in_=in_, axis=AX.X, op=op, negate=negate), R, W)

    def recip(self, out, in_, R, W):
        self.S.op("dve", lambda e: e.reciprocal(out=out, in_=in_), R, W)

    def memset(self, eng, ap, val, W):
        self.S.op(eng, lambda e: e.memset(ap, val), (), W)

    def dma(self, q, out, in_, R, W, semkey=None):
        self.S.dma(q, lambda e: e.dma_start(out=out, in_=in_), R, W, semkey=semkey)

    def sumsq(self, junk, in_, acc, R, W):
        self.act(junk, in_, AF.Square, R, W, accum_out=acc)

    def setup_consts(self, st):
        nc, S = self.nc, self.S
        self.identf = self.sb(st, [128, 128], F32, "identf")
        self.identb = self.sb(st, [128, 128], BF16, "identb")
        self.onesb = self.sb(st, [128, 128], BF16, "onesb")
        identf = self.identf
        self.memset("pool", identf[:], 0.0, [identf])
        S.op("pool", lambda e: e.affine_select(out=identf[:], in_=identf[:], pattern=[[-1, 128]], compare_op=ALU.not_equal,
                                               fill=1.0, base=0, channel_multiplier=1), [identf], [identf])
        self.cp("dve", self.identb[:], identf[:], [identf], [self.identb])
        self.memset("dve", self.onesb[:], 1.0, [self.onesb])
        self.epsc = self.sb(st, [128, 1], F32, "epsc")
        self.memset("dve", self.epsc[:], EPS, [self.epsc])

    def stage_mod(self):
        I, NB = self.I, self.NB
        with ExitStack() as st:
            cT = self.sb(st, [128, KC, 5], F32, "cT")
            cs = self.sb(st, [128, KC, 5], F32, "cs")
            self.memset("dve", cT[:], 0.0, [cT])
            for r in range(NB):
                self.dma("sp", cT[:, :, r], I["c"][r, :].rearrange("(c p) -> p c", p=128), [], [cT])
            self.dma("sp", cT[:, :, 4], I["c_ctx"].rearrange("(c p) -> p c", p=128), [], [cT])
            self.act(cs[:], cT[:], AF.Silu, [cT], [cs])
            wrot = self.sbr(st, 3, [128, KC, 512], F32, "adaw")
            ps = Rot([self.psb(st) for _ in range(2)])
            for l in range(2):
                bt = self.sb(st, [5, 6 * D], F32, "adab")
                ms = self.sb(st, [5, 6 * D], F32, "modsb")
                self.dma("sp", bt[:], I["ada_b"][l, :].partition_broadcast(5), [], [bt])
                for n in range(12):
                    w = wrot.next()
                    self.dma("sp", w[:], I["ada_w"][l, :, n * 512:(n + 1) * 512].rearrange("(c p) n -> p c n", p=128), [], [w])
                    p = ps.next()
                    for k in range(KC):
                        self.mm(p[0:5, :], cs[:, k, :], w[:, k, :], k == 0, k == KC - 1, [cs, w], [p])
                    self.tt("dve", ms[:, n * 512:(n + 1) * 512], p[0:5, :], bt[:, n * 512:(n + 1) * 512], ALU.add, [p, bt], [ms])
                self.dma("sp", self.mod[l], ms[:], [ms], [self.kmod], semkey=ms)
            return self.S.flush()

    def mod_cols(self, st, l, m, r):
        I = self.I
        g = I["norm_mix_g"] if m == 0 else I["norm_ffn_g"]
        gc = self.sb(st, [128, KC], F32, "gc")
        sc = self.sb(st, [128, KC], F32, "sc")
        sh = self.sb(st, [128, KC], F32, "sh")
        A = self.sb(st, [128, KC], F32, "A")
        self.dma("sp", gc[:], g[l, :].rearrange("(c p) -> p c", p=128), [], [gc])
        self.dma("sp", sc[:], self.mod[l, r, (3 * m + 1) * D:(3 * m + 2) * D].rearrange("(c p) -> p c", p=128), [self.kmod], [sc])
        self.dma("sp", sh[:], self.mod[l, r, (3 * m) * D:(3 * m + 1) * D].rearrange("(c p) -> p c", p=128), [self.kmod], [sh])
        self.stt(A[:], sc[:], 1.0, gc[:], ALU.add, ALU.mult, [sc, gc], [A])
        return A, sh

    def gate_tile(self, st, l, m, r):
        gt = self.sb(st, [128, D], F32, "gt")
        self.dma("sp", gt[:], self.mod[l, r, (3 * m + 2) * D:(3 * m + 3) * D].partition_broadcast(128), [self.kmod], [gt])
        return gt

    def norm_res(self, st, pbanks, junk=None, nxn=1, nhtf=1):
        R = {}
        R["xt"] = self.sbr(st, 2, [128, D], F32, "xt")
        R["xn"] = self.sbr(st, nxn, [128, D], F32, "xn")
        R["junk"] = junk if junk is not None else self.sb(st, [128, D], BF16, "junk")
        R["ss"] = self.sbr(st, 2, [128, 1], F32, "ss")
        R["sd"] = self.sbr(st, 2, [128, 1], F32, "sd")
        R["rs"] = self.sbr(st, 2, [128, 1], F32, "rs")
        R["hTf"] = self.sbr(st, nhtf, [128, KC, 128], F32, "hTf")
        R["pb"] = pbanks
        return R

    def norm_tile(self, R, src_ap, src_keys, A, Bc, dst_ap, dst_keys):
        xt = R["xt"].next()
        xn = R["xn"].next()
        ss = R["ss"].next()
        sd = R["sd"].next()
        rs = R["rs"].next()
        hTf = R["hTf"].next()
        junk = R["junk"]
        pa, pb = R["pb"]
        self.dma("sp", xt[:], src_ap, src_keys, [xt])
        self.sumsq(junk[:, 0:D], xt[:], ss[:], [xt], [junk, ss])
        self.act(sd[:], ss[:], AF.Sqrt, [ss, self.epsc], [sd], bias=self.epsc[:, 0:1], scale=1.0 / D)
        self.recip(rs[:], sd[:], [sd], [rs])
        self.act(xn[:], xt[:], AF.Copy, [xt, rs], [xn], scale=rs[:, 0:1])
        for k in range(KC):
            p = pa if k < 4 else pb
            self.tr(p[:, (k % 4) * 128:(k % 4 + 1) * 128], xn[:, k * 128:(k + 1) * 128], self.identf[:], [xn, self.identf], [p])
        for k in range(KC):
            p = pa if k < 4 else pb
            src = p[:, (k % 4) * 128:(k % 4 + 1) * 128]
            if k % 2 == 0:
                self.ts("dve", hTf[:, k, :], src, A[:, k:k + 1], Bc[:, k:k + 1], ALU.mult, ALU.add, [p, A, Bc], [hTf])
            else:
                self.act(hTf[:, k, :], src, AF.Identity, [p, A, Bc], [hTf], bias=Bc[:, k:k + 1], scale=A[:, k:k + 1])
        if dst_ap is not None:
            self.cp("dve", dst_ap, hTf[:], [hTf], dst_keys)
        self.last_xn = xn
        return hTf

    def src_l0(self, b, tile):
        if tile < 2:
            return self.I["ctx"][b, tile * 128:(tile + 1) * 128, :]
        return self.I["x"][b, (tile - 2) * 128:(tile - 1) * 128, :]

    def stage_hgrn(self, b):
        I, S = self.I, self.S
        l = 0
        with ExitStack() as st:
            PB = [self.psb(st) for _ in range(8)]
            hT = self.sb(st, [128, KC, T], BF16, "hT")
            hTk = S.keys(NT)
            ogT = self.sb(st, [128, KC, T], BF16, "ogT")
            ogk = S.keys(KC)
            maskf = self.sb(st, [128, 128], F32, "maskf")
            maskb = self.sb(st, [128, 128], F32, "maskb")
            bm = self.sb(st, [128, 4, 128], BF16, "bm")
            self.dma("sp", maskf[:], I["k_maskf"], [], [maskf])
            self.dma("sp", maskb[:], I["k_maskb"], [], [maskb])
            self.dma("pool", bm[:], I["k_bm"], [], [bm])
            m01 = self.sb(st, [128, T], BF16, "m01")
            self.memset("dve", m01[:], 1.0, [m01])
            self.memset("dve", m01[:, 0:T:32], 0.0, [m01])
            lbr = self.sb(st, [128, 2, 3, KC], F32, "lbr")
            with self.nc.allow_non_contiguous_dma(reason="tiny"):
                for d_ in range(2):
                    for j in range(3):
                        self.dma("sp", lbr[:, d_, j, :], I["hg_lb"][d_, j, :].rearrange("(h p) -> p h", p=128), [], [lbr])
            lbe = self.sb(st, [128, 2, 3, KC], F32, "lbe")
            self.act(lbe[:], lbr[:], AF.Exp, [lbr], [lbe])
            lbs = self.sb(st, [128, 2, KC], F32, "lbs")
            self.tt("dve", lbs[:], lbe[:, :, 0, :], lbe[:, :, 1, :], ALU.add, [lbe], [lbs])
            self.tt("dve", lbs[:], lbs[:], lbe[:, :, 2, :], ALU.add, [lbe, lbs], [lbs])
            lbi = self.sb(st, [128, 2, KC], F32, "lbi")
            self.recip(lbi[:], lbs[:], [lbs], [lbi])
            lb = self.sb(st, [128, 2, KC], F32, "lb")
            oml = self.sb(st, [128, 2, KC], F32, "oml")
            self.tt("dve", lb[:], lbe[:, :, 0, :], lbi[:], ALU.mult, [lbe, lbi], [lb])
            self.ts("dve", oml[:], lb[:], -1.0, 1.0, ALU.mult, ALU.add, [lb], [oml])
            ogc = self.sb(st, [128, 1], F32, "ogc")
            self.dma("sp", ogc[:], I["hg_out_norm_g"].rearrange("(p o) -> p o", o=1), [], [ogc])
            A_l, B_l = self.mod_cols(st, l, 0, b)
            A_c, B_c = self.mod_cols(st, l, 0, 4)
            gt_l = self.gate_tile(st, l, 0, b)
            gt_c = self.gate_tile(st, l, 0, 4)
            qdec = self.sb(st, [128, T], BF16, "qdec")
            NR = self.norm_res(st, (PB[0], PB[1]), junk=qdec)
            for tile in range(NT):
                A, Bc = (A_c, B_c) if tile < 2 else (A_l, B_l)
                self.norm_tile(NR, self.src_l0(b, tile), [], A, Bc, hT[:, :, tile * 128:(tile + 1) * 128], [hTk[tile]])
            wh = self.sbr(st, 1, [128, KC, 5, 128], BF16, "wh")
            Vh = self.sb(st, [128, NT, 128], BF16, "Vh")
            qs = self.sb(st, [128, T], BF16, "qs")
            sgate = self.sb(st, [128, T], BF16, "sgate")
            A1 = self.sb(st, [128, T], F32, "A1")
            A2 = self.sb(st, [128, T], F32, "A2")
            A3 = self.sb(st, [128, T], F32, "A3")
            kinc = self.sb(st, [128, T], BF16, "kinc")
            dec = self.sb(st, [128, T // 32], F32, "dec")
            tot = self.sb(st, [128, T // 32], F32, "tot")
            oacc = self.sb(st, [128, T], F32, "oacc")
            oak = S.keys(NT)
            sTm = self.sbr(st, 2, [128, 128], BF16, "sTm")
            kTs = self.sbr(st, 2, [128, 128], BF16, "kTs")
            Vbd = self.sbr(st, 2, [128, 4, 128], BF16, "Vbd")
            KVs = self.sbr(st, 2, [128, 4, 128], F32, "KVs")
            Sst = self.sb(st, [128, 8, 128], F32, "Sst")
            Sstk = S.keys(8)
            Sb = self.sb(st, [128, 8, 128], BF16, "Sb")
            Sbk = S.keys(2)
            pproj = Rot([PB[0], PB[1]])
            psT = Rot([PB[2], PB[3]])
            pkT = PB[4]
            pkTk = S.keys(2)
            pKV = PB[5]
            poT = Rot([PB[6], PB[7]])
            blocks = [(i * 512, 512) for i in range(4)] + [(2048, 256)]

            def proj(whh, sec, blk):
                t0, n = blk
                p = pproj.next()
                tiles = range(t0 // 128, (t0 + n) // 128)
                for k in range(KC):
                    self.mm(p[:, 0:n], whh[:, k, sec, :], hT[:, k, t0:t0 + n], k == 0, k == KC - 1,
                            [whh] + [hTk[t] for t in tiles], [p])
                return p

            for h in range(KC):
                whh = wh.next()
                for sec in range(5):
                    self.dma("pool", whh[:, :, sec, :],
                             I["hg_w_in"][:, sec * D + h * 128: sec * D + (h + 1) * 128].rearrange("(c p) e -> p c e", p=128), [], [whh])
                for tile in range(NT):
                    p = pproj.next()
                    for k in range(KC):
                        self.mm(p[:, 0:128], hT[:, k, tile * 128:(tile + 1) * 128], whh[:, k, 3, :], k == 0, k == KC - 1,
                                [whh, hTk[tile]], [p])
                    self.cp("act", Vh[:, tile, :], p[:, 0:128], [p], [Vh])
                for blk in blocks:
                    t0, n = blk
                    p = proj(whh, 0, blk)
                    self.act(qs[:, t0:t0 + n], p[:, 0:n], AF.Silu, [p], [qs])
                    p = proj(whh, 4, blk)
                    self.act(sgate[:, t0:t0 + n], p[:, 0:n], AF.Silu, [p], [sgate])
                for dr in range(2):
                    for blk in blocks:
                        t0, n = blk
                        p = proj(whh, 1 + dr, blk)
                        self.act(A1[:, t0:t0 + n], p[:, 0:n], AF.Sigmoid, [p], [A1])
                    self.ts("dve", A1[:], A1[:], oml[:, dr, h:h + 1], lb[:, dr, h:h + 1], ALU.mult, ALU.add, [A1, oml, lb], [A1])
                    self.act(A2[:], A1[:], AF.Ln, [A1], [A2])
                    self.ts("dve", A1[:], A1[:], -1.0, 1.0, ALU.mult, ALU.add, [A1], [A1])
                    S.op("dve", lambda e: e.tensor_tensor_scan(out=A3[:], data0=m01[:], data1=A2[:], initial=0.0,
                                                                op0=ALU.mult, op1=ALU.add), [m01, A2], [A3])
                    a3v = A3[:].rearrange("p (j i) -> p j i", i=32)
                    self.cp("dve", tot[:], a3v[:, :, 31], [A3], [tot])
                    self.act(dec[:], tot[:], AF.Exp, [tot], [dec])
                    if dr == 0:
                        barr, free = A3, A2
                    else:
                        a2v = A2[:].rearrange("p (j i) -> p j i", i=32)
                        self.tt("dve", A2[:], A2[:], A3[:], ALU.subtract, [A2, A3], [A2])
                        self.tt("dve", a2v, a2v, tot[:].unsqueeze(2).broadcast_to([128, T // 32, 32]), ALU.add, [A2, tot], [A2])
                        barr, free = A2, A3
                    self.act(free[:], barr[:], AF.Exp, [barr], [free], scale=-1.0)
                    self.act(barr[:], barr[:], AF.Exp, [barr], [barr])
                    self.tt("dve", qdec[:], qs[:], barr[:], ALU.mult, [qs, barr], [qdec])
                    self.tt("dve", kinc[:], A1[:], free[:], ALU.mult, [A1, free], [kinc])
                    order = list(range(NT)) if dr == 0 else [1, 0] + list(range(NT - 1, 1, -1))
                    mask = maskf if dr == 0 else maskb
                    self.memset("dve", Sst[:, 0, :], 0.0, [Sstk[0]])
                    for i, tile in enumerate(order):
                        base = 4 * (i % 2)
                        ts_ = slice(tile * 128, (tile + 1) * 128)
                        ps_ = psT.next()
                        self.mm(ps_[:, 0:128], kinc[:, ts_], qdec[:, ts_], True, True, [kinc, qdec], [ps_])
                        sm = sTm.next()
                        self.tt("dve", sm[:], ps_[:, 0:128], mask[:], ALU.mult, [ps_, mask], [sm])
                        pk_i = i % 2
                        pkv = pkT[:, pk_i * 64:(pk_i + 1) * 64].bitcast(BF16)
                        self.tr(pkv, kinc[:, ts_], self.identb[:], [kinc, self.identb], [pkTk[pk_i]])
                        kt = kTs.next()
                        self.cp("act", kt[:], pkv, [pkTk[pk_i]], [kt])
                        vb = Vbd.next()
                        self.tt("dve", vb[:], Vh[:, tile, :].unsqueeze(1).broadcast_to([128, 4, 128]), bm[:], ALU.mult, [Vh, bm], [vb])
                        self.mm(pKV[:, :], kt[:], vb[:].rearrange("p j v -> p (j v)"), True, True, [kt, vb], [pKV])
                        kv = KVs.next()
                        self.tt("dve", kv[:], pKV[:, :].rearrange("p (j v) -> p j v", j=4),
                                dec[:, tile * 4:(tile + 1) * 4].unsqueeze(2).broadcast_to([128, 4, 128]), ALU.mult, [pKV, dec], [kv])
                        corder = [0, 1, 2, 3] if dr == 0 else [3, 2, 1, 0]
                        for jj, c in enumerate(corder):
                            s_in = base + jj
                            s_out = (base + jj + 1) % 8
                            self.stt(Sst[:, s_out, :], Sst[:, s_in, :], dec[:, tile * 4 + c: tile * 4 + c + 1], kv[:, c, :],
                                     ALU.mult, ALU.add, [Sstk[s_in], dec, kv], [Sstk[s_out]])
                        self.cp("act", Sb[:, base:base + 4, :], Sst[:, base:base + 4, :], [Sstk[base + q_] for q_ in range(4)], [Sbk[i % 2]])
                        po = poT.next()
                        self.mm(po[:, 0:128], Vh[:, tile, :], sm[:], True, False, [Vh, sm], [po])
                        for jj, c in enumerate(corder):
                            self.mm(po[:, c * 32:(c + 1) * 32], Sb[:, base + jj, :], qdec[:, tile * 128 + c * 32: tile * 128 + (c + 1) * 32],
                                    False, jj == 3, [Sbk[i % 2], qdec], [po])
                        if dr == 0:
                            self.cp("act", oacc[:, ts_], po[:, 0:128], [po], [oak[tile]])
                        else:
                            self.tt("dve", oacc[:, ts_], oacc[:, ts_], po[:, 0:128], ALU.add, [po, oak[tile]], [oak[tile]])
                self.tt("dve", qdec[:], oacc[:], oacc[:], ALU.mult, oak, [qdec])
                for blk in blocks:
                    t0, n = blk
                    p = pproj.next()
                    self.mm(p[:, 0:n], self.onesb[:], qdec[:, t0:t0 + n], True, True, [self.onesb, qdec], [p])
                    self.act(A2[:, t0:t0 + n], p[:, 0:n], AF.Sqrt, [p, self.epsc], [A2], bias=self.epsc[:, 0:1], scale=1.0 / 128)
                self.recip(A3[:], A2[:], [A2], [A3])
                self.tt("dve", A3[:], A3[:], oacc[:], ALU.mult, [A3] + oak, [A3])
                self.stt(ogT[:, h, :], A3[:], ogc[:, 0:1], sgate[:], ALU.mult, ALU.mult, [A3, ogc, sgate], [ogk[h]])
            wo = self.sb(st, [128, KC, D], BF16, "wo")
            self.dma("pool", wo[:], I["hg_w_out"].rearrange("(c p) n -> p c n", p=128), [], [wo])
            xt2 = NR["xt"]
            tmp = NR["xn"]
            for tile in range(NT):
                gt = gt_c if tile < 2 else gt_l
                x_ = xt2.next()
                self.dma("sp", x_[:], self.src_l0(b, tile), [], [x_])
                t_ = tmp.next()
                for half in range(2):
                    p = pproj.next()
                    hs = slice(half * 512, (half + 1) * 512)
                    for k in range(KC):
                        self.mm(p[:, :], ogT[:, k, tile * 128:(tile + 1) * 128], wo[:, k, hs], k == 0, k == KC - 1, [ogk[k], wo], [p])
                    self.tt("dve", t_[:, hs], p[:, :], gt[:, hs], ALU.mult, [p, gt], [t_])
                self.tt("dve", t_[:], t_[:], x_[:], ALU.add, [t_, x_], [t_])
                self.dma("sp", self.xres[b, tile * 128:(tile + 1) * 128, :], t_[:], [t_], [self.kx[b]], semkey=t_)
                if ("xm0" in self.D_) and b == 0:
                    self.dma("sp", self.D_["xm0"][tile * 128:(tile + 1) * 128, :], t_[:], [t_], [self.kscr], semkey=t_)
            return S.flush()

    ROUTE_TMPS = (("lg", 36), ("gmax", 1), ("ngmax", 1), ("ge", 4), ("gsum", 1), ("pg", 1), ("gone", 4), ("pen", 4),
                  ("em", 32), ("m1", 1), ("oh1", 32), ("em2", 32), ("m2", 1), ("oh2", 32), ("dm", 1), ("e2", 1),
                  ("den", 1), ("rden", 1), ("w1", 1), ("w2", 1), ("tmpw", 32))

    def route_tile(self, sm, p):
        t = {nm: r.next() for nm, r in sm.items()}
        lg = t["lg"]
        self.cp("act", lg[:], p[:, 0:36], [p], [lg])
        self.red(t["gmax"][:], lg[:, 0:4], ALU.max, [lg], [t["gmax"]])
        self.ts("dve", t["ngmax"][:], t["gmax"][:], -1.0, None, ALU.mult, None, [t["gmax"]], [t["ngmax"]])
        self.act(t["ge"][:], lg[:, 0:4], AF.Exp, [lg, t["ngmax"]], [t["ge"], t["gsum"]], bias=t["ngmax"][:, 0:1], accum_out=t["gsum"][:])
        self.recip(t["pg"][:], t["gsum"][:], [t["gsum"]], [t["pg"]])
        self.ts("dve", t["gone"][:], lg[:, 0:4], t["gmax"][:, 0:1], None, ALU.is_ge, None, [lg, t["gmax"]], [t["gone"]])
        self.ts("dve", t["pen"][:], t["gone"][:], BIG, -BIG, ALU.mult, ALU.add, [t["gone"]], [t["pen"]])
        self.tt("dve", t["em"][:].rearrange("p (g j) -> p g j", g=4), lg[:, 4:36].rearrange("p (g j) -> p g j", g=4),
                t["pen"][:].unsqueeze(2).broadcast_to([128, 4, 8]), ALU.add, [lg, t["pen"]], [t["em"]])
        self.red(t["m1"][:], t["em"][:], ALU.max, [t["em"]], [t["m1"]])
        self.ts("dve", t["oh1"][:], t["em"][:], t["m1"][:, 0:1], None, ALU.is_ge, None, [t["em"], t["m1"]], [t["oh1"]])
        self.stt(t["em2"][:], t["oh1"][:], -BIG, t["em"][:], ALU.mult, ALU.add, [t["oh1"], t["em"]], [t["em2"]])
        self.red(t["m2"][:], t["em2"][:], ALU.max, [t["em2"]], [t["m2"]])
        self.ts("dve", t["oh2"][:], t["em2"][:], t["m2"][:, 0:1], None, ALU.is_ge, None, [t["em2"], t["m2"]], [t["oh2"]])
        self.tt("dve", t["dm"][:], t["m2"][:], t["m1"][:], ALU.subtract, [t["m2"], t["m1"]], [t["dm"]])
        self.act(t["e2"][:], t["dm"][:], AF.Exp, [t["dm"]], [t["e2"]])
        self.ts("dve", t["den"][:], t["e2"][:], 1.0, None, ALU.add, None, [t["e2"]], [t["den"]])
        self.recip(t["rden"][:], t["den"][:], [t["den"]], [t["rden"]])
        self.tt("dve", t["w1"][:], t["pg"][:], t["rden"][:], ALU.mult, [t["pg"], t["rden"]], [t["w1"]])
        self.tt("dve", t["w2"][:], t["w1"][:], t["e2"][:], ALU.mult, [t["w1"], t["e2"]], [t["w2"]])
        return t

    def stage_moe(self, l, b, half):
        I, S = self.I, self.S
        if l == 0:
            tiles = list(range(0, 9)) if half == 0 else list(range(9, 18))
        else:
            tiles = list(range(2, 10)) if half == 0 else list(range(10, 18))
        ntl = len(tiles)
        NTOK = ntl * 128
        with ExitStack() as st:
            PB = [self.psb(st) for _ in range(8)]
            hT = self.sb(st, [128, KC, NTOK], BF16, "hT")
            hTk = S.keys(ntl)
            acc = self.sb(st, [128, ntl, D], F32, "acc")
            acck = S.keys(ntl)
            Wt = self.sb(st, [128, ntl, NEXP], F32, "Wt")
            Wtk = S.keys(ntl)
            wr = self.sb(st, [128, KC, 36], F32, "wr")
            self.dma("sp", wr[:, :, 0:4], I["moe_w_group"][l].rearrange("(c p) g -> p c g", p=128), [], [wr])
            self.dma("sp", wr[:, :, 4:36], I["moe_w_expert"][l].rearrange("(c p) g -> p c g", p=128), [], [wr])
            A_l, B_l = self.mod_cols(st, l, 1, b)
            gt_l = self.gate_tile(st, l, 1, b)
            if l == 0 and half == 0:
                A_c, B_c = self.mod_cols(st, l, 1, 4)
                gt_c = self.gate_tile(st, l, 1, 4)
            NR = self.norm_res(st, (PB[0], PB[1]), nhtf=2)
            sm = {}
            for nm, w in (("lg", 36), ("gmax", 1), ("ngmax", 1), ("ge", 4), ("gsum", 1), ("pg", 1), ("gone", 4), ("pen", 4),
                          ("em", 32), ("m1", 1), ("oh1", 32), ("em2", 32), ("m2", 1), ("oh2", 32), ("dm", 1), ("e2", 1),
                          ("den", 1), ("rden", 1), ("w1", 1), ("w2", 1), ("tmpw", 32)):
                sm[nm] = self.sbr(st, 2, [128, w], F32, nm)
            for li, tile in enumerate(tiles):
                isctx = (l == 0 and tile < 2)
                A, Bc = (A_c, B_c) if isctx else (A_l, B_l)
                hTf = self.norm_tile(NR, self.xres[b, tile * 128:(tile + 1) * 128, :], [self.kx[b]], A, Bc,
                                     hT[:, :, li * 128:(li + 1) * 128], [hTk[li]])
                p = PB[2 + li % 2]
                for k in range(KC):
                    self.mm(p[:, 0:36], hTf[:, k, :], wr[:, k, :], k == 0, k == KC - 1, [hTf, wr], [p])
                t = self.route_tile(sm, p)
                self.ts("dve", t["tmpw"][:], t["oh1"][:], t["w1"][:, 0:1], None, ALU.mult, None, [t["oh1"], t["w1"]], [t["tmpw"]])
                self.stt(Wt[:, li, :], t["oh2"][:], t["w2"][:, 0:1], t["tmpw"][:], ALU.mult, ALU.add, [t["oh2"], t["w2"], t["tmpw"]], [Wtk[li]])
            wg = self.sbr(st, 2, [128, KC, FF], BF16, "wg")
            wu = self.sbr(st, 2, [128, KC, FF], BF16, "wu")
            wd = self.sbr(st, 2, [128, 4, D], BF16, "wd")
            sg = self.sbr(st, 2, [128, 512], BF16, "sg")
            actT = self.sbr(st, 2, [128, 4, 512], BF16, "actT")
            pgu = Rot([(PB[0], PB[1]), (PB[2], PB[3])])
            pyr = Rot([(PB[4], PB[5]), (PB[6], PB[7])])
            blocks = []
            t0 = 0
            while t0 < NTOK:
                n = min(512, NTOK - t0)
                blocks.append((t0, n))
                t0 += n
            for e in range(NEXP):
                g_, u_, d_ = wg.next(), wu.next(), wd.next()
                self.dma("pool", g_[:], I["moe_w_gate"][l, e].rearrange("(c p) f -> p c f", p=128), [], [g_])
                self.dma("pool", u_[:], I["moe_w_up"][l, e].rearrange("(c p) f -> p c f", p=128), [], [u_])
                self.dma("pool", d_[:], I["moe_w_down"][l, e].rearrange("(c p) f -> p c f", p=128), [], [d_])
                for (t0, n) in blocks:
                    at = actT.next()
                    hk = [hTk[t] for t in range(t0 // 128, (t0 + n) // 128)]
                    for f in range(4):
                        pg_, pu_ = pgu.next()
                        fs = slice(f * 128, (f + 1) * 128)
                        for k in range(KC):
                            self.mm(pg_[:, 0:n], g_[:, k, fs], hT[:, k, t0:t0 + n], k == 0, k == KC - 1, [g_] + hk, [pg_])
                        for k in range(KC):
                            self.mm(pu_[:, 0:n], u_[:, k, fs], hT[:, k, t0:t0 + n], k == 0, k == KC - 1, [u_] + hk, [pu_])
                        s_ = sg.next()
                        self.act(s_[:, 0:n], pg_[:, 0:n], AF.Silu, [pg_], [s_])
                        self.tt("dve", at[:, f, 0:n], s_[:, 0:n], pu_[:, 0:n], ALU.mult, [s_, pu_], [at])
                    for tt_ in range(n // 128):
                        li = t0 // 128 + tt_
                        pa, pb = pyr.next()
                        for hf, p in ((0, pa), (1, pb)):
                            hs = slice(hf * 512, (hf + 1) * 512)
                            for f in range(4):
                                self.mm(p[:, :], at[:, f, tt_ * 128:(tt_ + 1) * 128], d_[:, f, hs], f == 0, f == 3, [at, d_], [p])
                            if e == 0:
                                self.ts("dve", acc[:, li, hs], p[:, :], Wt[:, li, e:e + 1], None, ALU.mult, None, [p, Wtk[li]], [acck[li]])
                            else:
                                self.stt(acc[:, li, hs], p[:, :], Wt[:, li, e:e + 1], acc[:, li, hs], ALU.mult, ALU.add,
                                         [p, Wtk[li], acck[li]], [acck[li]])
            for li, tile in enumerate(tiles):
                isctx = (l == 0 and tile < 2)
                gt = gt_c if isctx else gt_l
                x_ = NR["xt"].next()
                self.dma("sp", x_[:], self.xres[b, tile * 128:(tile + 1) * 128, :], [self.kx[b]], [x_])
                t_ = NR["xn"].next()
                self.tt("dve", t_[:], acc[:, li, :], gt[:], ALU.mult, [acck[li], gt], [t_])
                self.tt("dve", t_[:], t_[:], x_[:], ALU.add, [t_, x_], [t_])
                if l == 0:
                    self.dma("sp", self.xres[b, tile * 128:(tile + 1) * 128, :], t_[:], [t_], [self.kx[b]], semkey=t_)
                    if ("xf0" in self.D_) and b == 0:
                        self.dma("sp", self.D_["xf0"][tile * 128:(tile + 1) * 128, :], t_[:], [t_], [self.kscr], semkey=t_)
                else:
                    self.dma("sp", self.out[b, (tile - 2) * 128:(tile - 1) * 128, :], t_[:], [t_], [self.kscr], semkey=t_)
            return S.flush()

    def rope(self, xin, xout, cos, sin, H, tm, R, W):
        x1, x2 = xin[:, :, :, 0, :], xin[:, :, :, 1, :]
        cb = cos.unsqueeze(1).broadcast_to([128, H, 2, 8])
        sb_ = sin.unsqueeze(1).broadcast_to([128, H, 2, 8])
        t1, t2 = tm
        v1 = t1[:, 0:H * 16].rearrange("p (h a f) -> p h a f", h=H, a=2)
        v2 = t2[:, 0:H * 16].rearrange("p (h a f) -> p h a f", h=H, a=2)
        self.tt("dve", v1, x1, cb, ALU.mult, R, [t1])
        self.tt("dve", v2, x2, sb_, ALU.mult, R, [t2])
        self.tt("dve", xout[:, :, :, 0, :], v1, v2, ALU.subtract, [t1, t2], W)
        self.tt("dve", v1, x2, cb, ALU.mult, R, [t1])
        self.tt("dve", v2, x1, sb_, ALU.mult, R, [t2])
        self.tt("dve", xout[:, :, :, 1, :], v1, v2, ALU.add, [t1, t2], W)

    def stage_mla(self, b):
        I, S = self.I, self.S
        l = 1
        NQT = SEQ // 128
        with ExitStack() as st:
            PB = [self.psb(st) for _ in range(8)]
            A_l, B_l = self.mod_cols(st, l, 0, b)
            A_c, B_c = self.mod_cols(st, l, 0, 4)
            gt_l = self.gate_tile(st, l, 0, b)
            NR = self.norm_res(st, (PB[0], PB[1]))
            win = self.sb(st, [128, KC, 416], BF16, "win")
            self.dma("pool", win[:], I["mla_w_in"].rearrange("(c p) n -> p c n", p=128), [], [win])
            cT = self.sb(st, [128, 3, T], BF16, "cT")
            cTk = S.keys(NT)
            krr = self.sb(st, [128, NT, 32], F32, "krr")
            krk = S.keys(NT)
            sskr = self.sb(st, [128, NT], F32, "sskr")
            ssk = S.keys(NT)
            gk = self.sb(st, [128, 96], F32, "gk")
            gq = self.sb(st, [128, 96], F32, "gq")
            self.dma("sp", gk[:], I["mla_k_qknorm_g"].partition_broadcast(128), [], [gk])
            self.dma("sp", gq[:], I["mla_q_qknorm_g"].partition_broadcast(128), [], [gq])
            self.ts("dve", gq[:], gq[:], float(96 ** -0.5), None, ALU.mult, None, [gq], [gq])
            qng = self.sb(st, [128, 2], F32, "qng")
            kvg = self.sb(st, [128, 1], F32, "kvg")
            self.dma("sp", qng[:], I["mla_q_norm_g"].rearrange("(k p) -> p k", p=128), [], [qng])
            self.dma("sp", kvg[:], I["mla_kv_norm_g"].rearrange("(p o) -> p o", o=1), [], [kvg])
            hTt = self.sbr(st, 2, [128, KC, 128], BF16, "hTt")
            csr = self.sbr(st, 2, [128, 416], F32, "cs")
            cnr = self.sbr(st, 2, [128, 384], BF16, "cn")
            junk2 = self.sb(st, [128, 256], BF16, "junk2")
            s1 = {nm: self.sbr(st, 2, [128, 1], F32, nm) for nm in ("ssq", "sskv", "sdq", "sdkv", "rsq", "rskv")}
            kr1 = self.sbr(st, 2, [128, 32], F32, "kr1")
            cosr = self.sbr(st, 2, [128, 16], F32, "cos")
            sinr = self.sbr(st, 2, [128, 16], F32, "sin")
            rt = (self.sb(st, [128, 64], F32, "rt1"), self.sb(st, [128, 64], F32, "rt2"))
            cost = {}
            for tile in range(NT):
                A, Bc = (A_c, B_c) if tile < 2 else (A_l, B_l)
                hb = hTt.next()
                self.norm_tile(NR, self.xres[b, tile * 128:(tile + 1) * 128, :], [self.kx[b]], A, Bc, hb[:], [hb])
                p = PB[2 + tile % 2]
                for k in range(KC):
                    self.mm(p[:, 0:416], hb[:, k, :], win[:, k, :], k == 0, k == KC - 1, [hb, win], [p])
                cs = csr.next()
                self.cp("act", cs[:], p[:, 0:416], [p], [cs])
                t = {nm: r.next() for nm, r in s1.items()}
                self.act(junk2[:, 0:256], cs[:, 0:256], AF.Square, [cs], [junk2, t["ssq"]], accum_out=t["ssq"][:])
                self.act(junk2[:, 0:128], cs[:, 256:384], AF.Square, [cs], [junk2, t["sskv"]], accum_out=t["sskv"][:])
                self.act(junk2[:, 0:32], cs[:, 384:416], AF.Square, [cs], [junk2, ssk[tile]], accum_out=sskr[:, tile:tile + 1])
                self.act(t["sdq"][:], t["ssq"][:], AF.Sqrt, [t["ssq"], self.epsc], [t["sdq"]], bias=self.epsc[:, 0:1], scale=1.0 / 256)
                self.act(t["sdkv"][:], t["sskv"][:], AF.Sqrt, [t["sskv"], self.epsc], [t["sdkv"]], bias=self.epsc[:, 0:1], scale=1.0 / 128)
                self.recip(t["rsq"][:], t["sdq"][:], [t["sdq"]], [t["rsq"]])
                self.recip(t["rskv"][:], t["sdkv"][:], [t["sdkv"]], [t["rskv"]])
                cn = cnr.next()
                self.act(cn[:, 0:256], cs[:, 0:256], AF.Copy, [cs, t["rsq"]], [cn], scale=t["rsq"][:, 0:1])
                self.act(cn[:, 256:384], cs[:, 256:384], AF.Copy, [cs, t["rskv"]], [cn], scale=t["rskv"][:, 0:1])
                pT = PB[4 + tile % 2]
                pv = pT[:, 0:192].bitcast(BF16).rearrange("p (j t) -> p j t", j=3)
                for j in range(3):
                    self.tr(pv[:, j, :], cn[:, j * 128:(j + 1) * 128], self.identb[:], [cn, self.identb], [pT])
                self.cp("dve", cT[:, :, tile * 128:(tile + 1) * 128], pv, [pT], [cTk[tile]])
                k1 = kr1.next()
                self.tt("dve", k1[:], cs[:, 384:416], gk[:, 64:96], ALU.mult, [cs, gk], [k1])
                if tile < 2:
                    self.cp("dve", krr[:, tile, :], k1[:], [k1], [krk[tile]])
                else:
                    co, si = cosr.next(), sinr.next()
                    self.dma("sp", co[:], I["k_cos"][(tile - 2) * 128:(tile - 1) * 128, :], [], [co])
                    self.dma("sp", si[:], I["k_sin"][(tile - 2) * 128:(tile - 1) * 128, :], [], [si])
                    self.rope(k1[:].rearrange("p (h a g f) -> p h a g f", h=1, a=2, g=2),
                              krr[:, tile, :].rearrange("p (h a g f) -> p h a g f", h=1, a=2, g=2),
                              co[:].rearrange("p (a f) -> p a f", a=2), si[:].rearrange("p (a f) -> p a f", a=2),
                              1, rt, [k1, co, si], [krk[tile]])
            HG = 4
            oat = self.sb(st, [128, NQT, D], BF16, "oat")
            oak = S.keys(NQT)
            QT = self.sb(st, [128, HG, SEQ], BF16, "QT")
            QTk = S.keys(NQT)
            KT = self.sb(st, [128, HG, T], BF16, "KT")
            KTk = S.keys(NT)
            Vx = self.sb(st, [128, NT, HG, 65], BF16, "Vx")
            Vxk = S.keys(NT)
            self.memset("dve", Vx[:], 1.0, Vxk)
            wqf = self.sb(st, [128, 2, 384], F32, "wqf")
            wqb = self.sb(st, [128, 2, 384], BF16, "wqb")
            wkf = self.sb(st, [128, 512], F32, "wkf")
            wkb = self.sb(st, [128, 512], BF16, "wkb")
            kvfr = self.sbr(st, 2, [128, HG, 128], F32, "kvf")
            sqk = self.sb(st, [128, HG, 96], F32, "sqk")
            tmpk = self.sb(st, [128, HG, 96], F32, "tmpk")
            s4 = {nm: self.sbr(st, 2, [128, HG], F32, nm) for nm in ("ssn", "ss", "sd", "rs", "ssq4", "sd4", "rs4")}
            kbr = self.sbr(st, 2, [128, HG, 96], BF16, "kb")
            qfr = self.sbr(st, 2, [128, HG, 96], F32, "qf")
            qnr = self.sbr(st, 2, [128, HG, 96], F32, "qn")
            qbr = self.sbr(st, 2, [128, HG, 96], BF16, "qb")
            ptr_ = self.sbr(st, 3, [128, 512], BF16, "pt")
            recr = self.sbr(st, 4, [128, 1], F32, "rec")
            for hg in range(16 // HG):
                self.dma("sp", wqf[:], I["mla_w_qb"][:, hg * HG * 96:(hg + 1) * HG * 96].rearrange("(k p) n -> p k n", p=128), [], [wqf])
                self.tt("dve", wqb[:], wqf[:], qng[:].unsqueeze(2).broadcast_to([128, 2, HG * 96]), ALU.mult, [wqf, qng], [wqb])
                self.dma("sp", wkf[:], I["mla_w_kvb"][:, hg * HG * 128:(hg + 1) * HG * 128], [], [wkf])
                self.ts("dve", wkb[:], wkf[:], kvg[:, 0:1], None, ALU.mult, None, [wkf, kvg], [wkb])
                for tile in range(NT):
                    ts_ = slice(tile * 128, (tile + 1) * 128)
                    p = PB[tile % 2]
                    self.mm(p[:, :], cT[:, 2, ts_], wkb[:], True, True, [cTk[tile], wkb], [p])
                    kvf = kvfr.next()
                    self.cp("act", kvf[:], p[:, :].rearrange("p (h e) -> p h e", h=HG), [p], [kvf])
                    t = {nm: r.next() for nm, r in s4.items()}
                    self.tt("dve", sqk[:, :, 0:64], kvf[:, :, 0:64], kvf[:, :, 0:64], ALU.mult, [kvf], [sqk])
                    self.red(t["ssn"][:], sqk[:, :, 0:64], ALU.add, [sqk], [t["ssn"]])
                    self.ts("dve", t["ss"][:], t["ssn"][:], sskr[:, tile:tile + 1], None, ALU.add, None, [t["ssn"], ssk[tile]], [t["ss"]])
                    self.act(t["sd"][:], t["ss"][:], AF.Sqrt, [t["ss"], self.epsc], [t["sd"]], bias=self.epsc[:, 0:1], scale=1.0 / 96)
                    self.recip(t["rs"][:], t["sd"][:], [t["sd"]], [t["rs"]])
                    kb = kbr.next()
                    self.tt("dve", tmpk[:, :, 0:64], kvf[:, :, 0:64], t["rs"][:].unsqueeze(2).broadcast_to([128, HG, 64]), ALU.mult,
                            [kvf, t["rs"]], [tmpk])
                    self.tt("dve", kb[:, :, 0:64], tmpk[:, :, 0:64], gk[:, 0:64].unsqueeze(1).broadcast_to([128, HG, 64]), ALU.mult,
                            [tmpk, gk], [kb])
                    self.tt("dve", kb[:, :, 64:96], krr[:, tile, :].unsqueeze(1).broadcast_to([128, HG, 32]),
                            t["rs"][:].unsqueeze(2).broadcast_to([128, HG, 32]), ALU.mult, [krk[tile], t["rs"]], [kb])
                    pk = PB[2 + tile % 2]
                    pkv = pk[:, 0:256].bitcast(BF16).rearrange("p (h t) -> p h t", h=HG)
                    for h in range(HG):
                        self.tr(pkv[0:96, h, :], kb[:, h, :], self.identb[:], [kb, self.identb], [pk])
                    self.cp("act", KT[0:96, :, ts_], pkv[0:96, :, :], [pk], [KTk[tile]])
                    self.cp("dve", Vx[:, tile, :, 0:64], kvf[:, :, 64:128], [kvf], [Vxk[tile]])
                    if tile >= 2:
                        qt_ = tile - 2
                        pq = PB[4 + tile % 2]
                        for k in range(2):
                            self.mm(pq[:, 0:HG * 96], cT[:, k, ts_], wqb[:, k, :], k == 0, k == 1, [cTk[tile], wqb], [pq])
                        qf = qfr.next()
                        self.cp("act", qf[:], pq[:, 0:HG * 96].rearrange("p (h e) -> p h e", h=HG), [pq], [qf])
                        self.tt("dve", sqk[:], qf[:], qf[:], ALU.mult, [qf], [sqk])
                        self.red(t["ssq4"][:], sqk[:], ALU.add, [sqk], [t["ssq4"]])
                        self.act(t["sd4"][:], t["ssq4"][:], AF.Sqrt, [t["ssq4"], self.epsc], [t["sd4"]], bias=self.epsc[:, 0:1], scale=1.0 / 96)
                        self.recip(t["rs4"][:], t["sd4"][:], [t["sd4"]], [t["rs4"]])
                        qn = qnr.next()
                        self.tt("dve", qn[:], qf[:], t["rs4"][:].unsqueeze(2).broadcast_to([128, HG, 96]), ALU.mult, [qf, t["rs4"]], [qn])
                        self.tt("dve", qn[:], qn[:], gq[:].unsqueeze(1).broadcast_to([128, HG, 96]), ALU.mult, [qn, gq], [qn])
                        qb = qbr.next()
                        self.cp("dve", qb[:, :, 0:64], qn[:, :, 0:64], [qn], [qb])
                        co, si = cosr.next(), sinr.next()
                        self.dma("sp", co[:], I["k_cos"][qt_ * 128:(qt_ + 1) * 128, :], [], [co])
                        self.dma("sp", si[:], I["k_sin"][qt_ * 128:(qt_ + 1) * 128, :], [], [si])
                        self.rope(qn[:, :, 64:96].rearrange("p h (a g f) -> p h a g f", a=2, g=2),
                                  qb[:, :, 64:96].rearrange("p h (a g f) -> p h a g f", a=2, g=2),
                                  co[:].rearrange("p (a f) -> p a f", a=2), si[:].rearrange("p (a f) -> p a f", a=2),
                                  HG, rt, [qn, co, si], [qb])
                        pqt = PB[6 + tile % 2]
                        pqv = pqt[:, 0:256].bitcast(BF16).rearrange("p (h t) -> p h t", h=HG)
                        for h in range(HG):
                            self.tr(pqv[0:96, h, :], qb[:, h, :], self.identb[:], [qb, self.identb], [pqt])
                        self.cp("act", QT[0:96, :, qt_ * 128:(qt_ + 1) * 128], pqv[0:96, :, :], [pqt], [QTk[qt_]])
                for h in range(HG):
                    hh = hg * HG + h
                    for qb_ in range(SEQ // 512):
                        po = PB[4:8]
                        qk = [QTk[qb_ * 4 + i] for i in range(4)]
                        for kt in range(NT):
                            ps_ = PB[kt % 3]
                            self.mm(ps_[:, :], KT[0:96, h, kt * 128:(kt + 1) * 128], QT[0:96, h, qb_ * 512:(qb_ + 1) * 512], True, True,
                                    [KTk[kt]] + qk, [ps_])
                            pt = ptr_.next()
                            self.act(pt[:], ps_[:, :], AF.Exp, [ps_], [pt])
                            for q4 in range(4):
                                self.mm(po[q4][:, 0:65], pt[:, q4 * 128:(q4 + 1) * 128], Vx[:, kt, h, :], kt == 0, kt == NT - 1,
                                        [pt, Vxk[kt]], [po[q4]])
                        for q4 in range(4):
                            rec = recr.next()
                            self.recip(rec[:], po[q4][:, 64:65], [po[q4]], [rec])
                            self.ts("dve", oat[:, qb_ * 4 + q4, hh * 64:(hh + 1) * 64], po[q4][:, 0:64], rec[:, 0:1], None, ALU.mult, None,
                                    [po[q4], rec], [oak[qb_ * 4 + q4]])
            wo = self.sb(st, [128, KC, D], BF16, "wo")
            self.dma("pool", wo[:], I["mla_w_out"].rearrange("(c p) n -> p c n", p=128), [], [wo])
            oTr = self.sbr(st, 2, [128, KC, 128], BF16, "oT")
            for qt_ in range(NQT):
                tile = qt_ + 2
                pT = PB[qt_ % 2]
                pv = pT[:, :].bitcast(BF16).rearrange("p (k t) -> p k t", k=KC)
                for k in range(KC):
                    self.tr(pv[:, k, :], oat[:, qt_, k * 128:(k + 1) * 128], self.identb[:], [oak[qt_], self.identb], [pT])
                oT = oTr.next()
                self.cp("act", oT[:], pv, [pT], [oT])
                x_ = NR["xt"].next()
                self.dma("sp", x_[:], self.xres[b, tile * 128:(tile + 1) * 128, :], [self.kx[b]], [x_])
                t_ = NR["xn"].next()
                for hf in range(2):
                    p = PB[2 + hf]
                    hs = slice(hf * 512, (hf + 1) * 512)
                    for k in range(KC):
                        self.mm(p[:, :], oT[:, k, :], wo[:, k, hs], k == 0, k == KC - 1, [oT, wo], [p])
                    self.tt("dve", t_[:, hs], p[:, :], gt_l[:, hs], ALU.mult, [p, gt_l], [t_])
                self.tt("dve", t_[:], t_[:], x_[:], ALU.add, [t_, x_], [t_])
                self.dma("sp", self.xres[b, tile * 128:(tile + 1) * 128, :], t_[:], [t_], [self.kx[b]], semkey=t_)
                if ("xm1" in self.D_) and b == 0:
                    self.dma("sp", self.D_["xm1"][qt_ * 128:(qt_ + 1) * 128, :], t_[:], [t_], [self.kscr], semkey=t_)
            return S.flush()

    def moe_sparse(self, l):
        I, S, NB = self.I, self.S, self.NB
        tiles = [(b, t) for b in range(NB) for t in (range(NT) if l == 0 else range(2, NT))]
        NTL = len(tiles)
        NBLK = 2 * NTL + NEXP
        info = {}
        with ExitStack() as pst:
            d_i = [self.sb(pst, [128, NTL], I32, "d%di" % k) for k in range(2)]
            w_a = [self.sb(pst, [128, NTL], F32, "w%da" % k) for k in range(2)]
            idxw = self.sb(pst, [128, NBLK], I32, "idxw")
            kh2 = S.keys(NTL)
            kxs, kys, kwb = S.key(), S.key(), S.key()
            with ExitStack() as st:
                PB = [self.psb(st) for _ in range(8)]
                for e in range(NEXP):
                    rows = slice(e * 128, (e + 1) * 128)
                    self.dma("pool", self.wgb[rows, :].rearrange("p (k f) -> p k f", k=8),
                             I["moe_w_gate"][l, e].rearrange("(k p) f -> p k f", p=128), [], [kwb])
                    self.dma("pool", self.wub[rows, :].rearrange("p (k f) -> p k f", k=8),
                             I["moe_w_up"][l, e].rearrange("(k p) f -> p k f", p=128), [], [kwb])
                    self.dma("pool", self.wdb[rows, :].rearrange("p (k f) -> p k f", k=4),
                             I["moe_w_down"][l, e].rearrange("(k p) f -> p k f", p=128), [], [kwb])
                Ltri = self.sb(st, [128, 128], F32, "Ltri")
                onesf = self.sb(st, [128, 128], F32, "onesf")
                self.memset("dve", onesf[:], 1.0, [onesf])
                self.memset("pool", Ltri[:], 1.0, [Ltri])
                S.op("pool", lambda e_: e_.affine_select(out=Ltri[:], in_=Ltri[:], pattern=[[1, 128]], compare_op=ALU.is_gt,
                                                         fill=0.0, base=0, channel_multiplier=-1), [Ltri], [Ltri])
                jvi = self.sb(st, [128, NBLK], I32, "jvi")
                jv = self.sb(st, [128, NBLK], F32, "jv")
                S.op("pool", lambda e_: e_.iota(jvi[:], pattern=[[128, NBLK]], base=0, channel_multiplier=0), [], [jvi])
                self.cp("dve", jv[:], jvi[:], [jvi], [jv])
                pii = self.sb(st, [128, 1], I32, "pii")
                pif = self.sb(st, [128, 1], F32, "pif")
                S.op("pool", lambda e_: e_.iota(pii[:], pattern=[[0, 1]], base=0, channel_multiplier=1), [], [pii])
                self.cp("dve", pif[:], pii[:], [pii], [pif])
                ones32 = self.sb(st, [128, NEXP], F32, "ones32")
                self.memset("dve", ones32[:], 1.0, [ones32])
                wr = self.sb(st, [128, KC, 36], F32, "wr")
                self.dma("sp", wr[:, :, 0:4], I["moe_w_group"][l].rearrange("(c p) g -> p c g", p=128), [], [wr])
                self.dma("sp", wr[:, :, 4:36], I["moe_w_expert"][l].rearrange("(c p) g -> p c g", p=128), [], [wr])
                grow = self.sb(st, [128, D], F32, "grow")
                self.dma("sp", grow[:], I["norm_ffn_g"][l, :].partition_broadcast(128), [], [grow])

                def rows_for(r):
                    Ar = self.sb(st, [128, D], F32, "Arow")
                    Br = self.sb(st, [128, D], F32, "Brow")
                    return Ar, Br

                def load_rows(Ar, Br, r):
                    self.dma("sp", Ar[:], self.mod[l, r, 4 * D:5 * D].partition_broadcast(128), [self.kmod], [Ar])
                    self.dma("sp", Br[:], self.mod[l, r, 3 * D:4 * D].partition_broadcast(128), [self.kmod], [Br])
                    self.stt(Ar[:], Ar[:], 1.0, grow[:], ALU.add, ALU.mult, [Ar, grow], [Ar])

                Ar_l, Br_l = rows_for(0)
                if l == 0:
                    Ar_c, Br_c = rows_for(4)
                    load_rows(Ar_c, Br_c, 4)
                    A_c, B_c = self.mod_cols(st, l, 1, 4)
                NR = self.norm_res(st, (PB[0], PB[1]), nhtf=2)
                sm = {nm: self.sbr(st, 2, [128, w], F32, nm) for nm, w in self.ROUTE_TMPS}
                OHs = self.sb(st, [128, NEXP], F32, "OHs")
                self.memset("dve", OHs[:], 0.0, [OHs])
                OHt = self.sbr(st, 2, [128, NEXP], F32, "OHt")
                Rall = self.sb(st, [128, NTL, NEXP], F32, "Rall")
                Rk = S.keys(NTL)
                oha = [self.sb(st, [128, NTL, NEXP], F32, "oh%da" % k) for k in range(2)]
                ohk = [S.keys(NTL) for _ in range(2)]
                t32 = self.sb(st, [128, D], F32, "t32")
                h2r = self.sbr(st, 2, [128, D], BF16, "h2b")
                cur_b = None
                cols = {}
                for ti, (b, tile) in enumerate(tiles):
                    if b != cur_b:
                        cur_b = b
                        load_rows(Ar_l, Br_l, b)
                        cols[b] = self.mod_cols(st, l, 1, b)
                    isctx = (l == 0 and tile < 2)
                    A, Bc = (A_c, B_c) if isctx else cols[b]
                    Ar, Br = (Ar_c, Br_c) if isctx else (Ar_l, Br_l)
                    hTf = self.norm_tile(NR, self.xres[b, tile * 128:(tile + 1) * 128, :], [self.kx[b]], A, Bc, None, [])
                    xn = self.last_xn
                    self.tt("dve", t32[:], xn[:], Ar[:], ALU.mult, [xn, Ar], [t32])
                    h2b = h2r.next()
                    self.tt("dve", h2b[:], t32[:], Br[:], ALU.add, [t32, Br], [h2b])
                    self.dma("sp", self.h2d[ti * 128:(ti + 1) * 128, :], h2b[:], [h2b], [kh2[ti]], semkey=h2b)
                    p = PB[2 + ti % 2]
                    for k in range(KC):
                        self.mm(p[:, 0:36], hTf[:, k, :], wr[:, k, :], k == 0, k == KC - 1, [hTf, wr], [p])
                    t = self.route_tile(sm, p)
                    self.cp("dve", oha[0][:, ti, :], t["oh1"][:], [t["oh1"]], [ohk[0][ti]])
                    self.cp("dve", oha[1][:, ti, :], t["oh2"][:], [t["oh2"]], [ohk[1][ti]])
                    self.cp("dve", w_a[0][:, ti:ti + 1], t["w1"][:], [t["w1"]], [w_a[0]])
                    self.cp("dve", w_a[1][:, ti:ti + 1], t["w2"][:], [t["w2"]], [w_a[1]])
                    oh = OHt.next()
                    self.tt("dve", oh[:], t["oh1"][:], t["oh2"][:], ALU.add, [t["oh1"], t["oh2"]], [oh])
                    pr = PB[4 + ti % 2]
                    self.mm(pr[:, 0:NEXP], Ltri[:], oh[:], True, False, [Ltri, oh], [pr])
                    self.mm(pr[:, 0:NEXP], onesf[:], OHs[:], False, True, [onesf, OHs], [pr])
                    self.cp("act", Rall[:, ti, :], pr[:, 0:NEXP], [pr], [Rk[ti]])
                    self.tt("dve", OHs[:], OHs[:], oh[:], ALU.add, [OHs, oh], [OHs])
                pc = PB[6]
                self.mm(pc[:, 0:NEXP], onesf[:], OHs[:], True, True, [onesf, OHs], [pc])
                cntf = self.sb(st, [128, NEXP], F32, "cntf")
                padf = self.sb(st, [128, NEXP], F32, "padf")
                pend = self.sb(st, [128, NEXP], F32, "pend")
                pstart = self.sb(st, [128, NEXP], F32, "pstart")
                cmpb = self.sb(st, [128, NBLK * NEXP], BF16, "cmpb")
                self.cp("dve", cntf[:], pc[:, 0:NEXP], [pc], [cntf])
                cv = cmpb[:].rearrange("p (e j) -> p e j", e=NEXP)
                self.tt("dve", cv, jv[:].unsqueeze(1).broadcast_to([128, NEXP, NBLK]),
                        cntf[:].unsqueeze(2).broadcast_to([128, NEXP, NBLK]), ALU.is_lt, [jv, cntf], [cmpb])
                self.red(padf[:], cv, ALU.add, [cmpb], [padf])
                self.ts("dve", padf[:], padf[:], 128.0, None, ALU.mult, None, [padf], [padf])
                S.op("dve", lambda e_: e_.tensor_tensor_scan(out=pend[:], data0=ones32[:], data1=padf[:], initial=0.0,
                                                             op0=ALU.mult, op1=ALU.add), [ones32, padf], [pend])
                self.tt("dve", pstart[:], pend[:], padf[:], ALU.subtract, [pend, padf], [pstart])
                bef = self.sb(st, [128, NBLK], F32, "bef")
                cv2 = cmpb[:].rearrange("p (j e) -> p j e", e=NEXP)
                self.tt("dve", cv2, pend[:].unsqueeze(1).broadcast_to([128, NBLK, NEXP]),
                        jv[:].unsqueeze(2).broadcast_to([128, NBLK, NEXP]), ALU.is_le, [pend, jv], [cmpb])
                self.red(bef[:], cv2, ALU.add, [cmpb], [bef])
                self.ts("dve", bef[:], bef[:], float(NEXP - 1), None, ALU.min, None, [bef], [bef])
                self.ts("dve", bef[:], bef[:], 128.0, pif[:, 0:1], ALU.mult, ALU.add, [bef, pif], [bef])
                self.cp("dve", idxw[:], bef[:], [bef], [idxw])
                dtmp = self.sbr(st, 2, [128, NEXP], F32, "dtmp")
                dtm2 = self.sbr(st, 2, [128, NEXP], F32, "dtm2")
                dfl = self.sbr(st, 2, [128, 1], F32, "dfl")
                for ti in range(NTL):
                    h2b = h2r.next()
                    self.dma("sp", h2b[:], self.h2d[ti * 128:(ti + 1) * 128, :], [kh2[ti]], [h2b])
                    d1 = dtmp.next()
                    self.tt("dve", d1[:], Rall[:, ti, :], pstart[:], ALU.add, [Rk[ti], pstart], [d1])
                    for k in range(2):
                        d2, df = dtm2.next(), dfl.next()
                        self.tt("dve", d2[:], d1[:], oha[k][:, ti, :], ALU.mult, [d1, ohk[k][ti]], [d2])
                        self.red(df[:], d2[:], ALU.add, [d2], [df])
                        self.cp("dve", d_i[k][:, ti:ti + 1], df[:], [df], [d_i[k]])
                        idx_ap = d_i[k][:, ti:ti + 1]
                        self._scatter(self.xs[:, :], idx_ap, h2b[:], [h2b, d_i[k]], [kxs])
                info["rs"] = S.flush()
            with ExitStack() as st:
                PB = [self.psb(st) for _ in range(8)]
                xbr = self.sbr(st, 2, [128, D], BF16, "xb")
                xTr = self.sbr(st, 2, [128, KC, 128], BF16, "xT")
                wgr = self.sbr(st, 2, [128, 4096], BF16, "wgs")
                wur = self.sbr(st, 2, [128, 4096], BF16, "wus")
                wdr = self.sbr(st, 2, [128, 4096], BF16, "wds")
                sgr = self.sbr(st, 2, [128, 512], BF16, "sgs")
                acr = self.sbr(st, 2, [128, 4, 128], BF16, "acs")
                ysr = self.sbr(st, 2, [128, D], F32, "ysb")
                pgu = Rot([(PB[2], PB[3]), (PB[4], PB[5])])
                for j in range(NBLK):
                    xb = xbr.next()
                    self.dma("sp", xb[:], self.xs[j * 128:(j + 1) * 128, :], [kxs], [xb])
                    wg, wu, wd = wgr.next(), wur.next(), wdr.next()
                    ia = idxw[:, j:j + 1]
                    self._gather(wg[:], self.wgb[:, :], ia, [idxw, kwb], [wg])
                    self._gather(wu[:], self.wub[:, :], ia, [idxw, kwb], [wu])
                    self._gather(wd[:], self.wdb[:, :], ia, [idxw, kwb], [wd])
                    pT = PB[j % 2]
                    pv = pT[:, :].bitcast(BF16).rearrange("p (k t) -> p k t", k=KC)
                    for k in range(KC):
                        self.tr(pv[:, k, :], xb[:, k * 128:(k + 1) * 128], self.identb[:], [xb, self.identb], [pT])
                    xT = xTr.next()
                    self.cp("act", xT[:], pv, [pT], [xT])
                    pg_, pu_ = pgu.next()
                    wgv = wg[:].rearrange("p (k f) -> p k f", k=KC)
                    wuv = wu[:].rearrange("p (k f) -> p k f", k=KC)
                    wdv = wd[:].rearrange("p (k f) -> p k f", k=4)
                    for fc in range(4):
                        fs = slice(fc * 128, (fc + 1) * 128)
                        for k in range(KC):
                            self.mm(pg_[:, fs], wgv[:, k, fs], xT[:, k, :], k == 0, k == KC - 1, [wg, xT], [pg_])
                    for fc in range(4):
                        fs = slice(fc * 128, (fc + 1) * 128)
                        for k in range(KC):
                            self.mm(pu_[:, fs], wuv[:, k, fs], xT[:, k, :], k == 0, k == KC - 1, [wu, xT], [pu_])
                    sg = sgr.next()
                    self.act(sg[:], pg_[:, :], AF.Silu, [pg_], [sg])
                    ac = acr.next()
                    self.tt("dve", ac[:].rearrange("p k t -> p (k t)"), sg[:], pu_[:, :], ALU.mult, [sg, pu_], [ac])
                    ysb = ysr.next()
                    for hf, p in ((0, PB[6]), (1, PB[7])):
                        hs = slice(hf * 512, (hf + 1) * 512)
                        for k in range(4):
                            self.mm(p[:, :], ac[:, k, :], wdv[:, k, hs], k == 0, k == 3, [ac, wd], [p])
                        if hf == 0:
                            self.cp("act", ysb[:, hs], p[:, :], [p], [ysb])
                        else:
                            self.cp("dve", ysb[:, hs], p[:, :], [p], [ysb])
                    self.dma("sp", self.ys[j * 128:(j + 1) * 128, :], ysb[:], [ysb], [kys], semkey=ysb)
                info["e"] = S.flush()
            with ExitStack() as st:
                gts = {}
                y1r = self.sbr(st, 2, [128, D], F32, "y1")
                y2r = self.sbr(st, 2, [128, D], F32, "y2")
                xr = self.sbr(st, 2, [128, D], F32, "xc")
                tr_ = self.sbr(st, 2, [128, D], F32, "tc")
                if l == 0:
                    gts[4] = self.gate_tile(st, l, 1, 4)
                for ti, (b, tile) in enumerate(tiles):
                    if b not in gts:
                        gts[b] = self.gate_tile(st, l, 1, b)
                    isctx = (l == 0 and tile < 2)
                    gt = gts[4] if isctx else gts[b]
                    y1, y2, x_, t_ = y1r.next(), y2r.next(), xr.next(), tr_.next()
                    self._gather(y1[:], self.ys[:, :], d_i[0][:, ti:ti + 1], [d_i[0], kys], [y1])
                    self._gather(y2[:], self.ys[:, :], d_i[1][:, ti:ti + 1], [d_i[1], kys], [y2])
                    self.dma("sp", x_[:], self.xres[b, tile * 128:(tile + 1) * 128, :], [self.kx[b]], [x_])
                    self.ts("dve", t_[:], y1[:], w_a[0][:, ti:ti + 1], None, ALU.mult, None, [y1, w_a[0]], [t_])
                    self.stt(t_[:], y2[:], w_a[1][:, ti:ti + 1], t_[:], ALU.mult, ALU.add, [y2, w_a[1], t_], [t_])
                    self.tt("dve", t_[:], t_[:], gt[:], ALU.mult, [t_, gt], [t_])
                    self.tt("dve", t_[:], t_[:], x_[:], ALU.add, [t_, x_], [t_])
                    if l == 0:
                        self.dma("sp", self.xres[b, tile * 128:(tile + 1) * 128, :], t_[:], [t_], [self.kx[b]], semkey=t_)
                        if ("xf0" in self.D_) and b == 0:
                            self.dma("sp", self.D_["xf0"][tile * 128:(tile + 1) * 128, :], t_[:], [t_], [self.kscr], semkey=t_)
                    else:
                        self.dma("sp", self.out[b, (tile - 2) * 128:(tile - 1) * 128, :], t_[:], [t_], [self.kscr], semkey=t_)
                info["c"] = S.flush()
        return info

    def _gather(self, out, src, idx_ap, R, W):
        nrow = src.shape[0]
        self.S.dma("pool", lambda e: e.indirect_dma_start(out=out, out_offset=None, in_=src,
                                                          in_offset=bass.IndirectOffsetOnAxis(ap=idx_ap, axis=0)), R, W)

    def _scatter(self, dst, idx_ap, in_, R, W):
        nrow = dst.shape[0]
        self.S.dma("pool", lambda e: e.indirect_dma_start(out=dst, out_offset=bass.IndirectOffsetOnAxis(ap=idx_ap, axis=0),
                                                          in_=in_, in_offset=None),
                   R, W, semkey=R[0])

    def build(self):
        with ExitStack() as gst:
            gst.enter_context(self.nc.allow_non_contiguous_dma(reason="small strided parameter loads"))
            self.setup_consts(gst)
            info = {}
            if "mod" in self.stages:
                info["mod"] = self.stage_mod()
            for b in range(self.NB):
                if "hgrn" in self.stages:
                    info["hgrn%d" % b] = self.stage_hgrn(b)
            if "moe0" in self.stages:
                info["moe0"] = self.moe_sparse(0)
            for b in range(self.NB):
                if "mla" in self.stages:
                    info["mla%d" % b] = self.stage_mla(b)
            if "moe1" in self.stages:
                info["moe1"] = self.moe_sparse(1)
            self.info = info
        self.S.close()
        return self.nc


def host_consts():
    s = np.arange(128)
    same = (s[:, None] // 32) == (s[None, :] // 32)
    maskf = (same & (s[:, None] <= s[None, :])).astype(np.float32)
    maskb = (same & (s[:, None] >= s[None, :])).astype(np.float32)
    bm = ((s[:, None] // 32) == np.arange(4)[None, :]).astype(np.float32)[:, :, None].repeat(128, axis=2)
    t = np.arange(SEQ)
    row, col = t // 64, t % 64
    inv = (10000.0 ** (-np.arange(0, 16, 2, dtype=np.float32) / 16)).astype(np.float32)
    ang = np.stack([row, col], axis=-1).astype(np.float32)[..., None] * inv
    cos = np.cos(ang).astype(np.float32).reshape(SEQ, 16)
    sin = np.sin(ang).astype(np.float32).reshape(SEQ, 16)
    return {"k_maskf": maskf, "k_maskb": maskb, "k_bm": np.ascontiguousarray(bm), "k_cos": cos, "k_sin": sin}


def make_in_maps(inputs, NB, ncores, used=None):
    sq = {"hg_w_in": "hg_w_in", "hg_lower_bounds": "hg_lb", "hg_out_norm_g": "hg_out_norm_g", "hg_w_out": "hg_w_out",
          "mla_w_in": "mla_w_in", "mla_q_norm_g": "mla_q_norm_g", "mla_kv_norm_g": "mla_kv_norm_g", "mla_w_qb": "mla_w_qb",
          "mla_w_kvb": "mla_w_kvb", "mla_q_qknorm_g": "mla_q_qknorm_g", "mla_k_qknorm_g": "mla_k_qknorm_g", "mla_w_out": "mla_w_out"}
    shared = {}
    for k, v in inputs.items():
        v = np.asarray(v, dtype=np.float32)
        if k in ("x", "c", "ctx"):
            continue
        if k == "hg_lower_bounds":
            shared["hg_lb"] = np.ascontiguousarray(v)
        elif k in sq:
            shared[sq[k]] = np.ascontiguousarray(v.reshape(v.shape[1:]))
        else:
            shared[k] = np.ascontiguousarray(v)
    shared.update(host_consts())
    maps = []
    for i in range(ncores):
        m = dict(shared)
        for k in ("x", "c", "ctx"):
            m[k] = np.ascontiguousarray(np.asarray(inputs[k], dtype=np.float32)[i * NB:(i + 1) * NB])
        if used is not None:
            m = {k: v for k, v in m.items() if k in used}
        maps.append(m)
    return maps


def kernel(**inputs):
    NB = 4
    kb = KB(NB=NB)
    nc = kb.build()
    maps = make_in_maps(inputs, NB, 8, used=set(kb.I.keys()))
    res = run_bass_kernel_spmd(nc, maps, core_ids=list(range(8)))
    return np.concatenate([r["out"] for r in res.results], axis=0).astype(np.float32)
```

```python
import numpy as np
import concourse.bass as bass
import concourse.mybir as mybir
from concourse.bass_utils import run_bass_kernel_spmd
from contextlib import ExitStack

F32 = mybir.dt.float32
BF16 = mybir.dt.bfloat16
I32 = mybir.dt.int32
AF = mybir.ActivationFunctionType
ALU = mybir.AluOpType
AX = mybir.AxisListType

ENGS = ("pe", "act", "dve", "pool", "sp")

D = 1024
KC = 8
CTX = 256
SEQ = 2048
T = CTX + SEQ
NT = T // 128
EPS = 1e-6
NEXP = 32
FF = 512
BIG = 1.0e30


class Key:
    __slots__ = ("w", "r", "dsem", "dcnt")

    def __init__(self):
        self.w = None
        self.r = []
        self.dsem = None
        self.dcnt = 0


class Tl:
    __slots__ = ("t", "k")

    def __init__(self, t, k):
        self.t = t
        self.k = k

    def __getitem__(self, idx):
        return self.t[idx]


def _k(x):
    return x.k if isinstance(x, Tl) else x


class Rot:
    def __init__(self, items):
        self.items = items
        self.i = 0

    def next(self):
        it = self.items[self.i % len(self.items)]
        self.i += 1
        return it


class Sched:
    def __init__(self, nc, n_dma_sems=80):
        self.nc = nc
        self.stack = ExitStack()
        self.esem = {e: self.stack.enter_context(nc.semaphore("es_" + e)) for e in ENGS}
        self.ecnt = {e: 0 for e in ENGS}
        self.dpool = [[self.stack.enter_context(nc.semaphore("ds%d" % i)), 0] for i in range(n_dma_sems)]
        self.dfree = list(range(n_dma_sems))
        self.all_keys = []
        self.reorder = True
        self._reset_stage()

    def _reset_stage(self):
        self.recs = []
        self.dlast = {}

    def key(self):
        k = Key()
        self.all_keys.append(k)
        return k

    def keys(self, n):
        return [self.key() for _ in range(n)]

    def _deps(self, reads, writes):
        deps = set()
        for t in reads:
            if t.w is not None:
                deps.add(t.w)
        for t in writes:
            if t.w is not None:
                deps.add(t.w)
            deps.update(t.r)
        return deps

    def _add(self, eng, fn, reads, writes, cost, dma, lat):
        reads = [_k(x) for x in reads]
        writes = [_k(x) for x in writes]
        deps = self._deps(reads, writes)
        i = len(self.recs)
        if dma is not None:
            prev = self.dlast.get(dma)
            if prev is not None:
                deps.add(prev)
            self.dlast[dma] = i
        self.recs.append({"eng": eng, "fn": fn, "deps": deps, "cost": cost, "dma": dma, "lat": lat, "inc": False})
        for t in reads:
            t.r.append(i)
        for t in writes:
            t.w = i
            t.r = []
        return i

    def op(self, eng, fn, reads=(), writes=(), cost=0.2):
        return self._add(eng, fn, reads, writes, cost, None, 0.0)

    def dma(self, eng, fn, reads=(), writes=(), semkey=None, nbytes=0, indirect=False):
        rk = [_k(x) for x in reads]
        wk = [_k(x) for x in writes]
        sk = _k(semkey) if semkey is not None else (wk[0] if wk else rk[0])
        if sk.dsem is None:
            sk.dsem = self.dfree.pop()
        i = self._add(eng, fn, rk, wk, 0.8 if indirect else 0.07, sk.dsem, 2.0 + nbytes / 150e3)
        return i

    def _schedule(self):
        recs = self.recs
        n = len(recs)
        users = [[] for _ in range(n)]
        ndep = [0] * n
        for i, r in enumerate(recs):
            ndep[i] = len(r["deps"])
            for d in r["deps"]:
                users[d].append(i)
        import heapq
        ready = {e: [] for e in ENGS}
        fin = [0.0] * n
        rt = [0.0] * n
        for i, r in enumerate(recs):
            if ndep[i] == 0:
                heapq.heappush(ready[r["eng"]], (0.0, i))
        free = {e: 0.0 for e in ENGS}
        order = {e: [] for e in ENGS}
        done = 0
        while done < n:
            best = None
            for e in ENGS:
                h = ready[e]
                if not h:
                    continue
                t0 = free[e]
                cand = None
                if h[0][0] <= t0:
                    tmp = []
                    while h and h[0][0] <= t0:
                        tmp.append(heapq.heappop(h))
                    ci = min(tmp, key=lambda x: x[1])
                    for x in tmp:
                        if x is not ci:
                            heapq.heappush(h, x)
                    cand = (t0, ci[1], ci)
                else:
                    x = h[0]
                    cand = (x[0], x[1], None)
                if best is None or (cand[0], cand[1]) < (best[0][0], best[0][1]):
                    if best is not None and best[0][2] is not None:
                        heapq.heappush(ready[best[1]], best[0][2])
                    best = (cand, e)
                elif cand[2] is not None:
                    heapq.heappush(h, cand[2])
            (start, i, popped), e = best
            if popped is None:
                heapq.heappop(ready[e])
            r = recs[i]
            free[e] = start + r["cost"]
            fin[i] = start + r["cost"] + r["lat"]
            order[e].append(i)
            done += 1
            for u in users[i]:
                ndep[u] -= 1
                ru = recs[u]
                lat = 0.05 if ru["eng"] == e else 0.25
                if fin[i] + lat > rt[u]:
                    rt[u] = fin[i] + lat
                if ndep[u] == 0:
                    heapq.heappush(ready[ru["eng"]], (rt[u], u))
        return order, max(fin) if n else 0.0

    def flush(self):
        nc = self.nc
        recs = self.recs
        if self.reorder:
            order, est = self._schedule()
        else:
            order = {e: [i for i, r in enumerate(recs) if r["eng"] == e] for e in ENGS}
            est = 0.0
        dval = {}
        dtot = {}
        for i, r in enumerate(recs):
            if r["dma"] is not None:
                c = self.dpool[r["dma"]][1] + 16
                self.dpool[r["dma"]][1] = c
                dval[i] = c
                dtot[r["dma"]] = c
        for i, r in enumerate(recs):
            for d in r["deps"]:
                rd = recs[d]
                if rd["dma"] is None and (rd["eng"] != r["eng"] or r["eng"] in ("act", "dve", "pool")):
                    rd["inc"] = True
        eval_ = {}
        for e in ENGS:
            c = self.ecnt[e]
            for i in order[e]:
                if recs[i]["inc"]:
                    c += 1
                eval_[i] = c
            self.ecnt[e] = c
        engobj = {"pe": "tensor", "act": "scalar", "dve": "vector", "pool": "gpsimd", "sp": "sync"}
        esem, dpool = self.esem, self.dpool

        def mk(e):
            def body(eng):
                seen = {}
                for i in order[e]:
                    r = recs[i]
                    waits = {}
                    for d in r["deps"]:
                        rd = recs[d]
                        if rd["dma"] is not None:
                            k, v = ("d", rd["dma"]), dval[d]
                        elif rd["eng"] != e or e in ("act", "dve", "pool"):
                            k, v = ("e", rd["eng"]), eval_[d]
                        else:
                            continue
                        if seen.get(k, -1) >= v:
                            continue
                        if waits.get(k, -1) < v:
                            waits[k] = v
                    for k, v in waits.items():
                        seen[k] = v
                        if k[0] == "e":
                            eng.wait_ge(esem[k[1]], v)
                        else:
                            eng.wait_ge(dpool[k[1]][0], v)
                    ins = r["fn"](eng)
                    if r["dma"] is not None:
                        ins.then_inc(dpool[r["dma"]][0], 16)
                    elif r["inc"]:
                        ins.then_inc(esem[e], 1)
                if e == "sp":
                    for idx, v in dtot.items():
                        if seen.get(("d", idx), -1) < v:
                            eng.wait_ge(dpool[idx][0], v)
            return body

        with nc.Block() as block:
            for e in ENGS:
                if order[e] or (e == "sp" and dtot):
                    getattr(block, engobj[e])(mk(e))
        for k in self.all_keys:
            if k.dsem is not None:
                self.dfree.append(k.dsem)
                k.dsem = None
            k.w = None
            k.r = []
        n = {e: len(order[e]) for e in ENGS}
        n["est_us"] = round(est, 1)
        self._reset_stage()
        return n

    def close(self):
        self.stack.close()


class KB:
    def __init__(self, NB=4, stages=("mod", "hgrn", "moe0", "mla", "moe1"), dbg=()):
        self.NB = NB
        self.stages = stages
        self.dbg = dbg
        nc = bass.Bass("TRN2", target_bir_lowering=False)
        self.nc = nc
        self.S = Sched(nc)
        self.uid = 0
        shapes = {
            "x": (NB, SEQ, D), "c": (NB, D), "ctx": (NB, CTX, D), "c_ctx": (D,),
            "ada_w": (2, D, 6 * D), "ada_b": (2, 6 * D), "norm_mix_g": (2, D), "norm_ffn_g": (2, D),
            "hg_w_in": (D, 5 * D), "hg_lb": (2, 3, D), "hg_out_norm_g": (128,), "hg_w_out": (D, D),
            "mla_w_in": (D, 416), "mla_q_norm_g": (256,), "mla_kv_norm_g": (128,), "mla_w_qb": (256, 1536),
            "mla_w_kvb": (128, 2048), "mla_q_qknorm_g": (96,), "mla_k_qknorm_g": (96,), "mla_w_out": (D, D),
            "moe_w_group": (2, D, 4), "moe_w_expert": (2, D, 32), "moe_w_gate": (2, NEXP, D, FF),
            "moe_w_up": (2, NEXP, D, FF), "moe_w_down": (2, NEXP, FF, D),
            "k_maskf": (128, 128), "k_maskb": (128, 128), "k_bm": (128, 4, 128), "k_cos": (SEQ, 16), "k_sin": (SEQ, 16),
        }

        class LazyIn(dict):
            def __missing__(d_, name):
                ap = nc.dram_tensor(name, list(shapes[name]), F32, kind="ExternalInput").ap()
                d_[name] = ap
                return ap

        I = LazyIn()
        self.I = I
        self.out = nc.dram_tensor("out", [NB, SEQ, D], F32, kind="ExternalOutput").ap()
        self.xres = nc.dram_tensor("xres", [NB, T, D], F32).ap()
        self.mod = nc.dram_tensor("modv", [2, 5, 6 * D], F32).ap()
        self.cT_d = nc.dram_tensor("cT_d", [128, 3, T], BF16).ap()
        self.krr_d = nc.dram_tensor("krr_d", [128, NT, 32], F32).ap()
        self.sskr_d = nc.dram_tensor("sskr_d", [128, NT], F32).ap()
        NTLmax = NB * NT
        self.NBLKmax = 2 * NTLmax + NEXP
        self.h2d = nc.dram_tensor("h2d", [NTLmax * 128, D], BF16).ap()
        self.xs = nc.dram_tensor("xs", [self.NBLKmax * 128, D], BF16).ap()
        self.ys = nc.dram_tensor("ys", [self.NBLKmax * 128, D], F32).ap()
        self.wgb = nc.dram_tensor("wgb", [NEXP * 128, 4096], BF16).ap()
        self.wub = nc.dram_tensor("wub", [NEXP * 128, 4096], BF16).ap()
        self.wdb = nc.dram_tensor("wdb", [NEXP * 128, 4096], BF16).ap()
        self.kx = self.S.keys(NB)
        self.kmod = self.S.key()
        self.kscr = self.S.key()
        self.D_ = {}
        for name, shape in dbg:
            self.D_[name] = nc.dram_tensor("dbg_" + name, list(shape), F32, kind="ExternalOutput").ap()

    def sb(self, st, shape, dt, nm="t"):
        self.uid += 1
        t = st.enter_context(self.nc.sbuf_tensor("%s_%d" % (nm, self.uid), list(shape), dt))
        return Tl(t, self.S.key())

    def sbr(self, st, n, shape, dt, nm="r"):
        return Rot([self.sb(st, shape, dt, nm) for _ in range(n)])

    def psb(self, st, nm="ps"):
        self.uid += 1
        t = st.enter_context(self.nc.psum_tensor("%s_%d" % (nm, self.uid), [128, 512], F32))
        return Tl(t, self.S.key())

    @staticmethod
    def _n(ap):
        n = 1
        for d in ap.shape[1:]:
            n *= d
        return n

    def mm(self, out, lhsT, rhs, start, stop, R, W):
        c = max(64, self._n(out)) / 2400.0 + 0.02
        if rhs.dtype == F32:
            c *= 4
        self.S.op("pe", lambda e: e.matmul(out, lhsT=lhsT, rhs=rhs, start=start, stop=stop), R, W, cost=c)

    def tr(self, out, in_, ident, R, W):
        self.S.op("pe", lambda e: e.transpose(out=out, in_=in_, identity=ident), R, W, cost=0.09)

    def act(self, out, in_, func, R, W, bias=None, scale=None, accum_out=None):
        kw = {}
        if bias is not None:
            kw["bias"] = bias
        if scale is not None:
            kw["scale"] = scale
        if accum_out is not None:
            kw["accum_out"] = accum_out
        self.S.op("act", lambda e: e.activation(out=out, in_=in_, func=func, **kw), R, W, cost=0.22 + self._n(out) / 1200.0)

    def ts(self, eng, out, in0, s1, s2, op0, op1, R, W):
        if op1 is None:
            self.S.op(eng, lambda e: e.tensor_scalar(out=out, in0=in0, scalar1=s1, scalar2=None, op0=op0), R, W, cost=self._c(eng, out))
        else:
            self.S.op(eng, lambda e: e.tensor_scalar(out=out, in0=in0, scalar1=s1, scalar2=s2, op0=op0, op1=op1), R, W, cost=self._c(eng, out))

    def tt(self, eng, out, in0, in1, op, R, W):
        self.S.op(eng, lambda e: e.tensor_tensor(out=out, in0=in0, in1=in1, op=op), R, W, cost=self._c(eng, out, 1.5))

    def stt(self, out, in0, scalar, in1, op0, op1, R, W):
        self.S.op("dve", lambda e: e.scalar_tensor_tensor(out=out, in0=in0, scalar=scalar, in1=in1, op0=op0, op1=op1), R, W,
                  cost=self._c("dve", out, 1.5))

    def cp(self, eng, out, in_, R, W):
        if eng == "act":
            self.S.op("act", lambda e: e.activation(out=out, in_=in_, func=AF.Copy), R, W, cost=0.22 + self._n(out) / 1200.0)
        else:
            self.S.op(eng, lambda e: e.tensor_copy(out=out, in_=in_), R, W, cost=self._c(eng, out))

    def red(self, out, in_, op, R, W, negate=None):
        self.S.op("dve", lambda e: e.tensor_reduce(out=out, in_=in_, axis=AX.X, op=op, negate=negate), R, W, cost=self._c("dve", in_))

    def recip(self, out, in_, R, W):
        self.S.op("dve", lambda e: e.reciprocal(out=out, in_=in_), R, W, cost=self._c("dve", out, 8.0))

    def memset(self, eng, ap, val, W):
        self.S.op(eng, lambda e: e.memset(ap, val), (), W, cost=self._c(eng, ap))

    def _c(self, eng, ap, mult=1.0):
        n = self._n(ap)
        if eng == "pool":
            return 0.25 + n * mult / 500.0
        return 0.1 + n * mult / 960.0

    def dma(self, q, out, in_, R, W, semkey=None):
        nb = out.shape[0] * self._n(out) * 4
        self.S.dma(q, lambda e: e.dma_start(out=out, in_=in_), R, W, semkey=semkey, nbytes=nb)

    def sumsq(self, junk, in_, acc, R, W):
        self.act(junk, in_, AF.Square, R, W, accum_out=acc)

    def setup_consts(self, st):
        nc, S = self.nc, self.S
        self.identf = self.sb(st, [128, 128], F32, "identf")
        self.identb = self.sb(st, [128, 128], BF16, "identb")
        self.onesb = self.sb(st, [128, 128], BF16, "onesb")
        identf = self.identf
        self.memset("pool", identf[:], 0.0, [identf])
        S.op("pool", lambda e: e.affine_select(out=identf[:], in_=identf[:], pattern=[[-1, 128]], compare_op=ALU.not_equal,
                                               fill=1.0, base=0, channel_multiplier=1), [identf], [identf])
        self.cp("dve", self.identb[:], identf[:], [identf], [self.identb])
        self.memset("dve", self.onesb[:], 1.0, [self.onesb])
        self.epsc = self.sb(st, [128, 1], F32, "epsc")
        self.memset("dve", self.epsc[:], EPS, [self.epsc])

    def stage_mod(self):
        I, NB = self.I, self.NB
        with ExitStack() as st:
            cT = self.sb(st, [128, KC, 5], F32, "cT")
            cs = self.sb(st, [128, KC, 5], F32, "cs")
            self.memset("dve", cT[:], 0.0, [cT])
            for r in range(NB):
                self.dma("sp", cT[:, :, r], I["c"][r, :].rearrange("(c p) -> p c", p=128), [], [cT])
            self.dma("sp", cT[:, :, 4], I["c_ctx"].rearrange("(c p) -> p c", p=128), [], [cT])
            self.act(cs[:], cT[:], AF.Silu, [cT], [cs])
            wrot = self.sbr(st, 3, [128, KC, 512], F32, "adaw")
            ps = Rot([self.psb(st) for _ in range(2)])
            for l in range(2):
                bt = self.sb(st, [5, 6 * D], F32, "adab")
                ms = self.sb(st, [5, 6 * D], F32, "modsb")
                self.dma("sp", bt[:], I["ada_b"][l, :].partition_broadcast(5), [], [bt])
                for n in range(12):
                    w = wrot.next()
                    self.dma("sp", w[:], I["ada_w"][l, :, n * 512:(n + 1) * 512].rearrange("(c p) n -> p c n", p=128), [], [w])
                    p = ps.next()
                    for k in range(KC):
                        self.mm(p[0:5, :], cs[:, k, :], w[:, k, :], k == 0, k == KC - 1, [cs, w], [p])
                    self.tt("dve", ms[:, n * 512:(n + 1) * 512], p[0:5, :], bt[:, n * 512:(n + 1) * 512], ALU.add, [p, bt], [ms])
                self.dma("sp", self.mod[l], ms[:], [ms], [self.kmod], semkey=ms)
            return self.S.flush()

    def mod_cols(self, st, l, m, r):
        I = self.I
        g = I["norm_mix_g"] if m == 0 else I["norm_ffn_g"]
        gc = self.sb(st, [128, KC], F32, "gc")
        sc = self.sb(st, [128, KC], F32, "sc")
        sh = self.sb(st, [128, KC], F32, "sh")
        A = self.sb(st, [128, KC], F32, "A")
        self.dma("sp", gc[:], g[l, :].rearrange("(c p) -> p c", p=128), [], [gc])
        self.dma("sp", sc[:], self.mod[l, r, (3 * m + 1) * D:(3 * m + 2) * D].rearrange("(c p) -> p c", p=128), [self.kmod], [sc])
        self.dma("sp", sh[:], self.mod[l, r, (3 * m) * D:(3 * m + 1) * D].rearrange("(c p) -> p c", p=128), [self.kmod], [sh])
        self.stt(A[:], sc[:], 1.0, gc[:], ALU.add, ALU.mult, [sc, gc], [A])
        return A, sh

    def gate_tile(self, st, l, m, r):
        gt = self.sb(st, [128, D], F32, "gt")
        self.dma("sp", gt[:], self.mod[l, r, (3 * m + 2) * D:(3 * m + 3) * D].partition_broadcast(128), [self.kmod], [gt])
        return gt

    def norm_res(self, st, pbanks, junk=None, nxn=1, nhtf=1):
        R = {}
        R["xt"] = self.sbr(st, 2, [128, D], F32, "xt")
        R["xn"] = self.sbr(st, nxn, [128, D], F32, "xn")
        R["junk"] = junk if junk is not None else self.sb(st, [128, D], BF16, "junk")
        R["ss"] = self.sbr(st, 2, [128, 1], F32, "ss")
        R["sd"] = self.sbr(st, 2, [128, 1], F32, "sd")
        R["rs"] = self.sbr(st, 2, [128, 1], F32, "rs")
        R["hTf"] = self.sbr(st, nhtf, [128, KC, 128], F32, "hTf")
        R["pb"] = pbanks
        return R

    def norm_tile(self, R, src_ap, src_keys, A, Bc, dst_ap, dst_keys):
        xt = R["xt"].next()
        xn = R["xn"].next()
        ss = R["ss"].next()
        sd = R["sd"].next()
        rs = R["rs"].next()
        hTf = R["hTf"].next()
        junk = R["junk"]
        pa, pb = R["pb"]
        self.dma("sp", xt[:], src_ap, src_keys, [xt])
        self.sumsq(junk[:, 0:D], xt[:], ss[:], [xt], [junk, ss])
        self.act(sd[:], ss[:], AF.Sqrt, [ss, self.epsc], [sd], bias=self.epsc[:, 0:1], scale=1.0 / D)
        self.recip(rs[:], sd[:], [sd], [rs])
        self.act(xn[:], xt[:], AF.Copy, [xt, rs], [xn], scale=rs[:, 0:1])
        for k in range(KC):
            p = pa if k < 4 else pb
            self.tr(p[:, (k % 4) * 128:(k % 4 + 1) * 128], xn[:, k * 128:(k + 1) * 128], self.identf[:], [xn, self.identf], [p])
        for k in range(KC):
            p = pa if k < 4 else pb
            src = p[:, (k % 4) * 128:(k % 4 + 1) * 128]
            if k % 2 == 0:
                self.ts("dve", hTf[:, k, :], src, A[:, k:k + 1], Bc[:, k:k + 1], ALU.mult, ALU.add, [p, A, Bc], [hTf])
            else:
                self.act(hTf[:, k, :], src, AF.Identity, [p, A, Bc], [hTf], bias=Bc[:, k:k + 1], scale=A[:, k:k + 1])
        if dst_ap is not None:
            self.cp("dve", dst_ap, hTf[:], [hTf], dst_keys)
        self.last_xn = xn
        return hTf

    def src_l0(self, b, tile):
        if tile < 2:
            return self.I["ctx"][b, tile * 128:(tile + 1) * 128, :]
        return self.I["x"][b, (tile - 2) * 128:(tile - 1) * 128, :]

    def stage_hgrn(self, b):
        I, S = self.I, self.S
        l = 0
        with ExitStack() as st:
            PB = [self.psb(st) for _ in range(8)]
            hT = self.sb(st, [128, KC, T], BF16, "hT")
            hTk = S.keys(NT)
            ogT = self.sb(st, [128, KC, T], BF16, "ogT")
            ogk = S.keys(KC)
            maskf = self.sb(st, [128, 128], F32, "maskf")
            maskb = self.sb(st, [128, 128], F32, "maskb")
            bm = self.sb(st, [128, 4, 128], BF16, "bm")
            self.dma("sp", maskf[:], I["k_maskf"], [], [maskf])
            self.dma("sp", maskb[:], I["k_maskb"], [], [maskb])
            self.dma("pool", bm[:], I["k_bm"], [], [bm])
            m01 = self.sb(st, [128, T], BF16, "m01")
            self.memset("dve", m01[:], 1.0, [m01])
            self.memset("dve", m01[:, 0:T:32], 0.0, [m01])
            lbr = self.sb(st, [128, 2, 3, KC], F32, "lbr")
            with self.nc.allow_non_contiguous_dma(reason="tiny"):
                for d_ in range(2):
                    for j in range(3):
                        self.dma("sp", lbr[:, d_, j, :], I["hg_lb"][d_, j, :].rearrange("(h p) -> p h", p=128), [], [lbr])
            lbe = self.sb(st, [128, 2, 3, KC], F32, "lbe")
            self.act(lbe[:], lbr[:], AF.Exp, [lbr], [lbe])
            lbs = self.sb(st, [128, 2, KC], F32, "lbs")
            self.tt("dve", lbs[:], lbe[:, :, 0, :], lbe[:, :, 1, :], ALU.add, [lbe], [lbs])
            self.tt("dve", lbs[:], lbs[:], lbe[:, :, 2, :], ALU.add, [lbe, lbs], [lbs])
            lbi = self.sb(st, [128, 2, KC], F32, "lbi")
            self.recip(lbi[:], lbs[:], [lbs], [lbi])
            lb = self.sb(st, [128, 2, KC], F32, "lb")
            oml = self.sb(st, [128, 2, KC], F32, "oml")
            self.tt("dve", lb[:], lbe[:, :, 0, :], lbi[:], ALU.mult, [lbe, lbi], [lb])
            self.ts("dve", oml[:], lb[:], -1.0, 1.0, ALU.mult, ALU.add, [lb], [oml])
            ogc = self.sb(st, [128, 1], F32, "ogc")
            self.dma("sp", ogc[:], I["hg_out_norm_g"].rearrange("(p o) -> p o", o=1), [], [ogc])
            A_l, B_l = self.mod_cols(st, l, 0, b)
            A_c, B_c = self.mod_cols(st, l, 0, 4)
            gt_l = self.gate_tile(st, l, 0, b)
            gt_c = self.gate_tile(st, l, 0, 4)
            qdec = self.sb(st, [128, T], BF16, "qdec")
            NR = self.norm_res(st, (PB[0], PB[1]), junk=qdec)
            for tile in range(NT):
                A, Bc = (A_c, B_c) if tile < 2 else (A_l, B_l)
                self.norm_tile(NR, self.src_l0(b, tile), [], A, Bc, hT[:, :, tile * 128:(tile + 1) * 128], [hTk[tile]])
            wh = self.sbr(st, 1, [128, KC, 5, 128], BF16, "wh")
            Vh = self.sb(st, [128, NT, 128], BF16, "Vh")
            qs = self.sb(st, [128, T], BF16, "qs")
            sgate = self.sb(st, [128, T], BF16, "sgate")
            A1 = self.sb(st, [128, T], F32, "A1")
            A2 = self.sb(st, [128, T], F32, "A2")
            A3 = self.sb(st, [128, T], F32, "A3")
            kinc = self.sb(st, [128, T], BF16, "kinc")
            dec = self.sb(st, [128, T // 32], F32, "dec")
            tot = self.sb(st, [128, T // 32], F32, "tot")
            oacc = self.sb(st, [128, T], F32, "oacc")
            oak = S.keys(NT)
            sTm = self.sbr(st, 2, [128, 128], BF16, "sTm")
            kTs = self.sbr(st, 2, [128, 128], BF16, "kTs")
            Vbd = self.sbr(st, 2, [128, 4, 128], BF16, "Vbd")
            KVs = self.sbr(st, 2, [128, 4, 128], F32, "KVs")
            Sst = self.sb(st, [128, 8, 128], F32, "Sst")
            Sstk = S.keys(8)
            Sb = self.sb(st, [128, 8, 128], BF16, "Sb")
            Sbk = S.keys(2)
            pproj = Rot([PB[0], PB[1]])
            psT = Rot([PB[2], PB[3]])
            pkT = PB[4]
            pkTk = S.keys(2)
            pKV = PB[5]
            poT = Rot([PB[6], PB[7]])
            blocks = [(i * 512, 512) for i in range(4)] + [(2048, 256)]

            def proj(whh, sec, blk):
                t0, n = blk
                p = pproj.next()
                tiles = range(t0 // 128, (t0 + n) // 128)
                for k in range(KC):
                    self.mm(p[:, 0:n], whh[:, k, sec, :], hT[:, k, t0:t0 + n], k == 0, k == KC - 1,
                            [whh] + [hTk[t] for t in tiles], [p])
                return p

            for h in range(KC):
                whh = wh.next()
                for sec in range(5):
                    self.dma("pool", whh[:, :, sec, :],
                             I["hg_w_in"][:, sec * D + h * 128: sec * D + (h + 1) * 128].rearrange("(c p) e -> p c e", p=128), [], [whh])
                for tile in range(NT):
                    p = pproj.next()
                    for k in range(KC):
                        self.mm(p[:, 0:128], hT[:, k, tile * 128:(tile + 1) * 128], whh[:, k, 3, :], k == 0, k == KC - 1,
                                [whh, hTk[tile]], [p])
                    self.cp("act", Vh[:, tile, :], p[:, 0:128], [p], [Vh])
                for blk in blocks:
                    t0, n = blk
                    p = proj(whh, 0, blk)
                    self.act(qs[:, t0:t0 + n], p[:, 0:n], AF.Silu, [p], [qs])
                    p = proj(whh, 4, blk)
                    self.act(sgate[:, t0:t0 + n], p[:, 0:n], AF.Silu, [p], [sgate])
                for dr in range(2):
                    for blk in blocks:
                        t0, n = blk
                        p = proj(whh, 1 + dr, blk)
                        self.act(A1[:, t0:t0 + n], p[:, 0:n], AF.Sigmoid, [p], [A1])
                    self.ts("dve", A1[:], A1[:], oml[:, dr, h:h + 1], lb[:, dr, h:h + 1], ALU.mult, ALU.add, [A1, oml, lb], [A1])
                    self.act(A2[:], A1[:], AF.Ln, [A1], [A2])
                    self.ts("dve", A1[:], A1[:], -1.0, 1.0, ALU.mult, ALU.add, [A1], [A1])
                    S.op("dve", lambda e: e.tensor_tensor_scan(out=A3[:], data0=m01[:], data1=A2[:], initial=0.0,
                                                                op0=ALU.mult, op1=ALU.add), [m01, A2], [A3], cost=0.1 + 2 * T / 960.0)
                    a3v = A3[:].rearrange("p (j i) -> p j i", i=32)
                    self.cp("dve", tot[:], a3v[:, :, 31], [A3], [tot])
                    self.act(dec[:], tot[:], AF.Exp, [tot], [dec])
                    if dr == 0:
                        barr, free = A3, A2
                    else:
                        a2v = A2[:].rearrange("p (j i) -> p j i", i=32)
                        self.tt("dve", A2[:], A2[:], A3[:], ALU.subtract, [A2, A3], [A2])
                        self.tt("dve", a2v, a2v, tot[:].unsqueeze(2).broadcast_to([128, T // 32, 32]), ALU.add, [A2, tot], [A2])
                        barr, free = A2, A3
                    self.act(free[:], barr[:], AF.Exp, [barr], [free], scale=-1.0)
                    self.act(barr[:], barr[:], AF.Exp, [barr], [barr])
                    self.tt("dve", qdec[:], qs[:], barr[:], ALU.mult, [qs, barr], [qdec])
                    self.tt("dve", kinc[:], A1[:], free[:], ALU.mult, [A1, free], [kinc])
                    order = list(range(NT)) if dr == 0 else [1, 0] + list(range(NT - 1, 1, -1))
                    mask = maskf if dr == 0 else maskb
                    self.memset("dve", Sst[:, 0, :], 0.0, [Sstk[0]])
                    for i, tile in enumerate(order):
                        base = 4 * (i % 2)
                        ts_ = slice(tile * 128, (tile + 1) * 128)
                        ps_ = psT.next()
                        self.mm(ps_[:, 0:128], kinc[:, ts_], qdec[:, ts_], True, True, [kinc, qdec], [ps_])
                        sm = sTm.next()
                        self.tt("dve", sm[:], ps_[:, 0:128], mask[:], ALU.mult, [ps_, mask], [sm])
                        pk_i = i % 2
                        pkv = pkT[:, pk_i * 64:(pk_i + 1) * 64].bitcast(BF16)
                        self.tr(pkv, kinc[:, ts_], self.identb[:], [kinc, self.identb], [pkTk[pk_i]])
                        kt = kTs.next()
                        self.cp("act", kt[:], pkv, [pkTk[pk_i]], [kt])
                        vb = Vbd.next()
                        self.tt("dve", vb[:], Vh[:, tile, :].unsqueeze(1).broadcast_to([128, 4, 128]), bm[:], ALU.mult, [Vh, bm], [vb])
                        self.mm(pKV[:, :], kt[:], vb[:].rearrange("p j v -> p (j v)"), True, True, [kt, vb], [pKV])
                        kv = KVs.next()
                        self.tt("dve", kv[:], pKV[:, :].rearrange("p (j v) -> p j v", j=4),
                                dec[:, tile * 4:(tile + 1) * 4].unsqueeze(2).broadcast_to([128, 4, 128]), ALU.mult, [pKV, dec], [kv])
                        corder = [0, 1, 2, 3] if dr == 0 else [3, 2, 1, 0]
                        for jj, c in enumerate(corder):
                            s_in = base + jj
                            s_out = (base + jj + 1) % 8
                            self.stt(Sst[:, s_out, :], Sst[:, s_in, :], dec[:, tile * 4 + c: tile * 4 + c + 1], kv[:, c, :],
                                     ALU.mult, ALU.add, [Sstk[s_in], dec, kv], [Sstk[s_out]])
                        self.cp("act", Sb[:, base:base + 4, :], Sst[:, base:base + 4, :], [Sstk[base + q_] for q_ in range(4)], [Sbk[i % 2]])
                        po = poT.next()
                        self.mm(po[:, 0:128], Vh[:, tile, :], sm[:], True, False, [Vh, sm], [po])
                        for jj, c in enumerate(corder):
                            self.mm(po[:, c * 32:(c + 1) * 32], Sb[:, base + jj, :], qdec[:, tile * 128 + c * 32: tile * 128 + (c + 1) * 32],
                                    False, jj == 3, [Sbk[i % 2], qdec], [po])
                        if dr == 0:
                            self.cp("act", oacc[:, ts_], po[:, 0:128], [po], [oak[tile]])
                        else:
                            self.tt("dve", oacc[:, ts_], oacc[:, ts_], po[:, 0:128], ALU.add, [po, oak[tile]], [oak[tile]])
                self.tt("dve", qdec[:], oacc[:], oacc[:], ALU.mult, oak, [qdec])
                for blk in blocks:
                    t0, n = blk
                    p = pproj.next()
                    self.mm(p[:, 0:n], self.onesb[:], qdec[:, t0:t0 + n], True, True, [self.onesb, qdec], [p])
                    self.act(A2[:, t0:t0 + n], p[:, 0:n], AF.Sqrt, [p, self.epsc], [A2], bias=self.epsc[:, 0:1], scale=1.0 / 128)
                self.recip(A3[:], A2[:], [A2], [A3])
                self.tt("dve", A3[:], A3[:], oacc[:], ALU.mult, [A3] + oak, [A3])
                self.stt(ogT[:, h, :], A3[:], ogc[:, 0:1], sgate[:], ALU.mult, ALU.mult, [A3, ogc, sgate], [ogk[h]])
            wo = self.sb(st, [128, KC, D], BF16, "wo")
            self.dma("pool", wo[:], I["hg_w_out"].rearrange("(c p) n -> p c n", p=128), [], [wo])
            xt2 = NR["xt"]
            tmp = NR["xn"]
            for tile in range(NT):
                gt = gt_c if tile < 2 else gt_l
                x_ = xt2.next()
                self.dma("sp", x_[:], self.src_l0(b, tile), [], [x_])
                t_ = tmp.next()
                for half in range(2):
                    p = pproj.next()
                    hs = slice(half * 512, (half + 1) * 512)
                    for k in range(KC):
                        self.mm(p[:, :], ogT[:, k, tile * 128:(tile + 1) * 128], wo[:, k, hs], k == 0, k == KC - 1, [ogk[k], wo], [p])
                    self.tt("dve", t_[:, hs], p[:, :], gt[:, hs], ALU.mult, [p, gt], [t_])
                self.tt("dve", t_[:], t_[:], x_[:], ALU.add, [t_, x_], [t_])
                self.dma("sp", self.xres[b, tile * 128:(tile + 1) * 128, :], t_[:], [t_], [self.kx[b]], semkey=t_)
                if ("xm0" in self.D_) and b == 0:
                    self.dma("sp", self.D_["xm0"][tile * 128:(tile + 1) * 128, :], t_[:], [t_], [self.kscr], semkey=t_)
            return S.flush()

    ROUTE_TMPS = (("lg", 36), ("gmax", 1), ("ngmax", 1), ("ge", 4), ("gsum", 1), ("pg", 1), ("gone", 4), ("pen", 4),
                  ("em", 32), ("m1", 1), ("oh1", 32), ("em2", 32), ("m2", 1), ("oh2", 32), ("dm", 1), ("e2", 1),
                  ("den", 1), ("rden", 1), ("w1", 1), ("w2", 1), ("tmpw", 32))

    def route_tile(self, sm, p):
        t = {nm: r.next() for nm, r in sm.items()}
        lg = t["lg"]
        self.cp("act", lg[:], p[:, 0:36], [p], [lg])
        self.red(t["gmax"][:], lg[:, 0:4], ALU.max, [lg], [t["gmax"]])
        self.ts("dve", t["ngmax"][:], t["gmax"][:], -1.0, None, ALU.mult, None, [t["gmax"]], [t["ngmax"]])
        self.act(t["ge"][:], lg[:, 0:4], AF.Exp, [lg, t["ngmax"]], [t["ge"], t["gsum"]], bias=t["ngmax"][:, 0:1], accum_out=t["gsum"][:])
        self.recip(t["pg"][:], t["gsum"][:], [t["gsum"]], [t["pg"]])
        self.ts("dve", t["gone"][:], lg[:, 0:4], t["gmax"][:, 0:1], None, ALU.is_ge, None, [lg, t["gmax"]], [t["gone"]])
        self.ts("dve", t["pen"][:], t["gone"][:], BIG, -BIG, ALU.mult, ALU.add, [t["gone"]], [t["pen"]])
        self.tt("dve", t["em"][:].rearrange("p (g j) -> p g j", g=4), lg[:, 4:36].rearrange("p (g j) -> p g j", g=4),
                t["pen"][:].unsqueeze(2).broadcast_to([128, 4, 8]), ALU.add, [lg, t["pen"]], [t["em"]])
        self.red(t["m1"][:], t["em"][:], ALU.max, [t["em"]], [t["m1"]])
        self.ts("dve", t["oh1"][:], t["em"][:], t["m1"][:, 0:1], None, ALU.is_ge, None, [t["em"], t["m1"]], [t["oh1"]])
        self.stt(t["em2"][:], t["oh1"][:], -BIG, t["em"][:], ALU.mult, ALU.add, [t["oh1"], t["em"]], [t["em2"]])
        self.red(t["m2"][:], t["em2"][:], ALU.max, [t["em2"]], [t["m2"]])
        self.ts("dve", t["oh2"][:], t["em2"][:], t["m2"][:, 0:1], None, ALU.is_ge, None, [t["em2"], t["m2"]], [t["oh2"]])
        self.tt("dve", t["dm"][:], t["m2"][:], t["m1"][:], ALU.subtract, [t["m2"], t["m1"]], [t["dm"]])
        self.act(t["e2"][:], t["dm"][:], AF.Exp, [t["dm"]], [t["e2"]])
        self.ts("dve", t["den"][:], t["e2"][:], 1.0, None, ALU.add, None, [t["e2"]], [t["den"]])
        self.recip(t["rden"][:], t["den"][:], [t["den"]], [t["rden"]])
        self.tt("dve", t["w1"][:], t["pg"][:], t["rden"][:], ALU.mult, [t["pg"], t["rden"]], [t["w1"]])
        self.tt("dve", t["w2"][:], t["w1"][:], t["e2"][:], ALU.mult, [t["w1"], t["e2"]], [t["w2"]])
        return t

    def stage_moe(self, l, b, half):
        I, S = self.I, self.S
        if l == 0:
            tiles = list(range(0, 9)) if half == 0 else list(range(9, 18))
        else:
            tiles = list(range(2, 10)) if half == 0 else list(range(10, 18))
        ntl = len(tiles)
        NTOK = ntl * 128
        with ExitStack() as st:
            PB = [self.psb(st) for _ in range(8)]
            hT = self.sb(st, [128, KC, NTOK], BF16, "hT")
            hTk = S.keys(ntl)
            acc = self.sb(st, [128, ntl, D], F32, "acc")
            acck = S.keys(ntl)
            Wt = self.sb(st, [128, ntl, NEXP], F32, "Wt")
            Wtk = S.keys(ntl)
            wr = self.sb(st, [128, KC, 36], F32, "wr")
            self.dma("sp", wr[:, :, 0:4], I["moe_w_group"][l].rearrange("(c p) g -> p c g", p=128), [], [wr])
            self.dma("sp", wr[:, :, 4:36], I["moe_w_expert"][l].rearrange("(c p) g -> p c g", p=128), [], [wr])
            A_l, B_l = self.mod_cols(st, l, 1, b)
            gt_l = self.gate_tile(st, l, 1, b)
            if l == 0 and half == 0:
                A_c, B_c = self.mod_cols(st, l, 1, 4)
                gt_c = self.gate_tile(st, l, 1, 4)
            NR = self.norm_res(st, (PB[0], PB[1]), nhtf=2)
            sm = {}
            for nm, w in (("lg", 36), ("gmax", 1), ("ngmax", 1), ("ge", 4), ("gsum", 1), ("pg", 1), ("gone", 4), ("pen", 4),
                          ("em", 32), ("m1", 1), ("oh1", 32), ("em2", 32), ("m2", 1), ("oh2", 32), ("dm", 1), ("e2", 1),
                          ("den", 1), ("rden", 1), ("w1", 1), ("w2", 1), ("tmpw", 32)):
                sm[nm] = self.sbr(st, 2, [128, w], F32, nm)
            for li, tile in enumerate(tiles):
                isctx = (l == 0 and tile < 2)
                A, Bc = (A_c, B_c) if isctx else (A_l, B_l)
                hTf = self.norm_tile(NR, self.xres[b, tile * 128:(tile + 1) * 128, :], [self.kx[b]], A, Bc,
                                     hT[:, :, li * 128:(li + 1) * 128], [hTk[li]])
                p = PB[2 + li % 2]
                for k in range(KC):
                    self.mm(p[:, 0:36], hTf[:, k, :], wr[:, k, :], k == 0, k == KC - 1, [hTf, wr], [p])
                t = self.route_tile(sm, p)
                self.ts("dve", t["tmpw"][:], t["oh1"][:], t["w1"][:, 0:1], None, ALU.mult, None, [t["oh1"], t["w1"]], [t["tmpw"]])
                self.stt(Wt[:, li, :], t["oh2"][:], t["w2"][:, 0:1], t["tmpw"][:], ALU.mult, ALU.add, [t["oh2"], t["w2"], t["tmpw"]], [Wtk[li]])
            wg = self.sbr(st, 2, [128, KC, FF], BF16, "wg")
            wu = self.sbr(st, 2, [128, KC, FF], BF16, "wu")
            wd = self.sbr(st, 2, [128, 4, D], BF16, "wd")
            sg = self.sbr(st, 2, [128, 512], BF16, "sg")
            actT = self.sbr(st, 2, [128, 4, 512], BF16, "actT")
            pgu = Rot([(PB[0], PB[1]), (PB[2], PB[3])])
            pyr = Rot([(PB[4], PB[5]), (PB[6], PB[7])])
            blocks = []
            t0 = 0
            while t0 < NTOK:
                n = min(512, NTOK - t0)
                blocks.append((t0, n))
                t0 += n
            for e in range(NEXP):
                g_, u_, d_ = wg.next(), wu.next(), wd.next()
                self.dma("pool", g_[:], I["moe_w_gate"][l, e].rearrange("(c p) f -> p c f", p=128), [], [g_])
                self.dma("pool", u_[:], I["moe_w_up"][l, e].rearrange("(c p) f -> p c f", p=128), [], [u_])
                self.dma("pool", d_[:], I["moe_w_down"][l, e].rearrange("(c p) f -> p c f", p=128), [], [d_])
                for (t0, n) in blocks:
                    at = actT.next()
                    hk = [hTk[t] for t in range(t0 // 128, (t0 + n) // 128)]
                    for f in range(4):
                        pg_, pu_ = pgu.next()
                        fs = slice(f * 128, (f + 1) * 128)
                        for k in range(KC):
                            self.mm(pg_[:, 0:n], g_[:, k, fs], hT[:, k, t0:t0 + n], k == 0, k == KC - 1, [g_] + hk, [pg_])
                        for k in range(KC):
                            self.mm(pu_[:, 0:n], u_[:, k, fs], hT[:, k, t0:t0 + n], k == 0, k == KC - 1, [u_] + hk, [pu_])
                        s_ = sg.next()
                        self.act(s_[:, 0:n], pg_[:, 0:n], AF.Silu, [pg_], [s_])
                        self.tt("dve", at[:, f, 0:n], s_[:, 0:n], pu_[:, 0:n], ALU.mult, [s_, pu_], [at])
                    for tt_ in range(n // 128):
                        li = t0 // 128 + tt_
                        pa, pb = pyr.next()
                        for hf, p in ((0, pa), (1, pb)):
                            hs = slice(hf * 512, (hf + 1) * 512)
                            for f in range(4):
                                self.mm(p[:, :], at[:, f, tt_ * 128:(tt_ + 1) * 128], d_[:, f, hs], f == 0, f == 3, [at, d_], [p])
                            if e == 0:
                                self.ts("dve", acc[:, li, hs], p[:, :], Wt[:, li, e:e + 1], None, ALU.mult, None, [p, Wtk[li]], [acck[li]])
                            else:
                                self.stt(acc[:, li, hs], p[:, :], Wt[:, li, e:e + 1], acc[:, li, hs], ALU.mult, ALU.add,
                                         [p, Wtk[li], acck[li]], [acck[li]])
            for li, tile in enumerate(tiles):
                isctx = (l == 0 and tile < 2)
                gt = gt_c if isctx else gt_l
                x_ = NR["xt"].next()
                self.dma("sp", x_[:], self.xres[b, tile * 128:(tile + 1) * 128, :], [self.kx[b]], [x_])
                t_ = NR["xn"].next()
                self.tt("dve", t_[:], acc[:, li, :], gt[:], ALU.mult, [acck[li], gt], [t_])
                self.tt("dve", t_[:], t_[:], x_[:], ALU.add, [t_, x_], [t_])
                if l == 0:
                    self.dma("sp", self.xres[b, tile * 128:(tile + 1) * 128, :], t_[:], [t_], [self.kx[b]], semkey=t_)
                    if ("xf0" in self.D_) and b == 0:
                        self.dma("sp", self.D_["xf0"][tile * 128:(tile + 1) * 128, :], t_[:], [t_], [self.kscr], semkey=t_)
                else:
                    self.dma("sp", self.out[b, (tile - 2) * 128:(tile - 1) * 128, :], t_[:], [t_], [self.kscr], semkey=t_)
            return S.flush()

    def rope(self, xin, xout, cos, sin, H, tm, R, W):
        x1, x2 = xin[:, :, :, 0, :], xin[:, :, :, 1, :]
        cb = cos.unsqueeze(1).broadcast_to([128, H, 2, 8])
        sb_ = sin.unsqueeze(1).broadcast_to([128, H, 2, 8])
        t1, t2 = tm
        v1 = t1[:, 0:H * 16].rearrange("p (h a f) -> p h a f", h=H, a=2)
        v2 = t2[:, 0:H * 16].rearrange("p (h a f) -> p h a f", h=H, a=2)
        self.tt("dve", v1, x1, cb, ALU.mult, R, [t1])
        self.tt("dve", v2, x2, sb_, ALU.mult, R, [t2])
        self.tt("dve", xout[:, :, :, 0, :], v1, v2, ALU.subtract, [t1, t2], W)
        self.tt("dve", v1, x2, cb, ALU.mult, R, [t1])
        self.tt("dve", v2, x1, sb_, ALU.mult, R, [t2])
        self.tt("dve", xout[:, :, :, 1, :], v1, v2, ALU.add, [t1, t2], W)

    def stage_mla(self, b):
        I, S = self.I, self.S
        l = 1
        NQT = SEQ // 128
        with ExitStack() as st:
            PB = [self.psb(st) for _ in range(8)]
            A_l, B_l = self.mod_cols(st, l, 0, b)
            A_c, B_c = self.mod_cols(st, l, 0, 4)
            gt_l = self.gate_tile(st, l, 0, b)
            NR = self.norm_res(st, (PB[0], PB[1]))
            win = self.sb(st, [128, KC, 416], BF16, "win")
            self.dma("pool", win[:], I["mla_w_in"].rearrange("(c p) n -> p c n", p=128), [], [win])
            cT = self.sb(st, [128, 3, T], BF16, "cT")
            cTk = S.keys(NT)
            krr = self.sb(st, [128, NT, 32], F32, "krr")
            krk = S.keys(NT)
            sskr = self.sb(st, [128, NT], F32, "sskr")
            ssk = S.keys(NT)
            gk = self.sb(st, [128, 96], F32, "gk")
            gq = self.sb(st, [128, 96], F32, "gq")
            self.dma("sp", gk[:], I["mla_k_qknorm_g"].partition_broadcast(128), [], [gk])
            self.dma("sp", gq[:], I["mla_q_qknorm_g"].partition_broadcast(128), [], [gq])
            self.ts("dve", gq[:], gq[:], float(96 ** -0.5), None, ALU.mult, None, [gq], [gq])
            qng = self.sb(st, [128, 2], F32, "qng")
            kvg = self.sb(st, [128, 1], F32, "kvg")
            self.dma("sp", qng[:], I["mla_q_norm_g"].rearrange("(k p) -> p k", p=128), [], [qng])
            self.dma("sp", kvg[:], I["mla_kv_norm_g"].rearrange("(p o) -> p o", o=1), [], [kvg])
            hTt = self.sbr(st, 2, [128, KC, 128], BF16, "hTt")
            csr = self.sbr(st, 2, [128, 416], F32, "cs")
            cnr = self.sbr(st, 2, [128, 384], BF16, "cn")
            junk2 = self.sb(st, [128, 256], BF16, "junk2")
            s1 = {nm: self.sbr(st, 2, [128, 1], F32, nm) for nm in ("ssq", "sskv", "sdq", "sdkv", "rsq", "rskv")}
            kr1 = self.sbr(st, 2, [128, 32], F32, "kr1")
            cosr = self.sbr(st, 2, [128, 16], F32, "cos")
            sinr = self.sbr(st, 2, [128, 16], F32, "sin")
            rt = (self.sb(st, [128, 64], F32, "rt1"), self.sb(st, [128, 64], F32, "rt2"))
            cost = {}
            for tile in range(NT):
                A, Bc = (A_c, B_c) if tile < 2 else (A_l, B_l)
                hb = hTt.next()
                self.norm_tile(NR, self.xres[b, tile * 128:(tile + 1) * 128, :], [self.kx[b]], A, Bc, hb[:], [hb])
                p = PB[2 + tile % 2]
                for k in range(KC):
                    self.mm(p[:, 0:416], hb[:, k, :], win[:, k, :], k == 0, k == KC - 1, [hb, win], [p])
                cs = csr.next()
                self.cp("act", cs[:], p[:, 0:416], [p], [cs])
                t = {nm: r.next() for nm, r in s1.items()}
                self.act(junk2[:, 0:256], cs[:, 0:256], AF.Square, [cs], [junk2, t["ssq"]], accum_out=t["ssq"][:])
                self.act(junk2[:, 0:128], cs[:, 256:384], AF.Square, [cs], [junk2, t["sskv"]], accum_out=t["sskv"][:])
                self.act(junk2[:, 0:32], cs[:, 384:416], AF.Square, [cs], [junk2, ssk[tile]], accum_out=sskr[:, tile:tile + 1])
                self.act(t["sdq"][:], t["ssq"][:], AF.Sqrt, [t["ssq"], self.epsc], [t["sdq"]], bias=self.epsc[:, 0:1], scale=1.0 / 256)
                self.act(t["sdkv"][:], t["sskv"][:], AF.Sqrt, [t["sskv"], self.epsc], [t["sdkv"]], bias=self.epsc[:, 0:1], scale=1.0 / 128)
                self.recip(t["rsq"][:], t["sdq"][:], [t["sdq"]], [t["rsq"]])
                self.recip(t["rskv"][:], t["sdkv"][:], [t["sdkv"]], [t["rskv"]])
                cn = cnr.next()
                self.act(cn[:, 0:256], cs[:, 0:256], AF.Copy, [cs, t["rsq"]], [cn], scale=t["rsq"][:, 0:1])
                self.act(cn[:, 256:384], cs[:, 256:384], AF.Copy, [cs, t["rskv"]], [cn], scale=t["rskv"][:, 0:1])
                pT = PB[4 + tile % 2]
                pv = pT[:, 0:192].bitcast(BF16).rearrange("p (j t) -> p j t", j=3)
                for j in range(3):
                    self.tr(pv[:, j, :], cn[:, j * 128:(j + 1) * 128], self.identb[:], [cn, self.identb], [pT])
                self.cp("dve", cT[:, :, tile * 128:(tile + 1) * 128], pv, [pT], [cTk[tile]])
                k1 = kr1.next()
                self.tt("dve", k1[:], cs[:, 384:416], gk[:, 64:96], ALU.mult, [cs, gk], [k1])
                if tile < 2:
                    self.cp("dve", krr[:, tile, :], k1[:], [k1], [krk[tile]])
                else:
                    co, si = cosr.next(), sinr.next()
                    self.dma("sp", co[:], I["k_cos"][(tile - 2) * 128:(tile - 1) * 128, :], [], [co])
                    self.dma("sp", si[:], I["k_sin"][(tile - 2) * 128:(tile - 1) * 128, :], [], [si])
                    self.rope(k1[:].rearrange("p (h a g f) -> p h a g f", h=1, a=2, g=2),
                              krr[:, tile, :].rearrange("p (h a g f) -> p h a g f", h=1, a=2, g=2),
                              co[:].rearrange("p (a f) -> p a f", a=2), si[:].rearrange("p (a f) -> p a f", a=2),
                              1, rt, [k1, co, si], [krk[tile]])
            HG = 4
            oat = self.sb(st, [128, NQT, D], BF16, "oat")
            oak = S.keys(NQT)
            QT = self.sb(st, [128, HG, SEQ], BF16, "QT")
            QTk = S.keys(NQT)
            KT = self.sb(st, [128, HG, T], BF16, "KT")
            KTk = S.keys(NT)
            Vx = self.sb(st, [128, NT, HG, 65], BF16, "Vx")
            Vxk = S.keys(NT)
            self.memset("dve", Vx[:], 1.0, Vxk)
            wqf = self.sb(st, [128, 2, 384], F32, "wqf")
            wqb = self.sb(st, [128, 2, 384], BF16, "wqb")
            wkf = self.sb(st, [128, 512], F32, "wkf")
            wkb = self.sb(st, [128, 512], BF16, "wkb")
            kvfr = self.sbr(st, 2, [128, HG, 128], F32, "kvf")
            sqk = self.sb(st, [128, HG, 96], F32, "sqk")
            tmpk = self.sb(st, [128, HG, 96], F32, "tmpk")
            s4 = {nm: self.sbr(st, 2, [128, HG], F32, nm) for nm in ("ssn", "ss", "sd", "rs", "ssq4", "sd4", "rs4")}
            kbr = self.sbr(st, 2, [128, HG, 96], BF16, "kb")
            qfr = self.sbr(st, 2, [128, HG, 96], F32, "qf")
            qnr = self.sbr(st, 2, [128, HG, 96], F32, "qn")
            qbr = self.sbr(st, 2, [128, HG, 96], BF16, "qb")
            ptr_ = self.sbr(st, 3, [128, 512], BF16, "pt")
            recr = self.sbr(st, 4, [128, 1], F32, "rec")
            for hg in range(16 // HG):
                self.dma("sp", wqf[:], I["mla_w_qb"][:, hg * HG * 96:(hg + 1) * HG * 96].rearrange("(k p) n -> p k n", p=128), [], [wqf])
                self.tt("dve", wqb[:], wqf[:], qng[:].unsqueeze(2).broadcast_to([128, 2, HG * 96]), ALU.mult, [wqf, qng], [wqb])
                self.dma("sp", wkf[:], I["mla_w_kvb"][:, hg * HG * 128:(hg + 1) * HG * 128], [], [wkf])
                self.ts("dve", wkb[:], wkf[:], kvg[:, 0:1], None, ALU.mult, None, [wkf, kvg], [wkb])
                for tile in range(NT):
                    ts_ = slice(tile * 128, (tile + 1) * 128)
                    p = PB[tile % 2]
                    self.mm(p[:, :], cT[:, 2, ts_], wkb[:], True, True, [cTk[tile], wkb], [p])
                    kvf = kvfr.next()
                    self.cp("act", kvf[:], p[:, :].rearrange("p (h e) -> p h e", h=HG), [p], [kvf])
                    t = {nm: r.next() for nm, r in s4.items()}
                    self.tt("dve", sqk[:, :, 0:64], kvf[:, :, 0:64], kvf[:, :, 0:64], ALU.mult, [kvf], [sqk])
                    self.red(t["ssn"][:], sqk[:, :, 0:64], ALU.add, [sqk], [t["ssn"]])
                    self.ts("dve", t["ss"][:], t["ssn"][:], sskr[:, tile:tile + 1], None, ALU.add, None, [t["ssn"], ssk[tile]], [t["ss"]])
                    self.act(t["sd"][:], t["ss"][:], AF.Sqrt, [t["ss"], self.epsc], [t["sd"]], bias=self.epsc[:, 0:1], scale=1.0 / 96)
                    self.recip(t["rs"][:], t["sd"][:], [t["sd"]], [t["rs"]])
                    kb = kbr.next()
                    self.tt("dve", tmpk[:, :, 0:64], kvf[:, :, 0:64], t["rs"][:].unsqueeze(2).broadcast_to([128, HG, 64]), ALU.mult,
                            [kvf, t["rs"]], [tmpk])
                    self.tt("dve", kb[:, :, 0:64], tmpk[:, :, 0:64], gk[:, 0:64].unsqueeze(1).broadcast_to([128, HG, 64]), ALU.mult,
                            [tmpk, gk], [kb])
                    self.tt("dve", kb[:, :, 64:96], krr[:, tile, :].unsqueeze(1).broadcast_to([128, HG, 32]),
                            t["rs"][:].unsqueeze(2).broadcast_to([128, HG, 32]), ALU.mult, [krk[tile], t["rs"]], [kb])
                    pk = PB[2 + tile % 2]
                    pkv = pk[:, 0:256].bitcast(BF16).rearrange("p (h t) -> p h t", h=HG)
                    for h in range(HG):
                        self.tr(pkv[0:96, h, :], kb[:, h, :], self.identb[:], [kb, self.identb], [pk])
                    self.cp("act", KT[0:96, :, ts_], pkv[0:96, :, :], [pk], [KTk[tile]])
                    self.cp("dve", Vx[:, tile, :, 0:64], kvf[:, :, 64:128], [kvf], [Vxk[tile]])
                    if tile >= 2:
                        qt_ = tile - 2
                        pq = PB[4 + tile % 2]
                        for k in range(2):
                            self.mm(pq[:, 0:HG * 96], cT[:, k, ts_], wqb[:, k, :], k == 0, k == 1, [cTk[tile], wqb], [pq])
                        qf = qfr.next()
                        self.cp("act", qf[:], pq[:, 0:HG * 96].rearrange("p (h e) -> p h e", h=HG), [pq], [qf])
                        self.tt("dve", sqk[:], qf[:], qf[:], ALU.mult, [qf], [sqk])
                        self.red(t["ssq4"][:], sqk[:], ALU.add, [sqk], [t["ssq4"]])
                        self.act(t["sd4"][:], t["ssq4"][:], AF.Sqrt, [t["ssq4"], self.epsc], [t["sd4"]], bias=self.epsc[:, 0:1], scale=1.0 / 96)
                        self.recip(t["rs4"][:], t["sd4"][:], [t["sd4"]], [t["rs4"]])
                        qn = qnr.next()
                        self.tt("dve", qn[:], qf[:], t["rs4"][:].unsqueeze(2).broadcast_to([128, HG, 96]), ALU.mult, [qf, t["rs4"]], [qn])
                        self.tt("dve", qn[:], qn[:], gq[:].unsqueeze(1).broadcast_to([128, HG, 96]), ALU.mult, [qn, gq], [qn])
                        qb = qbr.next()
                        self.cp("dve", qb[:, :, 0:64], qn[:, :, 0:64], [qn], [qb])
                        co, si = cosr.next(), sinr.next()
                        self.dma("sp", co[:], I["k_cos"][qt_ * 128:(qt_ + 1) * 128, :], [], [co])
                        self.dma("sp", si[:], I["k_sin"][qt_ * 128:(qt_ + 1) * 128, :], [], [si])
                        self.rope(qn[:, :, 64:96].rearrange("p h (a g f) -> p h a g f", a=2, g=2),
                                  qb[:, :, 64:96].rearrange("p h (a g f) -> p h a g f", a=2, g=2),
                                  co[:].rearrange("p (a f) -> p a f", a=2), si[:].rearrange("p (a f) -> p a f", a=2),
                                  HG, rt, [qn, co, si], [qb])
                        pqt = PB[6 + tile % 2]
                        pqv = pqt[:, 0:256].bitcast(BF16).rearrange("p (h t) -> p h t", h=HG)
                        for h in range(HG):
                            self.tr(pqv[0:96, h, :], qb[:, h, :], self.identb[:], [qb, self.identb], [pqt])
                        self.cp("act", QT[0:96, :, qt_ * 128:(qt_ + 1) * 128], pqv[0:96, :, :], [pqt], [QTk[qt_]])
                for h in range(HG):
                    hh = hg * HG + h
                    for qb_ in range(SEQ // 512):
                        po = PB[4:8]
                        qk = [QTk[qb_ * 4 + i] for i in range(4)]
                        for kt in range(NT):
                            ps_ = PB[kt % 3]
                            self.mm(ps_[:, :], KT[0:96, h, kt * 128:(kt + 1) * 128], QT[0:96, h, qb_ * 512:(qb_ + 1) * 512], True, True,
                                    [KTk[kt]] + qk, [ps_])
                            pt = ptr_.next()
                            self.act(pt[:], ps_[:, :], AF.Exp, [ps_], [pt])
                            for q4 in range(4):
                                self.mm(po[q4][:, 0:65], pt[:, q4 * 128:(q4 + 1) * 128], Vx[:, kt, h, :], kt == 0, kt == NT - 1,
                                        [pt, Vxk[kt]], [po[q4]])
                        for q4 in range(4):
                            rec = recr.next()
                            self.recip(rec[:], po[q4][:, 64:65], [po[q4]], [rec])
                            self.ts("dve", oat[:, qb_ * 4 + q4, hh * 64:(hh + 1) * 64], po[q4][:, 0:64], rec[:, 0:1], None, ALU.mult, None,
                                    [po[q4], rec], [oak[qb_ * 4 + q4]])
            wo = self.sb(st, [128, KC, D], BF16, "wo")
            self.dma("pool", wo[:], I["mla_w_out"].rearrange("(c p) n -> p c n", p=128), [], [wo])
            oTr = self.sbr(st, 2, [128, KC, 128], BF16, "oT")
            for qt_ in range(NQT):
                tile = qt_ + 2
                pT = PB[qt_ % 2]
                pv = pT[:, :].bitcast(BF16).rearrange("p (k t) -> p k t", k=KC)
                for k in range(KC):
                    self.tr(pv[:, k, :], oat[:, qt_, k * 128:(k + 1) * 128], self.identb[:], [oak[qt_], self.identb], [pT])
                oT = oTr.next()
                self.cp("act", oT[:], pv, [pT], [oT])
                x_ = NR["xt"].next()
                self.dma("sp", x_[:], self.xres[b, tile * 128:(tile + 1) * 128, :], [self.kx[b]], [x_])
                t_ = NR["xn"].next()
                for hf in range(2):
                    p = PB[2 + hf]
                    hs = slice(hf * 512, (hf + 1) * 512)
                    for k in range(KC):
                        self.mm(p[:, :], oT[:, k, :], wo[:, k, hs], k == 0, k == KC - 1, [oT, wo], [p])
                    self.tt("dve", t_[:, hs], p[:, :], gt_l[:, hs], ALU.mult, [p, gt_l], [t_])
                self.tt("dve", t_[:], t_[:], x_[:], ALU.add, [t_, x_], [t_])
                self.dma("sp", self.xres[b, tile * 128:(tile + 1) * 128, :], t_[:], [t_], [self.kx[b]], semkey=t_)
                if ("xm1" in self.D_) and b == 0:
                    self.dma("sp", self.D_["xm1"][qt_ * 128:(qt_ + 1) * 128, :], t_[:], [t_], [self.kscr], semkey=t_)
            return S.flush()

    def moe_sparse(self, l):
        I, S, NB = self.I, self.S, self.NB
        tiles = [(b, t) for b in range(NB) for t in (range(NT) if l == 0 else range(2, NT))]
        NTL = len(tiles)
        NBLK = 2 * NTL + NEXP
        info = {}
        with ExitStack() as pst:
            d_i = [self.sb(pst, [128, NTL], I32, "d%di" % k) for k in range(2)]
            w_a = [self.sb(pst, [128, NTL], F32, "w%da" % k) for k in range(2)]
            idxw = self.sb(pst, [128, NBLK], I32, "idxw")
            kh2 = S.keys(NTL)
            kxs, kys, kwb = S.key(), S.key(), S.key()
            with ExitStack() as st:
                PB = [self.psb(st) for _ in range(8)]
                for e in range(NEXP):
                    rows = slice(e * 128, (e + 1) * 128)
                    self.dma("pool", self.wgb[rows, :].rearrange("p (k f) -> p k f", k=8),
                             I["moe_w_gate"][l, e].rearrange("(k p) f -> p k f", p=128), [], [kwb])
                    self.dma("pool", self.wub[rows, :].rearrange("p (k f) -> p k f", k=8),
                             I["moe_w_up"][l, e].rearrange("(k p) f -> p k f", p=128), [], [kwb])
                    self.dma("pool", self.wdb[rows, :].rearrange("p (k f) -> p k f", k=4),
                             I["moe_w_down"][l, e].rearrange("(k p) f -> p k f", p=128), [], [kwb])
                Ltri = self.sb(st, [128, 128], F32, "Ltri")
                onesf = self.sb(st, [128, 128], F32, "onesf")
                self.memset("dve", onesf[:], 1.0, [onesf])
                self.memset("pool", Ltri[:], 1.0, [Ltri])
                S.op("pool", lambda e_: e_.affine_select(out=Ltri[:], in_=Ltri[:], pattern=[[1, 128]], compare_op=ALU.is_gt,
                                                         fill=0.0, base=0, channel_multiplier=-1), [Ltri], [Ltri])
                jvi = self.sb(st, [128, NBLK], I32, "jvi")
                jv = self.sb(st, [128, NBLK], F32, "jv")
                S.op("pool", lambda e_: e_.iota(jvi[:], pattern=[[128, NBLK]], base=0, channel_multiplier=0), [], [jvi])
                self.cp("dve", jv[:], jvi[:], [jvi], [jv])
                pii = self.sb(st, [128, 1], I32, "pii")
                pif = self.sb(st, [128, 1], F32, "pif")
                S.op("pool", lambda e_: e_.iota(pii[:], pattern=[[0, 1]], base=0, channel_multiplier=1), [], [pii])
                self.cp("dve", pif[:], pii[:], [pii], [pif])
                ones32 = self.sb(st, [128, NEXP], F32, "ones32")
                self.memset("dve", ones32[:], 1.0, [ones32])
                wr = self.sb(st, [128, KC, 36], F32, "wr")
                self.dma("sp", wr[:, :, 0:4], I["moe_w_group"][l].rearrange("(c p) g -> p c g", p=128), [], [wr])
                self.dma("sp", wr[:, :, 4:36], I["moe_w_expert"][l].rearrange("(c p) g -> p c g", p=128), [], [wr])
                grow = self.sb(st, [128, D], F32, "grow")
                self.dma("sp", grow[:], I["norm_ffn_g"][l, :].partition_broadcast(128), [], [grow])

                def rows_for(r):
                    Ar = self.sb(st, [128, D], F32, "Arow")
                    Br = self.sb(st, [128, D], F32, "Brow")
                    return Ar, Br

                def load_rows(Ar, Br, r):
                    self.dma("sp", Ar[:], self.mod[l, r, 4 * D:5 * D].partition_broadcast(128), [self.kmod], [Ar])
                    self.dma("sp", Br[:], self.mod[l, r, 3 * D:4 * D].partition_broadcast(128), [self.kmod], [Br])
                    self.stt(Ar[:], Ar[:], 1.0, grow[:], ALU.add, ALU.mult, [Ar, grow], [Ar])

                Ar_l, Br_l = rows_for(0)
                if l == 0:
                    Ar_c, Br_c = rows_for(4)
                    load_rows(Ar_c, Br_c, 4)
                    A_c, B_c = self.mod_cols(st, l, 1, 4)
                NR = self.norm_res(st, (PB[0], PB[1]), nhtf=2)
                sm = {nm: self.sbr(st, 2, [128, w], F32, nm) for nm, w in self.ROUTE_TMPS}
                OHs = self.sb(st, [128, NEXP], F32, "OHs")
                self.memset("dve", OHs[:], 0.0, [OHs])
                OHt = self.sbr(st, 2, [128, NEXP], F32, "OHt")
                Rall = self.sb(st, [128, NTL, NEXP], F32, "Rall")
                Rk = S.keys(NTL)
                oha = [self.sb(st, [128, NTL, NEXP], F32, "oh%da" % k) for k in range(2)]
                ohk = [S.keys(NTL) for _ in range(2)]
                t32 = self.sb(st, [128, D], F32, "t32")
                h2r = self.sbr(st, 2, [128, D], BF16, "h2b")
                cur_b = None
                cols = {}
                for ti, (b, tile) in enumerate(tiles):
                    if b != cur_b:
                        cur_b = b
                        load_rows(Ar_l, Br_l, b)
                        cols[b] = self.mod_cols(st, l, 1, b)
                    isctx = (l == 0 and tile < 2)
                    A, Bc = (A_c, B_c) if isctx else cols[b]
                    Ar, Br = (Ar_c, Br_c) if isctx else (Ar_l, Br_l)
                    hTf = self.norm_tile(NR, self.xres[b, tile * 128:(tile + 1) * 128, :], [self.kx[b]], A, Bc, None, [])
                    xn = self.last_xn
                    self.tt("dve", t32[:], xn[:], Ar[:], ALU.mult, [xn, Ar], [t32])
                    h2b = h2r.next()
                    self.tt("dve", h2b[:], t32[:], Br[:], ALU.add, [t32, Br], [h2b])
                    self.dma("sp", self.h2d[ti * 128:(ti + 1) * 128, :], h2b[:], [h2b], [kh2[ti]], semkey=h2b)
                    p = PB[2 + ti % 2]
                    for k in range(KC):
                        self.mm(p[:, 0:36], hTf[:, k, :], wr[:, k, :], k == 0, k == KC - 1, [hTf, wr], [p])
                    t = self.route_tile(sm, p)
                    self.cp("dve", oha[0][:, ti, :], t["oh1"][:], [t["oh1"]], [ohk[0][ti]])
                    self.cp("dve", oha[1][:, ti, :], t["oh2"][:], [t["oh2"]], [ohk[1][ti]])
                    self.cp("dve", w_a[0][:, ti:ti + 1], t["w1"][:], [t["w1"]], [w_a[0]])
                    self.cp("dve", w_a[1][:, ti:ti + 1], t["w2"][:], [t["w2"]], [w_a[1]])
                    oh = OHt.next()
                    self.tt("dve", oh[:], t["oh1"][:], t["oh2"][:], ALU.add, [t["oh1"], t["oh2"]], [oh])
                    pr = PB[4 + ti % 2]
                    self.mm(pr[:, 0:NEXP], Ltri[:], oh[:], True, False, [Ltri, oh], [pr])
                    self.mm(pr[:, 0:NEXP], onesf[:], OHs[:], False, True, [onesf, OHs], [pr])
                    self.cp("act", Rall[:, ti, :], pr[:, 0:NEXP], [pr], [Rk[ti]])
                    self.tt("dve", OHs[:], OHs[:], oh[:], ALU.add, [OHs, oh], [OHs])
                pc = PB[6]
                self.mm(pc[:, 0:NEXP], onesf[:], OHs[:], True, True, [onesf, OHs], [pc])
                cntf = self.sb(st, [128, NEXP], F32, "cntf")
                padf = self.sb(st, [128, NEXP], F32, "padf")
                pend = self.sb(st, [128, NEXP], F32, "pend")
                pstart = self.sb(st, [128, NEXP], F32, "pstart")
                cmpb = self.sb(st, [128, NBLK * NEXP], BF16, "cmpb")
                self.cp("dve", cntf[:], pc[:, 0:NEXP], [pc], [cntf])
                cv = cmpb[:].rearrange("p (e j) -> p e j", e=NEXP)
                self.tt("dve", cv, jv[:].unsqueeze(1).broadcast_to([128, NEXP, NBLK]),
                        cntf[:].unsqueeze(2).broadcast_to([128, NEXP, NBLK]), ALU.is_lt, [jv, cntf], [cmpb])
                self.red(padf[:], cv, ALU.add, [cmpb], [padf])
                self.ts("dve", padf[:], padf[:], 128.0, None, ALU.mult, None, [padf], [padf])
                S.op("dve", lambda e_: e_.tensor_tensor_scan(out=pend[:], data0=ones32[:], data1=padf[:], initial=0.0,
                                                             op0=ALU.mult, op1=ALU.add), [ones32, padf], [pend])
                self.tt("dve", pstart[:], pend[:], padf[:], ALU.subtract, [pend, padf], [pstart])
                bef = self.sb(st, [128, NBLK], F32, "bef")
                cv2 = cmpb[:].rearrange("p (j e) -> p j e", e=NEXP)
                self.tt("dve", cv2, pend[:].unsqueeze(1).broadcast_to([128, NBLK, NEXP]),
                        jv[:].unsqueeze(2).broadcast_to([128, NBLK, NEXP]), ALU.is_le, [pend, jv], [cmpb])
                self.red(bef[:], cv2, ALU.add, [cmpb], [bef])
                self.ts("dve", bef[:], bef[:], float(NEXP - 1), None, ALU.min, None, [bef], [bef])
                self.ts("dve", bef[:], bef[:], 128.0, pif[:, 0:1], ALU.mult, ALU.add, [bef, pif], [bef])
                self.cp("dve", idxw[:], bef[:], [bef], [idxw])
                dtmp = self.sbr(st, 2, [128, NEXP], F32, "dtmp")
                dtm2 = self.sbr(st, 2, [128, NEXP], F32, "dtm2")
                dfl = self.sbr(st, 2, [128, 1], F32, "dfl")
                for ti in range(NTL):
                    h2b = h2r.next()
                    self.dma("sp", h2b[:], self.h2d[ti * 128:(ti + 1) * 128, :], [kh2[ti]], [h2b])
                    d1 = dtmp.next()
                    self.tt("dve", d1[:], Rall[:, ti, :], pstart[:], ALU.add, [Rk[ti], pstart], [d1])
                    for k in range(2):
                        d2, df = dtm2.next(), dfl.next()
                        self.tt("dve", d2[:], d1[:], oha[k][:, ti, :], ALU.mult, [d1, ohk[k][ti]], [d2])
                        self.red(df[:], d2[:], ALU.add, [d2], [df])
                        self.cp("dve", d_i[k][:, ti:ti + 1], df[:], [df], [d_i[k]])
                        idx_ap = d_i[k][:, ti:ti + 1]
                        self._scatter(self.xs[:, :], idx_ap, h2b[:], [h2b, d_i[k]], [kxs])
                info["rs"] = S.flush()
            with ExitStack() as st:
                PB = [self.psb(st) for _ in range(8)]
                xbr = self.sbr(st, 2, [128, D], BF16, "xb")
                xTr = self.sbr(st, 2, [128, KC, 128], BF16, "xT")
                wgr = self.sbr(st, 2, [128, 4096], BF16, "wgs")
                wur = self.sbr(st, 2, [128, 4096], BF16, "wus")
                wdr = self.sbr(st, 2, [128, 4096], BF16, "wds")
                sgr = self.sbr(st, 2, [128, 512], BF16, "sgs")
                acr = self.sbr(st, 2, [128, 4, 128], BF16, "acs")
                ysr = self.sbr(st, 2, [128, D], F32, "ysb")
                pgu = Rot([(PB[2], PB[3]), (PB[4], PB[5])])
                for j in range(NBLK):
                    xb = xbr.next()
                    self.dma("sp", xb[:], self.xs[j * 128:(j + 1) * 128, :], [kxs], [xb])
                    wg, wu, wd = wgr.next(), wur.next(), wdr.next()
                    ia = idxw[:, j:j + 1]
                    self._gather(wg[:], self.wgb[:, :], ia, [idxw, kwb], [wg])
                    self._gather(wu[:], self.wub[:, :], ia, [idxw, kwb], [wu])
                    self._gather(wd[:], self.wdb[:, :], ia, [idxw, kwb], [wd])
                    pT = PB[j % 2]
                    pv = pT[:, :].bitcast(BF16).rearrange("p (k t) -> p k t", k=KC)
                    for k in range(KC):
                        self.tr(pv[:, k, :], xb[:, k * 128:(k + 1) * 128], self.identb[:], [xb, self.identb], [pT])
                    xT = xTr.next()
                    self.cp("act", xT[:], pv, [pT], [xT])
                    pg_, pu_ = pgu.next()
                    wgv = wg[:].rearrange("p (k f) -> p k f", k=KC)
                    wuv = wu[:].rearrange("p (k f) -> p k f", k=KC)
                    wdv = wd[:].rearrange("p (k f) -> p k f", k=4)
                    for fc in range(4):
                        fs = slice(fc * 128, (fc + 1) * 128)
                        for k in range(KC):
                            self.mm(pg_[:, fs], wgv[:, k, fs], xT[:, k, :], k == 0, k == KC - 1, [wg, xT], [pg_])
                    for fc in range(4):
                        fs = slice(fc * 128, (fc + 1) * 128)
                        for k in range(KC):
                            self.mm(pu_[:, fs], wuv[:, k, fs], xT[:, k, :], k == 0, k == KC - 1, [wu, xT], [pu_])
                    sg = sgr.next()
                    self.act(sg[:], pg_[:, :], AF.Silu, [pg_], [sg])
                    ac = acr.next()
                    self.tt("dve", ac[:].rearrange("p k t -> p (k t)"), sg[:], pu_[:, :], ALU.mult, [sg, pu_], [ac])
                    ysb = ysr.next()
                    for hf, p in ((0, PB[6]), (1, PB[7])):
                        hs = slice(hf * 512, (hf + 1) * 512)
                        for k in range(4):
                            self.mm(p[:, :], ac[:, k, :], wdv[:, k, hs], k == 0, k == 3, [ac, wd], [p])
                        if hf == 0:
                            self.cp("act", ysb[:, hs], p[:, :], [p], [ysb])
                        else:
                            self.cp("dve", ysb[:, hs], p[:, :], [p], [ysb])
                    self.dma("sp", self.ys[j * 128:(j + 1) * 128, :], ysb[:], [ysb], [kys], semkey=ysb)
                info["e"] = S.flush()
            with ExitStack() as st:
                gts = {}
                y1r = self.sbr(st, 2, [128, D], F32, "y1")
                y2r = self.sbr(st, 2, [128, D], F32, "y2")
                xr = self.sbr(st, 2, [128, D], F32, "xc")
                tr_ = self.sbr(st, 2, [128, D], F32, "tc")
                if l == 0:
                    gts[4] = self.gate_tile(st, l, 1, 4)
                for ti, (b, tile) in enumerate(tiles):
                    if b not in gts:
                        gts[b] = self.gate_tile(st, l, 1, b)
                    isctx = (l == 0 and tile < 2)
                    gt = gts[4] if isctx else gts[b]
                    y1, y2, x_, t_ = y1r.next(), y2r.next(), xr.next(), tr_.next()
                    self._gather(y1[:], self.ys[:, :], d_i[0][:, ti:ti + 1], [d_i[0], kys], [y1])
                    self._gather(y2[:], self.ys[:, :], d_i[1][:, ti:ti + 1], [d_i[1], kys], [y2])
                    self.dma("sp", x_[:], self.xres[b, tile * 128:(tile + 1) * 128, :], [self.kx[b]], [x_])
                    self.ts("dve", t_[:], y1[:], w_a[0][:, ti:ti + 1], None, ALU.mult, None, [y1, w_a[0]], [t_])
                    self.stt(t_[:], y2[:], w_a[1][:, ti:ti + 1], t_[:], ALU.mult, ALU.add, [y2, w_a[1], t_], [t_])
                    self.tt("dve", t_[:], t_[:], gt[:], ALU.mult, [t_, gt], [t_])
                    self.tt("dve", t_[:], t_[:], x_[:], ALU.add, [t_, x_], [t_])
                    if l == 0:
                        self.dma("sp", self.xres[b, tile * 128:(tile + 1) * 128, :], t_[:], [t_], [self.kx[b]], semkey=t_)
                        if ("xf0" in self.D_) and b == 0:
                            self.dma("sp", self.D_["xf0"][tile * 128:(tile + 1) * 128, :], t_[:], [t_], [self.kscr], semkey=t_)
                    else:
                        self.dma("sp", self.out[b, (tile - 2) * 128:(tile - 1) * 128, :], t_[:], [t_], [self.kscr], semkey=t_)
                info["c"] = S.flush()
        return info

    def _gather(self, out, src, idx_ap, R, W):
        nrow = src.shape[0]
        self.S.dma("pool", lambda e: e.indirect_dma_start(out=out, out_offset=None, in_=src,
                                                          in_offset=bass.IndirectOffsetOnAxis(ap=idx_ap, axis=0)), R, W,
                   nbytes=128 * self._n(out) * 2, indirect=True)

    def _scatter(self, dst, idx_ap, in_, R, W):
        nrow = dst.shape[0]
        self.S.dma("pool", lambda e: e.indirect_dma_start(out=dst, out_offset=bass.IndirectOffsetOnAxis(ap=idx_ap, axis=0),
                                                          in_=in_, in_offset=None),
                   R, W, semkey=R[0], nbytes=128 * 2048, indirect=True)

    def build(self):
        with ExitStack() as gst:
            gst.enter_context(self.nc.allow_non_contiguous_dma(reason="small strided parameter loads"))
            self.setup_consts(gst)
            info = {}
            if "mod" in self.stages:
                info["mod"] = self.stage_mod()
            for b in range(self.NB):
                if "hgrn" in self.stages:
                    info["hgrn%d" % b] = self.stage_hgrn(b)
            if "moe0" in self.stages:
                info["moe0"] = self.moe_sparse(0)
            for b in range(self.NB):
                if "mla" in self.stages:
                    info["mla%d" % b] = self.stage_mla(b)
            if "moe1" in self.stages:
                info["moe1"] = self.moe_sparse(1)
            self.info = info
        self.S.close()
        return self.nc


def host_consts():
    s = np.arange(128)
    same = (s[:, None] // 32) == (s[None, :] // 32)
    maskf = (same & (s[:, None] <= s[None, :])).astype(np.float32)
    maskb = (same & (s[:, None] >= s[None, :])).astype(np.float32)
    bm = ((s[:, None] // 32) == np.arange(4)[None, :]).astype(np.float32)[:, :, None].repeat(128, axis=2)
    t = np.arange(SEQ)
    row, col = t // 64, t % 64
    inv = (10000.0 ** (-np.arange(0, 16, 2, dtype=np.float32) / 16)).astype(np.float32)
    ang = np.stack([row, col], axis=-1).astype(np.float32)[..., None] * inv
    cos = np.cos(ang).astype(np.float32).reshape(SEQ, 16)
    sin = np.sin(ang).astype(np.float32).reshape(SEQ, 16)
    return {"k_maskf": maskf, "k_maskb": maskb, "k_bm": np.ascontiguousarray(bm), "k_cos": cos, "k_sin": sin}


def make_in_maps(inputs, NB, ncores, used=None):
    sq = {"hg_w_in": "hg_w_in", "hg_lower_bounds": "hg_lb", "hg_out_norm_g": "hg_out_norm_g", "hg_w_out": "hg_w_out",
          "mla_w_in": "mla_w_in", "mla_q_norm_g": "mla_q_norm_g", "mla_kv_norm_g": "mla_kv_norm_g", "mla_w_qb": "mla_w_qb",
          "mla_w_kvb": "mla_w_kvb", "mla_q_qknorm_g": "mla_q_qknorm_g", "mla_k_qknorm_g": "mla_k_qknorm_g", "mla_w_out": "mla_w_out"}
    shared = {}
    for k, v in inputs.items():
        v = np.asarray(v, dtype=np.float32)
        if k in ("x", "c", "ctx"):
            continue
        if k == "hg_lower_bounds":
            shared["hg_lb"] = np.ascontiguousarray(v)
        elif k in sq:
            shared[sq[k]] = np.ascontiguousarray(v.reshape(v.shape[1:]))
        else:
            shared[k] = np.ascontiguousarray(v)
    shared.update(host_consts())
    maps = []
    for i in range(ncores):
        m = dict(shared)
        for k in ("x", "c", "ctx"):
            m[k] = np.ascontiguousarray(np.asarray(inputs[k], dtype=np.float32)[i * NB:(i + 1) * NB])
        if used is not None:
            m = {k: v for k, v in m.items() if k in used}
        maps.append(m)
    return maps


def kernel(**inputs):
    NB = 4
    kb = KB(NB=NB)
    nc = kb.build()
    maps = make_in_maps(inputs, NB, 8, used=set(kb.I.keys()))
    res = run_bass_kernel_spmd(nc, maps, core_ids=list(range(8)))
    return np.concatenate([r["out"] for r in res.results], axis=0).astype(np.float32)
```

```python
import numpy as np
import os as _os
_F = lambda k: _os.environ.get(k, '1') == '1'
import concourse.bass as bass
import concourse.mybir as mybir
from concourse.bass_utils import run_bass_kernel_spmd
from contextlib import ExitStack

F32 = mybir.dt.float32
BF16 = mybir.dt.bfloat16
I32 = mybir.dt.int32
AF = mybir.ActivationFunctionType
ALU = mybir.AluOpType
AX = mybir.AxisListType

ENGS = ("pe", "act", "dve", "pool", "sp")

D = 1024
KC = 8
CTX = 256
SEQ = 2048
T = CTX + SEQ
NT = T // 128
EPS = 1e-6
NEXP = 32
FF = 512
BIG = 1.0e30


class Key:
    __slots__ = ("w", "r", "dsem", "dcnt", "excl")

    def __init__(self):
        self.w = None
        self.r = []
        self.dsem = None
        self.dcnt = 0
        self.excl = False


class Tl:
    __slots__ = ("t", "k")

    def __init__(self, t, k):
        self.t = t
        self.k = k

    def __getitem__(self, idx):
        return self.t[idx]


def _k(x):
    return x.k if isinstance(x, Tl) else x


class Rot:
    def __init__(self, items):
        self.items = items
        self.i = 0

    def next(self):
        it = self.items[self.i % len(self.items)]
        self.i += 1
        return it


class Sched:
    def __init__(self, nc, n_dma_sems=80):
        self.nc = nc
        self.stack = ExitStack()
        self.esem = {e: self.stack.enter_context(nc.semaphore("es_" + e)) for e in ENGS}
        self.ecnt = {e: 0 for e in ENGS}
        self.dpool = [[self.stack.enter_context(nc.semaphore("ds%d" % i)), 0] for i in range(n_dma_sems)]
        self.dfree = list(range(n_dma_sems))
        self.all_keys = []
        self.reorder = True
        self._reset_stage()

    def _reset_stage(self):
        self.recs = []
        self.dlast = {}

    def key(self):
        k = Key()
        self.all_keys.append(k)
        return k

    def keys(self, n):
        return [self.key() for _ in range(n)]

    def _deps(self, reads, writes):
        deps = set()
        for t in reads:
            if t.w is not None:
                deps.add(t.w)
        for t in writes:
            if t.w is not None:
                deps.add(t.w)
            deps.update(t.r)
        return deps

    def _add(self, eng, fn, reads, writes, cost, dma, lat):
        reads = [_k(x) for x in reads]
        writes = [_k(x) for x in writes]
        ex = [t for t in reads if t.excl and t not in writes]
        if ex:
            reads = [t for t in reads if not t.excl]
            writes = writes + ex
        deps = self._deps(reads, writes)
        i = len(self.recs)
        if dma is not None:
            prev = self.dlast.get(dma)
            if prev is not None:
                deps.add(prev)
            self.dlast[dma] = i
        self.recs.append({"eng": eng, "fn": fn, "deps": deps, "cost": cost, "dma": dma, "lat": lat, "inc": False})
        for t in reads:
            t.r.append(i)
        for t in writes:
            t.w = i
            t.r = []
        return i

    def op(self, eng, fn, reads=(), writes=(), cost=0.2):
        return self._add(eng, fn, reads, writes, cost, None, 0.0)

    def dma(self, eng, fn, reads=(), writes=(), semkey=None, nbytes=0, indirect=False):
        rk = [_k(x) for x in reads]
        wk = [_k(x) for x in writes]
        sk = _k(semkey) if semkey is not None else (wk[0] if wk else rk[0])
        if sk.dsem is None:
            sk.dsem = self.dfree.pop()
        i = self._add(eng, fn, rk, wk, 0.8 if indirect else 0.07, sk.dsem, 2.0 + nbytes / 150e3)
        return i

    def _schedule(self):
        recs = self.recs
        n = len(recs)
        users = [[] for _ in range(n)]
        ndep = [0] * n
        for i, r in enumerate(recs):
            ndep[i] = len(r["deps"])
            for d in r["deps"]:
                users[d].append(i)
        import heapq
        ready = {e: [] for e in ENGS}
        fin = [0.0] * n
        rt = [0.0] * n
        for i, r in enumerate(recs):
            if ndep[i] == 0:
                heapq.heappush(ready[r["eng"]], (0.0, i))
        free = {e: 0.0 for e in ENGS}
        order = {e: [] for e in ENGS}
        done = 0
        while done < n:
            best = None
            for e in ENGS:
                h = ready[e]
                if not h:
                    continue
                t0 = free[e]
                cand = None
                if h[0][0] <= t0:
                    tmp = []
                    while h and h[0][0] <= t0:
                        tmp.append(heapq.heappop(h))
                    ci = min(tmp, key=lambda x: x[1])
                    for x in tmp:
                        if x is not ci:
                            heapq.heappush(h, x)
                    cand = (t0, ci[1], ci)
                else:
                    x = h[0]
                    cand = (x[0], x[1], None)
                if best is None or (cand[0], cand[1]) < (best[0][0], best[0][1]):
                    if best is not None and best[0][2] is not None:
                        heapq.heappush(ready[best[1]], best[0][2])
                    best = (cand, e)
                elif cand[2] is not None:
                    heapq.heappush(h, cand[2])
            (start, i, popped), e = best
            if popped is None:
                heapq.heappop(ready[e])
            r = recs[i]
            free[e] = start + r["cost"]
            fin[i] = start + r["cost"] + r["lat"]
            order[e].append(i)
            done += 1
            for u in users[i]:
                ndep[u] -= 1
                ru = recs[u]
                lat = 0.05 if ru["eng"] == e else 0.25
                if fin[i] + lat > rt[u]:
                    rt[u] = fin[i] + lat
                if ndep[u] == 0:
                    heapq.heappush(ready[ru["eng"]], (rt[u], u))
        return order, max(fin) if n else 0.0

    def flush(self):
        nc = self.nc
        recs = self.recs
        if self.reorder:
            order, est = self._schedule()
        else:
            order = {e: [i for i, r in enumerate(recs) if r["eng"] == e] for e in ENGS}
            est = 0.0
        dval = {}
        dtot = {}
        for i, r in enumerate(recs):
            if r["dma"] is not None:
                c = self.dpool[r["dma"]][1] + 16
                self.dpool[r["dma"]][1] = c
                dval[i] = c
                dtot[r["dma"]] = c
        for i, r in enumerate(recs):
            for d in r["deps"]:
                rd = recs[d]
                if rd["dma"] is None and (rd["eng"] != r["eng"] or r["eng"] in ("act", "dve", "pool")):
                    rd["inc"] = True
        eval_ = {}
        for e in ENGS:
            c = self.ecnt[e]
            for i in order[e]:
                if recs[i]["inc"]:
                    c += 1
                eval_[i] = c
            self.ecnt[e] = c
        engobj = {"pe": "tensor", "act": "scalar", "dve": "vector", "pool": "gpsimd", "sp": "sync"}
        esem, dpool = self.esem, self.dpool

        def mk(e):
            def body(eng):
                seen = {}
                for i in order[e]:
                    r = recs[i]
                    waits = {}
                    for d in r["deps"]:
                        rd = recs[d]
                        if rd["dma"] is not None:
                            k, v = ("d", rd["dma"]), dval[d]
                        elif rd["eng"] != e or e in ("act", "dve", "pool"):
                            k, v = ("e", rd["eng"]), eval_[d]
                        else:
                            continue
                        if seen.get(k, -1) >= v:
                            continue
                        if waits.get(k, -1) < v:
                            waits[k] = v
                    for k, v in waits.items():
                        seen[k] = v
                        if k[0] == "e":
                            eng.wait_ge(esem[k[1]], v)
                        else:
                            eng.wait_ge(dpool[k[1]][0], v)
                    ins = r["fn"](eng)
                    if r["dma"] is not None:
                        ins.then_inc(dpool[r["dma"]][0], 16)
                    elif r["inc"]:
                        ins.then_inc(esem[e], 1)
                if e == "sp":
                    for idx, v in dtot.items():
                        if seen.get(("d", idx), -1) < v:
                            eng.wait_ge(dpool[idx][0], v)
            return body

        with nc.Block() as block:
            for e in ENGS:
                if order[e] or (e == "sp" and dtot):
                    getattr(block, engobj[e])(mk(e))
        for k in self.all_keys:
            if k.dsem is not None:
                self.dfree.append(k.dsem)
                k.dsem = None
            k.w = None
            k.r = []
        n = {e: len(order[e]) for e in ENGS}
        n["est_us"] = round(est, 1)
        self._reset_stage()
        return n

    def close(self):
        self.stack.close()


class KB:
    def __init__(self, NB=4, stages=("mod", "hgrn", "moe0", "mla", "moe1"), dbg=()):
        self.NB = NB
        self.stages = stages
        self.dbg = dbg
        nc = bass.Bass("TRN2", target_bir_lowering=False)
        self.nc = nc
        self.S = Sched(nc)
        self.uid = 0
        shapes = {
            "x": (NB, SEQ, D), "c": (NB, D), "ctx": (NB, CTX, D), "c_ctx": (D,),
            "ada_w": (2, D, 6 * D), "ada_b": (2, 6 * D), "norm_mix_g": (2, D), "norm_ffn_g": (2, D),
            "hg_w_in": (D, 5 * D), "hg_lb": (2, 3, D), "hg_out_norm_g": (128,), "hg_w_out": (D, D),
            "mla_w_in": (D, 416), "mla_q_norm_g": (256,), "mla_kv_norm_g": (128,), "mla_w_qb": (256, 1536),
            "mla_w_kvb": (128, 2048), "mla_q_qknorm_g": (96,), "mla_k_qknorm_g": (96,), "mla_w_out": (D, D),
            "moe_w_group": (2, D, 4), "moe_w_expert": (2, D, 32), "moe_w_gate": (2, NEXP, D, FF),
            "moe_w_up": (2, NEXP, D, FF), "moe_w_down": (2, NEXP, FF, D),
            "k_maskf": (128, 128), "k_maskb": (128, 128), "k_bm": (128, 4, 128), "k_bmc": (128, 4), "k_cos": (SEQ, 16), "k_sin": (SEQ, 16),
        }

        class LazyIn(dict):
            def __missing__(d_, name):
                ap = nc.dram_tensor(name, list(shapes[name]), F32, kind="ExternalInput").ap()
                d_[name] = ap
                return ap

        I = LazyIn()
        self.I = I
        self.out = nc.dram_tensor("out", [NB, SEQ, D], F32, kind="ExternalOutput").ap()
        self.xres = nc.dram_tensor("xres", [NB, T, D], F32).ap()
        self.mod = nc.dram_tensor("modv", [2, 5, 6 * D], F32).ap()
        self.cT_d = nc.dram_tensor("cT_d", [128, 3, T], BF16).ap()
        self.krr_d = nc.dram_tensor("krr_d", [128, NT, 32], F32).ap()
        self.sskr_d = nc.dram_tensor("sskr_d", [128, NT], F32).ap()
        NTLmax = NB * NT
        self.NBLKmax = 2 * NTLmax + NEXP
        self.h2d = nc.dram_tensor("h2d", [NTLmax * 128, D], BF16).ap()
        self.xs = nc.dram_tensor("xs", [self.NBLKmax * 128, D], BF16).ap()
        self.ys = nc.dram_tensor("ys", [self.NBLKmax * 128, D], F32).ap()
        self.wgb = [nc.dram_tensor("wgb%d" % l_, [NEXP * 128, 4096], BF16).ap() for l_ in range(2)]
        self.wub = [nc.dram_tensor("wub%d" % l_, [NEXP * 128, 4096], BF16).ap() for l_ in range(2)]
        self.wdb = [nc.dram_tensor("wdb%d" % l_, [NEXP * 128, 4096], BF16).ap() for l_ in range(2)]
        self.kwb = [self.S.key() for _ in range(2)]
        self.kx = self.S.keys(NB)
        self.kmod = self.S.key()
        self.kscr = self.S.key()
        self.D_ = {}
        for name, shape in dbg:
            self.D_[name] = nc.dram_tensor("dbg_" + name, list(shape), F32, kind="ExternalOutput").ap()

    def sb(self, st, shape, dt, nm="t"):
        self.uid += 1
        t = st.enter_context(self.nc.sbuf_tensor("%s_%d" % (nm, self.uid), list(shape), dt))
        return Tl(t, self.S.key())

    def sbr(self, st, n, shape, dt, nm="r"):
        return Rot([self.sb(st, shape, dt, nm) for _ in range(n)])

    def psb(self, st, nm="ps"):
        self.uid += 1
        t = st.enter_context(self.nc.psum_tensor("%s_%d" % (nm, self.uid), [128, 512], F32))
        k = self.S.key()
        k.excl = True
        return Tl(t, k)

    @staticmethod
    def _n(ap):
        n = 1
        for d in ap.shape[1:]:
            n *= d
        return n

    def mm(self, out, lhsT, rhs, start, stop, R, W):
        c = max(64, self._n(out)) / 2400.0 + 0.02
        if rhs.dtype == F32:
            c *= 4
        self.S.op("pe", lambda e: e.matmul(out, lhsT=lhsT, rhs=rhs, start=start, stop=stop), R, W, cost=c)

    def tr(self, out, in_, ident, R, W):
        self.S.op("pe", lambda e: e.transpose(out=out, in_=in_, identity=ident), R, W, cost=0.09)

    def act(self, out, in_, func, R, W, bias=None, scale=None, accum_out=None):
        kw = {}
        if bias is not None:
            kw["bias"] = bias
        if scale is not None:
            kw["scale"] = scale
        if accum_out is not None:
            kw["accum_out"] = accum_out
        self.S.op("act", lambda e: e.activation(out=out, in_=in_, func=func, **kw), R, W, cost=0.22 + self._n(out) / 1200.0)

    def ts(self, eng, out, in0, s1, s2, op0, op1, R, W):
        if op1 is None:
            self.S.op(eng, lambda e: e.tensor_scalar(out=out, in0=in0, scalar1=s1, scalar2=None, op0=op0), R, W, cost=self._c(eng, out))
        else:
            self.S.op(eng, lambda e: e.tensor_scalar(out=out, in0=in0, scalar1=s1, scalar2=s2, op0=op0, op1=op1), R, W, cost=self._c(eng, out))

    def tt(self, eng, out, in0, in1, op, R, W):
        self.S.op(eng, lambda e: e.tensor_tensor(out=out, in0=in0, in1=in1, op=op), R, W, cost=self._c(eng, out, 1.5))

    def stt(self, out, in0, scalar, in1, op0, op1, R, W):
        self.S.op("dve", lambda e: e.scalar_tensor_tensor(out=out, in0=in0, scalar=scalar, in1=in1, op0=op0, op1=op1), R, W,
                  cost=self._c("dve", out, 1.5))

    def cp(self, eng, out, in_, R, W):
        if eng == "act":
            self.S.op("act", lambda e: e.activation(out=out, in_=in_, func=AF.Copy), R, W, cost=0.22 + self._n(out) / 1200.0)
        else:
            self.S.op(eng, lambda e: e.tensor_copy(out=out, in_=in_), R, W, cost=self._c(eng, out))

    def red(self, out, in_, op, R, W, negate=None):
        self.S.op("dve", lambda e: e.tensor_reduce(out=out, in_=in_, axis=AX.X, op=op, negate=negate), R, W, cost=self._c("dve", in_))

    def recip(self, out, in_, R, W):
        self.S.op("dve", lambda e: e.reciprocal(out=out, in_=in_), R, W, cost=self._c("dve", out, 8.0))

    def memset(self, eng, ap, val, W):
        self.S.op(eng, lambda e: e.memset(ap, val), (), W, cost=self._c(eng, ap))

    def _c(self, eng, ap, mult=1.0):
        n = self._n(ap)
        if eng == "pool":
            return 0.25 + n * mult / 500.0
        return 0.1 + n * mult / 960.0

    def dma(self, q, out, in_, R, W, semkey=None):
        nb = out.shape[0] * self._n(out) * 4
        self.S.dma(q, lambda e: e.dma_start(out=out, in_=in_), R, W, semkey=semkey, nbytes=nb)

    def sumsq(self, junk, in_, acc, R, W):
        self.act(junk, in_, AF.Square, R, W, accum_out=acc)

    def setup_consts(self, st):
        nc, S = self.nc, self.S
        self.identf = self.sb(st, [128, 128], F32, "identf")
        self.identb = self.sb(st, [128, 128], BF16, "identb")
        self.onesb = self.sb(st, [128, 128], BF16, "onesb")
        identf = self.identf
        self.memset("pool", identf[:], 0.0, [identf])
        S.op("pool", lambda e: e.affine_select(out=identf[:], in_=identf[:], pattern=[[-1, 128]], compare_op=ALU.not_equal,
                                               fill=1.0, base=0, channel_multiplier=1), [identf], [identf])
        self.cp("dve", self.identb[:], identf[:], [identf], [self.identb])
        self.memset("dve", self.onesb[:], 1.0, [self.onesb])
        self.epsc = self.sb(st, [128, 1], F32, "epsc")
        self.memset("dve", self.epsc[:], EPS, [self.epsc])
        self.onec = self.sb(st, [128, 1], F32, "onec")
        self.memset("dve", self.onec[:], 1.0, [self.onec])

    def stage_mod(self):
        I, NB = self.I, self.NB
        with ExitStack() as st:
            cT = self.sb(st, [128, KC, 5], F32, "cT")
            cs = self.sb(st, [128, KC, 5], F32, "cs")
            self.memset("dve", cT[:], 0.0, [cT])
            for r in range(NB):
                self.dma("sp", cT[:, :, r], I["c"][r, :].rearrange("(c p) -> p c", p=128), [], [cT])
            self.dma("sp", cT[:, :, 4], I["c_ctx"].rearrange("(c p) -> p c", p=128), [], [cT])
            self.act(cs[:], cT[:], AF.Silu, [cT], [cs])
            wrot = self.sbr(st, 3, [128, KC, 512], F32, "adaw")
            ps = Rot([self.psb(st) for _ in range(2)])
            for l in range(2):
                bt = self.sb(st, [5, 6 * D], F32, "adab")
                ms = self.sb(st, [5, 6 * D], F32, "modsb")
                self.dma("sp", bt[:], I["ada_b"][l, :].partition_broadcast(5), [], [bt])
                for n in range(12):
                    w = wrot.next()
                    self.dma("sp", w[:], I["ada_w"][l, :, n * 512:(n + 1) * 512].rearrange("(c p) n -> p c n", p=128), [], [w])
                    p = ps.next()
                    for k in range(KC):
                        self.mm(p[0:5, :], cs[:, k, :], w[:, k, :], k == 0, k == KC - 1, [cs, w], [p])
                    self.tt("dve", ms[:, n * 512:(n + 1) * 512], p[0:5, :], bt[:, n * 512:(n + 1) * 512], ALU.add, [p, bt], [ms])
                self.dma("sp", self.mod[l], ms[:], [ms], [self.kmod], semkey=ms)
            return self.S.flush()

    def mod_cols(self, st, l, m, r):
        I = self.I
        g = I["norm_mix_g"] if m == 0 else I["norm_ffn_g"]
        gc = self.sb(st, [128, KC], F32, "gc")
        sc = self.sb(st, [128, KC], F32, "sc")
        sh = self.sb(st, [128, KC], F32, "sh")
        A = self.sb(st, [128, KC], F32, "A")
        self.dma("sp", gc[:], g[l, :].rearrange("(c p) -> p c", p=128), [], [gc])
        self.dma("sp", sc[:], self.mod[l, r, (3 * m + 1) * D:(3 * m + 2) * D].rearrange("(c p) -> p c", p=128), [self.kmod], [sc])
        self.dma("sp", sh[:], self.mod[l, r, (3 * m) * D:(3 * m + 1) * D].rearrange("(c p) -> p c", p=128), [self.kmod], [sh])
        self.stt(A[:], sc[:], 1.0, gc[:], ALU.add, ALU.mult, [sc, gc], [A])
        return A, sh

    def gate_tile(self, st, l, m, r):
        gt = self.sb(st, [128, D], F32, "gt")
        self.dma("sp", gt[:], self.mod[l, r, (3 * m + 2) * D:(3 * m + 3) * D].partition_broadcast(128), [self.kmod], [gt])
        return gt

    def norm_res(self, st, pbanks, junk=None, nxn=1, nhtf=1):
        R = {}
        R["xt"] = self.sbr(st, 2, [128, D], F32, "xt")
        R["xn"] = self.sbr(st, nxn, [128, D], F32, "xn")
        R["junk"] = junk if junk is not None else self.sb(st, [128, D], BF16, "junk")
        R["ss"] = self.sbr(st, 2, [128, 1], F32, "ss")
        R["sd"] = self.sbr(st, 2, [128, 1], F32, "sd")
        R["rs"] = self.sbr(st, 2, [128, 1], F32, "rs")
        R["hTf"] = self.sbr(st, nhtf, [128, KC, 128], F32, "hTf")
        R["pb"] = pbanks
        return R

    def norm_tile(self, R, src_ap, src_keys, A, Bc, dst_ap, dst_keys):
        xt = R["xt"].next()
        xn = R["xn"].next()
        ss = R["ss"].next()
        sd = R["sd"].next()
        rs = R["rs"].next()
        hTf = R["hTf"].next()
        junk = R["junk"]
        pa, pb = R["pb"]
        self.dma("sp", xt[:], src_ap, src_keys, [xt])
        self.sumsq(junk[:, 0:D], xt[:], ss[:], [xt], [junk, ss])
        self.act(sd[:], ss[:], AF.Sqrt, [ss, self.epsc], [sd], bias=self.epsc[:, 0:1], scale=1.0 / D)
        self.recip(rs[:], sd[:], [sd], [rs])
        self.act(xn[:], xt[:], AF.Copy, [xt, rs], [xn], scale=rs[:, 0:1])
        for k in range(KC):
            p = pa if k < 4 else pb
            self.tr(p[:, (k % 4) * 128:(k % 4 + 1) * 128], xn[:, k * 128:(k + 1) * 128], self.identf[:], [xn, self.identf], [p])
        for k in range(KC):
            p = pa if k < 4 else pb
            src = p[:, (k % 4) * 128:(k % 4 + 1) * 128]
            if k < 4:
                self.ts("dve", hTf[:, k, :], src, A[:, k:k + 1], Bc[:, k:k + 1], ALU.mult, ALU.add, [p, A, Bc], [hTf])
            else:
                self.act(hTf[:, k, :], src, AF.Identity, [p, A, Bc], [hTf], bias=Bc[:, k:k + 1], scale=A[:, k:k + 1])
        if dst_ap is not None:
            self.cp("dve", dst_ap, hTf[:], [hTf], dst_keys)
        self.last_xn = xn
        return hTf

    def src_l0(self, b, tile):
        if tile < 2:
            return self.I["ctx"][b, tile * 128:(tile + 1) * 128, :]
        return self.I["x"][b, (tile - 2) * 128:(tile - 1) * 128, :]

    def stage_hgrn(self, b):
        I, S = self.I, self.S
        l = 0
        with ExitStack() as st:
            PB = [self.psb(st) for _ in range(8)]
            if "moe0" in self.stages:
                self.precast(0, b)
            hT = self.sb(st, [128, KC, T], BF16, "hT")
            hTk = S.keys(NT)
            ogT = self.sb(st, [128, KC, T], BF16, "ogT")
            ogk = S.keys(KC)
            maskf = self.sb(st, [128, 128], F32, "maskf")
            maskb = self.sb(st, [128, 128], F32, "maskb")
            bmc = self.sb(st, [128, 4], F32, "bmc")
            if not _F("HG_A"):
                bm = self.sb(st, [128, 4, 128], BF16, "bm")
                self.dma("pool", bm[:], I["k_bm"], [], [bm])
                Vbd = self.sbr(st, 2, [128, 4, 128], BF16, "Vbd")
            self.dma("sp", maskf[:], I["k_maskf"], [], [maskf])
            self.dma("sp", maskb[:], I["k_maskb"], [], [maskb])
            self.dma("sp", bmc[:], I["k_bmc"], [], [bmc])
            m01 = self.sb(st, [128, T], BF16, "m01")
            self.memset("dve", m01[:], 1.0, [m01])
            self.memset("dve", m01[:, 0:T:32], 0.0, [m01])
            lbr = self.sb(st, [128, 2, 3, KC], F32, "lbr")
            with self.nc.allow_non_contiguous_dma(reason="tiny"):
                for d_ in range(2):
                    for j in range(3):
                        self.dma("sp", lbr[:, d_, j, :], I["hg_lb"][d_, j, :].rearrange("(h p) -> p h", p=128), [], [lbr])
            lbe = self.sb(st, [128, 2, 3, KC], F32, "lbe")
            self.act(lbe[:], lbr[:], AF.Exp, [lbr], [lbe])
            lbs = self.sb(st, [128, 2, KC], F32, "lbs")
            self.tt("dve", lbs[:], lbe[:, :, 0, :], lbe[:, :, 1, :], ALU.add, [lbe], [lbs])
            self.tt("dve", lbs[:], lbs[:], lbe[:, :, 2, :], ALU.add, [lbe, lbs], [lbs])
            lbi = self.sb(st, [128, 2, KC], F32, "lbi")
            self.recip(lbi[:], lbs[:], [lbs], [lbi])
            lb = self.sb(st, [128, 2, KC], F32, "lb")
            oml = self.sb(st, [128, 2, KC], F32, "oml")
            self.tt("dve", lb[:], lbe[:, :, 0, :], lbi[:], ALU.mult, [lbe, lbi], [lb])
            self.ts("dve", oml[:], lb[:], -1.0, 1.0, ALU.mult, ALU.add, [lb], [oml])
            ogc = self.sb(st, [128, 1], F32, "ogc")
            self.dma("sp", ogc[:], I["hg_out_norm_g"].rearrange("(p o) -> p o", o=1), [], [ogc])
            A_l, B_l = self.mod_cols(st, l, 0, b)
            A_c, B_c = self.mod_cols(st, l, 0, 4)
            gt_l = self.gate_tile(st, l, 0, b)
            gt_c = self.gate_tile(st, l, 0, 4)
            qdec = self.sb(st, [128, T], BF16, "qdec")
            NR = self.norm_res(st, (PB[0], PB[1]), junk=qdec)
            for tile in range(NT):
                A, Bc = (A_c, B_c) if tile < 2 else (A_l, B_l)
                self.norm_tile(NR, self.src_l0(b, tile), [], A, Bc, hT[:, :, tile * 128:(tile + 1) * 128], [hTk[tile]])
            wh = self.sbr(st, 1, [128, KC, 5, 128], BF16, "wh")
            Vh = self.sb(st, [128, NT, 128], BF16, "Vh")
            qs = self.sb(st, [128, T], BF16, "qs")
            sgate = self.sb(st, [128, T], BF16, "sgate")
            A1 = self.sb(st, [128, T], F32, "A1")
            A2 = self.sb(st, [128, T], F32, "A2")
            A3 = self.sb(st, [128, T], F32, "A3")
            kinc = self.sb(st, [128, T], BF16, "kinc")
            dec = self.sb(st, [128, T // 32], F32, "dec")
            tot = self.sb(st, [128, T // 32], F32, "tot")
            oacc = self.sb(st, [128, T], F32, "oacc")
            oak = S.keys(NT)
            sTm = self.sbr(st, 2, [128, 128], BF16, "sTm")
            kTs = self.sbr(st, 2, [128, 4, 128], BF16, "kTs")
            KVs = self.sbr(st, 2, [128, 4, 128], F32, "KVs")
            Sst = self.sb(st, [128, 8, 128], F32, "Sst")
            Sstk = S.keys(8)
            Sb = self.sb(st, [128, 8, 128], BF16, "Sb")
            Sbk = S.keys(2)
            pproj = Rot([PB[0], PB[1]])
            psT = Rot([PB[2], PB[3]])
            pkT = PB[4]
            pkTk = [PB[4].k, PB[4].k]
            pKV = PB[5]
            poT = Rot([PB[6], PB[7]])
            blocks = [(i * 512, 512) for i in range(4)] + [(2048, 256)]

            def proj(whh, sec, blk):
                t0, n = blk
                p = pproj.next()
                tiles = range(t0 // 128, (t0 + n) // 128)
                for k in range(KC):
                    self.mm(p[:, 0:n], whh[:, k, sec, :], hT[:, k, t0:t0 + n], k == 0, k == KC - 1,
                            [whh] + [hTk[t] for t in tiles], [p])
                return p

            for h in range(KC):
                whh = wh.next()
                for sec in range(5):
                    self.dma("pool", whh[:, :, sec, :],
                             I["hg_w_in"][:, sec * D + h * 128: sec * D + (h + 1) * 128].rearrange("(c p) e -> p c e", p=128), [], [whh])
                for tile in range(NT):
                    p = pproj.next()
                    for k in range(KC):
                        self.mm(p[:, 0:128], hT[:, k, tile * 128:(tile + 1) * 128], whh[:, k, 3, :], k == 0, k == KC - 1,
                                [whh, hTk[tile]], [p])
                    self.cp("act", Vh[:, tile, :], p[:, 0:128], [p], [Vh])
                for blk in blocks:
                    t0, n = blk
                    p = proj(whh, 0, blk)
                    self.act(qs[:, t0:t0 + n], p[:, 0:n], AF.Silu, [p], [qs])
                    p = proj(whh, 4, blk)
                    self.act(sgate[:, t0:t0 + n], p[:, 0:n], AF.Silu, [p], [sgate])
                for dr in range(2):
                    for blk in blocks:
                        t0, n = blk
                        p = proj(whh, 1 + dr, blk)
                        self.act(A1[:, t0:t0 + n], p[:, 0:n], AF.Sigmoid, [p], [A1])
                    self.ts("dve", A1[:], A1[:], oml[:, dr, h:h + 1], lb[:, dr, h:h + 1], ALU.mult, ALU.add, [A1, oml, lb], [A1])
                    self.act(A2[:], A1[:], AF.Ln, [A1], [A2])
                    if _F("HG_B"):
                        self.act(A1[:], A1[:], AF.Identity, [A1, self.onec], [A1], bias=self.onec[:, 0:1], scale=-1.0)
                    else:
                        self.ts("dve", A1[:], A1[:], -1.0, 1.0, ALU.mult, ALU.add, [A1], [A1])
                    S.op("dve", lambda e: e.tensor_tensor_scan(out=A3[:], data0=m01[:], data1=A2[:], initial=0.0,
                                                                op0=ALU.mult, op1=ALU.add), [m01, A2], [A3], cost=0.1 + 2 * T / 960.0)
                    a3v = A3[:].rearrange("p (j i) -> p j i", i=32)
                    self.cp("dve", tot[:], a3v[:, :, 31], [A3], [tot])
                    self.act(dec[:], tot[:], AF.Exp, [tot], [dec])
                    if dr == 0:
                        barr, free = A3, A2
                    else:
                        a2v = A2[:].rearrange("p (j i) -> p j i", i=32)
                        self.tt("dve", A2[:], A2[:], A3[:], ALU.subtract, [A2, A3], [A2])
                        self.tt("dve", a2v, a2v, tot[:].unsqueeze(2).broadcast_to([128, T // 32, 32]), ALU.add, [A2, tot], [A2])
                        barr, free = A2, A3
                    self.act(free[:], barr[:], AF.Exp, [barr], [free], scale=-1.0)
                    self.act(barr[:], barr[:], AF.Exp, [barr], [barr])
                    self.tt("dve", qdec[:], qs[:], barr[:], ALU.mult, [qs, barr], [qdec])
                    self.tt("dve", kinc[:], A1[:], free[:], ALU.mult, [A1, free], [kinc])
                    order = list(range(NT)) if dr == 0 else [1, 0] + list(range(NT - 1, 1, -1))
                    mask = maskf if dr == 0 else maskb
                    self.memset("dve", Sst[:, 0, :], 0.0, [Sstk[0]])
                    for i, tile in enumerate(order):
                        base = 4 * (i % 2)
                        ts_ = slice(tile * 128, (tile + 1) * 128)
                        ps_ = psT.next()
                        self.mm(ps_[:, 0:128], kinc[:, ts_], qdec[:, ts_], True, True, [kinc, qdec], [ps_])
                        sm = sTm.next()
                        self.tt("dve", sm[:], ps_[:, 0:128], mask[:], ALU.mult, [ps_, mask], [sm])
                        pk_i = i % 2
                        pkv = pkT[:, pk_i * 64:(pk_i + 1) * 64].bitcast(BF16)
                        self.tr(pkv, kinc[:, ts_], self.identb[:], [kinc, self.identb], [pkTk[pk_i]])
                        kt = kTs.next()
                        if _F("HG_A"):
                            for j in range(4):
                                self.act(kt[:, j, :], pkv, AF.Copy, [pkTk[pk_i], bmc], [kt], scale=bmc[:, j:j + 1])
                            for j in range(4):
                                self.mm(pKV[:, j * 128:(j + 1) * 128], kt[:, j, :], Vh[:, tile, :], True, True, [kt, Vh], [pKV])
                        else:
                            self.cp("act", kt[:, 0, :], pkv, [pkTk[pk_i]], [kt])
                            vb = Vbd.next()
                            self.tt("dve", vb[:], Vh[:, tile, :].unsqueeze(1).broadcast_to([128, 4, 128]), bm[:], ALU.mult, [Vh, bm], [vb])
                            self.mm(pKV[:, :], kt[:, 0, :], vb[:].rearrange("p j v -> p (j v)"), True, True, [kt, vb], [pKV])
                        kv = KVs.next()
                        self.tt("dve", kv[:], pKV[:, :].rearrange("p (j v) -> p j v", j=4),
                                dec[:, tile * 4:(tile + 1) * 4].unsqueeze(2).broadcast_to([128, 4, 128]), ALU.mult, [pKV, dec], [kv])
                        corder = [0, 1, 2, 3] if dr == 0 else [3, 2, 1, 0]
                        for jj, c in enumerate(corder):
                            s_in = base + jj
                            s_out = (base + jj + 1) % 8
                            self.stt(Sst[:, s_out, :], Sst[:, s_in, :], dec[:, tile * 4 + c: tile * 4 + c + 1], kv[:, c, :],
                                     ALU.mult, ALU.add, [Sstk[s_in], dec, kv], [Sstk[s_out]])
                        self.cp("act", Sb[:, base:base + 4, :], Sst[:, base:base + 4, :], [Sstk[base + q_] for q_ in range(4)], [Sbk[i % 2]])
                        po = poT.next()
                        self.mm(po[:, 0:128], Vh[:, tile, :], sm[:], True, False, [Vh, sm], [po])
                        for jj, c in enumerate(corder):
                            self.mm(po[:, c * 32:(c + 1) * 32], Sb[:, base + jj, :], qdec[:, tile * 128 + c * 32: tile * 128 + (c + 1) * 32],
                                    False, jj == 3, [Sbk[i % 2], qdec], [po])
                        if dr == 0:
                            self.cp("act", oacc[:, ts_], po[:, 0:128], [po], [oak[tile]])
                        else:
                            self.tt("dve", oacc[:, ts_], oacc[:, ts_], po[:, 0:128], ALU.add, [po, oak[tile]], [oak[tile]])
                if _F("HG_D"):
                    self.act(qdec[:], oacc[:], AF.Square, oak, [qdec])
                else:
                    self.tt("dve", qdec[:], oacc[:], oacc[:], ALU.mult, oak, [qdec])
                for blk in blocks:
                    t0, n = blk
                    p = pproj.next()
                    self.mm(p[:, 0:n], self.onesb[:], qdec[:, t0:t0 + n], True, True, [self.onesb, qdec], [p])
                    if _F("HG_C"):
                        self.act(A2[:, t0:t0 + n], p[:, 0:n], AF.Ln, [p, self.epsc], [A2], bias=self.epsc[:, 0:1], scale=1.0 / 128)
                    else:
                        self.act(A2[:, t0:t0 + n], p[:, 0:n], AF.Sqrt, [p, self.epsc], [A2], bias=self.epsc[:, 0:1], scale=1.0 / 128)
                if _F("HG_C"):
                    self.act(A3[:], A2[:], AF.Exp, [A2], [A3], scale=-0.5)
                else:
                    self.recip(A3[:], A2[:], [A2], [A3])
                self.tt("dve", A3[:], A3[:], oacc[:], ALU.mult, [A3] + oak, [A3])
                self.stt(ogT[:, h, :], A3[:], ogc[:, 0:1], sgate[:], ALU.mult, ALU.mult, [A3, ogc, sgate], [ogk[h]])
            wo = self.sb(st, [128, KC, D], BF16, "wo")
            self.dma("pool", wo[:], I["hg_w_out"].rearrange("(c p) n -> p c n", p=128), [], [wo])
            xt2 = NR["xt"]
            tmp = NR["xn"]
            for tile in range(NT):
                gt = gt_c if tile < 2 else gt_l
                x_ = xt2.next()
                self.dma("sp", x_[:], self.src_l0(b, tile), [], [x_])
                t_ = tmp.next()
                for half in range(2):
                    p = pproj.next()
                    hs = slice(half * 512, (half + 1) * 512)
                    for k in range(KC):
                        self.mm(p[:, :], ogT[:, k, tile * 128:(tile + 1) * 128], wo[:, k, hs], k == 0, k == KC - 1, [ogk[k], wo], [p])
                    self.tt("dve", t_[:, hs], p[:, :], gt[:, hs], ALU.mult, [p, gt], [t_])
                self.tt("dve", t_[:], t_[:], x_[:], ALU.add, [t_, x_], [t_])
                self.dma("sp", self.xres[b, tile * 128:(tile + 1) * 128, :], t_[:], [t_], [self.kx[b]], semkey=t_)
                if ("xm0" in self.D_) and b == 0:
                    self.dma("sp", self.D_["xm0"][tile * 128:(tile + 1) * 128, :], t_[:], [t_], [self.kscr], semkey=t_)
            return S.flush()

    ROUTE_TMPS = (("lg", 36), ("gmax", 1), ("ngmax", 1), ("ge", 4), ("gsum", 1), ("pg", 1), ("gone", 4), ("pen", 4),
                  ("em", 32), ("m1", 1), ("oh1", 32), ("em2", 32), ("m2", 1), ("oh2", 32), ("dm", 1), ("e2", 1),
                  ("den", 1), ("rden", 1), ("w1", 1), ("w2", 1), ("tmpw", 32))

    def route_tile(self, sm, p):
        t = {nm: r.next() for nm, r in sm.items()}
        lg = t["lg"]
        self.cp("act", lg[:], p[:, 0:36], [p], [lg])
        self.red(t["gmax"][:], lg[:, 0:4], ALU.max, [lg], [t["gmax"]])
        self.ts("dve", t["ngmax"][:], t["gmax"][:], -1.0, None, ALU.mult, None, [t["gmax"]], [t["ngmax"]])
        self.act(t["ge"][:], lg[:, 0:4], AF.Exp, [lg, t["ngmax"]], [t["ge"], t["gsum"]], bias=t["ngmax"][:, 0:1], accum_out=t["gsum"][:])
        self.recip(t["pg"][:], t["gsum"][:], [t["gsum"]], [t["pg"]])
        self.ts("dve", t["gone"][:], lg[:, 0:4], t["gmax"][:, 0:1], None, ALU.is_ge, None, [lg, t["gmax"]], [t["gone"]])
        self.ts("dve", t["pen"][:], t["gone"][:], BIG, -BIG, ALU.mult, ALU.add, [t["gone"]], [t["pen"]])
        self.tt("dve", t["em"][:].rearrange("p (g j) -> p g j", g=4), lg[:, 4:36].rearrange("p (g j) -> p g j", g=4),
                t["pen"][:].unsqueeze(2).broadcast_to([128, 4, 8]), ALU.add, [lg, t["pen"]], [t["em"]])
        self.red(t["m1"][:], t["em"][:], ALU.max, [t["em"]], [t["m1"]])
        self.ts("dve", t["oh1"][:], t["em"][:], t["m1"][:, 0:1], None, ALU.is_ge, None, [t["em"], t["m1"]], [t["oh1"]])
        self.stt(t["em2"][:], t["oh1"][:], -BIG, t["em"][:], ALU.mult, ALU.add, [t["oh1"], t["em"]], [t["em2"]])
        self.red(t["m2"][:], t["em2"][:], ALU.max, [t["em2"]], [t["m2"]])
        self.ts("dve", t["oh2"][:], t["em2"][:], t["m2"][:, 0:1], None, ALU.is_ge, None, [t["em2"], t["m2"]], [t["oh2"]])
        self.tt("dve", t["dm"][:], t["m2"][:], t["m1"][:], ALU.subtract, [t["m2"], t["m1"]], [t["dm"]])
        self.act(t["e2"][:], t["dm"][:], AF.Exp, [t["dm"]], [t["e2"]])
        self.ts("dve", t["den"][:], t["e2"][:], 1.0, None, ALU.add, None, [t["e2"]], [t["den"]])
        self.recip(t["rden"][:], t["den"][:], [t["den"]], [t["rden"]])
        self.tt("dve", t["w1"][:], t["pg"][:], t["rden"][:], ALU.mult, [t["pg"], t["rden"]], [t["w1"]])
        self.tt("dve", t["w2"][:], t["w1"][:], t["e2"][:], ALU.mult, [t["w1"], t["e2"]], [t["w2"]])
        return t

    def stage_moe(self, l, b, half):
        I, S = self.I, self.S
        if l == 0:
            tiles = list(range(0, 9)) if half == 0 else list(range(9, 18))
        else:
            tiles = list(range(2, 10)) if half == 0 else list(range(10, 18))
        ntl = len(tiles)
        NTOK = ntl * 128
        with ExitStack() as st:
            PB = [self.psb(st) for _ in range(8)]
            hT = self.sb(st, [128, KC, NTOK], BF16, "hT")
            hTk = S.keys(ntl)
            acc = self.sb(st, [128, ntl, D], F32, "acc")
            acck = S.keys(ntl)
            Wt = self.sb(st, [128, ntl, NEXP], F32, "Wt")
            Wtk = S.keys(ntl)
            wr = self.sb(st, [128, KC, 36], F32, "wr")
            self.dma("sp", wr[:, :, 0:4], I["moe_w_group"][l].rearrange("(c p) g -> p c g", p=128), [], [wr])
            self.dma("sp", wr[:, :, 4:36], I["moe_w_expert"][l].rearrange("(c p) g -> p c g", p=128), [], [wr])
            A_l, B_l = self.mod_cols(st, l, 1, b)
            gt_l = self.gate_tile(st, l, 1, b)
            if l == 0 and half == 0:
                A_c, B_c = self.mod_cols(st, l, 1, 4)
                gt_c = self.gate_tile(st, l, 1, 4)
            NR = self.norm_res(st, (PB[0], PB[1]), nhtf=2)
            sm = {}
            for nm, w in (("lg", 36), ("gmax", 1), ("ngmax", 1), ("ge", 4), ("gsum", 1), ("pg", 1), ("gone", 4), ("pen", 4),
                          ("em", 32), ("m1", 1), ("oh1", 32), ("em2", 32), ("m2", 1), ("oh2", 32), ("dm", 1), ("e2", 1),
                          ("den", 1), ("rden", 1), ("w1", 1), ("w2", 1), ("tmpw", 32)):
                sm[nm] = self.sbr(st, 2, [128, w], F32, nm)
            for li, tile in enumerate(tiles):
                isctx = (l == 0 and tile < 2)
                A, Bc = (A_c, B_c) if isctx else (A_l, B_l)
                hTf = self.norm_tile(NR, self.xres[b, tile * 128:(tile + 1) * 128, :], [self.kx[b]], A, Bc,
                                     hT[:, :, li * 128:(li + 1) * 128], [hTk[li]])
                p = PB[2 + li % 2]
                for k in range(KC):
                    self.mm(p[:, 0:36], hTf[:, k, :], wr[:, k, :], k == 0, k == KC - 1, [hTf, wr], [p])
                t = self.route_tile(sm, p)
                self.ts("dve", t["tmpw"][:], t["oh1"][:], t["w1"][:, 0:1], None, ALU.mult, None, [t["oh1"], t["w1"]], [t["tmpw"]])
                self.stt(Wt[:, li, :], t["oh2"][:], t["w2"][:, 0:1], t["tmpw"][:], ALU.mult, ALU.add, [t["oh2"], t["w2"], t["tmpw"]], [Wtk[li]])
            wg = self.sbr(st, 2, [128, KC, FF], BF16, "wg")
            wu = self.sbr(st, 2, [128, KC, FF], BF16, "wu")
            wd = self.sbr(st, 2, [128, 4, D], BF16, "wd")
            sg = self.sbr(st, 2, [128, 512], BF16, "sg")
            actT = self.sbr(st, 2, [128, 4, 512], BF16, "actT")
            pgu = Rot([(PB[0], PB[1]), (PB[2], PB[3])])
            pyr = Rot([(PB[4], PB[5]), (PB[6], PB[7])])
            blocks = []
            t0 = 0
            while t0 < NTOK:
                n = min(512, NTOK - t0)
                blocks.append((t0, n))
                t0 += n
            for e in range(NEXP):
                g_, u_, d_ = wg.next(), wu.next(), wd.next()
                self.dma("pool", g_[:], I["moe_w_gate"][l, e].rearrange("(c p) f -> p c f", p=128), [], [g_])
                self.dma("pool", u_[:], I["moe_w_up"][l, e].rearrange("(c p) f -> p c f", p=128), [], [u_])
                self.dma("pool", d_[:], I["moe_w_down"][l, e].rearrange("(c p) f -> p c f", p=128), [], [d_])
                for (t0, n) in blocks:
                    at = actT.next()
                    hk = [hTk[t] for t in range(t0 // 128, (t0 + n) // 128)]
                    for f in range(4):
                        pg_, pu_ = pgu.next()
                        fs = slice(f * 128, (f + 1) * 128)
                        for k in range(KC):
                            self.mm(pg_[:, 0:n], g_[:, k, fs], hT[:, k, t0:t0 + n], k == 0, k == KC - 1, [g_] + hk, [pg_])
                        for k in range(KC):
                            self.mm(pu_[:, 0:n], u_[:, k, fs], hT[:, k, t0:t0 + n], k == 0, k == KC - 1, [u_] + hk, [pu_])
                        s_ = sg.next()
                        self.act(s_[:, 0:n], pg_[:, 0:n], AF.Silu, [pg_], [s_])
                        self.tt("dve", at[:, f, 0:n], s_[:, 0:n], pu_[:, 0:n], ALU.mult, [s_, pu_], [at])
                    for tt_ in range(n // 128):
                        li = t0 // 128 + tt_
                        pa, pb = pyr.next()
                        for hf, p in ((0, pa), (1, pb)):
                            hs = slice(hf * 512, (hf + 1) * 512)
                            for f in range(4):
                                self.mm(p[:, :], at[:, f, tt_ * 128:(tt_ + 1) * 128], d_[:, f, hs], f == 0, f == 3, [at, d_], [p])
                            if e == 0:
                                self.ts("dve", acc[:, li, hs], p[:, :], Wt[:, li, e:e + 1], None, ALU.mult, None, [p, Wtk[li]], [acck[li]])
                            else:
                                self.stt(acc[:, li, hs], p[:, :], Wt[:, li, e:e + 1], acc[:, li, hs], ALU.mult, ALU.add,
                                         [p, Wtk[li], acck[li]], [acck[li]])
            for li, tile in enumerate(tiles):
                isctx = (l == 0 and tile < 2)
                gt = gt_c if isctx else gt_l
                x_ = NR["xt"].next()
                self.dma("sp", x_[:], self.xres[b, tile * 128:(tile + 1) * 128, :], [self.kx[b]], [x_])
                t_ = NR["xn"].next()
                self.tt("dve", t_[:], acc[:, li, :], gt[:], ALU.mult, [acck[li], gt], [t_])
                self.tt("dve", t_[:], t_[:], x_[:], ALU.add, [t_, x_], [t_])
                if l == 0:
                    self.dma("sp", self.xres[b, tile * 128:(tile + 1) * 128, :], t_[:], [t_], [self.kx[b]], semkey=t_)
                    if ("xf0" in self.D_) and b == 0:
                        self.dma("sp", self.D_["xf0"][tile * 128:(tile + 1) * 128, :], t_[:], [t_], [self.kscr], semkey=t_)
                else:
                    self.dma("sp", self.out[b, (tile - 2) * 128:(tile - 1) * 128, :], t_[:], [t_], [self.kscr], semkey=t_)
            return S.flush()

    def rope(self, xin, xout, cos, sin, H, tm, R, W):
        x1, x2 = xin[:, :, :, 0, :], xin[:, :, :, 1, :]
        cb = cos.unsqueeze(1).broadcast_to([128, H, 2, 8])
        sb_ = sin.unsqueeze(1).broadcast_to([128, H, 2, 8])
        t1, t2 = tm
        v1 = t1[:, 0:H * 16].rearrange("p (h a f) -> p h a f", h=H, a=2)
        v2 = t2[:, 0:H * 16].rearrange("p (h a f) -> p h a f", h=H, a=2)
        self.tt("dve", v1, x1, cb, ALU.mult, R, [t1])
        self.tt("dve", v2, x2, sb_, ALU.mult, R, [t2])
        self.tt("dve", xout[:, :, :, 0, :], v1, v2, ALU.subtract, [t1, t2], W)
        self.tt("dve", v1, x2, cb, ALU.mult, R, [t1])
        self.tt("dve", v2, x1, sb_, ALU.mult, R, [t2])
        self.tt("dve", xout[:, :, :, 1, :], v1, v2, ALU.add, [t1, t2], W)

    def stage_mla(self, b):
        I, S = self.I, self.S
        l = 1
        NQT = SEQ // 128
        with ExitStack() as st:
            PB = [self.psb(st) for _ in range(8)]
            if "moe1" in self.stages:
                self.precast(1, b)
            A_l, B_l = self.mod_cols(st, l, 0, b)
            A_c, B_c = self.mod_cols(st, l, 0, 4)
            gt_l = self.gate_tile(st, l, 0, b)
            NR = self.norm_res(st, (PB[0], PB[1]))
            win = self.sb(st, [128, KC, 416], BF16, "win")
            self.dma("pool", win[:], I["mla_w_in"].rearrange("(c p) n -> p c n", p=128), [], [win])
            cT = self.sb(st, [128, 3, T], BF16, "cT")
            cTk = S.keys(NT)
            krr = self.sb(st, [128, NT, 32], F32, "krr")
            krk = S.keys(NT)
            sskr = self.sb(st, [128, NT], F32, "sskr")
            ssk = S.keys(NT)
            gk = self.sb(st, [128, 96], F32, "gk")
            gq = self.sb(st, [128, 96], F32, "gq")
            self.dma("sp", gk[:], I["mla_k_qknorm_g"].partition_broadcast(128), [], [gk])
            self.dma("sp", gq[:], I["mla_q_qknorm_g"].partition_broadcast(128), [], [gq])
            self.ts("dve", gq[:], gq[:], float(96 ** -0.5), None, ALU.mult, None, [gq], [gq])
            qng = self.sb(st, [128, 2], F32, "qng")
            kvg = self.sb(st, [128, 1], F32, "kvg")
            self.dma("sp", qng[:], I["mla_q_norm_g"].rearrange("(k p) -> p k", p=128), [], [qng])
            self.dma("sp", kvg[:], I["mla_kv_norm_g"].rearrange("(p o) -> p o", o=1), [], [kvg])
            hTt = self.sbr(st, 2, [128, KC, 128], BF16, "hTt")
            csr = self.sbr(st, 2, [128, 416], F32, "cs")
            cnr = self.sbr(st, 2, [128, 384], BF16, "cn")
            junk2 = self.sb(st, [128, 256], BF16, "junk2")
            s1 = {nm: self.sbr(st, 2, [128, 1], F32, nm) for nm in ("ssq", "sskv", "sdq", "sdkv", "rsq", "rskv")}
            kr1 = self.sbr(st, 2, [128, 32], F32, "kr1")
            cosr = self.sbr(st, 2, [128, 16], F32, "cos")
            sinr = self.sbr(st, 2, [128, 16], F32, "sin")
            rt = (self.sb(st, [128, 64], F32, "rt1"), self.sb(st, [128, 64], F32, "rt2"))
            cost = {}
            for tile in range(NT):
                A, Bc = (A_c, B_c) if tile < 2 else (A_l, B_l)
                hb = hTt.next()
                self.norm_tile(NR, self.xres[b, tile * 128:(tile + 1) * 128, :], [self.kx[b]], A, Bc, hb[:], [hb])
                p = PB[2 + tile % 2]
                for k in range(KC):
                    self.mm(p[:, 0:416], hb[:, k, :], win[:, k, :], k == 0, k == KC - 1, [hb, win], [p])
                cs = csr.next()
                self.cp("act", cs[:], p[:, 0:416], [p], [cs])
                t = {nm: r.next() for nm, r in s1.items()}
                self.act(junk2[:, 0:256], cs[:, 0:256], AF.Square, [cs], [junk2, t["ssq"]], accum_out=t["ssq"][:])
                self.act(junk2[:, 0:128], cs[:, 256:384], AF.Square, [cs], [junk2, t["sskv"]], accum_out=t["sskv"][:])
                self.act(junk2[:, 0:32], cs[:, 384:416], AF.Square, [cs], [junk2, ssk[tile]], accum_out=sskr[:, tile:tile + 1])
                self.act(t["sdq"][:], t["ssq"][:], AF.Sqrt, [t["ssq"], self.epsc], [t["sdq"]], bias=self.epsc[:, 0:1], scale=1.0 / 256)
                self.act(t["sdkv"][:], t["sskv"][:], AF.Sqrt, [t["sskv"], self.epsc], [t["sdkv"]], bias=self.epsc[:, 0:1], scale=1.0 / 128)
                self.recip(t["rsq"][:], t["sdq"][:], [t["sdq"]], [t["rsq"]])
                self.recip(t["rskv"][:], t["sdkv"][:], [t["sdkv"]], [t["rskv"]])
                cn = cnr.next()
                self.act(cn[:, 0:256], cs[:, 0:256], AF.Copy, [cs, t["rsq"]], [cn], scale=t["rsq"][:, 0:1])
                self.act(cn[:, 256:384], cs[:, 256:384], AF.Copy, [cs, t["rskv"]], [cn], scale=t["rskv"][:, 0:1])
                pT = PB[4 + tile % 2]
                pv = pT[:, 0:192].bitcast(BF16).rearrange("p (j t) -> p j t", j=3)
                for j in range(3):
                    self.tr(pv[:, j, :], cn[:, j * 128:(j + 1) * 128], self.identb[:], [cn, self.identb], [pT])
                self.cp("dve", cT[:, :, tile * 128:(tile + 1) * 128], pv, [pT], [cTk[tile]])
                k1 = kr1.next()
                self.tt("dve", k1[:], cs[:, 384:416], gk[:, 64:96], ALU.mult, [cs, gk], [k1])
                if tile < 2:
                    self.cp("dve", krr[:, tile, :], k1[:], [k1], [krk[tile]])
                else:
                    co, si = cosr.next(), sinr.next()
                    self.dma("sp", co[:], I["k_cos"][(tile - 2) * 128:(tile - 1) * 128, :], [], [co])
                    self.dma("sp", si[:], I["k_sin"][(tile - 2) * 128:(tile - 1) * 128, :], [], [si])
                    self.rope(k1[:].rearrange("p (h a g f) -> p h a g f", h=1, a=2, g=2),
                              krr[:, tile, :].rearrange("p (h a g f) -> p h a g f", h=1, a=2, g=2),
                              co[:].rearrange("p (a f) -> p a f", a=2), si[:].rearrange("p (a f) -> p a f", a=2),
                              1, rt, [k1, co, si], [krk[tile]])
            HG = 4
            oat = self.sb(st, [128, NQT, D], BF16, "oat")
            oak = S.keys(NQT)
            QT = self.sb(st, [128, HG, SEQ], BF16, "QT")
            QTk = S.keys(NQT)
            KT = self.sb(st, [128, HG, T], BF16, "KT")
            KTk = S.keys(NT)
            Vx = self.sb(st, [128, NT, HG, 65], BF16, "Vx")
            Vxk = S.keys(NT)
            self.memset("dve", Vx[:], 1.0, Vxk)
            wqf = self.sb(st, [128, 2, 384], F32, "wqf")
            wqb = self.sb(st, [128, 2, 384], BF16, "wqb")
            wkf = self.sb(st, [128, 512], F32, "wkf")
            wkb = self.sb(st, [128, 512], BF16, "wkb")
            kvfr = self.sbr(st, 2, [128, HG, 128], F32, "kvf")
            sqk = self.sb(st, [128, HG, 96], F32, "sqk")
            tmpk = self.sb(st, [128, HG, 96], F32, "tmpk")
            s4 = {nm: self.sbr(st, 2, [128, HG], F32, nm) for nm in ("ssn", "ss", "sd", "rs", "ssq4", "sd4", "rs4")}
            kbr = self.sbr(st, 2, [128, HG, 96], BF16, "kb")
            qfr = self.sbr(st, 2, [128, HG, 96], F32, "qf")
            qnr = self.sbr(st, 2, [128, HG, 96], F32, "qn")
            qbr = self.sbr(st, 2, [128, HG, 96], BF16, "qb")
            ptr_ = self.sbr(st, 3, [128, 512], BF16, "pt")
            recr = self.sbr(st, 4, [128, 1], F32, "rec")
            for hg in range(16 // HG):
                self.dma("sp", wqf[:], I["mla_w_qb"][:, hg * HG * 96:(hg + 1) * HG * 96].rearrange("(k p) n -> p k n", p=128), [], [wqf])
                self.tt("dve", wqb[:], wqf[:], qng[:].unsqueeze(2).broadcast_to([128, 2, HG * 96]), ALU.mult, [wqf, qng], [wqb])
                self.dma("sp", wkf[:], I["mla_w_kvb"][:, hg * HG * 128:(hg + 1) * HG * 128], [], [wkf])
                self.ts("dve", wkb[:], wkf[:], kvg[:, 0:1], None, ALU.mult, None, [wkf, kvg], [wkb])
                for tile in range(NT):
                    ts_ = slice(tile * 128, (tile + 1) * 128)
                    p = PB[tile % 2]
                    self.mm(p[:, :], cT[:, 2, ts_], wkb[:], True, True, [cTk[tile], wkb], [p])
                    kvf = kvfr.next()
                    self.cp("act", kvf[:], p[:, :].rearrange("p (h e) -> p h e", h=HG), [p], [kvf])
                    t = {nm: r.next() for nm, r in s4.items()}
                    self.tt("dve", sqk[:, :, 0:64], kvf[:, :, 0:64], kvf[:, :, 0:64], ALU.mult, [kvf], [sqk])
                    self.red(t["ssn"][:], sqk[:, :, 0:64], ALU.add, [sqk], [t["ssn"]])
                    self.ts("dve", t["ss"][:], t["ssn"][:], sskr[:, tile:tile + 1], None, ALU.add, None, [t["ssn"], ssk[tile]], [t["ss"]])
                    self.act(t["sd"][:], t["ss"][:], AF.Sqrt, [t["ss"], self.epsc], [t["sd"]], bias=self.epsc[:, 0:1], scale=1.0 / 96)
                    self.recip(t["rs"][:], t["sd"][:], [t["sd"]], [t["rs"]])
                    kb = kbr.next()
                    self.tt("dve", tmpk[:, :, 0:64], kvf[:, :, 0:64], t["rs"][:].unsqueeze(2).broadcast_to([128, HG, 64]), ALU.mult,
                            [kvf, t["rs"]], [tmpk])
                    self.tt("dve", kb[:, :, 0:64], tmpk[:, :, 0:64], gk[:, 0:64].unsqueeze(1).broadcast_to([128, HG, 64]), ALU.mult,
                            [tmpk, gk], [kb])
                    self.tt("dve", kb[:, :, 64:96], krr[:, tile, :].unsqueeze(1).broadcast_to([128, HG, 32]),
                            t["rs"][:].unsqueeze(2).broadcast_to([128, HG, 32]), ALU.mult, [krk[tile], t["rs"]], [kb])
                    pk = PB[2 + tile % 2]
                    pkv = pk[:, 0:256].bitcast(BF16).rearrange("p (h t) -> p h t", h=HG)
                    for h in range(HG):
                        self.tr(pkv[0:96, h, :], kb[:, h, :], self.identb[:], [kb, self.identb], [pk])
                    self.cp("act", KT[0:96, :, ts_], pkv[0:96, :, :], [pk], [KTk[tile]])
                    self.cp("dve", Vx[:, tile, :, 0:64], kvf[:, :, 64:128], [kvf], [Vxk[tile]])
                    if tile >= 2:
                        qt_ = tile - 2
                        pq = PB[4 + tile % 2]
                        for k in range(2):
                            self.mm(pq[:, 0:HG * 96], cT[:, k, ts_], wqb[:, k, :], k == 0, k == 1, [cTk[tile], wqb], [pq])
                        qf = qfr.next()
                        self.cp("act", qf[:], pq[:, 0:HG * 96].rearrange("p (h e) -> p h e", h=HG), [pq], [qf])
                        self.tt("dve", sqk[:], qf[:], qf[:], ALU.mult, [qf], [sqk])
                        self.red(t["ssq4"][:], sqk[:], ALU.add, [sqk], [t["ssq4"]])
                        self.act(t["sd4"][:], t["ssq4"][:], AF.Sqrt, [t["ssq4"], self.epsc], [t["sd4"]], bias=self.epsc[:, 0:1], scale=1.0 / 96)
                        self.recip(t["rs4"][:], t["sd4"][:], [t["sd4"]], [t["rs4"]])
                        qn = qnr.next()
                        self.tt("dve", qn[:], qf[:], t["rs4"][:].unsqueeze(2).broadcast_to([128, HG, 96]), ALU.mult, [qf, t["rs4"]], [qn])
                        self.tt("dve", qn[:], qn[:], gq[:].unsqueeze(1).broadcast_to([128, HG, 96]), ALU.mult, [qn, gq], [qn])
                        qb = qbr.next()
                        self.cp("dve", qb[:, :, 0:64], qn[:, :, 0:64], [qn], [qb])
                        co, si = cosr.next(), sinr.next()
                        self.dma("sp", co[:], I["k_cos"][qt_ * 128:(qt_ + 1) * 128, :], [], [co])
                        self.dma("sp", si[:], I["k_sin"][qt_ * 128:(qt_ + 1) * 128, :], [], [si])
                        self.rope(qn[:, :, 64:96].rearrange("p h (a g f) -> p h a g f", a=2, g=2),
                                  qb[:, :, 64:96].rearrange("p h (a g f) -> p h a g f", a=2, g=2),
                                  co[:].rearrange("p (a f) -> p a f", a=2), si[:].rearrange("p (a f) -> p a f", a=2),
                                  HG, rt, [qn, co, si], [qb])
                        pqt = PB[6 + tile % 2]
                        pqv = pqt[:, 0:256].bitcast(BF16).rearrange("p (h t) -> p h t", h=HG)
                        for h in range(HG):
                            self.tr(pqv[0:96, h, :], qb[:, h, :], self.identb[:], [qb, self.identb], [pqt])
                        self.cp("act", QT[0:96, :, qt_ * 128:(qt_ + 1) * 128], pqv[0:96, :, :], [pqt], [QTk[qt_]])
                for h in range(HG):
                    hh = hg * HG + h
                    for qb_ in range(SEQ // 512):
                        po = PB[4:8]
                        qk = [QTk[qb_ * 4 + i] for i in range(4)]
                        for kt in range(NT):
                            ps_ = PB[kt % 3]
                            self.mm(ps_[:, :], KT[0:96, h, kt * 128:(kt + 1) * 128], QT[0:96, h, qb_ * 512:(qb_ + 1) * 512], True, True,
                                    [KTk[kt]] + qk, [ps_])
                            pt = ptr_.next()
                            self.act(pt[:], ps_[:, :], AF.Exp, [ps_], [pt])
                            for q4 in range(4):
                                self.mm(po[q4][:, 0:65], pt[:, q4 * 128:(q4 + 1) * 128], Vx[:, kt, h, :], kt == 0, kt == NT - 1,
                                        [pt, Vxk[kt]], [po[q4]])
                        for q4 in range(4):
                            rec = recr.next()
                            self.recip(rec[:], po[q4][:, 64:65], [po[q4]], [rec])
                            self.ts("dve", oat[:, qb_ * 4 + q4, hh * 64:(hh + 1) * 64], po[q4][:, 0:64], rec[:, 0:1], None, ALU.mult, None,
                                    [po[q4], rec], [oak[qb_ * 4 + q4]])
            wo = self.sb(st, [128, KC, D], BF16, "wo")
            self.dma("pool", wo[:], I["mla_w_out"].rearrange("(c p) n -> p c n", p=128), [], [wo])
            oTr = self.sbr(st, 2, [128, KC, 128], BF16, "oT")
            for qt_ in range(NQT):
                tile = qt_ + 2
                pT = PB[qt_ % 2]
                pv = pT[:, :].bitcast(BF16).rearrange("p (k t) -> p k t", k=KC)
                for k in range(KC):
                    self.tr(pv[:, k, :], oat[:, qt_, k * 128:(k + 1) * 128], self.identb[:], [oak[qt_], self.identb], [pT])
                oT = oTr.next()
                self.cp("act", oT[:], pv, [pT], [oT])
                x_ = NR["xt"].next()
                self.dma("sp", x_[:], self.xres[b, tile * 128:(tile + 1) * 128, :], [self.kx[b]], [x_])
                t_ = NR["xn"].next()
                for hf in range(2):
                    p = PB[2 + hf]
                    hs = slice(hf * 512, (hf + 1) * 512)
                    for k in range(KC):
                        self.mm(p[:, :], oT[:, k, :], wo[:, k, hs], k == 0, k == KC - 1, [oT, wo], [p])
                    self.tt("dve", t_[:, hs], p[:, :], gt_l[:, hs], ALU.mult, [p, gt_l], [t_])
                self.tt("dve", t_[:], t_[:], x_[:], ALU.add, [t_, x_], [t_])
                self.dma("sp", self.xres[b, tile * 128:(tile + 1) * 128, :], t_[:], [t_], [self.kx[b]], semkey=t_)
                if ("xm1" in self.D_) and b == 0:
                    self.dma("sp", self.D_["xm1"][qt_ * 128:(qt_ + 1) * 128, :], t_[:], [t_], [self.kscr], semkey=t_)
            return S.flush()

    def moe_sparse(self, l):
        I, S, NB = self.I, self.S, self.NB
        tiles = [(b, t) for b in range(NB) for t in (range(NT) if l == 0 else range(2, NT))]
        NTL = len(tiles)
        NBLK = 2 * NTL + NEXP
        info = {}
        with ExitStack() as pst:
            d_i = [self.sb(pst, [128, NTL], I32, "d%di" % k) for k in range(2)]
            w_a = [self.sb(pst, [128, NTL], F32, "w%da" % k) for k in range(2)]
            idxw = self.sb(pst, [128, NBLK], I32, "idxw")
            kh2 = S.keys(NTL)
            kxs, kys = S.key(), S.key()
            kwb = self.kwb[l]
            with ExitStack() as st:
                PB = [self.psb(st) for _ in range(8)]
                Ltri = self.sb(st, [128, 128], F32, "Ltri")
                onesf = self.sb(st, [128, 128], F32, "onesf")
                self.memset("dve", onesf[:], 1.0, [onesf])
                self.memset("pool", Ltri[:], 1.0, [Ltri])
                S.op("pool", lambda e_: e_.affine_select(out=Ltri[:], in_=Ltri[:], pattern=[[1, 128]], compare_op=ALU.is_gt,
                                                         fill=0.0, base=0, channel_multiplier=-1), [Ltri], [Ltri])
                jvi = self.sb(st, [128, NBLK], I32, "jvi")
                jv = self.sb(st, [128, NBLK], F32, "jv")
                S.op("pool", lambda e_: e_.iota(jvi[:], pattern=[[128, NBLK]], base=0, channel_multiplier=0), [], [jvi])
                self.cp("dve", jv[:], jvi[:], [jvi], [jv])
                pii = self.sb(st, [128, 1], I32, "pii")
                pif = self.sb(st, [128, 1], F32, "pif")
                S.op("pool", lambda e_: e_.iota(pii[:], pattern=[[0, 1]], base=0, channel_multiplier=1), [], [pii])
                self.cp("dve", pif[:], pii[:], [pii], [pif])
                ones32 = self.sb(st, [128, NEXP], F32, "ones32")
                self.memset("dve", ones32[:], 1.0, [ones32])
                wr = self.sb(st, [128, KC, 36], F32, "wr")
                self.dma("sp", wr[:, :, 0:4], I["moe_w_group"][l].rearrange("(c p) g -> p c g", p=128), [], [wr])
                self.dma("sp", wr[:, :, 4:36], I["moe_w_expert"][l].rearrange("(c p) g -> p c g", p=128), [], [wr])
                grow = self.sb(st, [128, D], F32, "grow")
                self.dma("sp", grow[:], I["norm_ffn_g"][l, :].partition_broadcast(128), [], [grow])

                def rows_for(r):
                    Ar = self.sb(st, [128, D], F32, "Arow")
                    Br = self.sb(st, [128, D], F32, "Brow")
                    return Ar, Br

                def load_rows(Ar, Br, r):
                    self.dma("sp", Ar[:], self.mod[l, r, 4 * D:5 * D].partition_broadcast(128), [self.kmod], [Ar])
                    self.dma("sp", Br[:], self.mod[l, r, 3 * D:4 * D].partition_broadcast(128), [self.kmod], [Br])
                    self.stt(Ar[:], Ar[:], 1.0, grow[:], ALU.add, ALU.mult, [Ar, grow], [Ar])

                Ar_l, Br_l = rows_for(0)
                if l == 0:
                    Ar_c, Br_c = rows_for(4)
                    load_rows(Ar_c, Br_c, 4)
                    A_c, B_c = self.mod_cols(st, l, 1, 4)
                NR = self.norm_res(st, (PB[0], PB[1]), nhtf=2)
                sm = {nm: self.sbr(st, 2, [128, w], F32, nm) for nm, w in self.ROUTE_TMPS}
                OHs = self.sb(st, [128, NEXP], F32, "OHs")
                self.memset("dve", OHs[:], 0.0, [OHs])
                OHt = self.sbr(st, 2, [128, NEXP], F32, "OHt")
                Rall = self.sb(st, [128, NTL, NEXP], F32, "Rall")
                Rk = S.keys(NTL)
                oha = [self.sb(st, [128, NTL, NEXP], F32, "oh%da" % k) for k in range(2)]
                ohk = [S.keys(NTL) for _ in range(2)]
                t32 = self.sb(st, [128, D], F32, "t32")
                h2r = self.sbr(st, 2, [128, D], BF16, "h2b")
                cur_b = None
                cols = {}
                for ti, (b, tile) in enumerate(tiles):
                    if b != cur_b:
                        cur_b = b
                        load_rows(Ar_l, Br_l, b)
                        cols[b] = self.mod_cols(st, l, 1, b)
                    isctx = (l == 0 and tile < 2)
                    A, Bc = (A_c, B_c) if isctx else cols[b]
                    Ar, Br = (Ar_c, Br_c) if isctx else (Ar_l, Br_l)
                    hTf = self.norm_tile(NR, self.xres[b, tile * 128:(tile + 1) * 128, :], [self.kx[b]], A, Bc, None, [])
                    xn = self.last_xn
                    self.tt("dve", t32[:], xn[:], Ar[:], ALU.mult, [xn, Ar], [t32])
                    h2b = h2r.next()
                    self.tt("dve", h2b[:], t32[:], Br[:], ALU.add, [t32, Br], [h2b])
                    self.dma("sp", self.h2d[ti * 128:(ti + 1) * 128, :], h2b[:], [h2b], [kh2[ti]], semkey=h2b)
                    p = PB[2 + ti % 2]
                    for k in range(KC):
                        self.mm(p[:, 0:36], hTf[:, k, :], wr[:, k, :], k == 0, k == KC - 1, [hTf, wr], [p])
                    t = self.route_tile(sm, p)
                    self.cp("dve", oha[0][:, ti, :], t["oh1"][:], [t["oh1"]], [ohk[0][ti]])
                    self.cp("dve", oha[1][:, ti, :], t["oh2"][:], [t["oh2"]], [ohk[1][ti]])
                    self.cp("dve", w_a[0][:, ti:ti + 1], t["w1"][:], [t["w1"]], [w_a[0]])
                    self.cp("dve", w_a[1][:, ti:ti + 1], t["w2"][:], [t["w2"]], [w_a[1]])
                    oh = OHt.next()
                    self.tt("dve", oh[:], t["oh1"][:], t["oh2"][:], ALU.add, [t["oh1"], t["oh2"]], [oh])
                    pr = PB[4 + ti % 2]
                    self.mm(pr[:, 0:NEXP], Ltri[:], oh[:], True, False, [Ltri, oh], [pr])
                    self.mm(pr[:, 0:NEXP], onesf[:], OHs[:], False, True, [onesf, OHs], [pr])
                    self.cp("act", Rall[:, ti, :], pr[:, 0:NEXP], [pr], [Rk[ti]])
                    self.tt("dve", OHs[:], OHs[:], oh[:], ALU.add, [OHs, oh], [OHs])
                pc = PB[6]
                self.mm(pc[:, 0:NEXP], onesf[:], OHs[:], True, True, [onesf, OHs], [pc])
                cntf = self.sb(st, [128, NEXP], F32, "cntf")
                padf = self.sb(st, [128, NEXP], F32, "padf")
                pend = self.sb(st, [128, NEXP], F32, "pend")
                pstart = self.sb(st, [128, NEXP], F32, "pstart")
                cmpb = self.sb(st, [128, NBLK * NEXP], BF16, "cmpb")
                self.cp("dve", cntf[:], pc[:, 0:NEXP], [pc], [cntf])
                cv = cmpb[:].rearrange("p (e j) -> p e j", e=NEXP)
                self.tt("dve", cv, jv[:].unsqueeze(1).broadcast_to([128, NEXP, NBLK]),
                        cntf[:].unsqueeze(2).broadcast_to([128, NEXP, NBLK]), ALU.is_lt, [jv, cntf], [cmpb])
                self.red(padf[:], cv, ALU.add, [cmpb], [padf])
                self.ts("dve", padf[:], padf[:], 128.0, None, ALU.mult, None, [padf], [padf])
                S.op("dve", lambda e_: e_.tensor_tensor_scan(out=pend[:], data0=ones32[:], data1=padf[:], initial=0.0,
                                                             op0=ALU.mult, op1=ALU.add), [ones32, padf], [pend])
                self.tt("dve", pstart[:], pend[:], padf[:], ALU.subtract, [pend, padf], [pstart])
                bef = self.sb(st, [128, NBLK], F32, "bef")
                cv2 = cmpb[:].rearrange("p (j e) -> p j e", e=NEXP)
                self.tt("dve", cv2, pend[:].unsqueeze(1).broadcast_to([128, NBLK, NEXP]),
                        jv[:].unsqueeze(2).broadcast_to([128, NBLK, NEXP]), ALU.is_le, [pend, jv], [cmpb])
                self.red(bef[:], cv2, ALU.add, [cmpb], [bef])
                self.ts("dve", bef[:], bef[:], float(NEXP - 1), None, ALU.min, None, [bef], [bef])
                self.ts("dve", bef[:], bef[:], 128.0, pif[:, 0:1], ALU.mult, ALU.add, [bef, pif], [bef])
                self.cp("dve", idxw[:], bef[:], [bef], [idxw])
                dtmp = self.sbr(st, 2, [128, NEXP], F32, "dtmp")
                dtm2 = self.sbr(st, 2, [128, NEXP], F32, "dtm2")
                dfl = self.sbr(st, 2, [128, 1], F32, "dfl")
                for ti in range(NTL):
                    h2b = h2r.next()
                    self.dma("sp", h2b[:], self.h2d[ti * 128:(ti + 1) * 128, :], [kh2[ti]], [h2b])
                    d1 = dtmp.next()
                    self.tt("dve", d1[:], Rall[:, ti, :], pstart[:], ALU.add, [Rk[ti], pstart], [d1])
                    for k in range(2):
                        d2, df = dtm2.next(), dfl.next()
                        self.tt("dve", d2[:], d1[:], oha[k][:, ti, :], ALU.mult, [d1, ohk[k][ti]], [d2])
                        self.red(df[:], d2[:], ALU.add, [d2], [df])
                        self.cp("dve", d_i[k][:, ti:ti + 1], df[:], [df], [d_i[k]])
                        idx_ap = d_i[k][:, ti:ti + 1]
                        self._scatter(self.xs[:, :], idx_ap, h2b[:], [h2b, d_i[k]], [kxs])
                info["rs"] = S.flush()
            with ExitStack() as st:
                PB = [self.psb(st) for _ in range(8)]
                xbr = self.sbr(st, 2, [128, D], BF16, "xb")
                xTr = self.sbr(st, 2, [128, KC, 128], BF16, "xT")
                wgr = self.sbr(st, 2, [128, 4096], BF16, "wgs")
                wur = self.sbr(st, 2, [128, 4096], BF16, "wus")
                wdr = self.sbr(st, 2, [128, 4096], BF16, "wds")
                sgr = self.sbr(st, 2, [128, 512], BF16, "sgs")
                acr = self.sbr(st, 2, [128, 4, 128], BF16, "acs")
                ysr = self.sbr(st, 2, [128, D], F32, "ysb")
                pgu = Rot([(PB[2], PB[3]), (PB[4], PB[5])])
                for j in range(NBLK):
                    xb = xbr.next()
                    self.dma("sp", xb[:], self.xs[j * 128:(j + 1) * 128, :], [kxs], [xb])
                    wg, wu, wd = wgr.next(), wur.next(), wdr.next()
                    ia = idxw[:, j:j + 1]
                    self._gather(wg[:], self.wgb[l][:, :], ia, [idxw, kwb], [wg])
                    self._gather(wu[:], self.wub[l][:, :], ia, [idxw, kwb], [wu])
                    self._gather(wd[:], self.wdb[l][:, :], ia, [idxw, kwb], [wd])
                    pT = PB[j % 2]
                    pv = pT[:, :].bitcast(BF16).rearrange("p (k t) -> p k t", k=KC)
                    for k in range(KC):
                        self.tr(pv[:, k, :], xb[:, k * 128:(k + 1) * 128], self.identb[:], [xb, self.identb], [pT])
                    xT = xTr.next()
                    self.cp("act", xT[:], pv, [pT], [xT])
                    pg_, pu_ = pgu.next()
                    wgv = wg[:].rearrange("p (k f) -> p k f", k=KC)
                    wuv = wu[:].rearrange("p (k f) -> p k f", k=KC)
                    wdv = wd[:].rearrange("p (k f) -> p k f", k=4)
                    for fc in range(4):
                        fs = slice(fc * 128, (fc + 1) * 128)
                        for k in range(KC):
                            self.mm(pg_[:, fs], wgv[:, k, fs], xT[:, k, :], k == 0, k == KC - 1, [wg, xT], [pg_])
                    for fc in range(4):
                        fs = slice(fc * 128, (fc + 1) * 128)
                        for k in range(KC):
                            self.mm(pu_[:, fs], wuv[:, k, fs], xT[:, k, :], k == 0, k == KC - 1, [wu, xT], [pu_])
                    sg = sgr.next()
                    self.act(sg[:], pg_[:, :], AF.Silu, [pg_], [sg])
                    ac = acr.next()
                    self.tt("dve", ac[:].rearrange("p k t -> p (k t)"), sg[:], pu_[:, :], ALU.mult, [sg, pu_], [ac])
                    ysb = ysr.next()
                    for hf, p in ((0, PB[6]), (1, PB[7])):
                        hs = slice(hf * 512, (hf + 1) * 512)
                        for k in range(4):
                            self.mm(p[:, :], ac[:, k, :], wdv[:, k, hs], k == 0, k == 3, [ac, wd], [p])
                        if hf == 0:
                            self.cp("act", ysb[:, hs], p[:, :], [p], [ysb])
                        else:
                            self.cp("dve", ysb[:, hs], p[:, :], [p], [ysb])
                    self.dma("sp", self.ys[j * 128:(j + 1) * 128, :], ysb[:], [ysb], [kys], semkey=ysb)
                info["e"] = S.flush()
            with ExitStack() as st:
                gts = {}
                y1r = self.sbr(st, 2, [128, D], F32, "y1")
                y2r = self.sbr(st, 2, [128, D], F32, "y2")
                xr = self.sbr(st, 2, [128, D], F32, "xc")
                tr_ = self.sbr(st, 2, [128, D], F32, "tc")
                if l == 0:
                    gts[4] = self.gate_tile(st, l, 1, 4)
                for ti, (b, tile) in enumerate(tiles):
                    if b not in gts:
                        gts[b] = self.gate_tile(st, l, 1, b)
                    isctx = (l == 0 and tile < 2)
                    gt = gts[4] if isctx else gts[b]
                    y1, y2, x_, t_ = y1r.next(), y2r.next(), xr.next(), tr_.next()
                    self._gather(y1[:], self.ys[:, :], d_i[0][:, ti:ti + 1], [d_i[0], kys], [y1])
                    self._gather(y2[:], self.ys[:, :], d_i[1][:, ti:ti + 1], [d_i[1], kys], [y2])
                    self.dma("sp", x_[:], self.xres[b, tile * 128:(tile + 1) * 128, :], [self.kx[b]], [x_])
                    self.ts("dve", t_[:], y1[:], w_a[0][:, ti:ti + 1], None, ALU.mult, None, [y1, w_a[0]], [t_])
                    self.stt(t_[:], y2[:], w_a[1][:, ti:ti + 1], t_[:], ALU.mult, ALU.add, [y2, w_a[1], t_], [t_])
                    self.tt("dve", t_[:], t_[:], gt[:], ALU.mult, [t_, gt], [t_])
                    self.tt("dve", t_[:], t_[:], x_[:], ALU.add, [t_, x_], [t_])
                    if l == 0:
                        self.dma("sp", self.xres[b, tile * 128:(tile + 1) * 128, :], t_[:], [t_], [self.kx[b]], semkey=t_)
                        if ("xf0" in self.D_) and b == 0:
                            self.dma("sp", self.D_["xf0"][tile * 128:(tile + 1) * 128, :], t_[:], [t_], [self.kscr], semkey=t_)
                    else:
                        self.dma("sp", self.out[b, (tile - 2) * 128:(tile - 1) * 128, :], t_[:], [t_], [self.kscr], semkey=t_)
                info["c"] = S.flush()
        return info

    def precast(self, l, b):
        I = self.I
        per = (NEXP + self.NB - 1) // self.NB
        if not hasattr(self, "pck"):
            self.pck = Rot(self.S.keys(8))
        for e in range(b * per, min(NEXP, (b + 1) * per)):
            rows = slice(e * 128, (e + 1) * 128)
            for dst, src, kk in ((self.wgb[l], "moe_w_gate", 8), (self.wub[l], "moe_w_up", 8), (self.wdb[l], "moe_w_down", 4)):
                self.dma("pool", dst[rows, :].rearrange("p (k f) -> p k f", k=kk),
                         I[src][l, e].rearrange("(k p) f -> p k f", p=128), [], [], semkey=self.pck.next())

    def _gather(self, out, src, idx_ap, R, W):
        nrow = src.shape[0]
        self.S.dma("pool", lambda e: e.indirect_dma_start(out=out, out_offset=None, in_=src,
                                                          in_offset=bass.IndirectOffsetOnAxis(ap=idx_ap, axis=0)), R, W,
                   nbytes=128 * self._n(out) * 2, indirect=True)

    def _scatter(self, dst, idx_ap, in_, R, W):
        nrow = dst.shape[0]
        self.S.dma("pool", lambda e: e.indirect_dma_start(out=dst, out_offset=bass.IndirectOffsetOnAxis(ap=idx_ap, axis=0),
                                                          in_=in_, in_offset=None),
                   R, W, semkey=R[0], nbytes=128 * 2048, indirect=True)

    def build(self):
        with ExitStack() as gst:
            gst.enter_context(self.nc.allow_non_contiguous_dma(reason="small strided parameter loads"))
            self.setup_consts(gst)
            info = {}
            if "mod" in self.stages:
                info["mod"] = self.stage_mod()
            for b in range(self.NB):
                if "hgrn" in self.stages:
                    info["hgrn%d" % b] = self.stage_hgrn(b)
            if "moe0" in self.stages:
                info["moe0"] = self.moe_sparse(0)
            for b in range(self.NB):
                if "mla" in self.stages:
                    info["mla%d" % b] = self.stage_mla(b)
            if "moe1" in self.stages:
                info["moe1"] = self.moe_sparse(1)
            self.info = info
        self.S.close()
        return self.nc


def host_consts():
    s = np.arange(128)
    same = (s[:, None] // 32) == (s[None, :] // 32)
    maskf = (same & (s[:, None] <= s[None, :])).astype(np.float32)
    maskb = (same & (s[:, None] >= s[None, :])).astype(np.float32)
    bm = ((s[:, None] // 32) == np.arange(4)[None, :]).astype(np.float32)[:, :, None].repeat(128, axis=2)
    t = np.arange(SEQ)
    row, col = t // 64, t % 64
    inv = (10000.0 ** (-np.arange(0, 16, 2, dtype=np.float32) / 16)).astype(np.float32)
    ang = np.stack([row, col], axis=-1).astype(np.float32)[..., None] * inv
    cos = np.cos(ang).astype(np.float32).reshape(SEQ, 16)
    sin = np.sin(ang).astype(np.float32).reshape(SEQ, 16)
    return {"k_maskf": maskf, "k_maskb": maskb, "k_bm": np.ascontiguousarray(bm), "k_bmc": np.ascontiguousarray(bm[:, :, 0]),
            "k_cos": cos, "k_sin": sin}


def make_in_maps(inputs, NB, ncores, used=None):
    sq = {"hg_w_in": "hg_w_in", "hg_lower_bounds": "hg_lb", "hg_out_norm_g": "hg_out_norm_g", "hg_w_out": "hg_w_out",
          "mla_w_in": "mla_w_in", "mla_q_norm_g": "mla_q_norm_g", "mla_kv_norm_g": "mla_kv_norm_g", "mla_w_qb": "mla_w_qb",
          "mla_w_kvb": "mla_w_kvb", "mla_q_qknorm_g": "mla_q_qknorm_g", "mla_k_qknorm_g": "mla_k_qknorm_g", "mla_w_out": "mla_w_out"}
    shared = {}
    for k, v in inputs.items():
        v = np.asarray(v, dtype=np.float32)
        if k in ("x", "c", "ctx"):
            continue
        if k == "hg_lower_bounds":
            shared["hg_lb"] = np.ascontiguousarray(v)
        elif k in sq:
            shared[sq[k]] = np.ascontiguousarray(v.reshape(v.shape[1:]))
        else:
            shared[k] = np.ascontiguousarray(v)
    shared.update(host_consts())
    maps = []
    for i in range(ncores):
        m = dict(shared)
        for k in ("x", "c", "ctx"):
            m[k] = np.ascontiguousarray(np.asarray(inputs[k], dtype=np.float32)[i * NB:(i + 1) * NB])
        if used is not None:
            m = {k: v for k, v in m.items() if k in used}
        maps.append(m)
    return maps


def kernel(**inputs):
    NB = 4
    kb = KB(NB=NB)
    nc = kb.build()
    maps = make_in_maps(inputs, NB, 8, used=set(kb.I.keys()))
    res = run_bass_kernel_spmd(nc, maps, core_ids=list(range(8)))
    return np.concatenate([r["out"] for r in res.results], axis=0).astype(np.float32)
```

```python
import numpy as np
import os as _os
_F = lambda k: _os.environ.get(k, '1') == '1'
import concourse.bass as bass
import concourse.mybir as mybir
from concourse.bass_utils import run_bass_kernel_spmd
from contextlib import ExitStack

F32 = mybir.dt.float32
BF16 = mybir.dt.bfloat16
I32 = mybir.dt.int32
AF = mybir.ActivationFunctionType
ALU = mybir.AluOpType
AX = mybir.AxisListType

ENGS = ("pe", "act", "dve", "pool", "sp")

D = 1024
KC = 8
CTX = 256
SEQ = 2048
T = CTX + SEQ
NT = T // 128
EPS = 1e-6
NEXP = 32
FF = 512
BIG = 1.0e30


class Key:
    __slots__ = ("w", "r", "dsem", "dcnt", "excl")

    def __init__(self):
        self.w = None
        self.r = []
        self.dsem = None
        self.dcnt = 0
        self.excl = False


class Tl:
    __slots__ = ("t", "k")

    def __init__(self, t, k):
        self.t = t
        self.k = k

    def __getitem__(self, idx):
        return self.t[idx]


def _k(x):
    return x.k if isinstance(x, Tl) else x


class Rot:
    def __init__(self, items):
        self.items = items
        self.i = 0

    def next(self):
        it = self.items[self.i % len(self.items)]
        self.i += 1
        return it


class Sched:
    def __init__(self, nc, n_dma_sems=80):
        self.nc = nc
        self.stack = ExitStack()
        self.esem = {e: self.stack.enter_context(nc.semaphore("es_" + e)) for e in ENGS}
        self.ecnt = {e: 0 for e in ENGS}
        self.dpool = [[self.stack.enter_context(nc.semaphore("ds%d" % i)), 0] for i in range(n_dma_sems)]
        self.dfree = list(range(n_dma_sems))
        self.all_keys = []
        self.reorder = True
        self._reset_stage()

    def _reset_stage(self):
        self.recs = []
        self.dlast = {}

    def key(self):
        k = Key()
        self.all_keys.append(k)
        return k

    def keys(self, n):
        return [self.key() for _ in range(n)]

    def _deps(self, reads, writes):
        deps = set()
        for t in reads:
            if t.w is not None:
                deps.add(t.w)
        for t in writes:
            if t.w is not None:
                deps.add(t.w)
            deps.update(t.r)
        return deps

    def _add(self, eng, fn, reads, writes, cost, dma, lat):
        reads = [_k(x) for x in reads]
        writes = [_k(x) for x in writes]
        ex = [t for t in reads if t.excl and t not in writes]
        if ex:
            reads = [t for t in reads if not t.excl]
            writes = writes + ex
        deps = self._deps(reads, writes)
        i = len(self.recs)
        if dma is not None:
            prev = self.dlast.get(dma)
            if prev is not None:
                deps.add(prev)
            self.dlast[dma] = i
        self.recs.append({"eng": eng, "fn": fn, "deps": deps, "cost": cost, "dma": dma, "lat": lat, "inc": False})
        for t in reads:
            t.r.append(i)
        for t in writes:
            t.w = i
            t.r = []
        return i

    def op(self, eng, fn, reads=(), writes=(), cost=0.2):
        return self._add(eng, fn, reads, writes, cost, None, 0.0)

    def dma(self, eng, fn, reads=(), writes=(), semkey=None, nbytes=0, indirect=False):
        rk = [_k(x) for x in reads]
        wk = [_k(x) for x in writes]
        sk = _k(semkey) if semkey is not None else (wk[0] if wk else rk[0])
        if sk.dsem is None:
            sk.dsem = self.dfree.pop()
        i = self._add(eng, fn, rk, wk, 0.8 if indirect else 0.07, sk.dsem, 2.0 + nbytes / 150e3)
        return i

    def _schedule(self):
        recs = self.recs
        n = len(recs)
        users = [[] for _ in range(n)]
        ndep = [0] * n
        for i, r in enumerate(recs):
            ndep[i] = len(r["deps"])
            for d in r["deps"]:
                users[d].append(i)
        import heapq
        ready = {e: [] for e in ENGS}
        fin = [0.0] * n
        rt = [0.0] * n
        for i, r in enumerate(recs):
            if ndep[i] == 0:
                heapq.heappush(ready[r["eng"]], (0.0, i))
        free = {e: 0.0 for e in ENGS}
        order = {e: [] for e in ENGS}
        done = 0
        while done < n:
            best = None
            for e in ENGS:
                h = ready[e]
                if not h:
                    continue
                t0 = free[e]
                cand = None
                if h[0][0] <= t0:
                    tmp = []
                    while h and h[0][0] <= t0:
                        tmp.append(heapq.heappop(h))
                    ci = min(tmp, key=lambda x: x[1])
                    for x in tmp:
                        if x is not ci:
                            heapq.heappush(h, x)
                    cand = (t0, ci[1], ci)
                else:
                    x = h[0]
                    cand = (x[0], x[1], None)
                if best is None or (cand[0], cand[1]) < (best[0][0], best[0][1]):
                    if best is not None and best[0][2] is not None:
                        heapq.heappush(ready[best[1]], best[0][2])
                    best = (cand, e)
                elif cand[2] is not None:
                    heapq.heappush(h, cand[2])
            (start, i, popped), e = best
            if popped is None:
                heapq.heappop(ready[e])
            r = recs[i]
            free[e] = start + r["cost"]
            fin[i] = start + r["cost"] + r["lat"]
            order[e].append(i)
            done += 1
            for u in users[i]:
                ndep[u] -= 1
                ru = recs[u]
                lat = 0.05 if ru["eng"] == e else 0.25
                if fin[i] + lat > rt[u]:
                    rt[u] = fin[i] + lat
                if ndep[u] == 0:
                    heapq.heappush(ready[ru["eng"]], (rt[u], u))
        return order, max(fin) if n else 0.0

    def flush(self):
        nc = self.nc
        recs = self.recs
        if self.reorder:
            order, est = self._schedule()
        else:
            order = {e: [i for i, r in enumerate(recs) if r["eng"] == e] for e in ENGS}
            est = 0.0
        dval = {}
        dtot = {}
        for i, r in enumerate(recs):
            if r["dma"] is not None:
                c = self.dpool[r["dma"]][1] + 16
                self.dpool[r["dma"]][1] = c
                dval[i] = c
                dtot[r["dma"]] = c
        for i, r in enumerate(recs):
            for d in r["deps"]:
                rd = recs[d]
                if rd["dma"] is None and (rd["eng"] != r["eng"] or r["eng"] in ("act", "dve", "pool")):
                    rd["inc"] = True
        eval_ = {}
        for e in ENGS:
            c = self.ecnt[e]
            for i in order[e]:
                if recs[i]["inc"]:
                    c += 1
                eval_[i] = c
            self.ecnt[e] = c
        engobj = {"pe": "tensor", "act": "scalar", "dve": "vector", "pool": "gpsimd", "sp": "sync"}
        esem, dpool = self.esem, self.dpool

        def mk(e):
            def body(eng):
                seen = {}
                for i in order[e]:
                    r = recs[i]
                    waits = {}
                    for d in r["deps"]:
                        rd = recs[d]
                        if rd["dma"] is not None:
                            k, v = ("d", rd["dma"]), dval[d]
                        elif rd["eng"] != e or e in ("act", "dve", "pool"):
                            k, v = ("e", rd["eng"]), eval_[d]
                        else:
                            continue
                        if seen.get(k, -1) >= v:
                            continue
                        if waits.get(k, -1) < v:
                            waits[k] = v
                    for k, v in waits.items():
                        seen[k] = v
                        if k[0] == "e":
                            eng.wait_ge(esem[k[1]], v)
                        else:
                            eng.wait_ge(dpool[k[1]][0], v)
                    ins = r["fn"](eng)
                    if r["dma"] is not None:
                        ins.then_inc(dpool[r["dma"]][0], 16)
                    elif r["inc"]:
                        ins.then_inc(esem[e], 1)
                if e == "sp":
                    for idx, v in dtot.items():
                        if seen.get(("d", idx), -1) < v:
                            eng.wait_ge(dpool[idx][0], v)
            return body

        with nc.Block() as block:
            for e in ENGS:
                if order[e] or (e == "sp" and dtot):
                    getattr(block, engobj[e])(mk(e))
        for k in self.all_keys:
            if k.dsem is not None:
                self.dfree.append(k.dsem)
                k.dsem = None
            k.w = None
            k.r = []
        n = {e: len(order[e]) for e in ENGS}
        n["est_us"] = round(est, 1)
        self._reset_stage()
        return n

    def close(self):
        self.stack.close()


class KB:
    def __init__(self, NB=4, stages=("mod", "hgrn", "moe0", "mla", "moe1"), dbg=()):
        self.NB = NB
        self.stages = stages
        self.dbg = dbg
        nc = bass.Bass("TRN2", target_bir_lowering=False)
        self.nc = nc
        self.S = Sched(nc)
        self.uid = 0
        shapes = {
            "x": (NB, SEQ, D), "c": (NB, D), "ctx": (NB, CTX, D), "c_ctx": (D,),
            "ada_w": (2, D, 6 * D), "ada_b": (2, 6 * D), "norm_mix_g": (2, D), "norm_ffn_g": (2, D),
            "hg_w_in": (D, 5 * D), "hg_lb": (2, 3, D), "hg_out_norm_g": (128,), "hg_w_out": (D, D),
            "mla_w_in": (D, 416), "mla_q_norm_g": (256,), "mla_kv_norm_g": (128,), "mla_w_qb": (256, 1536),
            "mla_w_kvb": (128, 2048), "mla_q_qknorm_g": (96,), "mla_k_qknorm_g": (96,), "mla_w_out": (D, D),
            "moe_w_group": (2, D, 4), "moe_w_expert": (2, D, 32), "moe_w_gate": (2, NEXP, D, FF),
            "moe_w_up": (2, NEXP, D, FF), "moe_w_down": (2, NEXP, FF, D),
            "k_maskf": (128, 128), "k_maskb": (128, 128), "k_bm": (128, 4, 128), "k_bmc": (128, 4), "k_cos": (SEQ, 16), "k_sin": (SEQ, 16),
        }

        class LazyIn(dict):
            def __missing__(d_, name):
                ap = nc.dram_tensor(name, list(shapes[name]), F32, kind="ExternalInput").ap()
                d_[name] = ap
                return ap

        I = LazyIn()
        self.I = I
        self.out = nc.dram_tensor("out", [NB, SEQ, D], F32, kind="ExternalOutput").ap()
        self.xres = nc.dram_tensor("xres", [NB, T, D], F32).ap()
        self.mod = nc.dram_tensor("modv", [2, 5, 6 * D], F32).ap()
        self.cT_d = nc.dram_tensor("cT_d", [128, 3, T], BF16).ap()
        self.krr_d = nc.dram_tensor("krr_d", [128, NT, 32], F32).ap()
        self.sskr_d = nc.dram_tensor("sskr_d", [128, NT], F32).ap()
        NTLmax = NB * NT
        self.NBLKmax = 2 * NTLmax + NEXP
        self.h2d = nc.dram_tensor("h2d", [NTLmax * 128, D], BF16).ap()
        self.xs = nc.dram_tensor("xs", [self.NBLKmax * 128, D], BF16).ap()
        self.ys = nc.dram_tensor("ys", [self.NBLKmax * 128, D], F32).ap()
        self.wgb = [nc.dram_tensor("wgb%d" % l_, [NEXP * 128, 4096], BF16).ap() for l_ in range(2)]
        self.wub = [nc.dram_tensor("wub%d" % l_, [NEXP * 128, 4096], BF16).ap() for l_ in range(2)]
        self.wdb = [nc.dram_tensor("wdb%d" % l_, [NEXP * 128, 4096], BF16).ap() for l_ in range(2)]
        self.kwb = [self.S.key() for _ in range(2)]
        self.kx = [self.S.keys(NT) for _ in range(NB)]
        self.kmod = self.S.key()
        self.kscr = self.S.key()
        self.D_ = {}
        for name, shape in dbg:
            self.D_[name] = nc.dram_tensor("dbg_" + name, list(shape), F32, kind="ExternalOutput").ap()

    def sb(self, st, shape, dt, nm="t"):
        self.uid += 1
        t = st.enter_context(self.nc.sbuf_tensor("%s_%d" % (nm, self.uid), list(shape), dt))
        return Tl(t, self.S.key())

    def sbr(self, st, n, shape, dt, nm="r"):
        return Rot([self.sb(st, shape, dt, nm) for _ in range(n)])

    def psb(self, st, nm="ps"):
        self.uid += 1
        t = st.enter_context(self.nc.psum_tensor("%s_%d" % (nm, self.uid), [128, 512], F32))
        k = self.S.key()
        k.excl = True
        return Tl(t, k)

    def psb2(self, st, nm="ps2"):
        self.uid += 1
        t = st.enter_context(self.nc.psum_tensor("%s_%d" % (nm, self.uid), [128, 1024], F32))
        k = self.S.key()
        k.excl = True
        return Tl(t, k)

    @staticmethod
    def _n(ap):
        n = 1
        for d in ap.shape[1:]:
            n *= d
        return n

    def mm(self, out, lhsT, rhs, start, stop, R, W):
        c = max(64, self._n(out)) / 2400.0 + 0.02
        if rhs.dtype == F32:
            c *= 4
        self.S.op("pe", lambda e: e.matmul(out, lhsT=lhsT, rhs=rhs, start=start, stop=stop), R, W, cost=c)

    def tr(self, out, in_, ident, R, W):
        self.S.op("pe", lambda e: e.transpose(out=out, in_=in_, identity=ident), R, W, cost=0.09)

    def act(self, out, in_, func, R, W, bias=None, scale=None, accum_out=None):
        kw = {}
        if bias is not None:
            kw["bias"] = bias
        if scale is not None:
            kw["scale"] = scale
        if accum_out is not None:
            kw["accum_out"] = accum_out
        self.S.op("act", lambda e: e.activation(out=out, in_=in_, func=func, **kw), R, W, cost=0.22 + self._n(out) / 1200.0)

    def ts(self, eng, out, in0, s1, s2, op0, op1, R, W):
        if op1 is None:
            self.S.op(eng, lambda e: e.tensor_scalar(out=out, in0=in0, scalar1=s1, scalar2=None, op0=op0), R, W, cost=self._c(eng, out))
        else:
            self.S.op(eng, lambda e: e.tensor_scalar(out=out, in0=in0, scalar1=s1, scalar2=s2, op0=op0, op1=op1), R, W, cost=self._c(eng, out))

    def tt(self, eng, out, in0, in1, op, R, W):
        self.S.op(eng, lambda e: e.tensor_tensor(out=out, in0=in0, in1=in1, op=op), R, W, cost=self._c(eng, out, 1.5))

    def stt(self, out, in0, scalar, in1, op0, op1, R, W):
        self.S.op("dve", lambda e: e.scalar_tensor_tensor(out=out, in0=in0, scalar=scalar, in1=in1, op0=op0, op1=op1), R, W,
                  cost=self._c("dve", out, 1.5))

    def cp(self, eng, out, in_, R, W):
        if eng == "act":
            self.S.op("act", lambda e: e.activation(out=out, in_=in_, func=AF.Copy), R, W, cost=0.22 + self._n(out) / 1200.0)
        else:
            self.S.op(eng, lambda e: e.tensor_copy(out=out, in_=in_), R, W, cost=self._c(eng, out))

    def red(self, out, in_, op, R, W, negate=None):
        self.S.op("dve", lambda e: e.tensor_reduce(out=out, in_=in_, axis=AX.X, op=op, negate=negate), R, W, cost=self._c("dve", in_))

    def recip(self, out, in_, R, W):
        self.S.op("dve", lambda e: e.reciprocal(out=out, in_=in_), R, W, cost=self._c("dve", out, 8.0))

    def memset(self, eng, ap, val, W):
        self.S.op(eng, lambda e: e.memset(ap, val), (), W, cost=self._c(eng, ap))

    def _c(self, eng, ap, mult=1.0):
        n = self._n(ap)
        if eng == "pool":
            return 0.25 + n * mult / 500.0
        return 0.1 + n * mult / 960.0

    def dma(self, q, out, in_, R, W, semkey=None):
        nb = out.shape[0] * self._n(out) * 4
        self.S.dma(q, lambda e: e.dma_start(out=out, in_=in_), R, W, semkey=semkey, nbytes=nb)

    def sumsq(self, junk, in_, acc, R, W):
        self.act(junk, in_, AF.Square, R, W, accum_out=acc)

    def setup_consts(self, st):
        nc, S = self.nc, self.S
        self.identf = self.sb(st, [128, 128], F32, "identf")
        self.identb = self.sb(st, [128, 128], BF16, "identb")
        self.onesb = self.sb(st, [128, 128], BF16, "onesb")
        identf = self.identf
        self.memset("pool", identf[:], 0.0, [identf])
        S.op("pool", lambda e: e.affine_select(out=identf[:], in_=identf[:], pattern=[[-1, 128]], compare_op=ALU.not_equal,
                                               fill=1.0, base=0, channel_multiplier=1), [identf], [identf])
        self.cp("dve", self.identb[:], identf[:], [identf], [self.identb])
        self.memset("dve", self.onesb[:], 1.0, [self.onesb])
        self.epsc = self.sb(st, [128, 1], F32, "epsc")
        self.memset("dve", self.epsc[:], EPS, [self.epsc])
        self.onec = self.sb(st, [128, 1], F32, "onec")
        self.memset("dve", self.onec[:], 1.0, [self.onec])

    def stage_mod(self):
        I, NB = self.I, self.NB
        with ExitStack() as st:
            cT = self.sb(st, [128, KC, 5], F32, "cT")
            cs = self.sb(st, [128, KC, 5], F32, "cs")
            self.memset("dve", cT[:], 0.0, [cT])
            for r in range(NB):
                self.dma("sp", cT[:, :, r], I["c"][r, :].rearrange("(c p) -> p c", p=128), [], [cT])
            self.dma("sp", cT[:, :, 4], I["c_ctx"].rearrange("(c p) -> p c", p=128), [], [cT])
            self.act(cs[:], cT[:], AF.Silu, [cT], [cs])
            wrot = self.sbr(st, 3, [128, KC, 512], F32, "adaw")
            ps = Rot([self.psb(st) for _ in range(2)])
            for l in range(2):
                bt = self.sb(st, [5, 6 * D], F32, "adab")
                ms = self.sb(st, [5, 6 * D], F32, "modsb")
                self.dma("sp", bt[:], I["ada_b"][l, :].partition_broadcast(5), [], [bt])
                for n in range(12):
                    w = wrot.next()
                    self.dma("sp", w[:], I["ada_w"][l, :, n * 512:(n + 1) * 512].rearrange("(c p) n -> p c n", p=128), [], [w])
                    p = ps.next()
                    for k in range(KC):
                        self.mm(p[0:5, :], cs[:, k, :], w[:, k, :], k == 0, k == KC - 1, [cs, w], [p])
                    self.tt("dve", ms[:, n * 512:(n + 1) * 512], p[0:5, :], bt[:, n * 512:(n + 1) * 512], ALU.add, [p, bt], [ms])
                self.dma("sp", self.mod[l], ms[:], [ms], [self.kmod], semkey=ms)
            return self.S.flush()

    def mod_cols(self, st, l, m, r):
        I = self.I
        g = I["norm_mix_g"] if m == 0 else I["norm_ffn_g"]
        gc = self.sb(st, [128, KC], F32, "gc")
        sc = self.sb(st, [128, KC], F32, "sc")
        sh = self.sb(st, [128, KC], F32, "sh")
        A = self.sb(st, [128, KC], F32, "A")
        self.dma("sp", gc[:], g[l, :].rearrange("(c p) -> p c", p=128), [], [gc])
        self.dma("sp", sc[:], self.mod[l, r, (3 * m + 1) * D:(3 * m + 2) * D].rearrange("(c p) -> p c", p=128), [self.kmod], [sc])
        self.dma("sp", sh[:], self.mod[l, r, (3 * m) * D:(3 * m + 1) * D].rearrange("(c p) -> p c", p=128), [self.kmod], [sh])
        self.stt(A[:], sc[:], 1.0, gc[:], ALU.add, ALU.mult, [sc, gc], [A])
        return A, sh

    def gate_tile(self, st, l, m, r):
        gt = self.sb(st, [128, D], F32, "gt")
        self.dma("sp", gt[:], self.mod[l, r, (3 * m + 2) * D:(3 * m + 3) * D].partition_broadcast(128), [self.kmod], [gt])
        return gt

    def norm_res(self, st, pbanks, junk=None, nxn=1, nhtf=1):
        R = {}
        R["xt"] = self.sbr(st, 2, [128, D], F32, "xt")
        R["xn"] = self.sbr(st, nxn, [128, D], F32, "xn")
        R["junk"] = junk if junk is not None else self.sb(st, [128, D], BF16, "junk")
        R["ss"] = self.sbr(st, 2, [128, 1], F32, "ss")
        R["sd"] = self.sbr(st, 2, [128, 1], F32, "sd")
        R["rs"] = self.sbr(st, 2, [128, 1], F32, "rs")
        R["hTf"] = self.sbr(st, nhtf, [128, KC, 128], F32, "hTf")
        R["pb"] = pbanks
        return R

    def norm_tile(self, R, src_ap, src_keys, A, Bc, dst_ap, dst_keys):
        xt = R["xt"].next()
        xn = R["xn"].next()
        ss = R["ss"].next()
        sd = R["sd"].next()
        rs = R["rs"].next()
        hTf = R["hTf"].next()
        junk = R["junk"]
        pa, pb = R["pb"]
        self.dma("sp", xt[:], src_ap, src_keys, [xt])
        self.sumsq(junk[:, 0:D], xt[:], ss[:], [xt], [junk, ss])
        self.act(sd[:], ss[:], AF.Sqrt, [ss, self.epsc], [sd], bias=self.epsc[:, 0:1], scale=1.0 / D)
        self.recip(rs[:], sd[:], [sd], [rs])
        self.act(xn[:], xt[:], AF.Copy, [xt, rs], [xn], scale=rs[:, 0:1])
        for k in range(KC):
            p = pa if k < 4 else pb
            self.tr(p[:, (k % 4) * 128:(k % 4 + 1) * 128], xn[:, k * 128:(k + 1) * 128], self.identf[:], [xn, self.identf], [p])
        for k in range(KC):
            p = pa if k < 4 else pb
            src = p[:, (k % 4) * 128:(k % 4 + 1) * 128]
            if k < 4:
                self.ts("dve", hTf[:, k, :], src, A[:, k:k + 1], Bc[:, k:k + 1], ALU.mult, ALU.add, [p, A, Bc], [hTf])
            else:
                self.act(hTf[:, k, :], src, AF.Identity, [p, A, Bc], [hTf], bias=Bc[:, k:k + 1], scale=A[:, k:k + 1])
        if dst_ap is not None:
            self.cp("dve", dst_ap, hTf[:], [hTf], dst_keys)
        self.last_xn = xn
        return hTf

    def src_l0(self, b, tile):
        if tile < 2:
            return self.I["ctx"][b, tile * 128:(tile + 1) * 128, :]
        return self.I["x"][b, (tile - 2) * 128:(tile - 1) * 128, :]

    def stage_hgrn(self, b):
        I, S = self.I, self.S
        l = 0
        with ExitStack() as st:
            PB = [self.psb(st) for _ in range(8)]
            if "moe0" in self.stages:
                self.precast(0, b)
            hT = self.sb(st, [128, KC, T], BF16, "hT")
            hTk = S.keys(NT)
            ogT = self.sb(st, [128, KC, T], BF16, "ogT")
            ogk = S.keys(KC)
            maskf = self.sb(st, [128, 128], F32, "maskf")
            maskb = self.sb(st, [128, 128], F32, "maskb")
            bmc = self.sb(st, [128, 4], F32, "bmc")
            if not _F("HG_A"):
                bm = self.sb(st, [128, 4, 128], BF16, "bm")
                self.dma("pool", bm[:], I["k_bm"], [], [bm])
                Vbd = self.sbr(st, 2, [128, 4, 128], BF16, "Vbd")
            self.dma("sp", maskf[:], I["k_maskf"], [], [maskf])
            self.dma("sp", maskb[:], I["k_maskb"], [], [maskb])
            self.dma("sp", bmc[:], I["k_bmc"], [], [bmc])
            m01 = self.sb(st, [128, T], BF16, "m01")
            self.memset("dve", m01[:], 1.0, [m01])
            self.memset("dve", m01[:, 0:T:32], 0.0, [m01])
            lbr = self.sb(st, [128, 2, 3, KC], F32, "lbr")
            with self.nc.allow_non_contiguous_dma(reason="tiny"):
                for d_ in range(2):
                    for j in range(3):
                        self.dma("sp", lbr[:, d_, j, :], I["hg_lb"][d_, j, :].rearrange("(h p) -> p h", p=128), [], [lbr])
            lbe = self.sb(st, [128, 2, 3, KC], F32, "lbe")
            self.act(lbe[:], lbr[:], AF.Exp, [lbr], [lbe])
            lbs = self.sb(st, [128, 2, KC], F32, "lbs")
            self.tt("dve", lbs[:], lbe[:, :, 0, :], lbe[:, :, 1, :], ALU.add, [lbe], [lbs])
            self.tt("dve", lbs[:], lbs[:], lbe[:, :, 2, :], ALU.add, [lbe, lbs], [lbs])
            lbi = self.sb(st, [128, 2, KC], F32, "lbi")
            self.recip(lbi[:], lbs[:], [lbs], [lbi])
            lb = self.sb(st, [128, 2, KC], F32, "lb")
            oml = self.sb(st, [128, 2, KC], F32, "oml")
            self.tt("dve", lb[:], lbe[:, :, 0, :], lbi[:], ALU.mult, [lbe, lbi], [lb])
            self.ts("dve", oml[:], lb[:], -1.0, 1.0, ALU.mult, ALU.add, [lb], [oml])
            ogc = self.sb(st, [128, 1], F32, "ogc")
            self.dma("sp", ogc[:], I["hg_out_norm_g"].rearrange("(p o) -> p o", o=1), [], [ogc])
            A_l, B_l = self.mod_cols(st, l, 0, b)
            A_c, B_c = self.mod_cols(st, l, 0, 4)
            gt_l = self.gate_tile(st, l, 0, b)
            gt_c = self.gate_tile(st, l, 0, 4)
            qdec = self.sb(st, [128, T], BF16, "qdec")
            NR = self.norm_res(st, (PB[0], PB[1]), junk=qdec)
            for tile in range(NT):
                A, Bc = (A_c, B_c) if tile < 2 else (A_l, B_l)
                self.norm_tile(NR, self.src_l0(b, tile), [], A, Bc, hT[:, :, tile * 128:(tile + 1) * 128], [hTk[tile]])
            wh = self.sbr(st, 1, [128, KC, 5, 128], BF16, "wh")
            Vh = self.sb(st, [128, NT, 128], BF16, "Vh")
            qs = self.sb(st, [128, T], BF16, "qs")
            sgate = self.sb(st, [128, T], BF16, "sgate")
            A1 = self.sb(st, [128, T], F32, "A1")
            A2 = self.sb(st, [128, T], F32, "A2")
            A3 = self.sb(st, [128, T], F32, "A3")
            kinc = self.sb(st, [128, T], BF16, "kinc")
            dec = self.sb(st, [128, T // 32], F32, "dec")
            tot = self.sb(st, [128, T // 32], F32, "tot")
            oacc = self.sb(st, [128, T], F32, "oacc")
            oak = S.keys(NT)
            sTm = self.sbr(st, 2, [128, 128], BF16, "sTm")
            kTs = self.sbr(st, 2, [128, 4, 128], BF16, "kTs")
            KVs = self.sbr(st, 2, [128, 4, 128], F32, "KVs")
            Sst = self.sb(st, [128, 8, 128], F32, "Sst")
            Sstk = S.keys(8)
            Sb = self.sb(st, [128, 8, 128], BF16, "Sb")
            Sbk = S.keys(2)
            pproj = Rot([PB[0], PB[1]])
            psT = Rot([PB[2], PB[3]])
            pkT = PB[4]
            pkTk = [PB[4].k, PB[4].k]
            pKV = PB[5]
            poT = Rot([PB[6], PB[7]])
            blocks = [(i * 512, 512) for i in range(4)] + [(2048, 256)]

            def proj(whh, sec, blk):
                t0, n = blk
                p = pproj.next()
                tiles = range(t0 // 128, (t0 + n) // 128)
                for k in range(KC):
                    self.mm(p[:, 0:n], whh[:, k, sec, :], hT[:, k, t0:t0 + n], k == 0, k == KC - 1,
                            [whh] + [hTk[t] for t in tiles], [p])
                return p

            for h in range(KC):
                whh = wh.next()
                for sec in range(5):
                    self.dma("pool", whh[:, :, sec, :],
                             I["hg_w_in"][:, sec * D + h * 128: sec * D + (h + 1) * 128].rearrange("(c p) e -> p c e", p=128), [], [whh])
                for tile in range(NT):
                    p = pproj.next()
                    for k in range(KC):
                        self.mm(p[:, 0:128], hT[:, k, tile * 128:(tile + 1) * 128], whh[:, k, 3, :], k == 0, k == KC - 1,
                                [whh, hTk[tile]], [p])
                    self.cp("act", Vh[:, tile, :], p[:, 0:128], [p], [Vh])
                for blk in blocks:
                    t0, n = blk
                    p = proj(whh, 0, blk)
                    self.act(qs[:, t0:t0 + n], p[:, 0:n], AF.Silu, [p], [qs])
                    p = proj(whh, 4, blk)
                    self.act(sgate[:, t0:t0 + n], p[:, 0:n], AF.Silu, [p], [sgate])
                for dr in range(2):
                    for blk in blocks:
                        t0, n = blk
                        p = proj(whh, 1 + dr, blk)
                        self.act(A1[:, t0:t0 + n], p[:, 0:n], AF.Sigmoid, [p], [A1])
                    self.ts("dve", A1[:], A1[:], oml[:, dr, h:h + 1], lb[:, dr, h:h + 1], ALU.mult, ALU.add, [A1, oml, lb], [A1])
                    self.act(A2[:], A1[:], AF.Ln, [A1], [A2])
                    if _F("HG_B"):
                        self.act(A1[:], A1[:], AF.Identity, [A1, self.onec], [A1], bias=self.onec[:, 0:1], scale=-1.0)
                    else:
                        self.ts("dve", A1[:], A1[:], -1.0, 1.0, ALU.mult, ALU.add, [A1], [A1])
                    S.op("dve", lambda e: e.tensor_tensor_scan(out=A3[:], data0=m01[:], data1=A2[:], initial=0.0,
                                                                op0=ALU.mult, op1=ALU.add), [m01, A2], [A3], cost=0.1 + 2 * T / 960.0)
                    a3v = A3[:].rearrange("p (j i) -> p j i", i=32)
                    self.cp("dve", tot[:], a3v[:, :, 31], [A3], [tot])
                    self.act(dec[:], tot[:], AF.Exp, [tot], [dec])
                    if dr == 0:
                        barr, free = A3, A2
                    else:
                        a2v = A2[:].rearrange("p (j i) -> p j i", i=32)
                        self.tt("dve", A2[:], A2[:], A3[:], ALU.subtract, [A2, A3], [A2])
                        self.tt("dve", a2v, a2v, tot[:].unsqueeze(2).broadcast_to([128, T // 32, 32]), ALU.add, [A2, tot], [A2])
                        barr, free = A2, A3
                    self.act(free[:], barr[:], AF.Exp, [barr], [free], scale=-1.0)
                    self.act(barr[:], barr[:], AF.Exp, [barr], [barr])
                    self.tt("dve", qdec[:], qs[:], barr[:], ALU.mult, [qs, barr], [qdec])
                    self.tt("dve", kinc[:], A1[:], free[:], ALU.mult, [A1, free], [kinc])
                    order = list(range(NT)) if dr == 0 else [1, 0] + list(range(NT - 1, 1, -1))
                    mask = maskf if dr == 0 else maskb
                    self.memset("dve", Sst[:, 0, :], 0.0, [Sstk[0]])
                    for i, tile in enumerate(order):
                        base = 4 * (i % 2)
                        ts_ = slice(tile * 128, (tile + 1) * 128)
                        ps_ = psT.next()
                        self.mm(ps_[:, 0:128], kinc[:, ts_], qdec[:, ts_], True, True, [kinc, qdec], [ps_])
                        sm = sTm.next()
                        self.tt("dve", sm[:], ps_[:, 0:128], mask[:], ALU.mult, [ps_, mask], [sm])
                        pk_i = i % 2
                        pkv = pkT[:, pk_i * 64:(pk_i + 1) * 64].bitcast(BF16)
                        self.tr(pkv, kinc[:, ts_], self.identb[:], [kinc, self.identb], [pkTk[pk_i]])
                        kt = kTs.next()
                        if _F("HG_A"):
                            for j in range(4):
                                self.act(kt[:, j, :], pkv, AF.Copy, [pkTk[pk_i], bmc], [kt], scale=bmc[:, j:j + 1])
                            for j in range(4):
                                self.mm(pKV[:, j * 128:(j + 1) * 128], kt[:, j, :], Vh[:, tile, :], True, True, [kt, Vh], [pKV])
                        else:
                            self.cp("act", kt[:, 0, :], pkv, [pkTk[pk_i]], [kt])
                            vb = Vbd.next()
                            self.tt("dve", vb[:], Vh[:, tile, :].unsqueeze(1).broadcast_to([128, 4, 128]), bm[:], ALU.mult, [Vh, bm], [vb])
                            self.mm(pKV[:, :], kt[:, 0, :], vb[:].rearrange("p j v -> p (j v)"), True, True, [kt, vb], [pKV])
                        kv = KVs.next()
                        self.tt("dve", kv[:], pKV[:, :].rearrange("p (j v) -> p j v", j=4),
                                dec[:, tile * 4:(tile + 1) * 4].unsqueeze(2).broadcast_to([128, 4, 128]), ALU.mult, [pKV, dec], [kv])
                        corder = [0, 1, 2, 3] if dr == 0 else [3, 2, 1, 0]
                        for jj, c in enumerate(corder):
                            s_in = base + jj
                            s_out = (base + jj + 1) % 8
                            self.stt(Sst[:, s_out, :], Sst[:, s_in, :], dec[:, tile * 4 + c: tile * 4 + c + 1], kv[:, c, :],
                                     ALU.mult, ALU.add, [Sstk[s_in], dec, kv], [Sstk[s_out]])
                        self.cp("pool" if not _F("HG_E") else "act", Sb[:, base:base + 4, :], Sst[:, base:base + 4, :],
                                [Sstk[base + q_] for q_ in range(4)], [Sbk[i % 2]])
                        po = poT.next()
                        self.mm(po[:, 0:128], Vh[:, tile, :], sm[:], True, False, [Vh, sm], [po])
                        for jj, c in enumerate(corder):
                            self.mm(po[:, c * 32:(c + 1) * 32], Sb[:, base + jj, :], qdec[:, tile * 128 + c * 32: tile * 128 + (c + 1) * 32],
                                    False, jj == 3, [Sbk[i % 2], qdec], [po])
                        if dr == 0:
                            self.cp("act", oacc[:, ts_], po[:, 0:128], [po], [oak[tile]])
                        else:
                            self.tt("dve", oacc[:, ts_], oacc[:, ts_], po[:, 0:128], ALU.add, [po, oak[tile]], [oak[tile]])
                if _F("HG_D"):
                    self.act(qdec[:], oacc[:], AF.Square, oak, [qdec])
                else:
                    self.tt("dve", qdec[:], oacc[:], oacc[:], ALU.mult, oak, [qdec])
                for blk in blocks:
                    t0, n = blk
                    p = pproj.next()
                    self.mm(p[:, 0:n], self.onesb[:], qdec[:, t0:t0 + n], True, True, [self.onesb, qdec], [p])
                    if _F("HG_C"):
                        self.act(A2[:, t0:t0 + n], p[:, 0:n], AF.Ln, [p, self.epsc], [A2], bias=self.epsc[:, 0:1], scale=1.0 / 128)
                    else:
                        self.act(A2[:, t0:t0 + n], p[:, 0:n], AF.Sqrt, [p, self.epsc], [A2], bias=self.epsc[:, 0:1], scale=1.0 / 128)
                if _F("HG_C"):
                    self.act(A3[:], A2[:], AF.Exp, [A2], [A3], scale=-0.5)
                else:
                    self.recip(A3[:], A2[:], [A2], [A3])
                self.tt("dve", A3[:], A3[:], oacc[:], ALU.mult, [A3] + oak, [A3])
                self.stt(ogT[:, h, :], A3[:], ogc[:, 0:1], sgate[:], ALU.mult, ALU.mult, [A3, ogc, sgate], [ogk[h]])
            wo = self.sb(st, [128, KC, D], BF16, "wo")
            self.dma("pool", wo[:], I["hg_w_out"].rearrange("(c p) n -> p c n", p=128), [], [wo])
            xt2 = NR["xt"]
            tmp = NR["xn"]
            for tile in range(NT):
                gt = gt_c if tile < 2 else gt_l
                x_ = xt2.next()
                self.dma("sp", x_[:], self.src_l0(b, tile), [], [x_])
                t_ = tmp.next()
                for half in range(2):
                    p = pproj.next()
                    hs = slice(half * 512, (half + 1) * 512)
                    for k in range(KC):
                        self.mm(p[:, :], ogT[:, k, tile * 128:(tile + 1) * 128], wo[:, k, hs], k == 0, k == KC - 1, [ogk[k], wo], [p])
                    self.tt("dve", t_[:, hs], p[:, :], gt[:, hs], ALU.mult, [p, gt], [t_])
                self.tt("dve", t_[:], t_[:], x_[:], ALU.add, [t_, x_], [t_])
                self.dma("sp", self.xres[b, tile * 128:(tile + 1) * 128, :], t_[:], [t_], [self.kx[b][tile]], semkey=t_)
                if ("xm0" in self.D_) and b == 0:
                    self.dma("sp", self.D_["xm0"][tile * 128:(tile + 1) * 128, :], t_[:], [t_], [self.kscr], semkey=t_)
            return S.flush()

    ROUTE_TMPS = (("lg", 36), ("gmax", 1), ("ngmax", 1), ("ge", 4), ("gsum", 1), ("pg", 1), ("gone", 4), ("pen", 4),
                  ("em", 32), ("m1", 1), ("oh1", 32), ("em2", 32), ("m2", 1), ("oh2", 32), ("dm", 1), ("e2", 1),
                  ("den", 1), ("rden", 1), ("w1", 1), ("w2", 1), ("tmpw", 32))

    def route_tile(self, sm, p):
        t = {nm: r.next() for nm, r in sm.items()}
        lg = t["lg"]
        self.cp("act", lg[:], p[:, 0:36], [p], [lg])
        self.red(t["gmax"][:], lg[:, 0:4], ALU.max, [lg], [t["gmax"]])
        self.ts("dve", t["ngmax"][:], t["gmax"][:], -1.0, None, ALU.mult, None, [t["gmax"]], [t["ngmax"]])
        self.act(t["ge"][:], lg[:, 0:4], AF.Exp, [lg, t["ngmax"]], [t["ge"], t["gsum"]], bias=t["ngmax"][:, 0:1], accum_out=t["gsum"][:])
        self.recip(t["pg"][:], t["gsum"][:], [t["gsum"]], [t["pg"]])
        self.ts("dve", t["gone"][:], lg[:, 0:4], t["gmax"][:, 0:1], None, ALU.is_ge, None, [lg, t["gmax"]], [t["gone"]])
        self.ts("dve", t["pen"][:], t["gone"][:], BIG, -BIG, ALU.mult, ALU.add, [t["gone"]], [t["pen"]])
        self.tt("dve", t["em"][:].rearrange("p (g j) -> p g j", g=4), lg[:, 4:36].rearrange("p (g j) -> p g j", g=4),
                t["pen"][:].unsqueeze(2).broadcast_to([128, 4, 8]), ALU.add, [lg, t["pen"]], [t["em"]])
        self.red(t["m1"][:], t["em"][:], ALU.max, [t["em"]], [t["m1"]])
        self.ts("dve", t["oh1"][:], t["em"][:], t["m1"][:, 0:1], None, ALU.is_ge, None, [t["em"], t["m1"]], [t["oh1"]])
        self.stt(t["em2"][:], t["oh1"][:], -BIG, t["em"][:], ALU.mult, ALU.add, [t["oh1"], t["em"]], [t["em2"]])
        self.red(t["m2"][:], t["em2"][:], ALU.max, [t["em2"]], [t["m2"]])
        self.ts("dve", t["oh2"][:], t["em2"][:], t["m2"][:, 0:1], None, ALU.is_ge, None, [t["em2"], t["m2"]], [t["oh2"]])
        self.tt("dve", t["dm"][:], t["m2"][:], t["m1"][:], ALU.subtract, [t["m2"], t["m1"]], [t["dm"]])
        self.act(t["e2"][:], t["dm"][:], AF.Exp, [t["dm"]], [t["e2"]])
        self.ts("dve", t["den"][:], t["e2"][:], 1.0, None, ALU.add, None, [t["e2"]], [t["den"]])
        self.recip(t["rden"][:], t["den"][:], [t["den"]], [t["rden"]])
        self.tt("dve", t["w1"][:], t["pg"][:], t["rden"][:], ALU.mult, [t["pg"], t["rden"]], [t["w1"]])
        self.tt("dve", t["w2"][:], t["w1"][:], t["e2"][:], ALU.mult, [t["w1"], t["e2"]], [t["w2"]])
        return t

    def stage_moe(self, l, b, half):
        I, S = self.I, self.S
        if l == 0:
            tiles = list(range(0, 9)) if half == 0 else list(range(9, 18))
        else:
            tiles = list(range(2, 10)) if half == 0 else list(range(10, 18))
        ntl = len(tiles)
        NTOK = ntl * 128
        with ExitStack() as st:
            PB = [self.psb(st) for _ in range(8)]
            hT = self.sb(st, [128, KC, NTOK], BF16, "hT")
            hTk = S.keys(ntl)
            acc = self.sb(st, [128, ntl, D], F32, "acc")
            acck = S.keys(ntl)
            Wt = self.sb(st, [128, ntl, NEXP], F32, "Wt")
            Wtk = S.keys(ntl)
            wr = self.sb(st, [128, KC, 36], F32, "wr")
            self.dma("sp", wr[:, :, 0:4], I["moe_w_group"][l].rearrange("(c p) g -> p c g", p=128), [], [wr])
            self.dma("sp", wr[:, :, 4:36], I["moe_w_expert"][l].rearrange("(c p) g -> p c g", p=128), [], [wr])
            A_l, B_l = self.mod_cols(st, l, 1, b)
            gt_l = self.gate_tile(st, l, 1, b)
            if l == 0 and half == 0:
                A_c, B_c = self.mod_cols(st, l, 1, 4)
                gt_c = self.gate_tile(st, l, 1, 4)
            NR = self.norm_res(st, (PB[0], PB[1]), nhtf=2)
            sm = {}
            for nm, w in (("lg", 36), ("gmax", 1), ("ngmax", 1), ("ge", 4), ("gsum", 1), ("pg", 1), ("gone", 4), ("pen", 4),
                          ("em", 32), ("m1", 1), ("oh1", 32), ("em2", 32), ("m2", 1), ("oh2", 32), ("dm", 1), ("e2", 1),
                          ("den", 1), ("rden", 1), ("w1", 1), ("w2", 1), ("tmpw", 32)):
                sm[nm] = self.sbr(st, 2, [128, w], F32, nm)
            for li, tile in enumerate(tiles):
                isctx = (l == 0 and tile < 2)
                A, Bc = (A_c, B_c) if isctx else (A_l, B_l)
                hTf = self.norm_tile(NR, self.xres[b, tile * 128:(tile + 1) * 128, :], [self.kx[b][tile]], A, Bc,
                                     hT[:, :, li * 128:(li + 1) * 128], [hTk[li]])
                p = PB[2 + li % 2]
                for k in range(KC):
                    self.mm(p[:, 0:36], hTf[:, k, :], wr[:, k, :], k == 0, k == KC - 1, [hTf, wr], [p])
                t = self.route_tile(sm, p)
                self.ts("dve", t["tmpw"][:], t["oh1"][:], t["w1"][:, 0:1], None, ALU.mult, None, [t["oh1"], t["w1"]], [t["tmpw"]])
                self.stt(Wt[:, li, :], t["oh2"][:], t["w2"][:, 0:1], t["tmpw"][:], ALU.mult, ALU.add, [t["oh2"], t["w2"], t["tmpw"]], [Wtk[li]])
            wg = self.sbr(st, 2, [128, KC, FF], BF16, "wg")
            wu = self.sbr(st, 2, [128, KC, FF], BF16, "wu")
            wd = self.sbr(st, 2, [128, 4, D], BF16, "wd")
            sg = self.sbr(st, 2, [128, 512], BF16, "sg")
            actT = self.sbr(st, 2, [128, 4, 512], BF16, "actT")
            pgu = Rot([(PB[0], PB[1]), (PB[2], PB[3])])
            pyr = Rot([(PB[4], PB[5]), (PB[6], PB[7])])
            blocks = []
            t0 = 0
            while t0 < NTOK:
                n = min(512, NTOK - t0)
                blocks.append((t0, n))
                t0 += n
            for e in range(NEXP):
                g_, u_, d_ = wg.next(), wu.next(), wd.next()
                self.dma("pool", g_[:], I["moe_w_gate"][l, e].rearrange("(c p) f -> p c f", p=128), [], [g_])
                self.dma("pool", u_[:], I["moe_w_up"][l, e].rearrange("(c p) f -> p c f", p=128), [], [u_])
                self.dma("pool", d_[:], I["moe_w_down"][l, e].rearrange("(c p) f -> p c f", p=128), [], [d_])
                for (t0, n) in blocks:
                    at = actT.next()
                    hk = [hTk[t] for t in range(t0 // 128, (t0 + n) // 128)]
                    for f in range(4):
                        pg_, pu_ = pgu.next()
                        fs = slice(f * 128, (f + 1) * 128)
                        for k in range(KC):
                            self.mm(pg_[:, 0:n], g_[:, k, fs], hT[:, k, t0:t0 + n], k == 0, k == KC - 1, [g_] + hk, [pg_])
                        for k in range(KC):
                            self.mm(pu_[:, 0:n], u_[:, k, fs], hT[:, k, t0:t0 + n], k == 0, k == KC - 1, [u_] + hk, [pu_])
                        s_ = sg.next()
                        self.act(s_[:, 0:n], pg_[:, 0:n], AF.Silu, [pg_], [s_])
                        self.tt("dve", at[:, f, 0:n], s_[:, 0:n], pu_[:, 0:n], ALU.mult, [s_, pu_], [at])
                    for tt_ in range(n // 128):
                        li = t0 // 128 + tt_
                        pa, pb = pyr.next()
                        for hf, p in ((0, pa), (1, pb)):
                            hs = slice(hf * 512, (hf + 1) * 512)
                            for f in range(4):
                                self.mm(p[:, :], at[:, f, tt_ * 128:(tt_ + 1) * 128], d_[:, f, hs], f == 0, f == 3, [at, d_], [p])
                            if e == 0:
                                self.ts("dve", acc[:, li, hs], p[:, :], Wt[:, li, e:e + 1], None, ALU.mult, None, [p, Wtk[li]], [acck[li]])
                            else:
                                self.stt(acc[:, li, hs], p[:, :], Wt[:, li, e:e + 1], acc[:, li, hs], ALU.mult, ALU.add,
                                         [p, Wtk[li], acck[li]], [acck[li]])
            for li, tile in enumerate(tiles):
                isctx = (l == 0 and tile < 2)
                gt = gt_c if isctx else gt_l
                x_ = NR["xt"].next()
                self.dma("sp", x_[:], self.xres[b, tile * 128:(tile + 1) * 128, :], [self.kx[b][tile]], [x_])
                t_ = NR["xn"].next()
                self.tt("dve", t_[:], acc[:, li, :], gt[:], ALU.mult, [acck[li], gt], [t_])
                self.tt("dve", t_[:], t_[:], x_[:], ALU.add, [t_, x_], [t_])
                if l == 0:
                    self.dma("sp", self.xres[b, tile * 128:(tile + 1) * 128, :], t_[:], [t_], [self.kx[b][tile]], semkey=t_)
                    if ("xf0" in self.D_) and b == 0:
                        self.dma("sp", self.D_["xf0"][tile * 128:(tile + 1) * 128, :], t_[:], [t_], [self.kscr], semkey=t_)
                else:
                    self.dma("sp", self.out[b, (tile - 2) * 128:(tile - 1) * 128, :], t_[:], [t_], [self.kscr], semkey=t_)
            return S.flush()

    def rope(self, xin, xout, cos, sin, H, tm, R, W):
        x1, x2 = xin[:, :, :, 0, :], xin[:, :, :, 1, :]
        cb = cos.unsqueeze(1).broadcast_to([128, H, 2, 8])
        sb_ = sin.unsqueeze(1).broadcast_to([128, H, 2, 8])
        t1, t2 = tm
        v1 = t1[:, 0:H * 16].rearrange("p (h a f) -> p h a f", h=H, a=2)
        v2 = t2[:, 0:H * 16].rearrange("p (h a f) -> p h a f", h=H, a=2)
        self.tt("dve", v1, x1, cb, ALU.mult, R, [t1])
        self.tt("dve", v2, x2, sb_, ALU.mult, R, [t2])
        self.tt("dve", xout[:, :, :, 0, :], v1, v2, ALU.subtract, [t1, t2], W)
        self.tt("dve", v1, x2, cb, ALU.mult, R, [t1])
        self.tt("dve", v2, x1, sb_, ALU.mult, R, [t2])
        self.tt("dve", xout[:, :, :, 1, :], v1, v2, ALU.add, [t1, t2], W)

    def stage_mla(self, b):
        I, S = self.I, self.S
        l = 1
        NQT = SEQ // 128
        with ExitStack() as st:
            PB = [self.psb(st) for _ in range(8)]
            if "moe1" in self.stages:
                self.precast(1, b)
            A_l, B_l = self.mod_cols(st, l, 0, b)
            A_c, B_c = self.mod_cols(st, l, 0, 4)
            gt_l = self.gate_tile(st, l, 0, b)
            NR = self.norm_res(st, (PB[0], PB[1]))
            win = self.sb(st, [128, KC, 416], BF16, "win")
            self.dma("pool", win[:], I["mla_w_in"].rearrange("(c p) n -> p c n", p=128), [], [win])
            cT = self.sb(st, [128, 3, T], BF16, "cT")
            cTk = S.keys(NT)
            krr = self.sb(st, [128, NT, 32], F32, "krr")
            krk = S.keys(NT)
            sskr = self.sb(st, [128, NT], F32, "sskr")
            ssk = S.keys(NT)
            gk = self.sb(st, [128, 96], F32, "gk")
            gq = self.sb(st, [128, 96], F32, "gq")
            self.dma("sp", gk[:], I["mla_k_qknorm_g"].partition_broadcast(128), [], [gk])
            self.dma("sp", gq[:], I["mla_q_qknorm_g"].partition_broadcast(128), [], [gq])
            self.ts("dve", gq[:], gq[:], float(96 ** -0.5), None, ALU.mult, None, [gq], [gq])
            qng = self.sb(st, [128, 2], F32, "qng")
            kvg = self.sb(st, [128, 1], F32, "kvg")
            self.dma("sp", qng[:], I["mla_q_norm_g"].rearrange("(k p) -> p k", p=128), [], [qng])
            self.dma("sp", kvg[:], I["mla_kv_norm_g"].rearrange("(p o) -> p o", o=1), [], [kvg])
            hTt = self.sbr(st, 2, [128, KC, 128], BF16, "hTt")
            csr = self.sbr(st, 2, [128, 416], F32, "cs")
            cnr = self.sbr(st, 2, [128, 384], BF16, "cn")
            junk2 = self.sb(st, [128, 256], BF16, "junk2")
            s1 = {nm: self.sbr(st, 2, [128, 1], F32, nm) for nm in ("ssq", "sskv", "sdq", "sdkv", "rsq", "rskv")}
            kr1 = self.sbr(st, 2, [128, 32], F32, "kr1")
            cosr = self.sbr(st, 2, [128, 16], F32, "cos")
            sinr = self.sbr(st, 2, [128, 16], F32, "sin")
            rt = (self.sb(st, [128, 64], F32, "rt1"), self.sb(st, [128, 64], F32, "rt2"))
            cost = {}
            for tile in range(NT):
                A, Bc = (A_c, B_c) if tile < 2 else (A_l, B_l)
                hb = hTt.next()
                self.norm_tile(NR, self.xres[b, tile * 128:(tile + 1) * 128, :], [self.kx[b][tile]], A, Bc, hb[:], [hb])
                p = PB[2 + tile % 2]
                for k in range(KC):
                    self.mm(p[:, 0:416], hb[:, k, :], win[:, k, :], k == 0, k == KC - 1, [hb, win], [p])
                cs = csr.next()
                self.cp("dve", cs[:], p[:, 0:416], [p], [cs])
                t = {nm: r.next() for nm, r in s1.items()}
                self.act(junk2[:, 0:256], cs[:, 0:256], AF.Square, [cs], [junk2, t["ssq"]], accum_out=t["ssq"][:])
                self.act(junk2[:, 0:128], cs[:, 256:384], AF.Square, [cs], [junk2, t["sskv"]], accum_out=t["sskv"][:])
                self.act(junk2[:, 0:32], cs[:, 384:416], AF.Square, [cs], [junk2, ssk[tile]], accum_out=sskr[:, tile:tile + 1])
                self.act(t["sdq"][:], t["ssq"][:], AF.Sqrt, [t["ssq"], self.epsc], [t["sdq"]], bias=self.epsc[:, 0:1], scale=1.0 / 256)
                self.act(t["sdkv"][:], t["sskv"][:], AF.Sqrt, [t["sskv"], self.epsc], [t["sdkv"]], bias=self.epsc[:, 0:1], scale=1.0 / 128)
                self.recip(t["rsq"][:], t["sdq"][:], [t["sdq"]], [t["rsq"]])
                self.recip(t["rskv"][:], t["sdkv"][:], [t["sdkv"]], [t["rskv"]])
                cn = cnr.next()
                self.act(cn[:, 0:256], cs[:, 0:256], AF.Copy, [cs, t["rsq"]], [cn], scale=t["rsq"][:, 0:1])
                self.act(cn[:, 256:384], cs[:, 256:384], AF.Copy, [cs, t["rskv"]], [cn], scale=t["rskv"][:, 0:1])
                pT = PB[4 + tile % 2]
                pv = pT[:, 0:192].bitcast(BF16).rearrange("p (j t) -> p j t", j=3)
                for j in range(3):
                    self.tr(pv[:, j, :], cn[:, j * 128:(j + 1) * 128], self.identb[:], [cn, self.identb], [pT])
                self.cp("dve", cT[:, :, tile * 128:(tile + 1) * 128], pv, [pT], [cTk[tile]])
                k1 = kr1.next()
                self.tt("dve", k1[:], cs[:, 384:416], gk[:, 64:96], ALU.mult, [cs, gk], [k1])
                if tile < 2:
                    self.cp("dve", krr[:, tile, :], k1[:], [k1], [krk[tile]])
                else:
                    co, si = cosr.next(), sinr.next()
                    self.dma("sp", co[:], I["k_cos"][(tile - 2) * 128:(tile - 1) * 128, :], [], [co])
                    self.dma("sp", si[:], I["k_sin"][(tile - 2) * 128:(tile - 1) * 128, :], [], [si])
                    self.rope(k1[:].rearrange("p (h a g f) -> p h a g f", h=1, a=2, g=2),
                              krr[:, tile, :].rearrange("p (h a g f) -> p h a g f", h=1, a=2, g=2),
                              co[:].rearrange("p (a f) -> p a f", a=2), si[:].rearrange("p (a f) -> p a f", a=2),
                              1, rt, [k1, co, si], [krk[tile]])
            HG = 4
            oat = self.sb(st, [128, NQT, D], BF16, "oat")
            oak = S.keys(NQT)
            QT = self.sb(st, [128, HG, SEQ], BF16, "QT")
            QTk = S.keys(NQT)
            KT = self.sb(st, [128, HG, T], BF16, "KT")
            KTk = S.keys(NT)
            Vx = self.sb(st, [128, NT, HG, 65], BF16, "Vx")
            Vxk = S.keys(NT)
            self.memset("dve", Vx[:], 1.0, Vxk)
            wqf = self.sb(st, [128, 2, 384], F32, "wqf")
            wqb = self.sb(st, [128, 2, 384], BF16, "wqb")
            wkf = self.sb(st, [128, 512], F32, "wkf")
            wkb = self.sb(st, [128, 512], BF16, "wkb")
            kvfr = self.sbr(st, 2, [128, HG, 128], F32, "kvf")
            sqk = self.sb(st, [128, HG, 96], F32, "sqk")
            tmpk = self.sb(st, [128, HG, 96], F32, "tmpk")
            s4 = {nm: self.sbr(st, 2, [128, HG], F32, nm) for nm in ("ssn", "ss", "sd", "rs", "ssq4", "sd4", "rs4")}
            kbr = self.sbr(st, 2, [128, HG, 96], BF16, "kb")
            qfr = self.sbr(st, 2, [128, HG, 96], F32, "qf")
            qnr = self.sbr(st, 2, [128, HG, 96], F32, "qn")
            qbr = self.sbr(st, 2, [128, HG, 96], BF16, "qb")
            ptr_ = self.sbr(st, 3, [128, 512], BF16, "pt")
            recr = self.sbr(st, 4, [128, 1], F32, "rec")
            for hg in range(16 // HG):
                self.dma("sp", wqf[:], I["mla_w_qb"][:, hg * HG * 96:(hg + 1) * HG * 96].rearrange("(k p) n -> p k n", p=128), [], [wqf])
                self.tt("dve", wqb[:], wqf[:], qng[:].unsqueeze(2).broadcast_to([128, 2, HG * 96]), ALU.mult, [wqf, qng], [wqb])
                self.dma("sp", wkf[:], I["mla_w_kvb"][:, hg * HG * 128:(hg + 1) * HG * 128], [], [wkf])
                self.ts("dve", wkb[:], wkf[:], kvg[:, 0:1], None, ALU.mult, None, [wkf, kvg], [wkb])
                for tile in range(NT):
                    ts_ = slice(tile * 128, (tile + 1) * 128)
                    p = PB[tile % 2]
                    self.mm(p[:, :], cT[:, 2, ts_], wkb[:], True, True, [cTk[tile], wkb], [p])
                    kvf = kvfr.next()
                    self.cp("dve", kvf[:], p[:, :].rearrange("p (h e) -> p h e", h=HG), [p], [kvf])
                    t = {nm: r.next() for nm, r in s4.items()}
                    self.tt("dve", sqk[:, :, 0:64], kvf[:, :, 0:64], kvf[:, :, 0:64], ALU.mult, [kvf], [sqk])
                    self.red(t["ssn"][:], sqk[:, :, 0:64], ALU.add, [sqk], [t["ssn"]])
                    self.ts("dve", t["ss"][:], t["ssn"][:], sskr[:, tile:tile + 1], None, ALU.add, None, [t["ssn"], ssk[tile]], [t["ss"]])
                    self.act(t["sd"][:], t["ss"][:], AF.Sqrt, [t["ss"], self.epsc], [t["sd"]], bias=self.epsc[:, 0:1], scale=1.0 / 96)
                    self.recip(t["rs"][:], t["sd"][:], [t["sd"]], [t["rs"]])
                    kb = kbr.next()
                    self.tt("dve", tmpk[:, :, 0:64], kvf[:, :, 0:64], t["rs"][:].unsqueeze(2).broadcast_to([128, HG, 64]), ALU.mult,
                            [kvf, t["rs"]], [tmpk])
                    self.tt("dve", kb[:, :, 0:64], tmpk[:, :, 0:64], gk[:, 0:64].unsqueeze(1).broadcast_to([128, HG, 64]), ALU.mult,
                            [tmpk, gk], [kb])
                    self.tt("dve", kb[:, :, 64:96], krr[:, tile, :].unsqueeze(1).broadcast_to([128, HG, 32]),
                            t["rs"][:].unsqueeze(2).broadcast_to([128, HG, 32]), ALU.mult, [krk[tile], t["rs"]], [kb])
                    pk = PB[2 + tile % 2]
                    pkv = pk[:, 0:256].bitcast(BF16).rearrange("p (h t) -> p h t", h=HG)
                    for h in range(HG):
                        self.tr(pkv[0:96, h, :], kb[:, h, :], self.identb[:], [kb, self.identb], [pk])
                    self.cp("dve", KT[0:96, :, ts_], pkv[0:96, :, :], [pk], [KTk[tile]])
                    self.cp("dve", Vx[:, tile, :, 0:64], kvf[:, :, 64:128], [kvf], [Vxk[tile]])
                    if tile >= 2:
                        qt_ = tile - 2
                        pq = PB[4 + tile % 2]
                        for k in range(2):
                            self.mm(pq[:, 0:HG * 96], cT[:, k, ts_], wqb[:, k, :], k == 0, k == 1, [cTk[tile], wqb], [pq])
                        qf = qfr.next()
                        self.cp("dve", qf[:], pq[:, 0:HG * 96].rearrange("p (h e) -> p h e", h=HG), [pq], [qf])
                        self.tt("dve", sqk[:], qf[:], qf[:], ALU.mult, [qf], [sqk])
                        self.red(t["ssq4"][:], sqk[:], ALU.add, [sqk], [t["ssq4"]])
                        self.act(t["sd4"][:], t["ssq4"][:], AF.Sqrt, [t["ssq4"], self.epsc], [t["sd4"]], bias=self.epsc[:, 0:1], scale=1.0 / 96)
                        self.recip(t["rs4"][:], t["sd4"][:], [t["sd4"]], [t["rs4"]])
                        qn = qnr.next()
                        self.tt("dve", qn[:], qf[:], t["rs4"][:].unsqueeze(2).broadcast_to([128, HG, 96]), ALU.mult, [qf, t["rs4"]], [qn])
                        self.tt("dve", qn[:], qn[:], gq[:].unsqueeze(1).broadcast_to([128, HG, 96]), ALU.mult, [qn, gq], [qn])
                        qb = qbr.next()
                        self.cp("dve", qb[:, :, 0:64], qn[:, :, 0:64], [qn], [qb])
                        co, si = cosr.next(), sinr.next()
                        self.dma("sp", co[:], I["k_cos"][qt_ * 128:(qt_ + 1) * 128, :], [], [co])
                        self.dma("sp", si[:], I["k_sin"][qt_ * 128:(qt_ + 1) * 128, :], [], [si])
                        self.rope(qn[:, :, 64:96].rearrange("p h (a g f) -> p h a g f", a=2, g=2),
                                  qb[:, :, 64:96].rearrange("p h (a g f) -> p h a g f", a=2, g=2),
                                  co[:].rearrange("p (a f) -> p a f", a=2), si[:].rearrange("p (a f) -> p a f", a=2),
                                  HG, rt, [qn, co, si], [qb])
                        pqt = PB[6 + tile % 2]
                        pqv = pqt[:, 0:256].bitcast(BF16).rearrange("p (h t) -> p h t", h=HG)
                        for h in range(HG):
                            self.tr(pqv[0:96, h, :], qb[:, h, :], self.identb[:], [qb, self.identb], [pqt])
                        self.cp("act", QT[0:96, :, qt_ * 128:(qt_ + 1) * 128], pqv[0:96, :, :], [pqt], [QTk[qt_]])
                for h in range(HG):
                    hh = hg * HG + h
                    for qb_ in range(SEQ // 512):
                        po = PB[4:8]
                        qk = [QTk[qb_ * 4 + i] for i in range(4)]
                        for kt in range(NT):
                            ps_ = PB[kt % 3]
                            self.mm(ps_[:, :], KT[0:96, h, kt * 128:(kt + 1) * 128], QT[0:96, h, qb_ * 512:(qb_ + 1) * 512], True, True,
                                    [KTk[kt]] + qk, [ps_])
                            pt = ptr_.next()
                            self.act(pt[:], ps_[:, :], AF.Exp, [ps_], [pt])
                            for q4 in range(4):
                                self.mm(po[q4][:, 0:65], pt[:, q4 * 128:(q4 + 1) * 128], Vx[:, kt, h, :], kt == 0, kt == NT - 1,
                                        [pt, Vxk[kt]], [po[q4]])
                        for q4 in range(4):
                            rec = recr.next()
                            self.recip(rec[:], po[q4][:, 64:65], [po[q4]], [rec])
                            self.ts("dve", oat[:, qb_ * 4 + q4, hh * 64:(hh + 1) * 64], po[q4][:, 0:64], rec[:, 0:1], None, ALU.mult, None,
                                    [po[q4], rec], [oak[qb_ * 4 + q4]])
            wo = self.sb(st, [128, KC, D], BF16, "wo")
            self.dma("pool", wo[:], I["mla_w_out"].rearrange("(c p) n -> p c n", p=128), [], [wo])
            oTr = self.sbr(st, 2, [128, KC, 128], BF16, "oT")
            for qt_ in range(NQT):
                tile = qt_ + 2
                pT = PB[qt_ % 2]
                pv = pT[:, :].bitcast(BF16).rearrange("p (k t) -> p k t", k=KC)
                for k in range(KC):
                    self.tr(pv[:, k, :], oat[:, qt_, k * 128:(k + 1) * 128], self.identb[:], [oak[qt_], self.identb], [pT])
                oT = oTr.next()
                self.cp("act", oT[:], pv, [pT], [oT])
                x_ = NR["xt"].next()
                self.dma("sp", x_[:], self.xres[b, tile * 128:(tile + 1) * 128, :], [self.kx[b][tile]], [x_])
                t_ = NR["xn"].next()
                for hf in range(2):
                    p = PB[2 + hf]
                    hs = slice(hf * 512, (hf + 1) * 512)
                    for k in range(KC):
                        self.mm(p[:, :], oT[:, k, :], wo[:, k, hs], k == 0, k == KC - 1, [oT, wo], [p])
                    self.tt("dve", t_[:, hs], p[:, :], gt_l[:, hs], ALU.mult, [p, gt_l], [t_])
                self.tt("dve", t_[:], t_[:], x_[:], ALU.add, [t_, x_], [t_])
                self.dma("sp", self.xres[b, tile * 128:(tile + 1) * 128, :], t_[:], [t_], [self.kx[b][tile]], semkey=t_)
                if ("xm1" in self.D_) and b == 0:
                    self.dma("sp", self.D_["xm1"][qt_ * 128:(qt_ + 1) * 128, :], t_[:], [t_], [self.kscr], semkey=t_)
            return S.flush()

    def moe_sparse(self, l):
        I, S, NB = self.I, self.S, self.NB
        tiles = [(b, t) for b in range(NB) for t in (range(NT) if l == 0 else range(2, NT))]
        NTL = len(tiles)
        NBLK = 2 * NTL + NEXP
        info = {}
        with ExitStack() as pst:
            d_i = [self.sb(pst, [128, NTL], I32, "d%di" % k) for k in range(2)]
            w_a = [self.sb(pst, [128, NTL], F32, "w%da" % k) for k in range(2)]
            idxw = self.sb(pst, [128, NBLK], I32, "idxw")
            kh2 = S.keys(NTL)
            kxs, kys = S.key(), S.key()
            kwb = self.kwb[l]
            with ExitStack() as st:
                PB = [self.psb(st) for _ in range(8)]
                Ltri = self.sb(st, [128, 128], F32, "Ltri")
                onesf = self.sb(st, [128, 128], F32, "onesf")
                self.memset("dve", onesf[:], 1.0, [onesf])
                self.memset("pool", Ltri[:], 1.0, [Ltri])
                S.op("pool", lambda e_: e_.affine_select(out=Ltri[:], in_=Ltri[:], pattern=[[1, 128]], compare_op=ALU.is_gt,
                                                         fill=0.0, base=0, channel_multiplier=-1), [Ltri], [Ltri])
                jvi = self.sb(st, [128, NBLK], I32, "jvi")
                jv = self.sb(st, [128, NBLK], F32, "jv")
                S.op("pool", lambda e_: e_.iota(jvi[:], pattern=[[128, NBLK]], base=0, channel_multiplier=0), [], [jvi])
                self.cp("dve", jv[:], jvi[:], [jvi], [jv])
                pii = self.sb(st, [128, 1], I32, "pii")
                pif = self.sb(st, [128, 1], F32, "pif")
                S.op("pool", lambda e_: e_.iota(pii[:], pattern=[[0, 1]], base=0, channel_multiplier=1), [], [pii])
                self.cp("dve", pif[:], pii[:], [pii], [pif])
                ones32 = self.sb(st, [128, NEXP], F32, "ones32")
                self.memset("dve", ones32[:], 1.0, [ones32])
                wr = self.sb(st, [128, KC, 36], F32, "wr")
                self.dma("sp", wr[:, :, 0:4], I["moe_w_group"][l].rearrange("(c p) g -> p c g", p=128), [], [wr])
                self.dma("sp", wr[:, :, 4:36], I["moe_w_expert"][l].rearrange("(c p) g -> p c g", p=128), [], [wr])
                grow = self.sb(st, [128, D], F32, "grow")
                self.dma("sp", grow[:], I["norm_ffn_g"][l, :].partition_broadcast(128), [], [grow])

                def rows_for(r):
                    Ar = self.sb(st, [128, D], F32, "Arow")
                    Br = self.sb(st, [128, D], F32, "Brow")
                    return Ar, Br

                def load_rows(Ar, Br, r):
                    self.dma("sp", Ar[:], self.mod[l, r, 4 * D:5 * D].partition_broadcast(128), [self.kmod], [Ar])
                    self.dma("sp", Br[:], self.mod[l, r, 3 * D:4 * D].partition_broadcast(128), [self.kmod], [Br])
                    self.stt(Ar[:], Ar[:], 1.0, grow[:], ALU.add, ALU.mult, [Ar, grow], [Ar])

                Ar_l, Br_l = rows_for(0)
                if l == 0:
                    Ar_c, Br_c = rows_for(4)
                    load_rows(Ar_c, Br_c, 4)
                    A_c, B_c = self.mod_cols(st, l, 1, 4)
                NR = self.norm_res(st, (PB[0], PB[1]), nhtf=2)
                sm = {nm: self.sbr(st, 2, [128, w], F32, nm) for nm, w in self.ROUTE_TMPS}
                OHs = self.sb(st, [128, NEXP], F32, "OHs")
                self.memset("dve", OHs[:], 0.0, [OHs])
                OHt = self.sbr(st, 2, [128, NEXP], F32, "OHt")
                Rall = self.sb(st, [128, NTL, NEXP], F32, "Rall")
                Rk = S.keys(NTL)
                oha = [self.sb(st, [128, NTL, NEXP], F32, "oh%da" % k) for k in range(2)]
                ohk = [S.keys(NTL) for _ in range(2)]
                t32 = self.sb(st, [128, D], F32, "t32")
                h2r = self.sbr(st, 2, [128, D], BF16, "h2b")
                cur_b = None
                cols = {}
                for ti, (b, tile) in enumerate(tiles):
                    if b != cur_b:
                        cur_b = b
                        load_rows(Ar_l, Br_l, b)
                        cols[b] = self.mod_cols(st, l, 1, b)
                    isctx = (l == 0 and tile < 2)
                    A, Bc = (A_c, B_c) if isctx else cols[b]
                    Ar, Br = (Ar_c, Br_c) if isctx else (Ar_l, Br_l)
                    hTf = self.norm_tile(NR, self.xres[b, tile * 128:(tile + 1) * 128, :], [self.kx[b][tile]], A, Bc, None, [])
                    xn = self.last_xn
                    self.tt("dve", t32[:], xn[:], Ar[:], ALU.mult, [xn, Ar], [t32])
                    h2b = h2r.next()
                    self.tt("dve", h2b[:], t32[:], Br[:], ALU.add, [t32, Br], [h2b])
                    self.dma("sp", self.h2d[ti * 128:(ti + 1) * 128, :], h2b[:], [h2b], [kh2[ti]], semkey=h2b)
                    p = PB[2 + ti % 2]
                    for k in range(KC):
                        self.mm(p[:, 0:36], hTf[:, k, :], wr[:, k, :], k == 0, k == KC - 1, [hTf, wr], [p])
                    t = self.route_tile(sm, p)
                    self.cp("dve", oha[0][:, ti, :], t["oh1"][:], [t["oh1"]], [ohk[0][ti]])
                    self.cp("dve", oha[1][:, ti, :], t["oh2"][:], [t["oh2"]], [ohk[1][ti]])
                    self.cp("dve", w_a[0][:, ti:ti + 1], t["w1"][:], [t["w1"]], [w_a[0]])
                    self.cp("dve", w_a[1][:, ti:ti + 1], t["w2"][:], [t["w2"]], [w_a[1]])
                    oh = OHt.next()
                    self.tt("dve", oh[:], t["oh1"][:], t["oh2"][:], ALU.add, [t["oh1"], t["oh2"]], [oh])
                    pr = PB[4 + ti % 2]
                    self.mm(pr[:, 0:NEXP], Ltri[:], oh[:], True, False, [Ltri, oh], [pr])
                    self.mm(pr[:, 0:NEXP], onesf[:], OHs[:], False, True, [onesf, OHs], [pr])
                    self.cp("act", Rall[:, ti, :], pr[:, 0:NEXP], [pr], [Rk[ti]])
                    self.tt("dve", OHs[:], OHs[:], oh[:], ALU.add, [OHs, oh], [OHs])
                pc = PB[6]
                self.mm(pc[:, 0:NEXP], onesf[:], OHs[:], True, True, [onesf, OHs], [pc])
                cntf = self.sb(st, [128, NEXP], F32, "cntf")
                padf = self.sb(st, [128, NEXP], F32, "padf")
                pend = self.sb(st, [128, NEXP], F32, "pend")
                pstart = self.sb(st, [128, NEXP], F32, "pstart")
                cmpb = self.sb(st, [128, NBLK * NEXP], BF16, "cmpb")
                self.cp("dve", cntf[:], pc[:, 0:NEXP], [pc], [cntf])
                cv = cmpb[:].rearrange("p (e j) -> p e j", e=NEXP)
                self.tt("dve", cv, jv[:].unsqueeze(1).broadcast_to([128, NEXP, NBLK]),
                        cntf[:].unsqueeze(2).broadcast_to([128, NEXP, NBLK]), ALU.is_lt, [jv, cntf], [cmpb])
                self.red(padf[:], cv, ALU.add, [cmpb], [padf])
                self.ts("dve", padf[:], padf[:], 128.0, None, ALU.mult, None, [padf], [padf])
                S.op("dve", lambda e_: e_.tensor_tensor_scan(out=pend[:], data0=ones32[:], data1=padf[:], initial=0.0,
                                                             op0=ALU.mult, op1=ALU.add), [ones32, padf], [pend])
                self.tt("dve", pstart[:], pend[:], padf[:], ALU.subtract, [pend, padf], [pstart])
                bef = self.sb(st, [128, NBLK], F32, "bef")
                cv2 = cmpb[:].rearrange("p (j e) -> p j e", e=NEXP)
                self.tt("dve", cv2, pend[:].unsqueeze(1).broadcast_to([128, NBLK, NEXP]),
                        jv[:].unsqueeze(2).broadcast_to([128, NBLK, NEXP]), ALU.is_le, [pend, jv], [cmpb])
                self.red(bef[:], cv2, ALU.add, [cmpb], [bef])
                self.ts("dve", bef[:], bef[:], float(NEXP - 1), None, ALU.min, None, [bef], [bef])
                same2 = self.sb(st, [128, NBLK], F32, "same2")
                self.memset("dve", same2[:], 0.0, [same2])
                self.tt("dve", same2[:, 2:NBLK], bef[:, 2:NBLK], bef[:, 0:NBLK - 2], ALU.is_equal, [bef], [same2])
                self.ts("dve", bef[:], bef[:], 128.0, pif[:, 0:1], ALU.mult, ALU.add, [bef, pif], [bef])
                self.stt(bef[:], same2[:], 1.0e6, bef[:], ALU.mult, ALU.add, [same2, bef], [bef])
                self.cp("dve", idxw[:], bef[:], [bef], [idxw])
                dtmp = self.sbr(st, 2, [128, NEXP], F32, "dtmp")
                dtm2 = self.sbr(st, 2, [128, NEXP], F32, "dtm2")
                dfl = self.sbr(st, 2, [128, 1], F32, "dfl")
                for ti in range(NTL):
                    h2b = h2r.next()
                    self.dma("sp", h2b[:], self.h2d[ti * 128:(ti + 1) * 128, :], [kh2[ti]], [h2b])
                    d1 = dtmp.next()
                    self.tt("dve", d1[:], Rall[:, ti, :], pstart[:], ALU.add, [Rk[ti], pstart], [d1])
                    for k in range(2):
                        d2, df = dtm2.next(), dfl.next()
                        self.tt("dve", d2[:], d1[:], oha[k][:, ti, :], ALU.mult, [d1, ohk[k][ti]], [d2])
                        self.red(df[:], d2[:], ALU.add, [d2], [df])
                        self.cp("dve", d_i[k][:, ti:ti + 1], df[:], [df], [d_i[k]])
                        idx_ap = d_i[k][:, ti:ti + 1]
                        self._scatter(self.xs[:, :], idx_ap, h2b[:], [h2b, d_i[k]], [kxs])
                info["rs"] = S.flush()
            with ExitStack() as st:
                PB = [self.psb(st) for _ in range(8)]
                xbr = self.sbr(st, 2, [128, D], BF16, "xb")
                xTr = self.sbr(st, 2, [128, KC, 128], BF16, "xT")
                wgr = self.sbr(st, 2, [128, 4096], BF16, "wgs")
                wur = self.sbr(st, 2, [128, 4096], BF16, "wus")
                wdr = self.sbr(st, 2, [128, 4096], BF16, "wds")
                sgr = self.sbr(st, 2, [128, 512], BF16, "sgs")
                acr = self.sbr(st, 2, [128, 4, 128], BF16, "acs")
                ysr = self.sbr(st, 2, [128, D], F32, "ysb")
                pgu = Rot([(PB[2], PB[3]), (PB[4], PB[5])])
                breg = {"v": NEXP * 128 - 1}
                for j in range(NBLK):
                    xb = xbr.next()
                    self.dma("sp", xb[:], self.xs[j * 128:(j + 1) * 128, :], [kxs], [xb])
                    wg, wu, wd = wgr.next(), wur.next(), wdr.next()
                    ia = idxw[:, j:j + 1]
                    self._gather(wg[:], self.wgb[l][:, :], ia, [idxw, kwb], [wg], bounds=breg)
                    self._gather(wu[:], self.wub[l][:, :], ia, [idxw, kwb], [wu], bounds=breg)
                    self._gather(wd[:], self.wdb[l][:, :], ia, [idxw, kwb], [wd], bounds=breg)
                    pT = PB[j % 2]
                    pv = pT[:, :].bitcast(BF16).rearrange("p (k t) -> p k t", k=KC)
                    for k in range(KC):
                        self.tr(pv[:, k, :], xb[:, k * 128:(k + 1) * 128], self.identb[:], [xb, self.identb], [pT])
                    xT = xTr.next()
                    self.cp("act", xT[:], pv, [pT], [xT])
                    pg_, pu_ = pgu.next()
                    wgv = wg[:].rearrange("p (k f) -> p k f", k=KC)
                    wuv = wu[:].rearrange("p (k f) -> p k f", k=KC)
                    wdv = wd[:].rearrange("p (k f) -> p k f", k=4)
                    for fc in range(4):
                        fs = slice(fc * 128, (fc + 1) * 128)
                        for k in range(KC):
                            self.mm(pg_[:, fs], wgv[:, k, fs], xT[:, k, :], k == 0, k == KC - 1, [wg, xT], [pg_])
                    for fc in range(4):
                        fs = slice(fc * 128, (fc + 1) * 128)
                        for k in range(KC):
                            self.mm(pu_[:, fs], wuv[:, k, fs], xT[:, k, :], k == 0, k == KC - 1, [wu, xT], [pu_])
                    sg = sgr.next()
                    self.act(sg[:], pg_[:, :], AF.Silu, [pg_], [sg])
                    ac = acr.next()
                    self.tt("dve", ac[:].rearrange("p k t -> p (k t)"), sg[:], pu_[:, :], ALU.mult, [sg, pu_], [ac])
                    ysb = ysr.next()
                    for hf, p in ((0, PB[6]), (1, PB[7])):
                        hs = slice(hf * 512, (hf + 1) * 512)
                        for k in range(4):
                            self.mm(p[:, :], ac[:, k, :], wdv[:, k, hs], k == 0, k == 3, [ac, wd], [p])
                        if hf == 0:
                            self.cp("act", ysb[:, hs], p[:, :], [p], [ysb])
                        else:
                            self.cp("dve", ysb[:, hs], p[:, :], [p], [ysb])
                    self.dma("sp", self.ys[j * 128:(j + 1) * 128, :], ysb[:], [ysb], [kys], semkey=ysb)
                info["e"] = S.flush()
            with ExitStack() as st:
                gts = {}
                y1r = self.sbr(st, 2, [128, D], F32, "y1")
                y2r = self.sbr(st, 2, [128, D], F32, "y2")
                xr = self.sbr(st, 2, [128, D], F32, "xc")
                tr_ = self.sbr(st, 2, [128, D], F32, "tc")
                if l == 0:
                    gts[4] = self.gate_tile(st, l, 1, 4)
                for ti, (b, tile) in enumerate(tiles):
                    if b not in gts:
                        gts[b] = self.gate_tile(st, l, 1, b)
                    isctx = (l == 0 and tile < 2)
                    gt = gts[4] if isctx else gts[b]
                    y1, y2, x_, t_ = y1r.next(), y2r.next(), xr.next(), tr_.next()
                    self._gather(y1[:], self.ys[:, :], d_i[0][:, ti:ti + 1], [d_i[0], kys], [y1])
                    self._gather(y2[:], self.ys[:, :], d_i[1][:, ti:ti + 1], [d_i[1], kys], [y2])
                    self.dma("sp", x_[:], self.xres[b, tile * 128:(tile + 1) * 128, :], [self.kx[b][tile]], [x_])
                    self.ts("dve", t_[:], y1[:], w_a[0][:, ti:ti + 1], None, ALU.mult, None, [y1, w_a[0]], [t_])
                    self.stt(t_[:], y2[:], w_a[1][:, ti:ti + 1], t_[:], ALU.mult, ALU.add, [y2, w_a[1], t_], [t_])
                    self.tt("dve", t_[:], t_[:], gt[:], ALU.mult, [t_, gt], [t_])
                    self.tt("dve", t_[:], t_[:], x_[:], ALU.add, [t_, x_], [t_])
                    if l == 0:
                        self.dma("sp", self.xres[b, tile * 128:(tile + 1) * 128, :], t_[:], [t_], [self.kx[b][tile]], semkey=t_)
                        if ("xf0" in self.D_) and b == 0:
                            self.dma("sp", self.D_["xf0"][tile * 128:(tile + 1) * 128, :], t_[:], [t_], [self.kscr], semkey=t_)
                    else:
                        self.dma("sp", self.out[b, (tile - 2) * 128:(tile - 1) * 128, :], t_[:], [t_], [self.kscr], semkey=t_)
                info["c"] = S.flush()
        return info

    def precast(self, l, b):
        I = self.I
        per = (NEXP + self.NB - 1) // self.NB
        if not hasattr(self, "pck"):
            self.pck = Rot(self.S.keys(8))
        for e in range(b * per, min(NEXP, (b + 1) * per)):
            rows = slice(e * 128, (e + 1) * 128)
            for dst, src, kk in ((self.wgb[l], "moe_w_gate", 8), (self.wub[l], "moe_w_up", 8), (self.wdb[l], "moe_w_down", 4)):
                self.dma("pool", dst[rows, :].rearrange("p (k f) -> p k f", k=kk),
                         I[src][l, e].rearrange("(k p) f -> p k f", p=128), [], [], semkey=self.pck.next())

    def _gather(self, out, src, idx_ap, R, W, bounds=None):
        def fn(e):
            if bounds is None:
                return e.indirect_dma_start(out=out, out_offset=None, in_=src,
                                            in_offset=bass.IndirectOffsetOnAxis(ap=idx_ap, axis=0))
            if "r" not in bounds:
                bounds["r"] = e.to_reg(bounds["v"])
            return e.indirect_dma_start(out=out, out_offset=None, in_=src,
                                        in_offset=bass.IndirectOffsetOnAxis(ap=idx_ap, axis=0),
                                        bounds_check=bounds["r"], oob_is_err=False)
        self.S.dma("pool", fn, R, W, nbytes=128 * self._n(out) * 2, indirect=True)

    def _scatter(self, dst, idx_ap, in_, R, W):
        nrow = dst.shape[0]
        self.S.dma("pool", lambda e: e.indirect_dma_start(out=dst, out_offset=bass.IndirectOffsetOnAxis(ap=idx_ap, axis=0),
                                                          in_=in_, in_offset=None),
                   R, W, semkey=R[0], nbytes=128 * 2048, indirect=True)

    def build(self):
        with ExitStack() as gst:
            gst.enter_context(self.nc.allow_non_contiguous_dma(reason="small strided parameter loads"))
            self.setup_consts(gst)
            info = {}
            if "mod" in self.stages:
                info["mod"] = self.stage_mod()
            for b in range(self.NB):
                if "hgrn" in self.stages:
                    info["hgrn%d" % b] = self.stage_hgrn(b)
            if "moe0" in self.stages:
                info["moe0"] = self.moe_sparse(0)
            for b in range(self.NB):
                if "mla" in self.stages:
                    info["mla%d" % b] = self.stage_mla(b)
            if "moe1" in self.stages:
                info["moe1"] = self.moe_sparse(1)
            self.info = info
        self.S.close()
        return self.nc


def host_consts():
    s = np.arange(128)
    same = (s[:, None] // 32) == (s[None, :] // 32)
    maskf = (same & (s[:, None] <= s[None, :])).astype(np.float32)
    maskb = (same & (s[:, None] >= s[None, :])).astype(np.float32)
    bm = ((s[:, None] // 32) == np.arange(4)[None, :]).astype(np.float32)[:, :, None].repeat(128, axis=2)
    t = np.arange(SEQ)
    row, col = t // 64, t % 64
    inv = (10000.0 ** (-np.arange(0, 16, 2, dtype=np.float32) / 16)).astype(np.float32)
    ang = np.stack([row, col], axis=-1).astype(np.float32)[..., None] * inv
    cos = np.cos(ang).astype(np.float32).reshape(SEQ, 16)
    sin = np.sin(ang).astype(np.float32).reshape(SEQ, 16)
    return {"k_maskf": maskf, "k_maskb": maskb, "k_bm": np.ascontiguousarray(bm), "k_bmc": np.ascontiguousarray(bm[:, :, 0]),
            "k_cos": cos, "k_sin": sin}


def make_in_maps(inputs, NB, ncores, used=None):
    sq = {"hg_w_in": "hg_w_in", "hg_lower_bounds": "hg_lb", "hg_out_norm_g": "hg_out_norm_g", "hg_w_out": "hg_w_out",
          "mla_w_in": "mla_w_in", "mla_q_norm_g": "mla_q_norm_g", "mla_kv_norm_g": "mla_kv_norm_g", "mla_w_qb": "mla_w_qb",
          "mla_w_kvb": "mla_w_kvb", "mla_q_qknorm_g": "mla_q_qknorm_g", "mla_k_qknorm_g": "mla_k_qknorm_g", "mla_w_out": "mla_w_out"}
    shared = {}
    for k, v in inputs.items():
        v = np.asarray(v, dtype=np.float32)
        if k in ("x", "c", "ctx"):
            continue
        if k == "hg_lower_bounds":
            shared["hg_lb"] = np.ascontiguousarray(v)
        elif k in sq:
            shared[sq[k]] = np.ascontiguousarray(v.reshape(v.shape[1:]))
        else:
            shared[k] = np.ascontiguousarray(v)
    shared.update(host_consts())
    maps = []
    for i in range(ncores):
        m = dict(shared)
        for k in ("x", "c", "ctx"):
            m[k] = np.ascontiguousarray(np.asarray(inputs[k], dtype=np.float32)[i * NB:(i + 1) * NB])
        if used is not None:
            m = {k: v for k, v in m.items() if k in used}
        maps.append(m)
    return maps


def kernel(**inputs):
    NB = 4
    kb = KB(NB=NB)
    nc = kb.build()
    maps = make_in_maps(inputs, NB, 8, used=set(kb.I.keys()))
    res = run_bass_kernel_spmd(nc, maps, core_ids=list(range(8)))
    return np.concatenate([r["out"] for r in res.results], axis=0).astype(np.float32)
```

```python
import numpy as np
import os as _os
_F = lambda k: _os.environ.get(k, '1') == '1'
import concourse.bass as bass
import concourse.mybir as mybir
from concourse.bass_utils import run_bass_kernel_spmd
from contextlib import ExitStack

F32 = mybir.dt.float32
BF16 = mybir.dt.bfloat16
I32 = mybir.dt.int32
AF = mybir.ActivationFunctionType
ALU = mybir.AluOpType
AX = mybir.AxisListType

ENGS = ("pe", "act", "dve", "pool", "sp")

D = 1024
KC = 8
CTX = 256
SEQ = 2048
T = CTX + SEQ
NT = T // 128
EPS = 1e-6
NEXP = 32
FF = 512
BIG = 1.0e30


class Key:
    __slots__ = ("w", "r", "dsem", "dcnt", "excl")

    def __init__(self):
        self.w = None
        self.r = []
        self.dsem = None
        self.dcnt = 0
        self.excl = False


class Tl:
    __slots__ = ("t", "k")

    def __init__(self, t, k):
        self.t = t
        self.k = k

    def __getitem__(self, idx):
        return self.t[idx]


def _k(x):
    return x.k if isinstance(x, Tl) else x


class Rot:
    def __init__(self, items):
        self.items = items
        self.i = 0

    def next(self):
        it = self.items[self.i % len(self.items)]
        self.i += 1
        return it


class Sched:
    def __init__(self, nc, n_dma_sems=80):
        self.nc = nc
        self.stack = ExitStack()
        self.esem = {e: self.stack.enter_context(nc.semaphore("es_" + e)) for e in ENGS}
        self.ecnt = {e: 0 for e in ENGS}
        self.dpool = [[self.stack.enter_context(nc.semaphore("ds%d" % i)), 0] for i in range(n_dma_sems)]
        self.dfree = list(range(n_dma_sems))
        self.all_keys = []
        self.reorder = True
        self._reset_stage()

    def _reset_stage(self):
        self.recs = []
        self.dlast = {}

    def key(self):
        k = Key()
        self.all_keys.append(k)
        return k

    def keys(self, n):
        return [self.key() for _ in range(n)]

    def _deps(self, reads, writes):
        deps = set()
        for t in reads:
            if t.w is not None:
                deps.add(t.w)
        for t in writes:
            if t.w is not None:
                deps.add(t.w)
            deps.update(t.r)
        return deps

    def _add(self, eng, fn, reads, writes, cost, dma, lat):
        reads = [_k(x) for x in reads]
        writes = [_k(x) for x in writes]
        ex = [t for t in reads if t.excl and t not in writes]
        if ex:
            reads = [t for t in reads if not t.excl]
            writes = writes + ex
        deps = self._deps(reads, writes)
        i = len(self.recs)
        if dma is not None:
            prev = self.dlast.get(dma)
            if prev is not None:
                deps.add(prev)
            self.dlast[dma] = i
        self.recs.append({"eng": eng, "fn": fn, "deps": deps, "cost": cost, "dma": dma, "lat": lat, "inc": False})
        for t in reads:
            t.r.append(i)
        for t in writes:
            t.w = i
            t.r = []
        return i

    def op(self, eng, fn, reads=(), writes=(), cost=0.2):
        return self._add(eng, fn, reads, writes, cost, None, 0.0)

    def dma(self, eng, fn, reads=(), writes=(), semkey=None, nbytes=0, indirect=False):
        rk = [_k(x) for x in reads]
        wk = [_k(x) for x in writes]
        sk = _k(semkey) if semkey is not None else (wk[0] if wk else rk[0])
        if sk.dsem is None:
            sk.dsem = self.dfree.pop()
        i = self._add(eng, fn, rk, wk, 0.8 if indirect else 0.07, sk.dsem, 2.0 + nbytes / 150e3)
        return i

    def _schedule(self):
        recs = self.recs
        n = len(recs)
        users = [[] for _ in range(n)]
        ndep = [0] * n
        for i, r in enumerate(recs):
            ndep[i] = len(r["deps"])
            for d in r["deps"]:
                users[d].append(i)
        import heapq
        ready = {e: [] for e in ENGS}
        fin = [0.0] * n
        rt = [0.0] * n
        for i, r in enumerate(recs):
            if ndep[i] == 0:
                heapq.heappush(ready[r["eng"]], (0.0, i))
        free = {e: 0.0 for e in ENGS}
        order = {e: [] for e in ENGS}
        done = 0
        while done < n:
            best = None
            for e in ENGS:
                h = ready[e]
                if not h:
                    continue
                t0 = free[e]
                cand = None
                if h[0][0] <= t0:
                    tmp = []
                    while h and h[0][0] <= t0:
                        tmp.append(heapq.heappop(h))
                    ci = min(tmp, key=lambda x: x[1])
                    for x in tmp:
                        if x is not ci:
                            heapq.heappush(h, x)
                    cand = (t0, ci[1], ci)
                else:
                    x = h[0]
                    cand = (x[0], x[1], None)
                if best is None or (cand[0], cand[1]) < (best[0][0], best[0][1]):
                    if best is not None and best[0][2] is not None:
                        heapq.heappush(ready[best[1]], best[0][2])
                    best = (cand, e)
                elif cand[2] is not None:
                    heapq.heappush(h, cand[2])
            (start, i, popped), e = best
            if popped is None:
                heapq.heappop(ready[e])
            r = recs[i]
            free[e] = start + r["cost"]
            fin[i] = start + r["cost"] + r["lat"]
            order[e].append(i)
            done += 1
            for u in users[i]:
                ndep[u] -= 1
                ru = recs[u]
                lat = 0.05 if ru["eng"] == e else 0.25
                if fin[i] + lat > rt[u]:
                    rt[u] = fin[i] + lat
                if ndep[u] == 0:
                    heapq.heappush(ready[ru["eng"]], (rt[u], u))
        return order, max(fin) if n else 0.0

    def flush(self):
        nc = self.nc
        recs = self.recs
        if self.reorder:
            order, est = self._schedule()
        else:
            order = {e: [i for i, r in enumerate(recs) if r["eng"] == e] for e in ENGS}
            est = 0.0
        dval = {}
        dtot = {}
        for i, r in enumerate(recs):
            if r["dma"] is not None:
                c = self.dpool[r["dma"]][1] + 16
                self.dpool[r["dma"]][1] = c
                dval[i] = c
                dtot[r["dma"]] = c
        for i, r in enumerate(recs):
            for d in r["deps"]:
                rd = recs[d]
                if rd["dma"] is None and (rd["eng"] != r["eng"] or r["eng"] in ("act", "dve", "pool")):
                    rd["inc"] = True
        eval_ = {}
        for e in ENGS:
            c = self.ecnt[e]
            for i in order[e]:
                if recs[i]["inc"]:
                    c += 1
                eval_[i] = c
            self.ecnt[e] = c
        engobj = {"pe": "tensor", "act": "scalar", "dve": "vector", "pool": "gpsimd", "sp": "sync"}
        esem, dpool = self.esem, self.dpool

        def mk(e):
            def body(eng):
                seen = {}
                for i in order[e]:
                    r = recs[i]
                    waits = {}
                    for d in r["deps"]:
                        rd = recs[d]
                        if rd["dma"] is not None:
                            k, v = ("d", rd["dma"]), dval[d]
                        elif rd["eng"] != e or e in ("act", "dve", "pool"):
                            k, v = ("e", rd["eng"]), eval_[d]
                        else:
                            continue
                        if seen.get(k, -1) >= v:
                            continue
                        if waits.get(k, -1) < v:
                            waits[k] = v
                    for k, v in waits.items():
                        seen[k] = v
                        if k[0] == "e":
                            eng.wait_ge(esem[k[1]], v)
                        else:
                            eng.wait_ge(dpool[k[1]][0], v)
                    ins = r["fn"](eng)
                    if r["dma"] is not None:
                        ins.then_inc(dpool[r["dma"]][0], 16)
                    elif r["inc"]:
                        ins.then_inc(esem[e], 1)
                if e == "sp":
                    for idx, v in dtot.items():
                        if seen.get(("d", idx), -1) < v:
                            eng.wait_ge(dpool[idx][0], v)
            return body

        with nc.Block() as block:
            for e in ENGS:
                if order[e] or (e == "sp" and dtot):
                    getattr(block, engobj[e])(mk(e))
        for k in self.all_keys:
            if k.dsem is not None:
                self.dfree.append(k.dsem)
                k.dsem = None
            k.w = None
            k.r = []
        n = {e: len(order[e]) for e in ENGS}
        n["est_us"] = round(est, 1)
        self._reset_stage()
        return n

    def close(self):
        self.stack.close()


class KB:
    def __init__(self, NB=4, stages=("mod", "hgrn", "moe0", "mla", "moe1"), dbg=()):
        self.NB = NB
        self.stages = stages
        self.dbg = dbg
        nc = bass.Bass("TRN2", target_bir_lowering=False)
        self.nc = nc
        self.S = Sched(nc)
        self.uid = 0
        shapes = {
            "x": (NB, SEQ, D), "c": (NB, D), "ctx": (NB, CTX, D), "c_ctx": (D,),
            "ada_w": (2, D, 6 * D), "ada_b": (2, 6 * D), "norm_mix_g": (2, D), "norm_ffn_g": (2, D),
            "hg_w_in": (D, 5 * D), "hg_lb": (2, 3, D), "hg_out_norm_g": (128,), "hg_w_out": (D, D),
            "mla_w_in": (D, 416), "mla_q_norm_g": (256,), "mla_kv_norm_g": (128,), "mla_w_qb": (256, 1536),
            "mla_w_kvb": (128, 2048), "mla_q_qknorm_g": (96,), "mla_k_qknorm_g": (96,), "mla_w_out": (D, D),
            "moe_w_group": (2, D, 4), "moe_w_expert": (2, D, 32), "moe_w_gate": (2, NEXP, D, FF),
            "moe_w_up": (2, NEXP, D, FF), "moe_w_down": (2, NEXP, FF, D),
            "k_maskf": (128, 128), "k_maskb": (128, 128), "k_bm": (128, 4, 128), "k_bmc": (128, 4), "k_cos": (SEQ, 16), "k_sin": (SEQ, 16),
        }

        class LazyIn(dict):
            def __missing__(d_, name):
                ap = nc.dram_tensor(name, list(shapes[name]), F32, kind="ExternalInput").ap()
                d_[name] = ap
                return ap

        I = LazyIn()
        self.I = I
        self.out = nc.dram_tensor("out", [NB, SEQ, D], F32, kind="ExternalOutput").ap()
        self.xres = nc.dram_tensor("xres", [NB, T, D], F32).ap()
        self.mod = nc.dram_tensor("modv", [2, 5, 6 * D], F32).ap()
        self.cT_d = nc.dram_tensor("cT_d", [128, 3, T], BF16).ap()
        self.krr_d = nc.dram_tensor("krr_d", [128, NT, 32], F32).ap()
        self.sskr_d = nc.dram_tensor("sskr_d", [128, NT], F32).ap()
        NTLmax = NB * NT
        self.NBLKmax = 2 * NTLmax + NEXP
        self.h2d = nc.dram_tensor("h2d", [NTLmax * 128, D], BF16).ap()
        self.xs = nc.dram_tensor("xs", [self.NBLKmax * 128, D], BF16).ap()
        self.ys = nc.dram_tensor("ys", [self.NBLKmax * 128, D], F32).ap()
        self.wgb = [nc.dram_tensor("wgb%d" % l_, [NEXP * 128, 4096], BF16).ap() for l_ in range(2)]
        self.wub = [nc.dram_tensor("wub%d" % l_, [NEXP * 128, 4096], BF16).ap() for l_ in range(2)]
        self.wdb = [nc.dram_tensor("wdb%d" % l_, [NEXP * 128, 4096], BF16).ap() for l_ in range(2)]
        self.kwb = [self.S.key() for _ in range(2)]
        self.kx = [self.S.keys(NT) for _ in range(NB)]
        self.kmod = self.S.key()
        self.kscr = self.S.key()
        self.D_ = {}
        for name, shape in dbg:
            self.D_[name] = nc.dram_tensor("dbg_" + name, list(shape), F32, kind="ExternalOutput").ap()

    def sb(self, st, shape, dt, nm="t"):
        self.uid += 1
        t = st.enter_context(self.nc.sbuf_tensor("%s_%d" % (nm, self.uid), list(shape), dt))
        return Tl(t, self.S.key())

    def sbr(self, st, n, shape, dt, nm="r"):
        return Rot([self.sb(st, shape, dt, nm) for _ in range(n)])

    def psb(self, st, nm="ps"):
        self.uid += 1
        t = st.enter_context(self.nc.psum_tensor("%s_%d" % (nm, self.uid), [128, 512], F32))
        k = self.S.key()
        k.excl = True
        return Tl(t, k)

    def psb2(self, st, nm="ps2"):
        self.uid += 1
        t = st.enter_context(self.nc.psum_tensor("%s_%d" % (nm, self.uid), [128, 1024], F32))
        k = self.S.key()
        k.excl = True
        return Tl(t, k)

    @staticmethod
    def _n(ap):
        n = 1
        for d in ap.shape[1:]:
            n *= d
        return n

    def mm(self, out, lhsT, rhs, start, stop, R, W):
        c = max(64, self._n(out)) / 2400.0 + 0.02
        if rhs.dtype == F32:
            c *= 4
        self.S.op("pe", lambda e: e.matmul(out, lhsT=lhsT, rhs=rhs, start=start, stop=stop), R, W, cost=c)

    def tr(self, out, in_, ident, R, W):
        self.S.op("pe", lambda e: e.transpose(out=out, in_=in_, identity=ident), R, W, cost=0.09)

    def act(self, out, in_, func, R, W, bias=None, scale=None, accum_out=None):
        kw = {}
        if bias is not None:
            kw["bias"] = bias
        if scale is not None:
            kw["scale"] = scale
        if accum_out is not None:
            kw["accum_out"] = accum_out
        self.S.op("act", lambda e: e.activation(out=out, in_=in_, func=func, **kw), R, W, cost=0.22 + self._n(out) / 1200.0)

    def ts(self, eng, out, in0, s1, s2, op0, op1, R, W):
        if op1 is None:
            self.S.op(eng, lambda e: e.tensor_scalar(out=out, in0=in0, scalar1=s1, scalar2=None, op0=op0), R, W, cost=self._c(eng, out))
        else:
            self.S.op(eng, lambda e: e.tensor_scalar(out=out, in0=in0, scalar1=s1, scalar2=s2, op0=op0, op1=op1), R, W, cost=self._c(eng, out))

    def tt(self, eng, out, in0, in1, op, R, W):
        self.S.op(eng, lambda e: e.tensor_tensor(out=out, in0=in0, in1=in1, op=op), R, W, cost=self._c(eng, out, 1.5))

    def stt(self, out, in0, scalar, in1, op0, op1, R, W):
        self.S.op("dve", lambda e: e.scalar_tensor_tensor(out=out, in0=in0, scalar=scalar, in1=in1, op0=op0, op1=op1), R, W,
                  cost=self._c("dve", out, 1.5))

    def cp(self, eng, out, in_, R, W):
        if eng == "act":
            self.S.op("act", lambda e: e.activation(out=out, in_=in_, func=AF.Copy), R, W, cost=0.22 + self._n(out) / 1200.0)
        else:
            self.S.op(eng, lambda e: e.tensor_copy(out=out, in_=in_), R, W, cost=self._c(eng, out))

    def red(self, out, in_, op, R, W, negate=None):
        self.S.op("dve", lambda e: e.tensor_reduce(out=out, in_=in_, axis=AX.X, op=op, negate=negate), R, W, cost=self._c("dve", in_))

    def recip(self, out, in_, R, W):
        self.S.op("dve", lambda e: e.reciprocal(out=out, in_=in_), R, W, cost=self._c("dve", out, 8.0))

    def memset(self, eng, ap, val, W):
        self.S.op(eng, lambda e: e.memset(ap, val), (), W, cost=self._c(eng, ap))

    def _c(self, eng, ap, mult=1.0):
        n = self._n(ap)
        if eng == "pool":
            return 0.25 + n * mult / 500.0
        return 0.1 + n * mult / 960.0

    def dma(self, q, out, in_, R, W, semkey=None):
        nb = out.shape[0] * self._n(out) * 4
        self.S.dma(q, lambda e: e.dma_start(out=out, in_=in_), R, W, semkey=semkey, nbytes=nb)

    def sumsq(self, junk, in_, acc, R, W):
        self.act(junk, in_, AF.Square, R, W, accum_out=acc)

    def setup_consts(self, st):
        nc, S = self.nc, self.S
        self.identf = self.sb(st, [128, 128], F32, "identf")
        self.identb = self.sb(st, [128, 128], BF16, "identb")
        self.onesb = self.sb(st, [128, 128], BF16, "onesb")
        identf = self.identf
        self.memset("pool", identf[:], 0.0, [identf])
        S.op("pool", lambda e: e.affine_select(out=identf[:], in_=identf[:], pattern=[[-1, 128]], compare_op=ALU.not_equal,
                                               fill=1.0, base=0, channel_multiplier=1), [identf], [identf])
        self.cp("dve", self.identb[:], identf[:], [identf], [self.identb])
        self.memset("dve", self.onesb[:], 1.0, [self.onesb])
        self.epsc = self.sb(st, [128, 1], F32, "epsc")
        self.memset("dve", self.epsc[:], EPS, [self.epsc])
        self.onec = self.sb(st, [128, 1], F32, "onec")
        self.memset("dve", self.onec[:], 1.0, [self.onec])

    def stage_mod(self):
        I, NB = self.I, self.NB
        with ExitStack() as st:
            cT = self.sb(st, [128, KC, 5], F32, "cT")
            cs = self.sb(st, [128, KC, 5], F32, "cs")
            self.memset("dve", cT[:], 0.0, [cT])
            for r in range(NB):
                self.dma("sp", cT[:, :, r], I["c"][r, :].rearrange("(c p) -> p c", p=128), [], [cT])
            self.dma("sp", cT[:, :, 4], I["c_ctx"].rearrange("(c p) -> p c", p=128), [], [cT])
            self.act(cs[:], cT[:], AF.Silu, [cT], [cs])
            wrot = self.sbr(st, 3, [128, KC, 512], F32, "adaw")
            ps = Rot([self.psb(st) for _ in range(2)])
            for l in range(2):
                bt = self.sb(st, [5, 6 * D], F32, "adab")
                ms = self.sb(st, [5, 6 * D], F32, "modsb")
                self.dma("sp", bt[:], I["ada_b"][l, :].partition_broadcast(5), [], [bt])
                for n in range(12):
                    w = wrot.next()
                    self.dma("sp", w[:], I["ada_w"][l, :, n * 512:(n + 1) * 512].rearrange("(c p) n -> p c n", p=128), [], [w])
                    p = ps.next()
                    for k in range(KC):
                        self.mm(p[0:5, :], cs[:, k, :], w[:, k, :], k == 0, k == KC - 1, [cs, w], [p])
                    self.tt("dve", ms[:, n * 512:(n + 1) * 512], p[0:5, :], bt[:, n * 512:(n + 1) * 512], ALU.add, [p, bt], [ms])
                self.dma("sp", self.mod[l], ms[:], [ms], [self.kmod], semkey=ms)
            return self.S.flush()

    def mod_cols(self, st, l, m, r):
        I = self.I
        g = I["norm_mix_g"] if m == 0 else I["norm_ffn_g"]
        gc = self.sb(st, [128, KC], F32, "gc")
        sc = self.sb(st, [128, KC], F32, "sc")
        sh = self.sb(st, [128, KC], F32, "sh")
        A = self.sb(st, [128, KC], F32, "A")
        self.dma("sp", gc[:], g[l, :].rearrange("(c p) -> p c", p=128), [], [gc])
        self.dma("sp", sc[:], self.mod[l, r, (3 * m + 1) * D:(3 * m + 2) * D].rearrange("(c p) -> p c", p=128), [self.kmod], [sc])
        self.dma("sp", sh[:], self.mod[l, r, (3 * m) * D:(3 * m + 1) * D].rearrange("(c p) -> p c", p=128), [self.kmod], [sh])
        self.stt(A[:], sc[:], 1.0, gc[:], ALU.add, ALU.mult, [sc, gc], [A])
        return A, sh

    def gate_tile(self, st, l, m, r):
        gt = self.sb(st, [128, D], F32, "gt")
        self.dma("sp", gt[:], self.mod[l, r, (3 * m + 2) * D:(3 * m + 3) * D].partition_broadcast(128), [self.kmod], [gt])
        return gt

    def norm_res(self, st, pbanks, junk=None, nxn=1, nhtf=1):
        R = {}
        R["xt"] = self.sbr(st, 2, [128, D], F32, "xt")
        R["xn"] = self.sbr(st, nxn, [128, D], F32, "xn")
        R["junk"] = junk if junk is not None else self.sb(st, [128, D], BF16, "junk")
        R["ss"] = self.sbr(st, 2, [128, 1], F32, "ss")
        R["sd"] = self.sbr(st, 2, [128, 1], F32, "sd")
        R["rs"] = self.sbr(st, 2, [128, 1], F32, "rs")
        R["hTf"] = self.sbr(st, nhtf, [128, KC, 128], F32, "hTf")
        R["pb"] = pbanks
        return R

    def norm_tile(self, R, src_ap, src_keys, A, Bc, dst_ap, dst_keys):
        xt = R["xt"].next()
        xn = R["xn"].next()
        ss = R["ss"].next()
        sd = R["sd"].next()
        rs = R["rs"].next()
        hTf = R["hTf"].next()
        junk = R["junk"]
        pa, pb = R["pb"]
        self.dma("sp", xt[:], src_ap, src_keys, [xt])
        self.sumsq(junk[:, 0:D], xt[:], ss[:], [xt], [junk, ss])
        self.act(sd[:], ss[:], AF.Sqrt, [ss, self.epsc], [sd], bias=self.epsc[:, 0:1], scale=1.0 / D)
        self.recip(rs[:], sd[:], [sd], [rs])
        self.act(xn[:], xt[:], AF.Copy, [xt, rs], [xn], scale=rs[:, 0:1])
        for k in range(KC):
            p = pa if k < 4 else pb
            self.tr(p[:, (k % 4) * 128:(k % 4 + 1) * 128], xn[:, k * 128:(k + 1) * 128], self.identf[:], [xn, self.identf], [p])
        for k in range(KC):
            p = pa if k < 4 else pb
            src = p[:, (k % 4) * 128:(k % 4 + 1) * 128]
            if k < 4:
                self.ts("dve", hTf[:, k, :], src, A[:, k:k + 1], Bc[:, k:k + 1], ALU.mult, ALU.add, [p, A, Bc], [hTf])
            else:
                self.act(hTf[:, k, :], src, AF.Identity, [p, A, Bc], [hTf], bias=Bc[:, k:k + 1], scale=A[:, k:k + 1])
        if dst_ap is not None:
            self.cp("dve", dst_ap, hTf[:], [hTf], dst_keys)
        self.last_xn = xn
        return hTf

    def src_l0(self, b, tile):
        if tile < 2:
            return self.I["ctx"][b, tile * 128:(tile + 1) * 128, :]
        return self.I["x"][b, (tile - 2) * 128:(tile - 1) * 128, :]

    def stage_hgrn(self, b):
        I, S = self.I, self.S
        l = 0
        with ExitStack() as st:
            PB = [self.psb(st) for _ in range(8)]
            if "moe0" in self.stages:
                self.precast(0, b)
            hT = self.sb(st, [128, KC, T], BF16, "hT")
            hTk = S.keys(NT)
            ogT = self.sb(st, [128, KC, T], BF16, "ogT")
            ogk = S.keys(KC)
            maskf = self.sb(st, [128, 128], F32, "maskf")
            maskb = self.sb(st, [128, 128], F32, "maskb")
            bmc = self.sb(st, [128, 4], F32, "bmc")
            if not _F("HG_A"):
                bm = self.sb(st, [128, 4, 128], BF16, "bm")
                self.dma("pool", bm[:], I["k_bm"], [], [bm])
                Vbd = self.sbr(st, 2, [128, 4, 128], BF16, "Vbd")
            self.dma("sp", maskf[:], I["k_maskf"], [], [maskf])
            self.dma("sp", maskb[:], I["k_maskb"], [], [maskb])
            self.dma("sp", bmc[:], I["k_bmc"], [], [bmc])
            m01 = self.sb(st, [128, T], BF16, "m01")
            self.memset("dve", m01[:], 1.0, [m01])
            self.memset("dve", m01[:, 0:T:32], 0.0, [m01])
            lbr = self.sb(st, [128, 2, 3, KC], F32, "lbr")
            with self.nc.allow_non_contiguous_dma(reason="tiny"):
                for d_ in range(2):
                    for j in range(3):
                        self.dma("sp", lbr[:, d_, j, :], I["hg_lb"][d_, j, :].rearrange("(h p) -> p h", p=128), [], [lbr])
            lbe = self.sb(st, [128, 2, 3, KC], F32, "lbe")
            self.act(lbe[:], lbr[:], AF.Exp, [lbr], [lbe])
            lbs = self.sb(st, [128, 2, KC], F32, "lbs")
            self.tt("dve", lbs[:], lbe[:, :, 0, :], lbe[:, :, 1, :], ALU.add, [lbe], [lbs])
            self.tt("dve", lbs[:], lbs[:], lbe[:, :, 2, :], ALU.add, [lbe, lbs], [lbs])
            lbi = self.sb(st, [128, 2, KC], F32, "lbi")
            self.recip(lbi[:], lbs[:], [lbs], [lbi])
            lb = self.sb(st, [128, 2, KC], F32, "lb")
            oml = self.sb(st, [128, 2, KC], F32, "oml")
            self.tt("dve", lb[:], lbe[:, :, 0, :], lbi[:], ALU.mult, [lbe, lbi], [lb])
            self.ts("dve", oml[:], lb[:], -1.0, 1.0, ALU.mult, ALU.add, [lb], [oml])
            ogc = self.sb(st, [128, 1], F32, "ogc")
            self.dma("sp", ogc[:], I["hg_out_norm_g"].rearrange("(p o) -> p o", o=1), [], [ogc])
            A_l, B_l = self.mod_cols(st, l, 0, b)
            A_c, B_c = self.mod_cols(st, l, 0, 4)
            gt_l = self.gate_tile(st, l, 0, b)
            gt_c = self.gate_tile(st, l, 0, 4)
            qdec = self.sb(st, [128, T], BF16, "qdec")
            NR = self.norm_res(st, (PB[0], PB[1]), junk=qdec)
            for tile in range(NT):
                A, Bc = (A_c, B_c) if tile < 2 else (A_l, B_l)
                self.norm_tile(NR, self.src_l0(b, tile), [], A, Bc, hT[:, :, tile * 128:(tile + 1) * 128], [hTk[tile]])
            wh = self.sbr(st, 1, [128, KC, 5, 128], BF16, "wh")
            Vh = self.sb(st, [128, NT, 128], BF16, "Vh")
            qs = self.sb(st, [128, T], BF16, "qs")
            sgate = self.sb(st, [128, T], BF16, "sgate")
            A1 = self.sb(st, [128, T], F32, "A1")
            A2 = self.sb(st, [128, T], F32, "A2")
            A3 = self.sb(st, [128, T], F32, "A3")
            kinc = self.sb(st, [128, T], BF16, "kinc")
            dec = self.sb(st, [128, T // 32], F32, "dec")
            tot = self.sb(st, [128, T // 32], F32, "tot")
            oacc = self.sb(st, [128, T], F32, "oacc")
            oak = S.keys(NT)
            sTm = self.sbr(st, 2, [128, 128], BF16, "sTm")
            kTs = self.sbr(st, 2, [128, 4, 128], BF16, "kTs")
            KVs = self.sbr(st, 2, [128, 4, 128], F32, "KVs")
            Sst = self.sb(st, [128, 8, 128], F32, "Sst")
            Sstk = S.keys(8)
            Sb = self.sb(st, [128, 8, 128], BF16, "Sb")
            Sbk = S.keys(2)
            pproj = Rot([PB[0], PB[1]])
            psT = Rot([PB[2], PB[3]])
            pkT = PB[4]
            pkTk = [PB[4].k, PB[4].k]
            pKV = PB[5]
            poT = Rot([PB[6], PB[7]])
            blocks = [(i * 512, 512) for i in range(4)] + [(2048, 256)]

            def proj(whh, sec, blk):
                t0, n = blk
                p = pproj.next()
                tiles = range(t0 // 128, (t0 + n) // 128)
                for k in range(KC):
                    self.mm(p[:, 0:n], whh[:, k, sec, :], hT[:, k, t0:t0 + n], k == 0, k == KC - 1,
                            [whh] + [hTk[t] for t in tiles], [p])
                return p

            for h in range(KC):
                whh = wh.next()
                for sec in range(5):
                    self.dma("pool", whh[:, :, sec, :],
                             I["hg_w_in"][:, sec * D + h * 128: sec * D + (h + 1) * 128].rearrange("(c p) e -> p c e", p=128), [], [whh])
                for tile in range(NT):
                    p = pproj.next()
                    for k in range(KC):
                        self.mm(p[:, 0:128], hT[:, k, tile * 128:(tile + 1) * 128], whh[:, k, 3, :], k == 0, k == KC - 1,
                                [whh, hTk[tile]], [p])
                    self.cp("act", Vh[:, tile, :], p[:, 0:128], [p], [Vh])
                for blk in blocks:
                    t0, n = blk
                    p = proj(whh, 0, blk)
                    self.act(qs[:, t0:t0 + n], p[:, 0:n], AF.Silu, [p], [qs])
                    p = proj(whh, 4, blk)
                    self.act(sgate[:, t0:t0 + n], p[:, 0:n], AF.Silu, [p], [sgate])
                for dr in range(2):
                    for blk in blocks:
                        t0, n = blk
                        p = proj(whh, 1 + dr, blk)
                        self.act(A1[:, t0:t0 + n], p[:, 0:n], AF.Sigmoid, [p], [A1])
                    self.ts("dve", A1[:], A1[:], oml[:, dr, h:h + 1], lb[:, dr, h:h + 1], ALU.mult, ALU.add, [A1, oml, lb], [A1])
                    self.act(A2[:], A1[:], AF.Ln, [A1], [A2])
                    if _F("HG_B"):
                        self.act(A1[:], A1[:], AF.Identity, [A1, self.onec], [A1], bias=self.onec[:, 0:1], scale=-1.0)
                    else:
                        self.ts("dve", A1[:], A1[:], -1.0, 1.0, ALU.mult, ALU.add, [A1], [A1])
                    S.op("dve", lambda e: e.tensor_tensor_scan(out=A3[:], data0=m01[:], data1=A2[:], initial=0.0,
                                                                op0=ALU.mult, op1=ALU.add), [m01, A2], [A3], cost=0.1 + 2 * T / 960.0)
                    a3v = A3[:].rearrange("p (j i) -> p j i", i=32)
                    self.cp("dve", tot[:], a3v[:, :, 31], [A3], [tot])
                    self.act(dec[:], tot[:], AF.Exp, [tot], [dec])
                    if dr == 0:
                        barr, free = A3, A2
                    else:
                        a2v = A2[:].rearrange("p (j i) -> p j i", i=32)
                        self.tt("dve", A2[:], A2[:], A3[:], ALU.subtract, [A2, A3], [A2])
                        self.tt("dve", a2v, a2v, tot[:].unsqueeze(2).broadcast_to([128, T // 32, 32]), ALU.add, [A2, tot], [A2])
                        barr, free = A2, A3
                    self.act(free[:], barr[:], AF.Exp, [barr], [free], scale=-1.0)
                    self.act(barr[:], barr[:], AF.Exp, [barr], [barr])
                    self.tt("dve", qdec[:], qs[:], barr[:], ALU.mult, [qs, barr], [qdec])
                    self.tt("dve", kinc[:], A1[:], free[:], ALU.mult, [A1, free], [kinc])
                    order = list(range(NT)) if dr == 0 else [1, 0] + list(range(NT - 1, 1, -1))
                    mask = maskf if dr == 0 else maskb
                    self.memset("dve", Sst[:, 0, :], 0.0, [Sstk[0]])
                    for i, tile in enumerate(order):
                        base = 4 * (i % 2)
                        ts_ = slice(tile * 128, (tile + 1) * 128)
                        ps_ = psT.next()
                        self.mm(ps_[:, 0:128], kinc[:, ts_], qdec[:, ts_], True, True, [kinc, qdec], [ps_])
                        sm = sTm.next()
                        self.tt("dve", sm[:], ps_[:, 0:128], mask[:], ALU.mult, [ps_, mask], [sm])
                        pk_i = i % 2
                        pkv = pkT[:, pk_i * 64:(pk_i + 1) * 64].bitcast(BF16)
                        self.tr(pkv, kinc[:, ts_], self.identb[:], [kinc, self.identb], [pkTk[pk_i]])
                        kt = kTs.next()
                        if _F("HG_A"):
                            for j in range(4):
                                self.act(kt[:, j, :], pkv, AF.Copy, [pkTk[pk_i], bmc], [kt], scale=bmc[:, j:j + 1])
                            for j in range(4):
                                self.mm(pKV[:, j * 128:(j + 1) * 128], kt[:, j, :], Vh[:, tile, :], True, True, [kt, Vh], [pKV])
                        else:
                            self.cp("act", kt[:, 0, :], pkv, [pkTk[pk_i]], [kt])
                            vb = Vbd.next()
                            self.tt("dve", vb[:], Vh[:, tile, :].unsqueeze(1).broadcast_to([128, 4, 128]), bm[:], ALU.mult, [Vh, bm], [vb])
                            self.mm(pKV[:, :], kt[:, 0, :], vb[:].rearrange("p j v -> p (j v)"), True, True, [kt, vb], [pKV])
                        kv = KVs.next()
                        self.tt("dve", kv[:], pKV[:, :].rearrange("p (j v) -> p j v", j=4),
                                dec[:, tile * 4:(tile + 1) * 4].unsqueeze(2).broadcast_to([128, 4, 128]), ALU.mult, [pKV, dec], [kv])
                        corder = [0, 1, 2, 3] if dr == 0 else [3, 2, 1, 0]
                        for jj, c in enumerate(corder):
                            s_in = base + jj
                            s_out = (base + jj + 1) % 8
                            self.stt(Sst[:, s_out, :], Sst[:, s_in, :], dec[:, tile * 4 + c: tile * 4 + c + 1], kv[:, c, :],
                                     ALU.mult, ALU.add, [Sstk[s_in], dec, kv], [Sstk[s_out]])
                        self.cp("pool" if not _F("HG_E") else "act", Sb[:, base:base + 4, :], Sst[:, base:base + 4, :],
                                [Sstk[base + q_] for q_ in range(4)], [Sbk[i % 2]])
                        po = poT.next()
                        self.mm(po[:, 0:128], Vh[:, tile, :], sm[:], True, False, [Vh, sm], [po])
                        for jj, c in enumerate(corder):
                            self.mm(po[:, c * 32:(c + 1) * 32], Sb[:, base + jj, :], qdec[:, tile * 128 + c * 32: tile * 128 + (c + 1) * 32],
                                    False, jj == 3, [Sbk[i % 2], qdec], [po])
                        if dr == 0:
                            self.cp("act", oacc[:, ts_], po[:, 0:128], [po], [oak[tile]])
                        else:
                            self.tt("dve", oacc[:, ts_], oacc[:, ts_], po[:, 0:128], ALU.add, [po, oak[tile]], [oak[tile]])
                if _F("HG_D"):
                    self.act(qdec[:], oacc[:], AF.Square, oak, [qdec])
                else:
                    self.tt("dve", qdec[:], oacc[:], oacc[:], ALU.mult, oak, [qdec])
                for blk in blocks:
                    t0, n = blk
                    p = pproj.next()
                    self.mm(p[:, 0:n], self.onesb[:], qdec[:, t0:t0 + n], True, True, [self.onesb, qdec], [p])
                    if _F("HG_C"):
                        self.act(A2[:, t0:t0 + n], p[:, 0:n], AF.Ln, [p, self.epsc], [A2], bias=self.epsc[:, 0:1], scale=1.0 / 128)
                    else:
                        self.act(A2[:, t0:t0 + n], p[:, 0:n], AF.Sqrt, [p, self.epsc], [A2], bias=self.epsc[:, 0:1], scale=1.0 / 128)
                if _F("HG_C"):
                    self.act(A3[:], A2[:], AF.Exp, [A2], [A3], scale=-0.5)
                else:
                    self.recip(A3[:], A2[:], [A2], [A3])
                self.tt("dve", A3[:], A3[:], oacc[:], ALU.mult, [A3] + oak, [A3])
                self.stt(ogT[:, h, :], A3[:], ogc[:, 0:1], sgate[:], ALU.mult, ALU.mult, [A3, ogc, sgate], [ogk[h]])
            wo = self.sb(st, [128, KC, D], BF16, "wo")
            self.dma("pool", wo[:], I["hg_w_out"].rearrange("(c p) n -> p c n", p=128), [], [wo])
            xt2 = NR["xt"]
            tmp = NR["xn"]
            for tile in range(NT):
                gt = gt_c if tile < 2 else gt_l
                x_ = xt2.next()
                self.dma("sp", x_[:], self.src_l0(b, tile), [], [x_])
                t_ = tmp.next()
                for half in range(2):
                    p = pproj.next()
                    hs = slice(half * 512, (half + 1) * 512)
                    for k in range(KC):
                        self.mm(p[:, :], ogT[:, k, tile * 128:(tile + 1) * 128], wo[:, k, hs], k == 0, k == KC - 1, [ogk[k], wo], [p])
                    self.tt("dve", t_[:, hs], p[:, :], gt[:, hs], ALU.mult, [p, gt], [t_])
                self.tt("dve", t_[:], t_[:], x_[:], ALU.add, [t_, x_], [t_])
                self.dma("sp", self.xres[b, tile * 128:(tile + 1) * 128, :], t_[:], [t_], [self.kx[b][tile]], semkey=t_)
                if ("xm0" in self.D_) and b == 0:
                    self.dma("sp", self.D_["xm0"][tile * 128:(tile + 1) * 128, :], t_[:], [t_], [self.kscr], semkey=t_)
            return S.flush()

    ROUTE_TMPS = (("lg", 36), ("gmax", 1), ("ngmax", 1), ("ge", 4), ("gsum", 1), ("pg", 1), ("gone", 4), ("pen", 4),
                  ("em", 32), ("m1", 1), ("oh1", 32), ("em2", 32), ("m2", 1), ("oh2", 32), ("dm", 1), ("e2", 1),
                  ("den", 1), ("rden", 1), ("w1", 1), ("w2", 1), ("tmpw", 32))

    def route_tile(self, sm, p):
        t = {nm: r.next() for nm, r in sm.items()}
        lg = t["lg"]
        self.cp("act", lg[:], p[:, 0:36], [p], [lg])
        self.red(t["gmax"][:], lg[:, 0:4], ALU.max, [lg], [t["gmax"]])
        self.ts("dve", t["ngmax"][:], t["gmax"][:], -1.0, None, ALU.mult, None, [t["gmax"]], [t["ngmax"]])
        self.act(t["ge"][:], lg[:, 0:4], AF.Exp, [lg, t["ngmax"]], [t["ge"], t["gsum"]], bias=t["ngmax"][:, 0:1], accum_out=t["gsum"][:])
        self.recip(t["pg"][:], t["gsum"][:], [t["gsum"]], [t["pg"]])
        self.ts("dve", t["gone"][:], lg[:, 0:4], t["gmax"][:, 0:1], None, ALU.is_ge, None, [lg, t["gmax"]], [t["gone"]])
        self.ts("dve", t["pen"][:], t["gone"][:], BIG, -BIG, ALU.mult, ALU.add, [t["gone"]], [t["pen"]])
        self.tt("dve", t["em"][:].rearrange("p (g j) -> p g j", g=4), lg[:, 4:36].rearrange("p (g j) -> p g j", g=4),
                t["pen"][:].unsqueeze(2).broadcast_to([128, 4, 8]), ALU.add, [lg, t["pen"]], [t["em"]])
        self.red(t["m1"][:], t["em"][:], ALU.max, [t["em"]], [t["m1"]])
        self.ts("dve", t["oh1"][:], t["em"][:], t["m1"][:, 0:1], None, ALU.is_ge, None, [t["em"], t["m1"]], [t["oh1"]])
        self.stt(t["em2"][:], t["oh1"][:], -BIG, t["em"][:], ALU.mult, ALU.add, [t["oh1"], t["em"]], [t["em2"]])
        self.red(t["m2"][:], t["em2"][:], ALU.max, [t["em2"]], [t["m2"]])
        self.ts("dve", t["oh2"][:], t["em2"][:], t["m2"][:, 0:1], None, ALU.is_ge, None, [t["em2"], t["m2"]], [t["oh2"]])
        self.tt("dve", t["dm"][:], t["m2"][:], t["m1"][:], ALU.subtract, [t["m2"], t["m1"]], [t["dm"]])
        self.act(t["e2"][:], t["dm"][:], AF.Exp, [t["dm"]], [t["e2"]])
        self.ts("dve", t["den"][:], t["e2"][:], 1.0, None, ALU.add, None, [t["e2"]], [t["den"]])
        self.recip(t["rden"][:], t["den"][:], [t["den"]], [t["rden"]])
        self.tt("dve", t["w1"][:], t["pg"][:], t["rden"][:], ALU.mult, [t["pg"], t["rden"]], [t["w1"]])
        self.tt("dve", t["w2"][:], t["w1"][:], t["e2"][:], ALU.mult, [t["w1"], t["e2"]], [t["w2"]])
        return t

    def stage_moe(self, l, b, half):
        I, S = self.I, self.S
        if l == 0:
            tiles = list(range(0, 9)) if half == 0 else list(range(9, 18))
        else:
            tiles = list(range(2, 10)) if half == 0 else list(range(10, 18))
        ntl = len(tiles)
        NTOK = ntl * 128
        with ExitStack() as st:
            PB = [self.psb(st) for _ in range(8)]
            hT = self.sb(st, [128, KC, NTOK], BF16, "hT")
            hTk = S.keys(ntl)
            acc = self.sb(st, [128, ntl, D], F32, "acc")
            acck = S.keys(ntl)
            Wt = self.sb(st, [128, ntl, NEXP], F32, "Wt")
            Wtk = S.keys(ntl)
            wr = self.sb(st, [128, KC, 36], F32, "wr")
            self.dma("sp", wr[:, :, 0:4], I["moe_w_group"][l].rearrange("(c p) g -> p c g", p=128), [], [wr])
            self.dma("sp", wr[:, :, 4:36], I["moe_w_expert"][l].rearrange("(c p) g -> p c g", p=128), [], [wr])
            A_l, B_l = self.mod_cols(st, l, 1, b)
            gt_l = self.gate_tile(st, l, 1, b)
            if l == 0 and half == 0:
                A_c, B_c = self.mod_cols(st, l, 1, 4)
                gt_c = self.gate_tile(st, l, 1, 4)
            NR = self.norm_res(st, (PB[0], PB[1]), nhtf=2)
            sm = {}
            for nm, w in (("lg", 36), ("gmax", 1), ("ngmax", 1), ("ge", 4), ("gsum", 1), ("pg", 1), ("gone", 4), ("pen", 4),
                          ("em", 32), ("m1", 1), ("oh1", 32), ("em2", 32), ("m2", 1), ("oh2", 32), ("dm", 1), ("e2", 1),
                          ("den", 1), ("rden", 1), ("w1", 1), ("w2", 1), ("tmpw", 32)):
                sm[nm] = self.sbr(st, 2, [128, w], F32, nm)
            for li, tile in enumerate(tiles):
                isctx = (l == 0 and tile < 2)
                A, Bc = (A_c, B_c) if isctx else (A_l, B_l)
                hTf = self.norm_tile(NR, self.xres[b, tile * 128:(tile + 1) * 128, :], [self.kx[b][tile]], A, Bc,
                                     hT[:, :, li * 128:(li + 1) * 128], [hTk[li]])
                p = PB[2 + li % 2]
                for k in range(KC):
                    self.mm(p[:, 0:36], hTf[:, k, :], wr[:, k, :], k == 0, k == KC - 1, [hTf, wr], [p])
                t = self.route_tile(sm, p)
                self.ts("dve", t["tmpw"][:], t["oh1"][:], t["w1"][:, 0:1], None, ALU.mult, None, [t["oh1"], t["w1"]], [t["tmpw"]])
                self.stt(Wt[:, li, :], t["oh2"][:], t["w2"][:, 0:1], t["tmpw"][:], ALU.mult, ALU.add, [t["oh2"], t["w2"], t["tmpw"]], [Wtk[li]])
            wg = self.sbr(st, 2, [128, KC, FF], BF16, "wg")
            wu = self.sbr(st, 2, [128, KC, FF], BF16, "wu")
            wd = self.sbr(st, 2, [128, 4, D], BF16, "wd")
            sg = self.sbr(st, 2, [128, 512], BF16, "sg")
            actT = self.sbr(st, 2, [128, 4, 512], BF16, "actT")
            pgu = Rot([(PB[0], PB[1]), (PB[2], PB[3])])
            pyr = Rot([(PB[4], PB[5]), (PB[6], PB[7])])
            blocks = []
            t0 = 0
            while t0 < NTOK:
                n = min(512, NTOK - t0)
                blocks.append((t0, n))
                t0 += n
            for e in range(NEXP):
                g_, u_, d_ = wg.next(), wu.next(), wd.next()
                self.dma("pool", g_[:], I["moe_w_gate"][l, e].rearrange("(c p) f -> p c f", p=128), [], [g_])
                self.dma("pool", u_[:], I["moe_w_up"][l, e].rearrange("(c p) f -> p c f", p=128), [], [u_])
                self.dma("pool", d_[:], I["moe_w_down"][l, e].rearrange("(c p) f -> p c f", p=128), [], [d_])
                for (t0, n) in blocks:
                    at = actT.next()
                    hk = [hTk[t] for t in range(t0 // 128, (t0 + n) // 128)]
                    for f in range(4):
                        pg_, pu_ = pgu.next()
                        fs = slice(f * 128, (f + 1) * 128)
                        for k in range(KC):
                            self.mm(pg_[:, 0:n], g_[:, k, fs], hT[:, k, t0:t0 + n], k == 0, k == KC - 1, [g_] + hk, [pg_])
                        for k in range(KC):
                            self.mm(pu_[:, 0:n], u_[:, k, fs], hT[:, k, t0:t0 + n], k == 0, k == KC - 1, [u_] + hk, [pu_])
                        s_ = sg.next()
                        self.act(s_[:, 0:n], pg_[:, 0:n], AF.Silu, [pg_], [s_])
                        self.tt("dve", at[:, f, 0:n], s_[:, 0:n], pu_[:, 0:n], ALU.mult, [s_, pu_], [at])
                    for tt_ in range(n // 128):
                        li = t0 // 128 + tt_
                        pa, pb = pyr.next()
                        for hf, p in ((0, pa), (1, pb)):
                            hs = slice(hf * 512, (hf + 1) * 512)
                            for f in range(4):
                                self.mm(p[:, :], at[:, f, tt_ * 128:(tt_ + 1) * 128], d_[:, f, hs], f == 0, f == 3, [at, d_], [p])
                            if e == 0:
                                self.ts("dve", acc[:, li, hs], p[:, :], Wt[:, li, e:e + 1], None, ALU.mult, None, [p, Wtk[li]], [acck[li]])
                            else:
                                self.stt(acc[:, li, hs], p[:, :], Wt[:, li, e:e + 1], acc[:, li, hs], ALU.mult, ALU.add,
                                         [p, Wtk[li], acck[li]], [acck[li]])
            for li, tile in enumerate(tiles):
                isctx = (l == 0 and tile < 2)
                gt = gt_c if isctx else gt_l
                x_ = NR["xt"].next()
                self.dma("sp", x_[:], self.xres[b, tile * 128:(tile + 1) * 128, :], [self.kx[b][tile]], [x_])
                t_ = NR["xn"].next()
                self.tt("dve", t_[:], acc[:, li, :], gt[:], ALU.mult, [acck[li], gt], [t_])
                self.tt("dve", t_[:], t_[:], x_[:], ALU.add, [t_, x_], [t_])
                if l == 0:
                    self.dma("sp", self.xres[b, tile * 128:(tile + 1) * 128, :], t_[:], [t_], [self.kx[b][tile]], semkey=t_)
                    if ("xf0" in self.D_) and b == 0:
                        self.dma("sp", self.D_["xf0"][tile * 128:(tile + 1) * 128, :], t_[:], [t_], [self.kscr], semkey=t_)
                else:
                    self.dma("sp", self.out[b, (tile - 2) * 128:(tile - 1) * 128, :], t_[:], [t_], [self.kscr], semkey=t_)
            return S.flush()

    def rope(self, xin, xout, cos, sin, H, tm, R, W):
        x1, x2 = xin[:, :, :, 0, :], xin[:, :, :, 1, :]
        cb = cos.unsqueeze(1).broadcast_to([128, H, 2, 8])
        sb_ = sin.unsqueeze(1).broadcast_to([128, H, 2, 8])
        t1, t2 = tm
        v1 = t1[:, 0:H * 16].rearrange("p (h a f) -> p h a f", h=H, a=2)
        v2 = t2[:, 0:H * 16].rearrange("p (h a f) -> p h a f", h=H, a=2)
        self.tt("dve", v1, x1, cb, ALU.mult, R, [t1])
        self.tt("dve", v2, x2, sb_, ALU.mult, R, [t2])
        self.tt("dve", xout[:, :, :, 0, :], v1, v2, ALU.subtract, [t1, t2], W)
        self.tt("dve", v1, x2, cb, ALU.mult, R, [t1])
        self.tt("dve", v2, x1, sb_, ALU.mult, R, [t2])
        self.tt("dve", xout[:, :, :, 1, :], v1, v2, ALU.add, [t1, t2], W)

    def stage_mla(self, b):
        I, S = self.I, self.S
        l = 1
        NQT = SEQ // 128
        with ExitStack() as st:
            PB = [self.psb(st) for _ in range(8)]
            if "moe1" in self.stages:
                self.precast(1, b)
            A_l, B_l = self.mod_cols(st, l, 0, b)
            A_c, B_c = self.mod_cols(st, l, 0, 4)
            gt_l = self.gate_tile(st, l, 0, b)
            NR = self.norm_res(st, (PB[0], PB[1]))
            win = self.sb(st, [128, KC, 416], BF16, "win")
            self.dma("pool", win[:], I["mla_w_in"].rearrange("(c p) n -> p c n", p=128), [], [win])
            cT = self.sb(st, [128, 3, T], BF16, "cT")
            cTk = S.keys(NT)
            krr = self.sb(st, [128, NT, 32], F32, "krr")
            krk = S.keys(NT)
            sskr = self.sb(st, [128, NT], F32, "sskr")
            ssk = S.keys(NT)
            gk = self.sb(st, [128, 96], F32, "gk")
            gq = self.sb(st, [128, 96], F32, "gq")
            self.dma("sp", gk[:], I["mla_k_qknorm_g"].partition_broadcast(128), [], [gk])
            self.dma("sp", gq[:], I["mla_q_qknorm_g"].partition_broadcast(128), [], [gq])
            self.ts("dve", gq[:], gq[:], float(96 ** -0.5), None, ALU.mult, None, [gq], [gq])
            qng = self.sb(st, [128, 2], F32, "qng")
            kvg = self.sb(st, [128, 1], F32, "kvg")
            self.dma("sp", qng[:], I["mla_q_norm_g"].rearrange("(k p) -> p k", p=128), [], [qng])
            self.dma("sp", kvg[:], I["mla_kv_norm_g"].rearrange("(p o) -> p o", o=1), [], [kvg])
            hTt = self.sbr(st, 2, [128, KC, 128], BF16, "hTt")
            csr = self.sbr(st, 2, [128, 416], F32, "cs")
            cnr = self.sbr(st, 2, [128, 384], BF16, "cn")
            junk2 = self.sb(st, [128, 256], BF16, "junk2")
            s1 = {nm: self.sbr(st, 2, [128, 1], F32, nm) for nm in ("ssq", "sskv", "sdq", "sdkv", "rsq", "rskv")}
            kr1 = self.sbr(st, 2, [128, 32], F32, "kr1")
            cosr = self.sbr(st, 2, [128, 16], F32, "cos")
            sinr = self.sbr(st, 2, [128, 16], F32, "sin")
            rt = (self.sb(st, [128, 64], F32, "rt1"), self.sb(st, [128, 64], F32, "rt2"))
            cost = {}
            for tile in range(NT):
                A, Bc = (A_c, B_c) if tile < 2 else (A_l, B_l)
                hb = hTt.next()
                self.norm_tile(NR, self.xres[b, tile * 128:(tile + 1) * 128, :], [self.kx[b][tile]], A, Bc, hb[:], [hb])
                p = PB[2 + tile % 2]
                for k in range(KC):
                    self.mm(p[:, 0:416], hb[:, k, :], win[:, k, :], k == 0, k == KC - 1, [hb, win], [p])
                cs = csr.next()
                self.cp("dve", cs[:], p[:, 0:416], [p], [cs])
                t = {nm: r.next() for nm, r in s1.items()}
                self.act(junk2[:, 0:256], cs[:, 0:256], AF.Square, [cs], [junk2, t["ssq"]], accum_out=t["ssq"][:])
                self.act(junk2[:, 0:128], cs[:, 256:384], AF.Square, [cs], [junk2, t["sskv"]], accum_out=t["sskv"][:])
                self.act(junk2[:, 0:32], cs[:, 384:416], AF.Square, [cs], [junk2, ssk[tile]], accum_out=sskr[:, tile:tile + 1])
                self.act(t["sdq"][:], t["ssq"][:], AF.Sqrt, [t["ssq"], self.epsc], [t["sdq"]], bias=self.epsc[:, 0:1], scale=1.0 / 256)
                self.act(t["sdkv"][:], t["sskv"][:], AF.Sqrt, [t["sskv"], self.epsc], [t["sdkv"]], bias=self.epsc[:, 0:1], scale=1.0 / 128)
                self.recip(t["rsq"][:], t["sdq"][:], [t["sdq"]], [t["rsq"]])
                self.recip(t["rskv"][:], t["sdkv"][:], [t["sdkv"]], [t["rskv"]])
                cn = cnr.next()
                self.act(cn[:, 0:256], cs[:, 0:256], AF.Copy, [cs, t["rsq"]], [cn], scale=t["rsq"][:, 0:1])
                self.act(cn[:, 256:384], cs[:, 256:384], AF.Copy, [cs, t["rskv"]], [cn], scale=t["rskv"][:, 0:1])
                pT = PB[4 + tile % 2]
                pv = pT[:, 0:192].bitcast(BF16).rearrange("p (j t) -> p j t", j=3)
                for j in range(3):
                    self.tr(pv[:, j, :], cn[:, j * 128:(j + 1) * 128], self.identb[:], [cn, self.identb], [pT])
                self.cp("dve", cT[:, :, tile * 128:(tile + 1) * 128], pv, [pT], [cTk[tile]])
                k1 = kr1.next()
                self.tt("dve", k1[:], cs[:, 384:416], gk[:, 64:96], ALU.mult, [cs, gk], [k1])
                if tile < 2:
                    self.cp("dve", krr[:, tile, :], k1[:], [k1], [krk[tile]])
                else:
                    co, si = cosr.next(), sinr.next()
                    self.dma("sp", co[:], I["k_cos"][(tile - 2) * 128:(tile - 1) * 128, :], [], [co])
                    self.dma("sp", si[:], I["k_sin"][(tile - 2) * 128:(tile - 1) * 128, :], [], [si])
                    self.rope(k1[:].rearrange("p (h a g f) -> p h a g f", h=1, a=2, g=2),
                              krr[:, tile, :].rearrange("p (h a g f) -> p h a g f", h=1, a=2, g=2),
                              co[:].rearrange("p (a f) -> p a f", a=2), si[:].rearrange("p (a f) -> p a f", a=2),
                              1, rt, [k1, co, si], [krk[tile]])
            HG = 2
            NG = 16 // HG
            oat = self.sb(st, [128, NQT, D], BF16, "oat")
            oak = S.keys(NQT)
            QTs = [self.sb(st, [128, HG, SEQ], BF16, "QT") for _ in range(2)]
            QTks = [S.keys(NQT) for _ in range(2)]
            KTs = [self.sb(st, [128, HG, T], BF16, "KT") for _ in range(2)]
            KTks = [S.keys(NT) for _ in range(2)]
            Vxs = [self.sb(st, [128, NT, HG, 65], BF16, "Vx") for _ in range(2)]
            Vxks = [S.keys(NT) for _ in range(2)]
            for s_ in range(2):
                self.memset("dve", Vxs[s_][:], 1.0, Vxks[s_])
            wqfr = self.sbr(st, 2, [128, 2, HG * 96], F32, "wqf")
            wqbr = self.sbr(st, 2, [128, 2, HG * 96], BF16, "wqb")
            wkfr = self.sbr(st, 2, [128, HG * 128], F32, "wkf")
            wkbr = self.sbr(st, 2, [128, HG * 128], BF16, "wkb")
            kvfr = self.sbr(st, 2, [128, HG, 128], F32, "kvf")
            sqk = self.sb(st, [128, HG, 96], F32, "sqk")
            tmpk = self.sb(st, [128, HG, 96], F32, "tmpk")
            s4 = {nm: self.sbr(st, 2, [128, HG], F32, nm) for nm in ("ssn", "ss", "sd", "rs", "ssq4", "sd4", "rs4")}
            kbr = self.sbr(st, 2, [128, HG, 96], BF16, "kb")
            qfr = self.sbr(st, 2, [128, HG, 96], F32, "qf")
            qnr = self.sbr(st, 2, [128, HG, 96], F32, "qn")
            qbr = self.sbr(st, 2, [128, HG, 96], BF16, "qb")
            ptr_ = self.sbr(st, 3, [128, 512], BF16, "pt")
            recr = self.sbr(st, 4, [128, 1], F32, "rec")
            pA, pB_ = PB[2], PB[3]

            def rstd(out, ss, tmp, n):
                self.act(tmp[:], ss[:], AF.Ln, [ss, self.epsc], [tmp], bias=self.epsc[:, 0:1], scale=1.0 / n)
                self.act(out[:], tmp[:], AF.Exp, [tmp], [out], scale=-0.5)

            for hg in range(NG):
                s_ = hg % 2
                QT, QTk, KT, KTk, Vx, Vxk = QTs[s_], QTks[s_], KTs[s_], KTks[s_], Vxs[s_], Vxks[s_]
                wqf, wqb, wkf, wkb = wqfr.next(), wqbr.next(), wkfr.next(), wkbr.next()
                self.dma("sp", wqf[:], I["mla_w_qb"][:, hg * HG * 96:(hg + 1) * HG * 96].rearrange("(k p) n -> p k n", p=128), [], [wqf])
                self.tt("dve", wqb[:], wqf[:], qng[:].unsqueeze(2).broadcast_to([128, 2, HG * 96]), ALU.mult, [wqf, qng], [wqb])
                self.dma("sp", wkf[:], I["mla_w_kvb"][:, hg * HG * 128:(hg + 1) * HG * 128], [], [wkf])
                self.ts("dve", wkb[:], wkf[:], kvg[:, 0:1], None, ALU.mult, None, [wkf, kvg], [wkb])
                for tile in range(NT):
                    ts_ = slice(tile * 128, (tile + 1) * 128)
                    self.mm(pA[:, 0:HG * 128], cT[:, 2, ts_], wkb[:], True, True, [cTk[tile], wkb], [pA])
                    kvf = kvfr.next()
                    self.cp("dve", kvf[:], pA[:, 0:HG * 128].rearrange("p (h e) -> p h e", h=HG), [pA], [kvf])
                    t = {nm: r.next() for nm, r in s4.items()}
                    self.tt("dve", sqk[:, :, 0:64], kvf[:, :, 0:64], kvf[:, :, 0:64], ALU.mult, [kvf], [sqk])
                    self.red(t["ssn"][:], sqk[:, :, 0:64], ALU.add, [sqk], [t["ssn"]])
                    self.ts("dve", t["ss"][:], t["ssn"][:], sskr[:, tile:tile + 1], None, ALU.add, None, [t["ssn"], ssk[tile]], [t["ss"]])
                    rstd(t["rs"], t["ss"], t["sd"], 96)
                    kb = kbr.next()
                    self.tt("dve", tmpk[:, :, 0:64], kvf[:, :, 0:64], t["rs"][:].unsqueeze(2).broadcast_to([128, HG, 64]), ALU.mult,
                            [kvf, t["rs"]], [tmpk])
                    self.tt("dve", kb[:, :, 0:64], tmpk[:, :, 0:64], gk[:, 0:64].unsqueeze(1).broadcast_to([128, HG, 64]), ALU.mult,
                            [tmpk, gk], [kb])
                    self.tt("dve", kb[:, :, 64:96], krr[:, tile, :].unsqueeze(1).broadcast_to([128, HG, 32]),
                            t["rs"][:].unsqueeze(2).broadcast_to([128, HG, 32]), ALU.mult, [krk[tile], t["rs"]], [kb])
                    pkv = pB_[:, 0:HG * 64].bitcast(BF16).rearrange("p (h t) -> p h t", h=HG)
                    for h in range(HG):
                        self.tr(pkv[0:96, h, :], kb[:, h, :], self.identb[:], [kb, self.identb], [pB_])
                    self.cp("dve", KT[0:96, :, ts_], pkv[0:96, :, :], [pB_], [KTk[tile]])
                    self.cp("dve", Vx[:, tile, :, 0:64], kvf[:, :, 64:128], [kvf], [Vxk[tile]])
                    if tile >= 2:
                        qt_ = tile - 2
                        for k in range(2):
                            self.mm(pA[:, 0:HG * 96], cT[:, k, ts_], wqb[:, k, :], k == 0, k == 1, [cTk[tile], wqb], [pA])
                        qf = qfr.next()
                        self.cp("dve", qf[:], pA[:, 0:HG * 96].rearrange("p (h e) -> p h e", h=HG), [pA], [qf])
                        self.tt("dve", sqk[:], qf[:], qf[:], ALU.mult, [qf], [sqk])
                        self.red(t["ssq4"][:], sqk[:], ALU.add, [sqk], [t["ssq4"]])
                        rstd(t["rs4"], t["ssq4"], t["sd4"], 96)
                        qn = qnr.next()
                        self.tt("dve", qn[:], qf[:], t["rs4"][:].unsqueeze(2).broadcast_to([128, HG, 96]), ALU.mult, [qf, t["rs4"]], [qn])
                        self.tt("dve", qn[:], qn[:], gq[:].unsqueeze(1).broadcast_to([128, HG, 96]), ALU.mult, [qn, gq], [qn])
                        qb = qbr.next()
                        self.cp("dve", qb[:, :, 0:64], qn[:, :, 0:64], [qn], [qb])
                        co, si = cosr.next(), sinr.next()
                        self.dma("sp", co[:], I["k_cos"][qt_ * 128:(qt_ + 1) * 128, :], [], [co])
                        self.dma("sp", si[:], I["k_sin"][qt_ * 128:(qt_ + 1) * 128, :], [], [si])
                        self.rope(qn[:, :, 64:96].rearrange("p h (a g f) -> p h a g f", a=2, g=2),
                                  qb[:, :, 64:96].rearrange("p h (a g f) -> p h a g f", a=2, g=2),
                                  co[:].rearrange("p (a f) -> p a f", a=2), si[:].rearrange("p (a f) -> p a f", a=2),
                                  HG, rt, [qn, co, si], [qb])
                        pqv = pB_[:, 0:HG * 64].bitcast(BF16).rearrange("p (h t) -> p h t", h=HG)
                        for h in range(HG):
                            self.tr(pqv[0:96, h, :], qb[:, h, :], self.identb[:], [qb, self.identb], [pB_])
                        self.cp("dve", QT[0:96, :, qt_ * 128:(qt_ + 1) * 128], pqv[0:96, :, :], [pB_], [QTk[qt_]])
                for h in range(HG):
                    hh = hg * HG + h
                    for qb_ in range(SEQ // 512):
                        po = PB[4:8]
                        qk = [QTk[qb_ * 4 + i] for i in range(4)]
                        for kt in range(NT):
                            ps_ = PB[kt % 2]
                            self.mm(ps_[:, :], KT[0:96, h, kt * 128:(kt + 1) * 128], QT[0:96, h, qb_ * 512:(qb_ + 1) * 512], True, True,
                                    [KTk[kt]] + qk, [ps_])
                            pt = ptr_.next()
                            self.act(pt[:], ps_[:, :], AF.Exp, [ps_], [pt])
                            for q4 in range(4):
                                self.mm(po[q4][:, 0:65], pt[:, q4 * 128:(q4 + 1) * 128], Vx[:, kt, h, :], kt == 0, kt == NT - 1,
                                        [pt, Vxk[kt]], [po[q4]])
                        for q4 in range(4):
                            rec = recr.next()
                            self.recip(rec[:], po[q4][:, 64:65], [po[q4]], [rec])
                            self.ts("dve", oat[:, qb_ * 4 + q4, hh * 64:(hh + 1) * 64], po[q4][:, 0:64], rec[:, 0:1], None, ALU.mult, None,
                                    [po[q4], rec], [oak[qb_ * 4 + q4]])
            wo = self.sb(st, [128, KC, D], BF16, "wo")
            self.dma("pool", wo[:], I["mla_w_out"].rearrange("(c p) n -> p c n", p=128), [], [wo])
            oTr = self.sbr(st, 2, [128, KC, 128], BF16, "oT")
            for qt_ in range(NQT):
                tile = qt_ + 2
                pT = PB[qt_ % 2]
                pv = pT[:, :].bitcast(BF16).rearrange("p (k t) -> p k t", k=KC)
                for k in range(KC):
                    self.tr(pv[:, k, :], oat[:, qt_, k * 128:(k + 1) * 128], self.identb[:], [oak[qt_], self.identb], [pT])
                oT = oTr.next()
                self.cp("act", oT[:], pv, [pT], [oT])
                x_ = NR["xt"].next()
                self.dma("sp", x_[:], self.xres[b, tile * 128:(tile + 1) * 128, :], [self.kx[b][tile]], [x_])
                t_ = NR["xn"].next()
                for hf in range(2):
                    p = PB[2 + hf]
                    hs = slice(hf * 512, (hf + 1) * 512)
                    for k in range(KC):
                        self.mm(p[:, :], oT[:, k, :], wo[:, k, hs], k == 0, k == KC - 1, [oT, wo], [p])
                    self.tt("dve", t_[:, hs], p[:, :], gt_l[:, hs], ALU.mult, [p, gt_l], [t_])
                self.tt("dve", t_[:], t_[:], x_[:], ALU.add, [t_, x_], [t_])
                self.dma("sp", self.xres[b, tile * 128:(tile + 1) * 128, :], t_[:], [t_], [self.kx[b][tile]], semkey=t_)
                if ("xm1" in self.D_) and b == 0:
                    self.dma("sp", self.D_["xm1"][qt_ * 128:(qt_ + 1) * 128, :], t_[:], [t_], [self.kscr], semkey=t_)
            return S.flush()

    def moe_sparse(self, l):
        I, S, NB = self.I, self.S, self.NB
        tiles = [(b, t) for b in range(NB) for t in (range(NT) if l == 0 else range(2, NT))]
        NTL = len(tiles)
        NBLK = 2 * NTL + NEXP
        info = {}
        with ExitStack() as pst:
            d_i = [self.sb(pst, [128, NTL], I32, "d%di" % k) for k in range(2)]
            w_a = [self.sb(pst, [128, NTL], F32, "w%da" % k) for k in range(2)]
            idxw = self.sb(pst, [128, NBLK], I32, "idxw")
            kh2 = S.keys(NTL)
            kxs, kys = S.key(), S.key()
            kwb = self.kwb[l]
            with ExitStack() as st:
                PB = [self.psb(st) for _ in range(8)]
                Ltri = self.sb(st, [128, 128], F32, "Ltri")
                onesf = self.sb(st, [128, 128], F32, "onesf")
                self.memset("dve", onesf[:], 1.0, [onesf])
                self.memset("pool", Ltri[:], 1.0, [Ltri])
                S.op("pool", lambda e_: e_.affine_select(out=Ltri[:], in_=Ltri[:], pattern=[[1, 128]], compare_op=ALU.is_gt,
                                                         fill=0.0, base=0, channel_multiplier=-1), [Ltri], [Ltri])
                jvi = self.sb(st, [128, NBLK], I32, "jvi")
                jv = self.sb(st, [128, NBLK], F32, "jv")
                S.op("pool", lambda e_: e_.iota(jvi[:], pattern=[[128, NBLK]], base=0, channel_multiplier=0), [], [jvi])
                self.cp("dve", jv[:], jvi[:], [jvi], [jv])
                pii = self.sb(st, [128, 1], I32, "pii")
                pif = self.sb(st, [128, 1], F32, "pif")
                S.op("pool", lambda e_: e_.iota(pii[:], pattern=[[0, 1]], base=0, channel_multiplier=1), [], [pii])
                self.cp("dve", pif[:], pii[:], [pii], [pif])
                ones32 = self.sb(st, [128, NEXP], F32, "ones32")
                self.memset("dve", ones32[:], 1.0, [ones32])
                wr = self.sb(st, [128, KC, 36], F32, "wr")
                self.dma("sp", wr[:, :, 0:4], I["moe_w_group"][l].rearrange("(c p) g -> p c g", p=128), [], [wr])
                self.dma("sp", wr[:, :, 4:36], I["moe_w_expert"][l].rearrange("(c p) g -> p c g", p=128), [], [wr])
                grow = self.sb(st, [128, D], F32, "grow")
                self.dma("sp", grow[:], I["norm_ffn_g"][l, :].partition_broadcast(128), [], [grow])

                def rows_for(r):
                    Ar = self.sb(st, [128, D], F32, "Arow")
                    Br = self.sb(st, [128, D], F32, "Brow")
                    return Ar, Br

                def load_rows(Ar, Br, r):
                    self.dma("sp", Ar[:], self.mod[l, r, 4 * D:5 * D].partition_broadcast(128), [self.kmod], [Ar])
                    self.dma("sp", Br[:], self.mod[l, r, 3 * D:4 * D].partition_broadcast(128), [self.kmod], [Br])
                    self.stt(Ar[:], Ar[:], 1.0, grow[:], ALU.add, ALU.mult, [Ar, grow], [Ar])

                Ar_l, Br_l = rows_for(0)
                if l == 0:
                    Ar_c, Br_c = rows_for(4)
                    load_rows(Ar_c, Br_c, 4)
                    A_c, B_c = self.mod_cols(st, l, 1, 4)
                NR = self.norm_res(st, (PB[0], PB[1]), nhtf=2)
                sm = {nm: self.sbr(st, 2, [128, w], F32, nm) for nm, w in self.ROUTE_TMPS}
                OHs = self.sb(st, [128, NEXP], F32, "OHs")
                self.memset("dve", OHs[:], 0.0, [OHs])
                OHt = self.sbr(st, 2, [128, NEXP], F32, "OHt")
                Rall = self.sb(st, [128, NTL, NEXP], F32, "Rall")
                Rk = S.keys(NTL)
                oha = [self.sb(st, [128, NTL, NEXP], F32, "oh%da" % k) for k in range(2)]
                ohk = [S.keys(NTL) for _ in range(2)]
                t32 = self.sb(st, [128, D], F32, "t32")
                h2r = self.sbr(st, 2, [128, D], BF16, "h2b")
                cur_b = None
                cols = {}
                for ti, (b, tile) in enumerate(tiles):
                    if b != cur_b:
                        cur_b = b
                        load_rows(Ar_l, Br_l, b)
                        cols[b] = self.mod_cols(st, l, 1, b)
                    isctx = (l == 0 and tile < 2)
                    A, Bc = (A_c, B_c) if isctx else cols[b]
                    Ar, Br = (Ar_c, Br_c) if isctx else (Ar_l, Br_l)
                    hTf = self.norm_tile(NR, self.xres[b, tile * 128:(tile + 1) * 128, :], [self.kx[b][tile]], A, Bc, None, [])
                    xn = self.last_xn
                    self.tt("dve", t32[:], xn[:], Ar[:], ALU.mult, [xn, Ar], [t32])
                    h2b = h2r.next()
                    self.tt("dve", h2b[:], t32[:], Br[:], ALU.add, [t32, Br], [h2b])
                    self.dma("sp", self.h2d[ti * 128:(ti + 1) * 128, :], h2b[:], [h2b], [kh2[ti]], semkey=h2b)
                    p = PB[2 + ti % 2]
                    for k in range(KC):
                        self.mm(p[:, 0:36], hTf[:, k, :], wr[:, k, :], k == 0, k == KC - 1, [hTf, wr], [p])
                    t = self.route_tile(sm, p)
                    self.cp("dve", oha[0][:, ti, :], t["oh1"][:], [t["oh1"]], [ohk[0][ti]])
                    self.cp("dve", oha[1][:, ti, :], t["oh2"][:], [t["oh2"]], [ohk[1][ti]])
                    self.cp("dve", w_a[0][:, ti:ti + 1], t["w1"][:], [t["w1"]], [w_a[0]])
                    self.cp("dve", w_a[1][:, ti:ti + 1], t["w2"][:], [t["w2"]], [w_a[1]])
                    oh = OHt.next()
                    self.tt("dve", oh[:], t["oh1"][:], t["oh2"][:], ALU.add, [t["oh1"], t["oh2"]], [oh])
                    pr = PB[4 + ti % 2]
                    self.mm(pr[:, 0:NEXP], Ltri[:], oh[:], True, False, [Ltri, oh], [pr])
                    self.mm(pr[:, 0:NEXP], onesf[:], OHs[:], False, True, [onesf, OHs], [pr])
                    self.cp("act", Rall[:, ti, :], pr[:, 0:NEXP], [pr], [Rk[ti]])
                    self.tt("dve", OHs[:], OHs[:], oh[:], ALU.add, [OHs, oh], [OHs])
                pc = PB[6]
                self.mm(pc[:, 0:NEXP], onesf[:], OHs[:], True, True, [onesf, OHs], [pc])
                cntf = self.sb(st, [128, NEXP], F32, "cntf")
                padf = self.sb(st, [128, NEXP], F32, "padf")
                pend = self.sb(st, [128, NEXP], F32, "pend")
                pstart = self.sb(st, [128, NEXP], F32, "pstart")
                cmpb = self.sb(st, [128, NBLK * NEXP], BF16, "cmpb")
                self.cp("dve", cntf[:], pc[:, 0:NEXP], [pc], [cntf])
                cv = cmpb[:].rearrange("p (e j) -> p e j", e=NEXP)
                self.tt("dve", cv, jv[:].unsqueeze(1).broadcast_to([128, NEXP, NBLK]),
                        cntf[:].unsqueeze(2).broadcast_to([128, NEXP, NBLK]), ALU.is_lt, [jv, cntf], [cmpb])
                self.red(padf[:], cv, ALU.add, [cmpb], [padf])
                self.ts("dve", padf[:], padf[:], 128.0, None, ALU.mult, None, [padf], [padf])
                S.op("dve", lambda e_: e_.tensor_tensor_scan(out=pend[:], data0=ones32[:], data1=padf[:], initial=0.0,
                                                             op0=ALU.mult, op1=ALU.add), [ones32, padf], [pend])
                self.tt("dve", pstart[:], pend[:], padf[:], ALU.subtract, [pend, padf], [pstart])
                bef = self.sb(st, [128, NBLK], F32, "bef")
                cv2 = cmpb[:].rearrange("p (j e) -> p j e", e=NEXP)
                self.tt("dve", cv2, pend[:].unsqueeze(1).broadcast_to([128, NBLK, NEXP]),
                        jv[:].unsqueeze(2).broadcast_to([128, NBLK, NEXP]), ALU.is_le, [pend, jv], [cmpb])
                self.red(bef[:], cv2, ALU.add, [cmpb], [bef])
                self.ts("dve", bef[:], bef[:], float(NEXP - 1), None, ALU.min, None, [bef], [bef])
                same2 = self.sb(st, [128, NBLK], F32, "same2")
                self.memset("dve", same2[:], 0.0, [same2])
                self.tt("dve", same2[:, 2:NBLK], bef[:, 2:NBLK], bef[:, 0:NBLK - 2], ALU.is_equal, [bef], [same2])
                self.ts("dve", bef[:], bef[:], 128.0, pif[:, 0:1], ALU.mult, ALU.add, [bef, pif], [bef])
                self.stt(bef[:], same2[:], 1.0e6, bef[:], ALU.mult, ALU.add, [same2, bef], [bef])
                self.cp("dve", idxw[:], bef[:], [bef], [idxw])
                dtmp = self.sbr(st, 2, [128, NEXP], F32, "dtmp")
                dtm2 = self.sbr(st, 2, [128, NEXP], F32, "dtm2")
                dfl = self.sbr(st, 2, [128, 1], F32, "dfl")
                for ti in range(NTL):
                    h2b = h2r.next()
                    self.dma("sp", h2b[:], self.h2d[ti * 128:(ti + 1) * 128, :], [kh2[ti]], [h2b])
                    d1 = dtmp.next()
                    self.tt("dve", d1[:], Rall[:, ti, :], pstart[:], ALU.add, [Rk[ti], pstart], [d1])
                    for k in range(2):
                        d2, df = dtm2.next(), dfl.next()
                        self.tt("dve", d2[:], d1[:], oha[k][:, ti, :], ALU.mult, [d1, ohk[k][ti]], [d2])
                        self.red(df[:], d2[:], ALU.add, [d2], [df])
                        self.cp("dve", d_i[k][:, ti:ti + 1], df[:], [df], [d_i[k]])
                        idx_ap = d_i[k][:, ti:ti + 1]
                        self._scatter(self.xs[:, :], idx_ap, h2b[:], [h2b, d_i[k]], [kxs])
                info["rs"] = S.flush()
            with ExitStack() as st:
                PB = [self.psb(st) for _ in range(8)]
                xbr = self.sbr(st, 2, [128, D], BF16, "xb")
                xTr = self.sbr(st, 2, [128, KC, 128], BF16, "xT")
                wgr = self.sbr(st, 2, [128, 4096], BF16, "wgs")
                wur = self.sbr(st, 2, [128, 4096], BF16, "wus")
                wdr = self.sbr(st, 2, [128, 4096], BF16, "wds")
                sgr = self.sbr(st, 2, [128, 512], BF16, "sgs")
                acr = self.sbr(st, 2, [128, 4, 128], BF16, "acs")
                ysr = self.sbr(st, 2, [128, D], F32, "ysb")
                pgu = Rot([(PB[2], PB[3]), (PB[4], PB[5])])
                breg = {"v": NEXP * 128 - 1}
                for j in range(NBLK):
                    xb = xbr.next()
                    self.dma("sp", xb[:], self.xs[j * 128:(j + 1) * 128, :], [kxs], [xb])
                    wg, wu, wd = wgr.next(), wur.next(), wdr.next()
                    ia = idxw[:, j:j + 1]
                    self._gather(wg[:], self.wgb[l][:, :], ia, [idxw, kwb], [wg], bounds=breg)
                    self._gather(wu[:], self.wub[l][:, :], ia, [idxw, kwb], [wu], bounds=breg)
                    self._gather(wd[:], self.wdb[l][:, :], ia, [idxw, kwb], [wd], bounds=breg)
                    pT = PB[j % 2]
                    pv = pT[:, :].bitcast(BF16).rearrange("p (k t) -> p k t", k=KC)
                    for k in range(KC):
                        self.tr(pv[:, k, :], xb[:, k * 128:(k + 1) * 128], self.identb[:], [xb, self.identb], [pT])
                    xT = xTr.next()
                    self.cp("act", xT[:], pv, [pT], [xT])
                    pg_, pu_ = pgu.next()
                    wgv = wg[:].rearrange("p (k f) -> p k f", k=KC)
                    wuv = wu[:].rearrange("p (k f) -> p k f", k=KC)
                    wdv = wd[:].rearrange("p (k f) -> p k f", k=4)
                    for fc in range(4):
                        fs = slice(fc * 128, (fc + 1) * 128)
                        for k in range(KC):
                            self.mm(pg_[:, fs], wgv[:, k, fs], xT[:, k, :], k == 0, k == KC - 1, [wg, xT], [pg_])
                    for fc in range(4):
                        fs = slice(fc * 128, (fc + 1) * 128)
                        for k in range(KC):
                            self.mm(pu_[:, fs], wuv[:, k, fs], xT[:, k, :], k == 0, k == KC - 1, [wu, xT], [pu_])
                    sg = sgr.next()
                    self.act(sg[:], pg_[:, :], AF.Silu, [pg_], [sg])
                    ac = acr.next()
                    self.tt("dve", ac[:].rearrange("p k t -> p (k t)"), sg[:], pu_[:, :], ALU.mult, [sg, pu_], [ac])
                    ysb = ysr.next()
                    for hf, p in ((0, PB[6]), (1, PB[7])):
                        hs = slice(hf * 512, (hf + 1) * 512)
                        for k in range(4):
                            self.mm(p[:, :], ac[:, k, :], wdv[:, k, hs], k == 0, k == 3, [ac, wd], [p])
                        if hf == 0:
                            self.cp("act", ysb[:, hs], p[:, :], [p], [ysb])
                        else:
                            self.cp("dve", ysb[:, hs], p[:, :], [p], [ysb])
                    self.dma("sp", self.ys[j * 128:(j + 1) * 128, :], ysb[:], [ysb], [kys], semkey=ysb)
                info["e"] = S.flush()
            with ExitStack() as st:
                gts = {}
                y1r = self.sbr(st, 2, [128, D], F32, "y1")
                y2r = self.sbr(st, 2, [128, D], F32, "y2")
                xr = self.sbr(st, 2, [128, D], F32, "xc")
                tr_ = self.sbr(st, 2, [128, D], F32, "tc")
                if l == 0:
                    gts[4] = self.gate_tile(st, l, 1, 4)
                for ti, (b, tile) in enumerate(tiles):
                    if b not in gts:
                        gts[b] = self.gate_tile(st, l, 1, b)
                    isctx = (l == 0 and tile < 2)
                    gt = gts[4] if isctx else gts[b]
                    y1, y2, x_, t_ = y1r.next(), y2r.next(), xr.next(), tr_.next()
                    self._gather(y1[:], self.ys[:, :], d_i[0][:, ti:ti + 1], [d_i[0], kys], [y1])
                    self._gather(y2[:], self.ys[:, :], d_i[1][:, ti:ti + 1], [d_i[1], kys], [y2])
                    self.dma("sp", x_[:], self.xres[b, tile * 128:(tile + 1) * 128, :], [self.kx[b][tile]], [x_])
                    self.ts("dve", t_[:], y1[:], w_a[0][:, ti:ti + 1], None, ALU.mult, None, [y1, w_a[0]], [t_])
                    self.stt(t_[:], y2[:], w_a[1][:, ti:ti + 1], t_[:], ALU.mult, ALU.add, [y2, w_a[1], t_], [t_])
                    self.tt("dve", t_[:], t_[:], gt[:], ALU.mult, [t_, gt], [t_])
                    self.tt("dve", t_[:], t_[:], x_[:], ALU.add, [t_, x_], [t_])
                    if l == 0:
                        self.dma("sp", self.xres[b, tile * 128:(tile + 1) * 128, :], t_[:], [t_], [self.kx[b][tile]], semkey=t_)
                        if ("xf0" in self.D_) and b == 0:
                            self.dma("sp", self.D_["xf0"][tile * 128:(tile + 1) * 128, :], t_[:], [t_], [self.kscr], semkey=t_)
                    else:
                        self.dma("sp", self.out[b, (tile - 2) * 128:(tile - 1) * 128, :], t_[:], [t_], [self.kscr], semkey=t_)
                info["c"] = S.flush()
        return info

    def precast(self, l, b):
        I = self.I
        per = (NEXP + self.NB - 1) // self.NB
        if not hasattr(self, "pck"):
            self.pck = Rot(self.S.keys(8))
        for e in range(b * per, min(NEXP, (b + 1) * per)):
            rows = slice(e * 128, (e + 1) * 128)
            for dst, src, kk in ((self.wgb[l], "moe_w_gate", 8), (self.wub[l], "moe_w_up", 8), (self.wdb[l], "moe_w_down", 4)):
                self.dma("pool", dst[rows, :].rearrange("p (k f) -> p k f", k=kk),
                         I[src][l, e].rearrange("(k p) f -> p k f", p=128), [], [], semkey=self.pck.next())

    def _gather(self, out, src, idx_ap, R, W, bounds=None):
        def fn(e):
            if bounds is None:
                return e.indirect_dma_start(out=out, out_offset=None, in_=src,
                                            in_offset=bass.IndirectOffsetOnAxis(ap=idx_ap, axis=0))
            if "r" not in bounds:
                bounds["r"] = e.to_reg(bounds["v"])
            return e.indirect_dma_start(out=out, out_offset=None, in_=src,
                                        in_offset=bass.IndirectOffsetOnAxis(ap=idx_ap, axis=0),
                                        bounds_check=bounds["r"], oob_is_err=False)
        self.S.dma("pool", fn, R, W, nbytes=128 * self._n(out) * 2, indirect=True)

    def _scatter(self, dst, idx_ap, in_, R, W):
        nrow = dst.shape[0]
        self.S.dma("pool", lambda e: e.indirect_dma_start(out=dst, out_offset=bass.IndirectOffsetOnAxis(ap=idx_ap, axis=0),
                                                          in_=in_, in_offset=None),
                   R, W, semkey=R[0], nbytes=128 * 2048, indirect=True)

    def build(self):
        with ExitStack() as gst:
            gst.enter_context(self.nc.allow_non_contiguous_dma(reason="small strided parameter loads"))
            self.setup_consts(gst)
            info = {}
            if "mod" in self.stages:
                info["mod"] = self.stage_mod()
            for b in range(self.NB):
                if "hgrn" in self.stages:
                    info["hgrn%d" % b] = self.stage_hgrn(b)
            if "moe0" in self.stages:
                info["moe0"] = self.moe_sparse(0)
            for b in range(self.NB):
                if "mla" in self.stages:
                    info["mla%d" % b] = self.stage_mla(b)
            if "moe1" in self.stages:
                info["moe1"] = self.moe_sparse(1)
            self.info = info
        self.S.close()
        return self.nc


def host_consts():
    s = np.arange(128)
    same = (s[:, None] // 32) == (s[None, :] // 32)
    maskf = (same & (s[:, None] <= s[None, :])).astype(np.float32)
    maskb = (same & (s[:, None] >= s[None, :])).astype(np.float32)
    bm = ((s[:, None] // 32) == np.arange(4)[None, :]).astype(np.float32)[:, :, None].repeat(128, axis=2)
    t = np.arange(SEQ)
    row, col = t // 64, t % 64
    inv = (10000.0 ** (-np.arange(0, 16, 2, dtype=np.float32) / 16)).astype(np.float32)
    ang = np.stack([row, col], axis=-1).astype(np.float32)[..., None] * inv
    cos = np.cos(ang).astype(np.float32).reshape(SEQ, 16)
    sin = np.sin(ang).astype(np.float32).reshape(SEQ, 16)
    return {"k_maskf": maskf, "k_maskb": maskb, "k_bm": np.ascontiguousarray(bm), "k_bmc": np.ascontiguousarray(bm[:, :, 0]),
            "k_cos": cos, "k_sin": sin}


def make_in_maps(inputs, NB, ncores, used=None):
    sq = {"hg_w_in": "hg_w_in", "hg_lower_bounds": "hg_lb", "hg_out_norm_g": "hg_out_norm_g", "hg_w_out": "hg_w_out",
          "mla_w_in": "mla_w_in", "mla_q_norm_g": "mla_q_norm_g", "mla_kv_norm_g": "mla_kv_norm_g", "mla_w_qb": "mla_w_qb",
          "mla_w_kvb": "mla_w_kvb", "mla_q_qknorm_g": "mla_q_qknorm_g", "mla_k_qknorm_g": "mla_k_qknorm_g", "mla_w_out": "mla_w_out"}
    shared = {}
    for k, v in inputs.items():
        v = np.asarray(v, dtype=np.float32)
        if k in ("x", "c", "ctx"):
            continue
        if k == "hg_lower_bounds":
            shared["hg_lb"] = np.ascontiguousarray(v)
        elif k in sq:
            shared[sq[k]] = np.ascontiguousarray(v.reshape(v.shape[1:]))
        else:
            shared[k] = np.ascontiguousarray(v)
    shared.update(host_consts())
    maps = []
    for i in range(ncores):
        m = dict(shared)
        for k in ("x", "c", "ctx"):
            m[k] = np.ascontiguousarray(np.asarray(inputs[k], dtype=np.float32)[i * NB:(i + 1) * NB])
        if used is not None:
            m = {k: v for k, v in m.items() if k in used}
        maps.append(m)
    return maps


def kernel(**inputs):
    NB = 4
    kb = KB(NB=NB)
    nc = kb.build()
    maps = make_in_maps(inputs, NB, 8, used=set(kb.I.keys()))
    res = run_bass_kernel_spmd(nc, maps, core_ids=list(range(8)))
    return np.concatenate([r["out"] for r in res.results], axis=0).astype(np.float32)
```

```python
import numpy as np
import os as _os
_F = lambda k: _os.environ.get(k, '1') == '1'
import concourse.bass as bass
import concourse.mybir as mybir
from concourse.bass_utils import run_bass_kernel_spmd
from contextlib import ExitStack

F32 = mybir.dt.float32
BF16 = mybir.dt.bfloat16
I32 = mybir.dt.int32
AF = mybir.ActivationFunctionType
ALU = mybir.AluOpType
AX = mybir.AxisListType

ENGS = ("pe", "act", "dve", "pool", "sp")

D = 1024
KC = 8
CTX = 256
SEQ = 2048
T = CTX + SEQ
NT = T // 128
EPS = 1e-6
NEXP = 32
FF = 512
BIG = 1.0e30


class Key:
    __slots__ = ("w", "r", "dsem", "dcnt", "excl")

    def __init__(self):
        self.w = None
        self.r = []
        self.dsem = None
        self.dcnt = 0
        self.excl = False


class Tl:
    __slots__ = ("t", "k")

    def __init__(self, t, k):
        self.t = t
        self.k = k

    def __getitem__(self, idx):
        return self.t[idx]


def _k(x):
    return x.k if isinstance(x, Tl) else x


class Rot:
    def __init__(self, items):
        self.items = items
        self.i = 0

    def next(self):
        it = self.items[self.i % len(self.items)]
        self.i += 1
        return it


class Sched:
    def __init__(self, nc, n_dma_sems=80):
        self.nc = nc
        self.stack = ExitStack()
        self.esem = {e: self.stack.enter_context(nc.semaphore("es_" + e)) for e in ENGS}
        self.ecnt = {e: 0 for e in ENGS}
        self.dpool = [[self.stack.enter_context(nc.semaphore("ds%d" % i)), 0] for i in range(n_dma_sems)]
        self.dfree = list(range(n_dma_sems))
        self.all_keys = []
        self.reorder = True
        self._reset_stage()

    def _reset_stage(self):
        self.recs = []
        self.dlast = {}

    def key(self):
        k = Key()
        self.all_keys.append(k)
        return k

    def keys(self, n):
        return [self.key() for _ in range(n)]

    def _deps(self, reads, writes):
        deps = set()
        for t in reads:
            if t.w is not None:
                deps.add(t.w)
        for t in writes:
            if t.w is not None:
                deps.add(t.w)
            deps.update(t.r)
        return deps

    def _add(self, eng, fn, reads, writes, cost, dma, lat):
        reads = [_k(x) for x in reads]
        writes = [_k(x) for x in writes]
        ex = [t for t in reads if t.excl and t not in writes]
        if ex:
            reads = [t for t in reads if not t.excl]
            writes = writes + ex
        deps = self._deps(reads, writes)
        i = len(self.recs)
        if dma is not None:
            prev = self.dlast.get(dma)
            if prev is not None:
                deps.add(prev)
            self.dlast[dma] = i
        self.recs.append({"eng": eng, "fn": fn, "deps": deps, "cost": cost, "dma": dma, "lat": lat, "inc": False})
        for t in reads:
            t.r.append(i)
        for t in writes:
            t.w = i
            t.r = []
        return i

    def op(self, eng, fn, reads=(), writes=(), cost=0.2):
        return self._add(eng, fn, reads, writes, cost, None, 0.0)

    def dma(self, eng, fn, reads=(), writes=(), semkey=None, nbytes=0, indirect=False):
        rk = [_k(x) for x in reads]
        wk = [_k(x) for x in writes]
        sk = _k(semkey) if semkey is not None else (wk[0] if wk else rk[0])
        if sk.dsem is None:
            sk.dsem = self.dfree.pop()
        i = self._add(eng, fn, rk, wk, 0.8 if indirect else 0.07, sk.dsem, 2.0 + nbytes / 150e3)
        return i

    def _schedule(self):
        recs = self.recs
        n = len(recs)
        users = [[] for _ in range(n)]
        ndep = [0] * n
        for i, r in enumerate(recs):
            ndep[i] = len(r["deps"])
            for d in r["deps"]:
                users[d].append(i)
        import heapq
        ready = {e: [] for e in ENGS}
        fin = [0.0] * n
        rt = [0.0] * n
        for i, r in enumerate(recs):
            if ndep[i] == 0:
                heapq.heappush(ready[r["eng"]], (0.0, i))
        free = {e: 0.0 for e in ENGS}
        order = {e: [] for e in ENGS}
        done = 0
        while done < n:
            best = None
            for e in ENGS:
                h = ready[e]
                if not h:
                    continue
                t0 = free[e]
                cand = None
                if h[0][0] <= t0:
                    tmp = []
                    while h and h[0][0] <= t0:
                        tmp.append(heapq.heappop(h))
                    ci = min(tmp, key=lambda x: x[1])
                    for x in tmp:
                        if x is not ci:
                            heapq.heappush(h, x)
                    cand = (t0, ci[1], ci)
                else:
                    x = h[0]
                    cand = (x[0], x[1], None)
                if best is None or (cand[0], cand[1]) < (best[0][0], best[0][1]):
                    if best is not None and best[0][2] is not None:
                        heapq.heappush(ready[best[1]], best[0][2])
                    best = (cand, e)
                elif cand[2] is not None:
                    heapq.heappush(h, cand[2])
            (start, i, popped), e = best
            if popped is None:
                heapq.heappop(ready[e])
            r = recs[i]
            free[e] = start + r["cost"]
            fin[i] = start + r["cost"] + r["lat"]
            order[e].append(i)
            done += 1
            for u in users[i]:
                ndep[u] -= 1
                ru = recs[u]
                if ru["eng"] == e:
                    lat = 0.0 if e in ("pe", "sp") else 0.22
                else:
                    lat = 0.3
                if fin[i] + lat > rt[u]:
                    rt[u] = fin[i] + lat
                if ndep[u] == 0:
                    heapq.heappush(ready[ru["eng"]], (rt[u], u))
        return order, max(fin) if n else 0.0

    def flush(self):
        nc = self.nc
        recs = self.recs
        if self.reorder:
            order, est = self._schedule()
        else:
            order = {e: [i for i, r in enumerate(recs) if r["eng"] == e] for e in ENGS}
            est = 0.0
        dval = {}
        dtot = {}
        for i, r in enumerate(recs):
            if r["dma"] is not None:
                c = self.dpool[r["dma"]][1] + 16
                self.dpool[r["dma"]][1] = c
                dval[i] = c
                dtot[r["dma"]] = c
        for i, r in enumerate(recs):
            for d in r["deps"]:
                rd = recs[d]
                if rd["dma"] is None and (rd["eng"] != r["eng"] or r["eng"] in ("act", "dve", "pool")):
                    rd["inc"] = True
        eval_ = {}
        for e in ENGS:
            c = self.ecnt[e]
            for i in order[e]:
                if recs[i]["inc"]:
                    c += 1
                eval_[i] = c
            self.ecnt[e] = c
        engobj = {"pe": "tensor", "act": "scalar", "dve": "vector", "pool": "gpsimd", "sp": "sync"}
        esem, dpool = self.esem, self.dpool

        def mk(e):
            def body(eng):
                seen = {}
                for i in order[e]:
                    r = recs[i]
                    waits = {}
                    for d in r["deps"]:
                        rd = recs[d]
                        if rd["dma"] is not None:
                            k, v = ("d", rd["dma"]), dval[d]
                        elif rd["eng"] != e or e in ("act", "dve", "pool"):
                            k, v = ("e", rd["eng"]), eval_[d]
                        else:
                            continue
                        if seen.get(k, -1) >= v:
                            continue
                        if waits.get(k, -1) < v:
                            waits[k] = v
                    for k, v in waits.items():
                        seen[k] = v
                        if k[0] == "e":
                            eng.wait_ge(esem[k[1]], v)
                        else:
                            eng.wait_ge(dpool[k[1]][0], v)
                    ins = r["fn"](eng)
                    if r["dma"] is not None:
                        ins.then_inc(dpool[r["dma"]][0], 16)
                    elif r["inc"]:
                        ins.then_inc(esem[e], 1)
                if e == "sp":
                    for idx, v in dtot.items():
                        if seen.get(("d", idx), -1) < v:
                            eng.wait_ge(dpool[idx][0], v)
            return body

        with nc.Block() as block:
            for e in ENGS:
                if order[e] or (e == "sp" and dtot):
                    getattr(block, engobj[e])(mk(e))
        for k in self.all_keys:
            if k.dsem is not None:
                self.dfree.append(k.dsem)
                k.dsem = None
            k.w = None
            k.r = []
        n = {e: len(order[e]) for e in ENGS}
        n["est_us"] = round(est, 1)
        self._reset_stage()
        return n

    def close(self):
        self.stack.close()


class KB:
    def __init__(self, NB=4, stages=("mod", "hgrn", "moe0", "mla", "moe1"), dbg=()):
        self.NB = NB
        self.stages = stages
        self.dbg = dbg
        nc = bass.Bass("TRN2", target_bir_lowering=False)
        self.nc = nc
        self.S = Sched(nc)
        self.uid = 0
        shapes = {
            "x": (NB, SEQ, D), "c": (NB, D), "ctx": (NB, CTX, D), "c_ctx": (D,),
            "ada_w": (2, D, 6 * D), "ada_b": (2, 6 * D), "norm_mix_g": (2, D), "norm_ffn_g": (2, D),
            "hg_w_in": (D, 5 * D), "hg_lb": (2, 3, D), "hg_out_norm_g": (128,), "hg_w_out": (D, D),
            "mla_w_in": (D, 416), "mla_q_norm_g": (256,), "mla_kv_norm_g": (128,), "mla_w_qb": (256, 1536),
            "mla_w_kvb": (128, 2048), "mla_q_qknorm_g": (96,), "mla_k_qknorm_g": (96,), "mla_w_out": (D, D),
            "moe_w_group": (2, D, 4), "moe_w_expert": (2, D, 32), "moe_w_gate": (2, NEXP, D, FF),
            "moe_w_up": (2, NEXP, D, FF), "moe_w_down": (2, NEXP, FF, D),
            "k_maskf": (128, 128), "k_maskb": (128, 128), "k_bm": (128, 4, 128), "k_bmc": (128, 4), "k_cos": (SEQ, 16), "k_sin": (SEQ, 16),
        }

        class LazyIn(dict):
            def __missing__(d_, name):
                ap = nc.dram_tensor(name, list(shapes[name]), F32, kind="ExternalInput").ap()
                d_[name] = ap
                return ap

        I = LazyIn()
        self.I = I
        self.out = nc.dram_tensor("out", [NB, SEQ, D], F32, kind="ExternalOutput").ap()
        self.xres = nc.dram_tensor("xres", [NB, T, D], F32).ap()
        self.mod = nc.dram_tensor("modv", [2, 5, 6 * D], F32).ap()
        self.cT_d = nc.dram_tensor("cT_d", [128, 3, T], BF16).ap()
        self.krr_d = nc.dram_tensor("krr_d", [128, NT, 32], F32).ap()
        self.sskr_d = nc.dram_tensor("sskr_d", [128, NT], F32).ap()
        NTLmax = NB * NT
        self.NBLKmax = 2 * NTLmax + NEXP
        self.h2d = nc.dram_tensor("h2d", [NTLmax * 128, D], BF16).ap()
        self.xs = nc.dram_tensor("xs", [self.NBLKmax * 128, D], BF16).ap()
        self.ys = nc.dram_tensor("ys", [self.NBLKmax * 128, D], F32).ap()
        self.wgb = [nc.dram_tensor("wgb%d" % l_, [NEXP * 128, 4096], BF16).ap() for l_ in range(2)]
        self.wub = [nc.dram_tensor("wub%d" % l_, [NEXP * 128, 4096], BF16).ap() for l_ in range(2)]
        self.wdb = [nc.dram_tensor("wdb%d" % l_, [NEXP * 128, 4096], BF16).ap() for l_ in range(2)]
        self.kwb = [self.S.key() for _ in range(2)]
        self.kx = [self.S.keys(NT) for _ in range(NB)]
        self.kmod = self.S.key()
        self.kscr = self.S.key()
        self.D_ = {}
        for name, shape in dbg:
            self.D_[name] = nc.dram_tensor("dbg_" + name, list(shape), F32, kind="ExternalOutput").ap()

    def sb(self, st, shape, dt, nm="t"):
        self.uid += 1
        t = st.enter_context(self.nc.sbuf_tensor("%s_%d" % (nm, self.uid), list(shape), dt))
        return Tl(t, self.S.key())

    def sbr(self, st, n, shape, dt, nm="r"):
        return Rot([self.sb(st, shape, dt, nm) for _ in range(n)])

    def psb(self, st, nm="ps"):
        self.uid += 1
        t = st.enter_context(self.nc.psum_tensor("%s_%d" % (nm, self.uid), [128, 512], F32))
        k = self.S.key()
        k.excl = True
        return Tl(t, k)

    def psb2(self, st, nm="ps2"):
        self.uid += 1
        t = st.enter_context(self.nc.psum_tensor("%s_%d" % (nm, self.uid), [128, 1024], F32))
        k = self.S.key()
        k.excl = True
        return Tl(t, k)

    @staticmethod
    def _n(ap):
        n = 1
        for d in ap.shape[1:]:
            n *= d
        return n

    def mm(self, out, lhsT, rhs, start, stop, R, W):
        c = max(64, self._n(out)) / 2400.0 + 0.02
        if rhs.dtype == F32:
            c *= 4
        self.S.op("pe", lambda e: e.matmul(out, lhsT=lhsT, rhs=rhs, start=start, stop=stop), R, W, cost=c)

    def tr(self, out, in_, ident, R, W):
        self.S.op("pe", lambda e: e.transpose(out=out, in_=in_, identity=ident), R, W, cost=0.09)

    def act(self, out, in_, func, R, W, bias=None, scale=None, accum_out=None):
        kw = {}
        if bias is not None:
            kw["bias"] = bias
        if scale is not None:
            kw["scale"] = scale
        if accum_out is not None:
            kw["accum_out"] = accum_out
        self.S.op("act", lambda e: e.activation(out=out, in_=in_, func=func, **kw), R, W, cost=0.22 + self._n(out) / 1200.0)

    def ts(self, eng, out, in0, s1, s2, op0, op1, R, W):
        if op1 is None:
            self.S.op(eng, lambda e: e.tensor_scalar(out=out, in0=in0, scalar1=s1, scalar2=None, op0=op0), R, W, cost=self._c(eng, out))
        else:
            self.S.op(eng, lambda e: e.tensor_scalar(out=out, in0=in0, scalar1=s1, scalar2=s2, op0=op0, op1=op1), R, W, cost=self._c(eng, out))

    def tt(self, eng, out, in0, in1, op, R, W):
        self.S.op(eng, lambda e: e.tensor_tensor(out=out, in0=in0, in1=in1, op=op), R, W, cost=self._c(eng, out, 1.5))

    def stt(self, out, in0, scalar, in1, op0, op1, R, W):
        self.S.op("dve", lambda e: e.scalar_tensor_tensor(out=out, in0=in0, scalar=scalar, in1=in1, op0=op0, op1=op1), R, W,
                  cost=self._c("dve", out, 1.5))

    def cp(self, eng, out, in_, R, W):
        if eng == "act":
            self.S.op("act", lambda e: e.activation(out=out, in_=in_, func=AF.Copy), R, W, cost=0.22 + self._n(out) / 1200.0)
        else:
            self.S.op(eng, lambda e: e.tensor_copy(out=out, in_=in_), R, W, cost=self._c(eng, out))

    def red(self, out, in_, op, R, W, negate=None):
        self.S.op("dve", lambda e: e.tensor_reduce(out=out, in_=in_, axis=AX.X, op=op, negate=negate), R, W, cost=self._c("dve", in_))

    def recip(self, out, in_, R, W):
        self.S.op("dve", lambda e: e.reciprocal(out=out, in_=in_), R, W, cost=self._c("dve", out, 8.0))

    def memset(self, eng, ap, val, W):
        self.S.op(eng, lambda e: e.memset(ap, val), (), W, cost=self._c(eng, ap))

    def _c(self, eng, ap, mult=1.0):
        n = self._n(ap)
        if eng == "pool":
            return 0.25 + n * mult / 500.0
        return 0.1 + n * mult / 960.0

    def dma(self, q, out, in_, R, W, semkey=None):
        nb = out.shape[0] * self._n(out) * 4
        self.S.dma(q, lambda e: e.dma_start(out=out, in_=in_), R, W, semkey=semkey, nbytes=nb)

    def sumsq(self, junk, in_, acc, R, W):
        self.act(junk, in_, AF.Square, R, W, accum_out=acc)

    def setup_consts(self, st):
        nc, S = self.nc, self.S
        self.identf = self.sb(st, [128, 128], F32, "identf")
        self.identb = self.sb(st, [128, 128], BF16, "identb")
        self.onesb = self.sb(st, [128, 128], BF16, "onesb")
        identf = self.identf
        self.memset("pool", identf[:], 0.0, [identf])
        S.op("pool", lambda e: e.affine_select(out=identf[:], in_=identf[:], pattern=[[-1, 128]], compare_op=ALU.not_equal,
                                               fill=1.0, base=0, channel_multiplier=1), [identf], [identf])
        self.cp("dve", self.identb[:], identf[:], [identf], [self.identb])
        self.memset("dve", self.onesb[:], 1.0, [self.onesb])
        self.epsc = self.sb(st, [128, 1], F32, "epsc")
        self.memset("dve", self.epsc[:], EPS, [self.epsc])
        self.onec = self.sb(st, [128, 1], F32, "onec")
        self.memset("dve", self.onec[:], 1.0, [self.onec])

    def stage_mod(self):
        I, NB = self.I, self.NB
        with ExitStack() as st:
            cT = self.sb(st, [128, KC, 5], F32, "cT")
            cs = self.sb(st, [128, KC, 5], F32, "cs")
            self.memset("dve", cT[:], 0.0, [cT])
            for r in range(NB):
                self.dma("sp", cT[:, :, r], I["c"][r, :].rearrange("(c p) -> p c", p=128), [], [cT])
            self.dma("sp", cT[:, :, 4], I["c_ctx"].rearrange("(c p) -> p c", p=128), [], [cT])
            self.act(cs[:], cT[:], AF.Silu, [cT], [cs])
            wrot = self.sbr(st, 3, [128, KC, 512], F32, "adaw")
            ps = Rot([self.psb(st) for _ in range(2)])
            for l in range(2):
                bt = self.sb(st, [5, 6 * D], F32, "adab")
                ms = self.sb(st, [5, 6 * D], F32, "modsb")
                self.dma("sp", bt[:], I["ada_b"][l, :].partition_broadcast(5), [], [bt])
                for n in range(12):
                    w = wrot.next()
                    self.dma("sp", w[:], I["ada_w"][l, :, n * 512:(n + 1) * 512].rearrange("(c p) n -> p c n", p=128), [], [w])
                    p = ps.next()
                    for k in range(KC):
                        self.mm(p[0:5, :], cs[:, k, :], w[:, k, :], k == 0, k == KC - 1, [cs, w], [p])
                    self.tt("dve", ms[:, n * 512:(n + 1) * 512], p[0:5, :], bt[:, n * 512:(n + 1) * 512], ALU.add, [p, bt], [ms])
                self.dma("sp", self.mod[l], ms[:], [ms], [self.kmod], semkey=ms)
            return self.S.flush()

    def mod_cols(self, st, l, m, r):
        I = self.I
        g = I["norm_mix_g"] if m == 0 else I["norm_ffn_g"]
        gc = self.sb(st, [128, KC], F32, "gc")
        sc = self.sb(st, [128, KC], F32, "sc")
        sh = self.sb(st, [128, KC], F32, "sh")
        A = self.sb(st, [128, KC], F32, "A")
        self.dma("sp", gc[:], g[l, :].rearrange("(c p) -> p c", p=128), [], [gc])
        self.dma("sp", sc[:], self.mod[l, r, (3 * m + 1) * D:(3 * m + 2) * D].rearrange("(c p) -> p c", p=128), [self.kmod], [sc])
        self.dma("sp", sh[:], self.mod[l, r, (3 * m) * D:(3 * m + 1) * D].rearrange("(c p) -> p c", p=128), [self.kmod], [sh])
        self.stt(A[:], sc[:], 1.0, gc[:], ALU.add, ALU.mult, [sc, gc], [A])
        return A, sh

    def gate_tile(self, st, l, m, r):
        gt = self.sb(st, [128, D], F32, "gt")
        self.dma("sp", gt[:], self.mod[l, r, (3 * m + 2) * D:(3 * m + 3) * D].partition_broadcast(128), [self.kmod], [gt])
        return gt

    def norm_res(self, st, pbanks, junk=None, nxn=1, nhtf=1, nxt=2):
        R = {}
        R["xt"] = self.sbr(st, nxt, [128, D], F32, "xt")
        R["xn"] = self.sbr(st, nxn, [128, D], F32, "xn")
        R["junk"] = junk if junk is not None else self.sb(st, [128, D], BF16, "junk")
        R["ss"] = self.sbr(st, 3, [128, 1], F32, "ss")
        R["sd"] = self.sbr(st, 3, [128, 1], F32, "sd")
        R["rs"] = self.sbr(st, 3, [128, 1], F32, "rs")
        R["hTf"] = self.sbr(st, nhtf, [128, KC, 128], F32, "hTf")
        R["pb"] = pbanks
        return R

    def norm_tile(self, R, src_ap, src_keys, A, Bc, dst_ap, dst_keys):
        xt = R["xt"].next()
        xn = R["xn"].next()
        ss = R["ss"].next()
        sd = R["sd"].next()
        rs = R["rs"].next()
        hTf = R["hTf"].next()
        junk = R["junk"]
        pa, pb = R["pb"]
        self.dma("sp", xt[:], src_ap, src_keys, [xt])
        self.sumsq(junk[:, 0:D], xt[:], ss[:], [xt], [junk, ss])
        self.act(sd[:], ss[:], AF.Sqrt, [ss, self.epsc], [sd], bias=self.epsc[:, 0:1], scale=1.0 / D)
        self.recip(rs[:], sd[:], [sd], [rs])
        self.act(xn[:], xt[:], AF.Copy, [xt, rs], [xn], scale=rs[:, 0:1])
        for k in range(KC):
            p = pa if k < 4 else pb
            self.tr(p[:, (k % 4) * 128:(k % 4 + 1) * 128], xn[:, k * 128:(k + 1) * 128], self.identf[:], [xn, self.identf], [p])
        for k in range(KC):
            p = pa if k < 4 else pb
            src = p[:, (k % 4) * 128:(k % 4 + 1) * 128]
            if k < 4:
                self.ts("dve", hTf[:, k, :], src, A[:, k:k + 1], Bc[:, k:k + 1], ALU.mult, ALU.add, [p, A, Bc], [hTf])
            else:
                self.act(hTf[:, k, :], src, AF.Identity, [p, A, Bc], [hTf], bias=Bc[:, k:k + 1], scale=A[:, k:k + 1])
        if dst_ap is not None:
            self.cp("dve", dst_ap, hTf[:], [hTf], dst_keys)
        self.last_xn = xn
        return hTf

    def src_l0(self, b, tile):
        if tile < 2:
            return self.I["ctx"][b, tile * 128:(tile + 1) * 128, :]
        return self.I["x"][b, (tile - 2) * 128:(tile - 1) * 128, :]

    def stage_hgrn(self, b):
        I, S = self.I, self.S
        l = 0
        with ExitStack() as st:
            PB = [self.psb(st) for _ in range(8)]
            if "moe0" in self.stages:
                self.precast(0, b)
            hT = self.sb(st, [128, KC, T], BF16, "hT")
            hTk = S.keys(NT)
            ogT = self.sb(st, [128, KC, T], BF16, "ogT")
            ogk = S.keys(KC)
            maskf = self.sb(st, [128, 128], F32, "maskf")
            maskb = self.sb(st, [128, 128], F32, "maskb")
            bmc = self.sb(st, [128, 4], F32, "bmc")
            if not _F("HG_A"):
                bm = self.sb(st, [128, 4, 128], BF16, "bm")
                self.dma("pool", bm[:], I["k_bm"], [], [bm])
                Vbd = self.sbr(st, 2, [128, 4, 128], BF16, "Vbd")
            self.dma("sp", maskf[:], I["k_maskf"], [], [maskf])
            self.dma("sp", maskb[:], I["k_maskb"], [], [maskb])
            self.dma("sp", bmc[:], I["k_bmc"], [], [bmc])
            m01 = self.sb(st, [128, T], BF16, "m01")
            self.memset("dve", m01[:], 1.0, [m01])
            self.memset("dve", m01[:, 0:T:32], 0.0, [m01])
            lbr = self.sb(st, [128, 2, 3, KC], F32, "lbr")
            with self.nc.allow_non_contiguous_dma(reason="tiny"):
                for d_ in range(2):
                    for j in range(3):
                        self.dma("sp", lbr[:, d_, j, :], I["hg_lb"][d_, j, :].rearrange("(h p) -> p h", p=128), [], [lbr])
            lbe = self.sb(st, [128, 2, 3, KC], F32, "lbe")
            self.act(lbe[:], lbr[:], AF.Exp, [lbr], [lbe])
            lbs = self.sb(st, [128, 2, KC], F32, "lbs")
            self.tt("dve", lbs[:], lbe[:, :, 0, :], lbe[:, :, 1, :], ALU.add, [lbe], [lbs])
            self.tt("dve", lbs[:], lbs[:], lbe[:, :, 2, :], ALU.add, [lbe, lbs], [lbs])
            lbi = self.sb(st, [128, 2, KC], F32, "lbi")
            self.recip(lbi[:], lbs[:], [lbs], [lbi])
            lb = self.sb(st, [128, 2, KC], F32, "lb")
            oml = self.sb(st, [128, 2, KC], F32, "oml")
            self.tt("dve", lb[:], lbe[:, :, 0, :], lbi[:], ALU.mult, [lbe, lbi], [lb])
            self.ts("dve", oml[:], lb[:], -1.0, 1.0, ALU.mult, ALU.add, [lb], [oml])
            ogc = self.sb(st, [128, 1], F32, "ogc")
            self.dma("sp", ogc[:], I["hg_out_norm_g"].rearrange("(p o) -> p o", o=1), [], [ogc])
            A_l, B_l = self.mod_cols(st, l, 0, b)
            A_c, B_c = self.mod_cols(st, l, 0, 4)
            gt_l = self.gate_tile(st, l, 0, b)
            gt_c = self.gate_tile(st, l, 0, 4)
            qdec = self.sb(st, [128, T], BF16, "qdec")
            NR = self.norm_res(st, (PB[0], PB[1]), junk=qdec)
            for tile in range(NT):
                A, Bc = (A_c, B_c) if tile < 2 else (A_l, B_l)
                self.norm_tile(NR, self.src_l0(b, tile), [], A, Bc, hT[:, :, tile * 128:(tile + 1) * 128], [hTk[tile]])
            wh = self.sbr(st, 1, [128, KC, 5, 128], BF16, "wh")
            Vh = self.sb(st, [128, NT, 128], BF16, "Vh")
            qs = self.sb(st, [128, T], BF16, "qs")
            sgate = self.sb(st, [128, T], BF16, "sgate")
            A1 = self.sb(st, [128, T], F32, "A1")
            A2 = self.sb(st, [128, T], F32, "A2")
            A3 = self.sb(st, [128, T], F32, "A3")
            kinc = self.sb(st, [128, T], BF16, "kinc")
            dec = self.sb(st, [128, T // 32], F32, "dec")
            tot = self.sb(st, [128, T // 32], F32, "tot")
            oacc = self.sb(st, [128, T], F32, "oacc")
            oak = S.keys(NT)
            sTm = self.sbr(st, 2, [128, 128], BF16, "sTm")
            kTs = self.sbr(st, 2, [128, 4, 128], BF16, "kTs")
            KVs = self.sbr(st, 2, [128, 4, 128], F32, "KVs")
            Sst = self.sb(st, [128, 8, 128], F32, "Sst")
            Sstk = S.keys(8)
            Sb = self.sb(st, [128, 8, 128], BF16, "Sb")
            Sbk = S.keys(2)
            pproj = Rot([PB[0], PB[1]])
            psT = Rot([PB[2], PB[3]])
            pkT = PB[4]
            pkTk = [PB[4].k, PB[4].k]
            pKV = PB[5]
            poT = Rot([PB[6], PB[7]])
            blocks = [(i * 512, 512) for i in range(4)] + [(2048, 256)]

            def proj(whh, sec, blk):
                t0, n = blk
                p = pproj.next()
                tiles = range(t0 // 128, (t0 + n) // 128)
                for k in range(KC):
                    self.mm(p[:, 0:n], whh[:, k, sec, :], hT[:, k, t0:t0 + n], k == 0, k == KC - 1,
                            [whh] + [hTk[t] for t in tiles], [p])
                return p

            for h in range(KC):
                whh = wh.next()
                for sec in range(5):
                    self.dma("pool", whh[:, :, sec, :],
                             I["hg_w_in"][:, sec * D + h * 128: sec * D + (h + 1) * 128].rearrange("(c p) e -> p c e", p=128), [], [whh])
                for tile in range(NT):
                    p = pproj.next()
                    for k in range(KC):
                        self.mm(p[:, 0:128], hT[:, k, tile * 128:(tile + 1) * 128], whh[:, k, 3, :], k == 0, k == KC - 1,
                                [whh, hTk[tile]], [p])
                    self.cp("act", Vh[:, tile, :], p[:, 0:128], [p], [Vh])
                for blk in blocks:
                    t0, n = blk
                    p = proj(whh, 0, blk)
                    self.act(qs[:, t0:t0 + n], p[:, 0:n], AF.Silu, [p], [qs])
                    p = proj(whh, 4, blk)
                    self.act(sgate[:, t0:t0 + n], p[:, 0:n], AF.Silu, [p], [sgate])
                for dr in range(2):
                    for blk in blocks:
                        t0, n = blk
                        p = proj(whh, 1 + dr, blk)
                        self.act(A1[:, t0:t0 + n], p[:, 0:n], AF.Sigmoid, [p], [A1])
                    self.ts("dve", A1[:], A1[:], oml[:, dr, h:h + 1], lb[:, dr, h:h + 1], ALU.mult, ALU.add, [A1, oml, lb], [A1])
                    self.act(A2[:], A1[:], AF.Ln, [A1], [A2])
                    if _F("HG_B"):
                        self.act(A1[:], A1[:], AF.Identity, [A1, self.onec], [A1], bias=self.onec[:, 0:1], scale=-1.0)
                    else:
                        self.ts("dve", A1[:], A1[:], -1.0, 1.0, ALU.mult, ALU.add, [A1], [A1])
                    S.op("dve", lambda e: e.tensor_tensor_scan(out=A3[:], data0=m01[:], data1=A2[:], initial=0.0,
                                                                op0=ALU.mult, op1=ALU.add), [m01, A2], [A3], cost=0.1 + 2 * T / 960.0)
                    a3v = A3[:].rearrange("p (j i) -> p j i", i=32)
                    self.cp("dve", tot[:], a3v[:, :, 31], [A3], [tot])
                    self.act(dec[:], tot[:], AF.Exp, [tot], [dec])
                    if dr == 0:
                        barr, free = A3, A2
                    else:
                        a2v = A2[:].rearrange("p (j i) -> p j i", i=32)
                        self.tt("dve", A2[:], A2[:], A3[:], ALU.subtract, [A2, A3], [A2])
                        self.tt("dve", a2v, a2v, tot[:].unsqueeze(2).broadcast_to([128, T // 32, 32]), ALU.add, [A2, tot], [A2])
                        barr, free = A2, A3
                    self.act(free[:], barr[:], AF.Exp, [barr], [free], scale=-1.0)
                    self.act(barr[:], barr[:], AF.Exp, [barr], [barr])
                    self.tt("dve", qdec[:], qs[:], barr[:], ALU.mult, [qs, barr], [qdec])
                    self.tt("dve", kinc[:], A1[:], free[:], ALU.mult, [A1, free], [kinc])
                    order = list(range(NT)) if dr == 0 else [1, 0] + list(range(NT - 1, 1, -1))
                    mask = maskf if dr == 0 else maskb
                    self.memset("dve", Sst[:, 0, :], 0.0, [Sstk[0]])
                    for i, tile in enumerate(order):
                        base = 4 * (i % 2)
                        ts_ = slice(tile * 128, (tile + 1) * 128)
                        ps_ = psT.next()
                        self.mm(ps_[:, 0:128], kinc[:, ts_], qdec[:, ts_], True, True, [kinc, qdec], [ps_])
                        sm = sTm.next()
                        self.tt("dve", sm[:], ps_[:, 0:128], mask[:], ALU.mult, [ps_, mask], [sm])
                        pk_i = i % 2
                        pkv = pkT[:, pk_i * 64:(pk_i + 1) * 64].bitcast(BF16)
                        self.tr(pkv, kinc[:, ts_], self.identb[:], [kinc, self.identb], [pkTk[pk_i]])
                        kt = kTs.next()
                        if _F("HG_A"):
                            for j in range(4):
                                self.act(kt[:, j, :], pkv, AF.Copy, [pkTk[pk_i], bmc], [kt], scale=bmc[:, j:j + 1])
                            for j in range(4):
                                self.mm(pKV[:, j * 128:(j + 1) * 128], kt[:, j, :], Vh[:, tile, :], True, True, [kt, Vh], [pKV])
                        else:
                            self.cp("act", kt[:, 0, :], pkv, [pkTk[pk_i]], [kt])
                            vb = Vbd.next()
                            self.tt("dve", vb[:], Vh[:, tile, :].unsqueeze(1).broadcast_to([128, 4, 128]), bm[:], ALU.mult, [Vh, bm], [vb])
                            self.mm(pKV[:, :], kt[:, 0, :], vb[:].rearrange("p j v -> p (j v)"), True, True, [kt, vb], [pKV])
                        kv = KVs.next()
                        self.tt("dve", kv[:], pKV[:, :].rearrange("p (j v) -> p j v", j=4),
                                dec[:, tile * 4:(tile + 1) * 4].unsqueeze(2).broadcast_to([128, 4, 128]), ALU.mult, [pKV, dec], [kv])
                        corder = [0, 1, 2, 3] if dr == 0 else [3, 2, 1, 0]
                        for jj, c in enumerate(corder):
                            s_in = base + jj
                            s_out = (base + jj + 1) % 8
                            self.stt(Sst[:, s_out, :], Sst[:, s_in, :], dec[:, tile * 4 + c: tile * 4 + c + 1], kv[:, c, :],
                                     ALU.mult, ALU.add, [Sstk[s_in], dec, kv], [Sstk[s_out]])
                        self.cp("pool" if not _F("HG_E") else "act", Sb[:, base:base + 4, :], Sst[:, base:base + 4, :],
                                [Sstk[base + q_] for q_ in range(4)], [Sbk[i % 2]])
                        po = poT.next()
                        self.mm(po[:, 0:128], Vh[:, tile, :], sm[:], True, False, [Vh, sm], [po])
                        for jj, c in enumerate(corder):
                            self.mm(po[:, c * 32:(c + 1) * 32], Sb[:, base + jj, :], qdec[:, tile * 128 + c * 32: tile * 128 + (c + 1) * 32],
                                    False, jj == 3, [Sbk[i % 2], qdec], [po])
                        if dr == 0:
                            self.cp("act", oacc[:, ts_], po[:, 0:128], [po], [oak[tile]])
                        else:
                            self.tt("dve", oacc[:, ts_], oacc[:, ts_], po[:, 0:128], ALU.add, [po, oak[tile]], [oak[tile]])
                if _F("HG_D"):
                    self.act(qdec[:], oacc[:], AF.Square, oak, [qdec])
                else:
                    self.tt("dve", qdec[:], oacc[:], oacc[:], ALU.mult, oak, [qdec])
                for blk in blocks:
                    t0, n = blk
                    p = pproj.next()
                    self.mm(p[:, 0:n], self.onesb[:], qdec[:, t0:t0 + n], True, True, [self.onesb, qdec], [p])
                    if _F("HG_C"):
                        self.act(A2[:, t0:t0 + n], p[:, 0:n], AF.Ln, [p, self.epsc], [A2], bias=self.epsc[:, 0:1], scale=1.0 / 128)
                    else:
                        self.act(A2[:, t0:t0 + n], p[:, 0:n], AF.Sqrt, [p, self.epsc], [A2], bias=self.epsc[:, 0:1], scale=1.0 / 128)
                if _F("HG_C"):
                    self.act(A3[:], A2[:], AF.Exp, [A2], [A3], scale=-0.5)
                else:
                    self.recip(A3[:], A2[:], [A2], [A3])
                self.tt("dve", A3[:], A3[:], oacc[:], ALU.mult, [A3] + oak, [A3])
                self.stt(ogT[:, h, :], A3[:], ogc[:, 0:1], sgate[:], ALU.mult, ALU.mult, [A3, ogc, sgate], [ogk[h]])
            wo = self.sb(st, [128, KC, D], BF16, "wo")
            self.dma("pool", wo[:], I["hg_w_out"].rearrange("(c p) n -> p c n", p=128), [], [wo])
            xt2 = NR["xt"]
            tmp = NR["xn"]
            for tile in range(NT):
                gt = gt_c if tile < 2 else gt_l
                x_ = xt2.next()
                self.dma("sp", x_[:], self.src_l0(b, tile), [], [x_])
                t_ = tmp.next()
                for half in range(2):
                    p = pproj.next()
                    hs = slice(half * 512, (half + 1) * 512)
                    for k in range(KC):
                        self.mm(p[:, :], ogT[:, k, tile * 128:(tile + 1) * 128], wo[:, k, hs], k == 0, k == KC - 1, [ogk[k], wo], [p])
                    self.tt("dve", t_[:, hs], p[:, :], gt[:, hs], ALU.mult, [p, gt], [t_])
                self.tt("dve", t_[:], t_[:], x_[:], ALU.add, [t_, x_], [t_])
                self.dma("sp", self.xres[b, tile * 128:(tile + 1) * 128, :], t_[:], [t_], [self.kx[b][tile]], semkey=t_)
                if ("xm0" in self.D_) and b == 0:
                    self.dma("sp", self.D_["xm0"][tile * 128:(tile + 1) * 128, :], t_[:], [t_], [self.kscr], semkey=t_)
            return S.flush()

    ROUTE_TMPS = (("lg", 36), ("gmax", 1), ("ngmax", 1), ("ge", 4), ("gsum", 1), ("pg", 1), ("gone", 4), ("pen", 4),
                  ("em", 32), ("m1", 1), ("oh1", 32), ("em2", 32), ("m2", 1), ("oh2", 32), ("dm", 1), ("e2", 1),
                  ("den", 1), ("rden", 1), ("w1", 1), ("w2", 1), ("tmpw", 32))

    def route_tile(self, sm, p):
        t = {nm: r.next() for nm, r in sm.items()}
        lg = t["lg"]
        self.cp("act", lg[:], p[:, 0:36], [p], [lg])
        self.red(t["gmax"][:], lg[:, 0:4], ALU.max, [lg], [t["gmax"]])
        self.ts("dve", t["ngmax"][:], t["gmax"][:], -1.0, None, ALU.mult, None, [t["gmax"]], [t["ngmax"]])
        self.act(t["ge"][:], lg[:, 0:4], AF.Exp, [lg, t["ngmax"]], [t["ge"], t["gsum"]], bias=t["ngmax"][:, 0:1], accum_out=t["gsum"][:])
        self.recip(t["pg"][:], t["gsum"][:], [t["gsum"]], [t["pg"]])
        self.ts("dve", t["gone"][:], lg[:, 0:4], t["gmax"][:, 0:1], None, ALU.is_ge, None, [lg, t["gmax"]], [t["gone"]])
        self.ts("dve", t["pen"][:], t["gone"][:], BIG, -BIG, ALU.mult, ALU.add, [t["gone"]], [t["pen"]])
        self.tt("dve", t["em"][:].rearrange("p (g j) -> p g j", g=4), lg[:, 4:36].rearrange("p (g j) -> p g j", g=4),
                t["pen"][:].unsqueeze(2).broadcast_to([128, 4, 8]), ALU.add, [lg, t["pen"]], [t["em"]])
        self.red(t["m1"][:], t["em"][:], ALU.max, [t["em"]], [t["m1"]])
        self.ts("dve", t["oh1"][:], t["em"][:], t["m1"][:, 0:1], None, ALU.is_ge, None, [t["em"], t["m1"]], [t["oh1"]])
        self.stt(t["em2"][:], t["oh1"][:], -BIG, t["em"][:], ALU.mult, ALU.add, [t["oh1"], t["em"]], [t["em2"]])
        self.red(t["m2"][:], t["em2"][:], ALU.max, [t["em2"]], [t["m2"]])
        self.ts("dve", t["oh2"][:], t["em2"][:], t["m2"][:, 0:1], None, ALU.is_ge, None, [t["em2"], t["m2"]], [t["oh2"]])
        self.tt("dve", t["dm"][:], t["m2"][:], t["m1"][:], ALU.subtract, [t["m2"], t["m1"]], [t["dm"]])
        self.act(t["e2"][:], t["dm"][:], AF.Exp, [t["dm"]], [t["e2"]])
        self.ts("dve", t["den"][:], t["e2"][:], 1.0, None, ALU.add, None, [t["e2"]], [t["den"]])
        self.recip(t["rden"][:], t["den"][:], [t["den"]], [t["rden"]])
        self.tt("dve", t["w1"][:], t["pg"][:], t["rden"][:], ALU.mult, [t["pg"], t["rden"]], [t["w1"]])
        self.tt("dve", t["w2"][:], t["w1"][:], t["e2"][:], ALU.mult, [t["w1"], t["e2"]], [t["w2"]])
        return t

    def stage_moe(self, l, b, half):
        I, S = self.I, self.S
        if l == 0:
            tiles = list(range(0, 9)) if half == 0 else list(range(9, 18))
        else:
            tiles = list(range(2, 10)) if half == 0 else list(range(10, 18))
        ntl = len(tiles)
        NTOK = ntl * 128
        with ExitStack() as st:
            PB = [self.psb(st) for _ in range(8)]
            hT = self.sb(st, [128, KC, NTOK], BF16, "hT")
            hTk = S.keys(ntl)
            acc = self.sb(st, [128, ntl, D], F32, "acc")
            acck = S.keys(ntl)
            Wt = self.sb(st, [128, ntl, NEXP], F32, "Wt")
            Wtk = S.keys(ntl)
            wr = self.sb(st, [128, KC, 36], F32, "wr")
            self.dma("sp", wr[:, :, 0:4], I["moe_w_group"][l].rearrange("(c p) g -> p c g", p=128), [], [wr])
            self.dma("sp", wr[:, :, 4:36], I["moe_w_expert"][l].rearrange("(c p) g -> p c g", p=128), [], [wr])
            A_l, B_l = self.mod_cols(st, l, 1, b)
            gt_l = self.gate_tile(st, l, 1, b)
            if l == 0 and half == 0:
                A_c, B_c = self.mod_cols(st, l, 1, 4)
                gt_c = self.gate_tile(st, l, 1, 4)
            NR = self.norm_res(st, (PB[0], PB[1]), nhtf=2)
            sm = {}
            for nm, w in (("lg", 36), ("gmax", 1), ("ngmax", 1), ("ge", 4), ("gsum", 1), ("pg", 1), ("gone", 4), ("pen", 4),
                          ("em", 32), ("m1", 1), ("oh1", 32), ("em2", 32), ("m2", 1), ("oh2", 32), ("dm", 1), ("e2", 1),
                          ("den", 1), ("rden", 1), ("w1", 1), ("w2", 1), ("tmpw", 32)):
                sm[nm] = self.sbr(st, 2, [128, w], F32, nm)
            for li, tile in enumerate(tiles):
                isctx = (l == 0 and tile < 2)
                A, Bc = (A_c, B_c) if isctx else (A_l, B_l)
                hTf = self.norm_tile(NR, self.xres[b, tile * 128:(tile + 1) * 128, :], [self.kx[b][tile]], A, Bc,
                                     hT[:, :, li * 128:(li + 1) * 128], [hTk[li]])
                p = PB[2 + li % 2]
                for k in range(KC):
                    self.mm(p[:, 0:36], hTf[:, k, :], wr[:, k, :], k == 0, k == KC - 1, [hTf, wr], [p])
                t = self.route_tile(sm, p)
                self.ts("dve", t["tmpw"][:], t["oh1"][:], t["w1"][:, 0:1], None, ALU.mult, None, [t["oh1"], t["w1"]], [t["tmpw"]])
                self.stt(Wt[:, li, :], t["oh2"][:], t["w2"][:, 0:1], t["tmpw"][:], ALU.mult, ALU.add, [t["oh2"], t["w2"], t["tmpw"]], [Wtk[li]])
            wg = self.sbr(st, 2, [128, KC, FF], BF16, "wg")
            wu = self.sbr(st, 2, [128, KC, FF], BF16, "wu")
            wd = self.sbr(st, 2, [128, 4, D], BF16, "wd")
            sg = self.sbr(st, 2, [128, 512], BF16, "sg")
            actT = self.sbr(st, 2, [128, 4, 512], BF16, "actT")
            pgu = Rot([(PB[0], PB[1]), (PB[2], PB[3])])
            pyr = Rot([(PB[4], PB[5]), (PB[6], PB[7])])
            blocks = []
            t0 = 0
            while t0 < NTOK:
                n = min(512, NTOK - t0)
                blocks.append((t0, n))
                t0 += n
            for e in range(NEXP):
                g_, u_, d_ = wg.next(), wu.next(), wd.next()
                self.dma("pool", g_[:], I["moe_w_gate"][l, e].rearrange("(c p) f -> p c f", p=128), [], [g_])
                self.dma("pool", u_[:], I["moe_w_up"][l, e].rearrange("(c p) f -> p c f", p=128), [], [u_])
                self.dma("pool", d_[:], I["moe_w_down"][l, e].rearrange("(c p) f -> p c f", p=128), [], [d_])
                for (t0, n) in blocks:
                    at = actT.next()
                    hk = [hTk[t] for t in range(t0 // 128, (t0 + n) // 128)]
                    for f in range(4):
                        pg_, pu_ = pgu.next()
                        fs = slice(f * 128, (f + 1) * 128)
                        for k in range(KC):
                            self.mm(pg_[:, 0:n], g_[:, k, fs], hT[:, k, t0:t0 + n], k == 0, k == KC - 1, [g_] + hk, [pg_])
                        for k in range(KC):
                            self.mm(pu_[:, 0:n], u_[:, k, fs], hT[:, k, t0:t0 + n], k == 0, k == KC - 1, [u_] + hk, [pu_])
                        s_ = sg.next()
                        self.act(s_[:, 0:n], pg_[:, 0:n], AF.Silu, [pg_], [s_])
                        self.tt("dve", at[:, f, 0:n], s_[:, 0:n], pu_[:, 0:n], ALU.mult, [s_, pu_], [at])
                    for tt_ in range(n // 128):
                        li = t0 // 128 + tt_
                        pa, pb = pyr.next()
                        for hf, p in ((0, pa), (1, pb)):
                            hs = slice(hf * 512, (hf + 1) * 512)
                            for f in range(4):
                                self.mm(p[:, :], at[:, f, tt_ * 128:(tt_ + 1) * 128], d_[:, f, hs], f == 0, f == 3, [at, d_], [p])
                            if e == 0:
                                self.ts("dve", acc[:, li, hs], p[:, :], Wt[:, li, e:e + 1], None, ALU.mult, None, [p, Wtk[li]], [acck[li]])
                            else:
                                self.stt(acc[:, li, hs], p[:, :], Wt[:, li, e:e + 1], acc[:, li, hs], ALU.mult, ALU.add,
                                         [p, Wtk[li], acck[li]], [acck[li]])
            for li, tile in enumerate(tiles):
                isctx = (l == 0 and tile < 2)
                gt = gt_c if isctx else gt_l
                x_ = NR["xt"].next()
                self.dma("sp", x_[:], self.xres[b, tile * 128:(tile + 1) * 128, :], [self.kx[b][tile]], [x_])
                t_ = NR["xn"].next()
                self.tt("dve", t_[:], acc[:, li, :], gt[:], ALU.mult, [acck[li], gt], [t_])
                self.tt("dve", t_[:], t_[:], x_[:], ALU.add, [t_, x_], [t_])
                if l == 0:
                    self.dma("sp", self.xres[b, tile * 128:(tile + 1) * 128, :], t_[:], [t_], [self.kx[b][tile]], semkey=t_)
                    if ("xf0" in self.D_) and b == 0:
                        self.dma("sp", self.D_["xf0"][tile * 128:(tile + 1) * 128, :], t_[:], [t_], [self.kscr], semkey=t_)
                else:
                    self.dma("sp", self.out[b, (tile - 2) * 128:(tile - 1) * 128, :], t_[:], [t_], [], semkey=t_)
            return S.flush()

    def rope(self, xin, xout, cos, sin, H, tm, R, W):
        x1, x2 = xin[:, :, :, 0, :], xin[:, :, :, 1, :]
        cb = cos.unsqueeze(1).broadcast_to([128, H, 2, 8])
        sb_ = sin.unsqueeze(1).broadcast_to([128, H, 2, 8])
        t1, t2 = tm
        v1 = t1[:, 0:H * 16].rearrange("p (h a f) -> p h a f", h=H, a=2)
        v2 = t2[:, 0:H * 16].rearrange("p (h a f) -> p h a f", h=H, a=2)
        self.tt("dve", v1, x1, cb, ALU.mult, R, [t1])
        self.tt("dve", v2, x2, sb_, ALU.mult, R, [t2])
        self.tt("dve", xout[:, :, :, 0, :], v1, v2, ALU.subtract, [t1, t2], W)
        self.tt("dve", v1, x2, cb, ALU.mult, R, [t1])
        self.tt("dve", v2, x1, sb_, ALU.mult, R, [t2])
        self.tt("dve", xout[:, :, :, 1, :], v1, v2, ALU.add, [t1, t2], W)

    def stage_mla(self, b):
        I, S = self.I, self.S
        l = 1
        NQT = SEQ // 128
        with ExitStack() as st:
            PB = [self.psb(st) for _ in range(8)]
            if "moe1" in self.stages:
                self.precast(1, b)
            A_l, B_l = self.mod_cols(st, l, 0, b)
            A_c, B_c = self.mod_cols(st, l, 0, 4)
            gt_l = self.gate_tile(st, l, 0, b)
            NR = self.norm_res(st, (PB[0], PB[1]), nxn=2, nhtf=2)
            win = self.sb(st, [128, KC, 416], BF16, "win")
            self.dma("pool", win[:], I["mla_w_in"].rearrange("(c p) n -> p c n", p=128), [], [win])
            cT = self.sb(st, [128, 3, T], BF16, "cT")
            cTk = S.keys(NT)
            krr = self.sb(st, [128, NT, 32], F32, "krr")
            krk = S.keys(NT)
            sskr = self.sb(st, [128, NT], F32, "sskr")
            ssk = S.keys(NT)
            gk = self.sb(st, [128, 96], F32, "gk")
            gq = self.sb(st, [128, 96], F32, "gq")
            self.dma("sp", gk[:], I["mla_k_qknorm_g"].partition_broadcast(128), [], [gk])
            self.dma("sp", gq[:], I["mla_q_qknorm_g"].partition_broadcast(128), [], [gq])
            self.ts("dve", gq[:], gq[:], float(96 ** -0.5), None, ALU.mult, None, [gq], [gq])
            qng = self.sb(st, [128, 2], F32, "qng")
            kvg = self.sb(st, [128, 1], F32, "kvg")
            self.dma("sp", qng[:], I["mla_q_norm_g"].rearrange("(k p) -> p k", p=128), [], [qng])
            self.dma("sp", kvg[:], I["mla_kv_norm_g"].rearrange("(p o) -> p o", o=1), [], [kvg])
            hTt = self.sbr(st, 2, [128, KC, 128], BF16, "hTt")
            csr = self.sbr(st, 2, [128, 416], F32, "cs")
            cnr = self.sbr(st, 2, [128, 384], BF16, "cn")
            junk2 = self.sb(st, [128, 256], BF16, "junk2")
            s1 = {nm: self.sbr(st, 2, [128, 1], F32, nm) for nm in ("ssq", "sskv", "sdq", "sdkv", "rsq", "rskv")}
            kr1 = self.sbr(st, 2, [128, 32], F32, "kr1")
            cosr = self.sbr(st, 2, [128, 16], F32, "cos")
            sinr = self.sbr(st, 2, [128, 16], F32, "sin")
            rt = (self.sb(st, [128, 64], F32, "rt1"), self.sb(st, [128, 64], F32, "rt2"))
            cost = {}
            for tile in range(NT):
                A, Bc = (A_c, B_c) if tile < 2 else (A_l, B_l)
                hb = hTt.next()
                self.norm_tile(NR, self.xres[b, tile * 128:(tile + 1) * 128, :], [self.kx[b][tile]], A, Bc, hb[:], [hb])
                p = PB[2 + tile % 2]
                for k in range(KC):
                    self.mm(p[:, 0:416], hb[:, k, :], win[:, k, :], k == 0, k == KC - 1, [hb, win], [p])
                cs = csr.next()
                self.cp("dve", cs[:], p[:, 0:416], [p], [cs])
                t = {nm: r.next() for nm, r in s1.items()}
                self.act(junk2[:, 0:256], cs[:, 0:256], AF.Square, [cs], [junk2, t["ssq"]], accum_out=t["ssq"][:])
                self.act(junk2[:, 0:128], cs[:, 256:384], AF.Square, [cs], [junk2, t["sskv"]], accum_out=t["sskv"][:])
                self.act(junk2[:, 0:32], cs[:, 384:416], AF.Square, [cs], [junk2, ssk[tile]], accum_out=sskr[:, tile:tile + 1])
                self.act(t["sdq"][:], t["ssq"][:], AF.Sqrt, [t["ssq"], self.epsc], [t["sdq"]], bias=self.epsc[:, 0:1], scale=1.0 / 256)
                self.act(t["sdkv"][:], t["sskv"][:], AF.Sqrt, [t["sskv"], self.epsc], [t["sdkv"]], bias=self.epsc[:, 0:1], scale=1.0 / 128)
                self.recip(t["rsq"][:], t["sdq"][:], [t["sdq"]], [t["rsq"]])
                self.recip(t["rskv"][:], t["sdkv"][:], [t["sdkv"]], [t["rskv"]])
                cn = cnr.next()
                self.act(cn[:, 0:256], cs[:, 0:256], AF.Copy, [cs, t["rsq"]], [cn], scale=t["rsq"][:, 0:1])
                self.act(cn[:, 256:384], cs[:, 256:384], AF.Copy, [cs, t["rskv"]], [cn], scale=t["rskv"][:, 0:1])
                pT = PB[4 + tile % 2]
                pv = pT[:, 0:192].bitcast(BF16).rearrange("p (j t) -> p j t", j=3)
                for j in range(3):
                    self.tr(pv[:, j, :], cn[:, j * 128:(j + 1) * 128], self.identb[:], [cn, self.identb], [pT])
                self.cp("dve", cT[:, :, tile * 128:(tile + 1) * 128], pv, [pT], [cTk[tile]])
                k1 = kr1.next()
                self.tt("dve", k1[:], cs[:, 384:416], gk[:, 64:96], ALU.mult, [cs, gk], [k1])
                if tile < 2:
                    self.cp("dve", krr[:, tile, :], k1[:], [k1], [krk[tile]])
                else:
                    co, si = cosr.next(), sinr.next()
                    self.dma("sp", co[:], I["k_cos"][(tile - 2) * 128:(tile - 1) * 128, :], [], [co])
                    self.dma("sp", si[:], I["k_sin"][(tile - 2) * 128:(tile - 1) * 128, :], [], [si])
                    self.rope(k1[:].rearrange("p (h a g f) -> p h a g f", h=1, a=2, g=2),
                              krr[:, tile, :].rearrange("p (h a g f) -> p h a g f", h=1, a=2, g=2),
                              co[:].rearrange("p (a f) -> p a f", a=2), si[:].rearrange("p (a f) -> p a f", a=2),
                              1, rt, [k1, co, si], [krk[tile]])
            HG = 2
            NG = 16 // HG
            oat = self.sb(st, [128, NQT, D], BF16, "oat")
            oak = S.keys(NQT)
            QTs = [self.sb(st, [128, HG, SEQ], BF16, "QT") for _ in range(2)]
            QTks = [S.keys(NQT) for _ in range(2)]
            KTs = [self.sb(st, [128, HG, T], BF16, "KT") for _ in range(2)]
            KTks = [S.keys(NT) for _ in range(2)]
            Vxs = [self.sb(st, [128, NT, HG, 65], BF16, "Vx") for _ in range(2)]
            Vxks = [S.keys(NT) for _ in range(2)]
            for s_ in range(2):
                self.memset("dve", Vxs[s_][:], 1.0, Vxks[s_])
            wqfr = self.sbr(st, 2, [128, 2, HG * 96], F32, "wqf")
            wqbr = self.sbr(st, 2, [128, 2, HG * 96], BF16, "wqb")
            wkfr = self.sbr(st, 2, [128, HG * 128], F32, "wkf")
            wkbr = self.sbr(st, 2, [128, HG * 128], BF16, "wkb")
            kvfr = self.sbr(st, 2, [128, HG, 128], F32, "kvf")
            sqk = self.sb(st, [128, HG, 96], F32, "sqk")
            tmpk = self.sb(st, [128, HG, 96], F32, "tmpk")
            s4 = {nm: self.sbr(st, 2, [128, HG], F32, nm) for nm in ("ssn", "ss", "sd", "rs", "ssq4", "sd4", "rs4")}
            kbr = self.sbr(st, 2, [128, HG, 96], BF16, "kb")
            qfr = self.sbr(st, 2, [128, HG, 96], F32, "qf")
            qnr = self.sbr(st, 2, [128, HG, 96], F32, "qn")
            qbr = self.sbr(st, 2, [128, HG, 96], BF16, "qb")
            ptr_ = self.sbr(st, 3, [128, 512], BF16, "pt")
            recr = self.sbr(st, 4, [128, 1], F32, "rec")
            pA, pB_ = PB[2], PB[3]

            def rstd(out, ss, tmp, n):
                self.act(tmp[:], ss[:], AF.Ln, [ss, self.epsc], [tmp], bias=self.epsc[:, 0:1], scale=1.0 / n)
                self.act(out[:], tmp[:], AF.Exp, [tmp], [out], scale=-0.5)

            for hg in range(NG):
                s_ = hg % 2
                QT, QTk, KT, KTk, Vx, Vxk = QTs[s_], QTks[s_], KTs[s_], KTks[s_], Vxs[s_], Vxks[s_]
                wqf, wqb, wkf, wkb = wqfr.next(), wqbr.next(), wkfr.next(), wkbr.next()
                self.dma("sp", wqf[:], I["mla_w_qb"][:, hg * HG * 96:(hg + 1) * HG * 96].rearrange("(k p) n -> p k n", p=128), [], [wqf])
                self.tt("dve", wqb[:], wqf[:], qng[:].unsqueeze(2).broadcast_to([128, 2, HG * 96]), ALU.mult, [wqf, qng], [wqb])
                self.dma("sp", wkf[:], I["mla_w_kvb"][:, hg * HG * 128:(hg + 1) * HG * 128], [], [wkf])
                self.ts("dve", wkb[:], wkf[:], kvg[:, 0:1], None, ALU.mult, None, [wkf, kvg], [wkb])
                for tile in range(NT):
                    ts_ = slice(tile * 128, (tile + 1) * 128)
                    self.mm(pA[:, 0:HG * 128], cT[:, 2, ts_], wkb[:], True, True, [cTk[tile], wkb], [pA])
                    kvf = kvfr.next()
                    self.cp("dve", kvf[:], pA[:, 0:HG * 128].rearrange("p (h e) -> p h e", h=HG), [pA], [kvf])
                    t = {nm: r.next() for nm, r in s4.items()}
                    self.tt("dve", sqk[:, :, 0:64], kvf[:, :, 0:64], kvf[:, :, 0:64], ALU.mult, [kvf], [sqk])
                    self.red(t["ssn"][:], sqk[:, :, 0:64], ALU.add, [sqk], [t["ssn"]])
                    self.ts("dve", t["ss"][:], t["ssn"][:], sskr[:, tile:tile + 1], None, ALU.add, None, [t["ssn"], ssk[tile]], [t["ss"]])
                    rstd(t["rs"], t["ss"], t["sd"], 96)
                    kb = kbr.next()
                    self.tt("dve", tmpk[:, :, 0:64], kvf[:, :, 0:64], t["rs"][:].unsqueeze(2).broadcast_to([128, HG, 64]), ALU.mult,
                            [kvf, t["rs"]], [tmpk])
                    self.tt("dve", kb[:, :, 0:64], tmpk[:, :, 0:64], gk[:, 0:64].unsqueeze(1).broadcast_to([128, HG, 64]), ALU.mult,
                            [tmpk, gk], [kb])
                    self.tt("dve", kb[:, :, 64:96], krr[:, tile, :].unsqueeze(1).broadcast_to([128, HG, 32]),
                            t["rs"][:].unsqueeze(2).broadcast_to([128, HG, 32]), ALU.mult, [krk[tile], t["rs"]], [kb])
                    pkv = pB_[:, 0:HG * 64].bitcast(BF16).rearrange("p (h t) -> p h t", h=HG)
                    for h in range(HG):
                        self.tr(pkv[0:96, h, :], kb[:, h, :], self.identb[:], [kb, self.identb], [pB_])
                    self.cp("dve", KT[0:96, :, ts_], pkv[0:96, :, :], [pB_], [KTk[tile]])
                    self.cp("dve", Vx[:, tile, :, 0:64], kvf[:, :, 64:128], [kvf], [Vxk[tile]])
                    if tile >= 2:
                        qt_ = tile - 2
                        for k in range(2):
                            self.mm(pA[:, 0:HG * 96], cT[:, k, ts_], wqb[:, k, :], k == 0, k == 1, [cTk[tile], wqb], [pA])
                        qf = qfr.next()
                        self.cp("dve", qf[:], pA[:, 0:HG * 96].rearrange("p (h e) -> p h e", h=HG), [pA], [qf])
                        self.tt("dve", sqk[:], qf[:], qf[:], ALU.mult, [qf], [sqk])
                        self.red(t["ssq4"][:], sqk[:], ALU.add, [sqk], [t["ssq4"]])
                        rstd(t["rs4"], t["ssq4"], t["sd4"], 96)
                        qn = qnr.next()
                        self.tt("dve", qn[:], qf[:], t["rs4"][:].unsqueeze(2).broadcast_to([128, HG, 96]), ALU.mult, [qf, t["rs4"]], [qn])
                        self.tt("dve", qn[:], qn[:], gq[:].unsqueeze(1).broadcast_to([128, HG, 96]), ALU.mult, [qn, gq], [qn])
                        qb = qbr.next()
                        self.cp("dve", qb[:, :, 0:64], qn[:, :, 0:64], [qn], [qb])
                        co, si = cosr.next(), sinr.next()
                        self.dma("sp", co[:], I["k_cos"][qt_ * 128:(qt_ + 1) * 128, :], [], [co])
                        self.dma("sp", si[:], I["k_sin"][qt_ * 128:(qt_ + 1) * 128, :], [], [si])
                        self.rope(qn[:, :, 64:96].rearrange("p h (a g f) -> p h a g f", a=2, g=2),
                                  qb[:, :, 64:96].rearrange("p h (a g f) -> p h a g f", a=2, g=2),
                                  co[:].rearrange("p (a f) -> p a f", a=2), si[:].rearrange("p (a f) -> p a f", a=2),
                                  HG, rt, [qn, co, si], [qb])
                        pqv = pB_[:, 0:HG * 64].bitcast(BF16).rearrange("p (h t) -> p h t", h=HG)
                        for h in range(HG):
                            self.tr(pqv[0:96, h, :], qb[:, h, :], self.identb[:], [qb, self.identb], [pB_])
                        self.cp("dve", QT[0:96, :, qt_ * 128:(qt_ + 1) * 128], pqv[0:96, :, :], [pB_], [QTk[qt_]])
                for h in range(HG):
                    hh = hg * HG + h
                    for qb_ in range(SEQ // 512):
                        po = PB[4:8]
                        qk = [QTk[qb_ * 4 + i] for i in range(4)]
                        for kt in range(NT):
                            ps_ = PB[kt % 2]
                            self.mm(ps_[:, :], KT[0:96, h, kt * 128:(kt + 1) * 128], QT[0:96, h, qb_ * 512:(qb_ + 1) * 512], True, True,
                                    [KTk[kt]] + qk, [ps_])
                            pt = ptr_.next()
                            self.act(pt[:], ps_[:, :], AF.Exp, [ps_], [pt])
                            for q4 in range(4):
                                self.mm(po[q4][:, 0:65], pt[:, q4 * 128:(q4 + 1) * 128], Vx[:, kt, h, :], kt == 0, kt == NT - 1,
                                        [pt, Vxk[kt]], [po[q4]])
                        for q4 in range(4):
                            rec = recr.next()
                            self.recip(rec[:], po[q4][:, 64:65], [po[q4]], [rec])
                            self.ts("dve", oat[:, qb_ * 4 + q4, hh * 64:(hh + 1) * 64], po[q4][:, 0:64], rec[:, 0:1], None, ALU.mult, None,
                                    [po[q4], rec], [oak[qb_ * 4 + q4]])
            wo = self.sb(st, [128, KC, D], BF16, "wo")
            self.dma("pool", wo[:], I["mla_w_out"].rearrange("(c p) n -> p c n", p=128), [], [wo])
            oTr = self.sbr(st, 2, [128, KC, 128], BF16, "oT")
            for qt_ in range(NQT):
                tile = qt_ + 2
                pT = PB[qt_ % 2]
                pv = pT[:, :].bitcast(BF16).rearrange("p (k t) -> p k t", k=KC)
                for k in range(KC):
                    self.tr(pv[:, k, :], oat[:, qt_, k * 128:(k + 1) * 128], self.identb[:], [oak[qt_], self.identb], [pT])
                oT = oTr.next()
                self.cp("act", oT[:], pv, [pT], [oT])
                x_ = NR["xt"].next()
                self.dma("sp", x_[:], self.xres[b, tile * 128:(tile + 1) * 128, :], [self.kx[b][tile]], [x_])
                t_ = NR["xn"].next()
                for hf in range(2):
                    p = PB[2 + hf]
                    hs = slice(hf * 512, (hf + 1) * 512)
                    for k in range(KC):
                        self.mm(p[:, :], oT[:, k, :], wo[:, k, hs], k == 0, k == KC - 1, [oT, wo], [p])
                    self.tt("dve", t_[:, hs], p[:, :], gt_l[:, hs], ALU.mult, [p, gt_l], [t_])
                self.tt("dve", t_[:], t_[:], x_[:], ALU.add, [t_, x_], [t_])
                self.dma("sp", self.xres[b, tile * 128:(tile + 1) * 128, :], t_[:], [t_], [self.kx[b][tile]], semkey=t_)
                if ("xm1" in self.D_) and b == 0:
                    self.dma("sp", self.D_["xm1"][qt_ * 128:(qt_ + 1) * 128, :], t_[:], [t_], [self.kscr], semkey=t_)
            return S.flush()

    def moe_sparse(self, l):
        I, S, NB = self.I, self.S, self.NB
        tiles = [(b, t) for b in range(NB) for t in (range(NT) if l == 0 else range(2, NT))]
        NTL = len(tiles)
        NBLK = 2 * NTL + NEXP
        info = {}
        with ExitStack() as pst:
            d_i = [self.sb(pst, [128, NTL], I32, "d%di" % k) for k in range(2)]
            w_a = [self.sb(pst, [128, NTL], F32, "w%da" % k) for k in range(2)]
            idxw = self.sb(pst, [128, NBLK], I32, "idxw")
            kh2 = S.keys(NTL)
            dik = [S.keys(NTL) for _ in range(2)]
            kxs, kys = S.key(), S.key()
            kwb = self.kwb[l]
            with ExitStack() as st:
                PB = [self.psb(st) for _ in range(8)]
                Ltri = self.sb(st, [128, 128], BF16, "Ltri")
                onesf = self.sb(st, [128, 128], BF16, "onesf")
                self.memset("dve", onesf[:], 1.0, [onesf])
                self.memset("pool", Ltri[:], 1.0, [Ltri])
                S.op("pool", lambda e_: e_.affine_select(out=Ltri[:], in_=Ltri[:], pattern=[[1, 128]], compare_op=ALU.is_gt,
                                                         fill=0.0, base=0, channel_multiplier=-1), [Ltri], [Ltri])
                jvi = self.sb(st, [128, NBLK], I32, "jvi")
                jv = self.sb(st, [128, NBLK], F32, "jv")
                S.op("pool", lambda e_: e_.iota(jvi[:], pattern=[[128, NBLK]], base=0, channel_multiplier=0), [], [jvi])
                self.cp("dve", jv[:], jvi[:], [jvi], [jv])
                pii = self.sb(st, [128, 1], I32, "pii")
                pif = self.sb(st, [128, 1], F32, "pif")
                S.op("pool", lambda e_: e_.iota(pii[:], pattern=[[0, 1]], base=0, channel_multiplier=1), [], [pii])
                self.cp("dve", pif[:], pii[:], [pii], [pif])
                ones32 = self.sb(st, [128, NEXP], F32, "ones32")
                self.memset("dve", ones32[:], 1.0, [ones32])
                wr = self.sb(st, [128, KC, 36], F32, "wr")
                self.dma("sp", wr[:, :, 0:4], I["moe_w_group"][l].rearrange("(c p) g -> p c g", p=128), [], [wr])
                self.dma("sp", wr[:, :, 4:36], I["moe_w_expert"][l].rearrange("(c p) g -> p c g", p=128), [], [wr])
                grow = self.sb(st, [128, D], F32, "grow")
                self.dma("sp", grow[:], I["norm_ffn_g"][l, :].partition_broadcast(128), [], [grow])

                def rows_for(r):
                    Ar = self.sb(st, [128, D], F32, "Arow")
                    Br = self.sb(st, [128, D], F32, "Brow")
                    return Ar, Br

                def load_rows(Ar, Br, r):
                    self.dma("sp", Ar[:], self.mod[l, r, 4 * D:5 * D].partition_broadcast(128), [self.kmod], [Ar])
                    self.dma("sp", Br[:], self.mod[l, r, 3 * D:4 * D].partition_broadcast(128), [self.kmod], [Br])
                    self.stt(Ar[:], Ar[:], 1.0, grow[:], ALU.add, ALU.mult, [Ar, grow], [Ar])

                Ar_l, Br_l = rows_for(0)
                if l == 0:
                    Ar_c, Br_c = rows_for(4)
                    load_rows(Ar_c, Br_c, 4)
                    A_c, B_c = self.mod_cols(st, l, 1, 4)
                NR = self.norm_res(st, (PB[0], PB[1]), nhtf=3, nxn=3, nxt=3)
                sm = {nm: self.sbr(st, 3, [128, w], F32, nm) for nm, w in self.ROUTE_TMPS}
                lTr = self.sbr(st, 2, [36, 128], F32, "lT")
                OHs = self.sb(st, [128, NEXP], BF16, "OHs")
                self.memset("dve", OHs[:], 0.0, [OHs])
                Rall = self.sb(st, [128, NTL, NEXP], F32, "Rall")
                Rk = S.keys(NTL)
                oha = [self.sb(st, [128, NTL, NEXP], F32, "oh%da" % k) for k in range(2)]
                ohk = [S.keys(NTL) for _ in range(2)]
                t32r = self.sbr(st, 2, [128, D], F32, "t32")
                h2r = self.sbr(st, 3, [128, D], BF16, "h2b")
                G = 4
                gt_ = {}
                for nm, w in (("lg", 36), ("t4", 4), ("ge", 4), ("gone", 4), ("pen", 4), ("em", 32), ("oh1", 32), ("em2", 32), ("oh2", 32)):
                    gt_[nm] = self.sbr(st, 2, [128, G, w], F32, "g" + nm)
                for nm in ("gmax", "gsum", "pg", "m1", "m2", "dm", "e2", "den", "rden", "w1", "w2"):
                    gt_[nm] = self.sbr(st, 2, [128, G], F32, "g" + nm)
                OHg = self.sbr(st, 2, [128, G, NEXP], BF16, "OHg")
                ohsum = self.sbr(st, 2, [128, NEXP], F32, "ohsum")
                cur_b = None
                cols = {}
                groups = [list(range(i, min(i + G, NTL))) for i in range(0, NTL, G)]
                for gi, grp in enumerate(groups):
                    g = len(grp)
                    ti0 = grp[0]
                    p = PB[2 + gi % 2]
                    for i, ti in enumerate(grp):
                        b, tile = tiles[ti]
                        if b != cur_b:
                            cur_b = b
                            load_rows(Ar_l, Br_l, b)
                            cols[b] = self.mod_cols(st, l, 1, b)
                        isctx = (l == 0 and tile < 2)
                        A, Bc = (A_c, B_c) if isctx else cols[b]
                        Ar, Br = (Ar_c, Br_c) if isctx else (Ar_l, Br_l)
                        hTf = self.norm_tile(NR, self.xres[b, tile * 128:(tile + 1) * 128, :], [self.kx[b][tile]], A, Bc, None, [])
                        xn = self.last_xn
                        t32 = t32r.next()
                        self.tt("dve", t32[:], xn[:], Ar[:], ALU.mult, [xn, Ar], [t32])
                        h2b = h2r.next()
                        self.tt("dve", h2b[:], t32[:], Br[:], ALU.add, [t32, Br], [h2b])
                        self.dma("sp", self.h2d[ti * 128:(ti + 1) * 128, :], h2b[:], [h2b], [kh2[ti]], semkey=h2b)
                        pl = PB[6 + ti % 2]
                        for k in range(KC):
                            self.mm(pl[0:36, 0:128], wr[:, k, :], hTf[:, k, :], k == 0, k == KC - 1, [hTf, wr], [pl])
                        lT = lTr.next()
                        self.cp("act", lT[:], pl[0:36, 0:128], [pl], [lT])
                        self.tr(p[:, i * 36:(i + 1) * 36], lT[:], self.identf[0:36, 0:36], [lT, self.identf], [p])
                    t = {nm: r.next() for nm, r in gt_.items()}
                    v3 = lambda x, w: x[:, 0:g, 0:w]
                    lg = t["lg"]
                    self.cp("act", lg[:, 0:g, :], p[:, 0:g * 36].rearrange("p (g e) -> p g e", g=g), [p], [lg])
                    lgg, lge = lg[:, 0:g, 0:4], lg[:, 0:g, 4:36]
                    bc = lambda x, w: x[:, 0:g].unsqueeze(2).broadcast_to([128, g, w])
                    self.red(t["gmax"][:, 0:g], lgg, ALU.max, [lg], [t["gmax"]])
                    self.tt("dve", v3(t["t4"], 4), lgg, bc(t["gmax"], 4), ALU.subtract, [lg, t["gmax"]], [t["t4"]])
                    self.act(v3(t["ge"], 4), v3(t["t4"], 4), AF.Exp, [t["t4"]], [t["ge"]])
                    self.red(t["gsum"][:, 0:g], v3(t["ge"], 4), ALU.add, [t["ge"]], [t["gsum"]])
                    self.recip(t["pg"][:, 0:g], t["gsum"][:, 0:g], [t["gsum"]], [t["pg"]])
                    self.tt("dve", v3(t["gone"], 4), lgg, bc(t["gmax"], 4), ALU.is_ge, [lg, t["gmax"]], [t["gone"]])
                    self.ts("dve", v3(t["pen"], 4), v3(t["gone"], 4), BIG, -BIG, ALU.mult, ALU.add, [t["gone"]], [t["pen"]])
                    self.tt("dve", v3(t["em"], 32).rearrange("p g (a j) -> p g a j", a=4), lge.rearrange("p g (a j) -> p g a j", a=4),
                            v3(t["pen"], 4).unsqueeze(3).broadcast_to([128, g, 4, 8]), ALU.add, [lg, t["pen"]], [t["em"]])
                    self.red(t["m1"][:, 0:g], v3(t["em"], 32), ALU.max, [t["em"]], [t["m1"]])
                    self.tt("dve", v3(t["oh1"], 32), v3(t["em"], 32), bc(t["m1"], 32), ALU.is_ge, [t["em"], t["m1"]], [t["oh1"]])
                    self.stt(t["em2"][:, 0:g, :].rearrange("p g e -> p (g e)"), t["oh1"][:, 0:g, :].rearrange("p g e -> p (g e)"), -BIG,
                             t["em"][:, 0:g, :].rearrange("p g e -> p (g e)"), ALU.mult, ALU.add, [t["oh1"], t["em"]], [t["em2"]])
                    self.red(t["m2"][:, 0:g], v3(t["em2"], 32), ALU.max, [t["em2"]], [t["m2"]])
                    self.tt("dve", v3(t["oh2"], 32), v3(t["em2"], 32), bc(t["m2"], 32), ALU.is_ge, [t["em2"], t["m2"]], [t["oh2"]])
                    self.tt("dve", t["dm"][:, 0:g], t["m2"][:, 0:g], t["m1"][:, 0:g], ALU.subtract, [t["m2"], t["m1"]], [t["dm"]])
                    self.act(t["e2"][:, 0:g], t["dm"][:, 0:g], AF.Exp, [t["dm"]], [t["e2"]])
                    self.ts("dve", t["den"][:, 0:g], t["e2"][:, 0:g], 1.0, None, ALU.add, None, [t["e2"]], [t["den"]])
                    self.recip(t["rden"][:, 0:g], t["den"][:, 0:g], [t["den"]], [t["rden"]])
                    self.tt("dve", t["w1"][:, 0:g], t["pg"][:, 0:g], t["rden"][:, 0:g], ALU.mult, [t["pg"], t["rden"]], [t["w1"]])
                    self.tt("dve", t["w2"][:, 0:g], t["w1"][:, 0:g], t["e2"][:, 0:g], ALU.mult, [t["w1"], t["e2"]], [t["w2"]])
                    gk = lambda ks: [ks[ti] for ti in grp]
                    self.cp("dve", oha[0][:, ti0:ti0 + g, :], v3(t["oh1"], 32), [t["oh1"]], gk(ohk[0]))
                    self.cp("dve", oha[1][:, ti0:ti0 + g, :], v3(t["oh2"], 32), [t["oh2"]], gk(ohk[1]))
                    self.cp("dve", w_a[0][:, ti0:ti0 + g], t["w1"][:, 0:g], [t["w1"]], [w_a[0]])
                    self.cp("dve", w_a[1][:, ti0:ti0 + g], t["w2"][:, 0:g], [t["w2"]], [w_a[1]])
                    oh = OHg.next()
                    self.tt("dve", oh[:, 0:g, :], v3(t["oh1"], 32), v3(t["oh2"], 32), ALU.add, [t["oh1"], t["oh2"]], [oh])
                    pr = PB[4 + gi % 2]
                    for i in range(g):
                        cs_ = slice(i * NEXP, (i + 1) * NEXP)
                        self.mm(pr[:, cs_], Ltri[:], oh[:, i, :], True, False, [Ltri, oh], [pr])
                        for i2 in range(i):
                            self.mm(pr[:, cs_], onesf[:], oh[:, i2, :], False, False, [onesf, oh], [pr])
                        self.mm(pr[:, cs_], onesf[:], OHs[:], False, True, [onesf, OHs], [pr])
                    self.cp("act", Rall[:, ti0:ti0 + g, :], pr[:, 0:g * NEXP].rearrange("p (g e) -> p g e", g=g), [pr], gk(Rk))
                    osum = ohsum.next()
                    self.red(osum[:], oh[:, 0:g, :].rearrange("p g e -> p e g"), ALU.add, [oh], [osum])
                    self.tt("dve", OHs[:], OHs[:], osum[:], ALU.add, [OHs, osum], [OHs])
                pc = PB[5]
                self.mm(pc[:, 0:NEXP], onesf[:], OHs[:], True, True, [onesf, OHs], [pc])
                cntf = self.sb(st, [128, NEXP], F32, "cntf")
                padf = self.sb(st, [128, NEXP], F32, "padf")
                pend = self.sb(st, [128, NEXP], F32, "pend")
                pstart = self.sb(st, [128, NEXP], F32, "pstart")
                cmpb = self.sb(st, [128, NBLK * NEXP], BF16, "cmpb")
                self.cp("dve", cntf[:], pc[:, 0:NEXP], [pc], [cntf])
                cv = cmpb[:].rearrange("p (e j) -> p e j", e=NEXP)
                self.tt("dve", cv, jv[:].unsqueeze(1).broadcast_to([128, NEXP, NBLK]),
                        cntf[:].unsqueeze(2).broadcast_to([128, NEXP, NBLK]), ALU.is_lt, [jv, cntf], [cmpb])
                self.red(padf[:], cv, ALU.add, [cmpb], [padf])
                self.ts("dve", padf[:], padf[:], 128.0, None, ALU.mult, None, [padf], [padf])
                S.op("dve", lambda e_: e_.tensor_tensor_scan(out=pend[:], data0=ones32[:], data1=padf[:], initial=0.0,
                                                             op0=ALU.mult, op1=ALU.add), [ones32, padf], [pend])
                self.tt("dve", pstart[:], pend[:], padf[:], ALU.subtract, [pend, padf], [pstart])
                bef = self.sb(st, [128, NBLK], F32, "bef")
                cv2 = cmpb[:].rearrange("p (j e) -> p j e", e=NEXP)
                self.tt("dve", cv2, pend[:].unsqueeze(1).broadcast_to([128, NBLK, NEXP]),
                        jv[:].unsqueeze(2).broadcast_to([128, NBLK, NEXP]), ALU.is_le, [pend, jv], [cmpb])
                self.red(bef[:], cv2, ALU.add, [cmpb], [bef])
                self.ts("dve", bef[:], bef[:], float(NEXP - 1), None, ALU.min, None, [bef], [bef])
                same2 = self.sb(st, [128, NBLK], F32, "same2")
                self.memset("dve", same2[:], 0.0, [same2])
                self.tt("dve", same2[:, 2:NBLK], bef[:, 2:NBLK], bef[:, 0:NBLK - 2], ALU.is_equal, [bef], [same2])
                self.ts("dve", bef[:], bef[:], 128.0, pif[:, 0:1], ALU.mult, ALU.add, [bef, pif], [bef])
                self.stt(bef[:], same2[:], 1.0e6, bef[:], ALU.mult, ALU.add, [same2, bef], [bef])
                self.cp("dve", idxw[:], bef[:], [bef], [idxw])
                sck = S.keys(4)
                dtmp = self.sbr(st, 3, [128, NEXP], F32, "dtmp")
                dtm2 = self.sbr(st, 4, [128, NEXP], F32, "dtm2")
                dfl = self.sbr(st, 4, [128, 1], F32, "dfl")
                for ti in range(NTL):
                    h2b = h2r.next()
                    self.dma("sp", h2b[:], self.h2d[ti * 128:(ti + 1) * 128, :], [kh2[ti]], [h2b])
                    d1 = dtmp.next()
                    self.tt("dve", d1[:], Rall[:, ti, :], pstart[:], ALU.add, [Rk[ti], pstart], [d1])
                    for k in range(2):
                        d2, df = dtm2.next(), dfl.next()
                        self.tt("dve", d2[:], d1[:], oha[k][:, ti, :], ALU.mult, [d1, ohk[k][ti]], [d2])
                        self.red(df[:], d2[:], ALU.add, [d2], [df])
                        self.cp("dve", d_i[k][:, ti:ti + 1], df[:], [df], [dik[k][ti]])
                        idx_ap = d_i[k][:, ti:ti + 1]
                        self._scatter(self.xs[:, :], idx_ap, h2b[:], [h2b, dik[k][ti]], [sck[(2 * ti + k) % 4]])
                info["rs"] = S.flush()
            with ExitStack() as st:
                PB = [self.psb(st) for _ in range(8)]
                xbr = self.sbr(st, 2, [128, D], BF16, "xb")
                xTr = self.sbr(st, 2, [128, KC, 128], BF16, "xT")
                wgr = self.sbr(st, 2, [128, 4096], BF16, "wgs")
                wur = self.sbr(st, 2, [128, 4096], BF16, "wus")
                wdr = self.sbr(st, 2, [128, 4096], BF16, "wds")
                sgr = self.sbr(st, 2, [128, 512], BF16, "sgs")
                acr = self.sbr(st, 2, [128, 4, 128], BF16, "acs")
                ysr = self.sbr(st, 2, [128, D], F32, "ysb")
                pgu = Rot([(PB[2], PB[3]), (PB[4], PB[5])])
                breg = {"v": NEXP * 128 - 1}
                for j in range(NBLK):
                    xb = xbr.next()
                    self.dma("sp", xb[:], self.xs[j * 128:(j + 1) * 128, :], [kxs], [xb])
                    wg, wu, wd = wgr.next(), wur.next(), wdr.next()
                    ia = idxw[:, j:j + 1]
                    self._gather(wg[:], self.wgb[l][:, :], ia, [idxw, kwb], [wg], bounds=breg)
                    self._gather(wu[:], self.wub[l][:, :], ia, [idxw, kwb], [wu], bounds=breg)
                    self._gather(wd[:], self.wdb[l][:, :], ia, [idxw, kwb], [wd], bounds=breg)
                    pT = PB[j % 2]
                    pv = pT[:, :].bitcast(BF16).rearrange("p (k t) -> p k t", k=KC)
                    for k in range(KC):
                        self.tr(pv[:, k, :], xb[:, k * 128:(k + 1) * 128], self.identb[:], [xb, self.identb], [pT])
                    xT = xTr.next()
                    self.cp("act", xT[:], pv, [pT], [xT])
                    pg_, pu_ = pgu.next()
                    wgv = wg[:].rearrange("p (k f) -> p k f", k=KC)
                    wuv = wu[:].rearrange("p (k f) -> p k f", k=KC)
                    wdv = wd[:].rearrange("p (k f) -> p k f", k=4)
                    for fc in range(4):
                        fs = slice(fc * 128, (fc + 1) * 128)
                        for k in range(KC):
                            self.mm(pg_[:, fs], wgv[:, k, fs], xT[:, k, :], k == 0, k == KC - 1, [wg, xT], [pg_])
                    for fc in range(4):
                        fs = slice(fc * 128, (fc + 1) * 128)
                        for k in range(KC):
                            self.mm(pu_[:, fs], wuv[:, k, fs], xT[:, k, :], k == 0, k == KC - 1, [wu, xT], [pu_])
                    sg = sgr.next()
                    self.act(sg[:], pg_[:, :], AF.Silu, [pg_], [sg])
                    ac = acr.next()
                    self.tt("dve", ac[:].rearrange("p k t -> p (k t)"), sg[:], pu_[:, :], ALU.mult, [sg, pu_], [ac])
                    ysb = ysr.next()
                    for hf, p in ((0, PB[6]), (1, PB[7])):
                        hs = slice(hf * 512, (hf + 1) * 512)
                        for k in range(4):
                            self.mm(p[:, :], ac[:, k, :], wdv[:, k, hs], k == 0, k == 3, [ac, wd], [p])
                        if hf == 0:
                            self.cp("act", ysb[:, hs], p[:, :], [p], [ysb])
                        else:
                            self.cp("dve", ysb[:, hs], p[:, :], [p], [ysb])
                    self.dma("sp", self.ys[j * 128:(j + 1) * 128, :], ysb[:], [ysb], [], semkey=ysb)
                info["e"] = S.flush()
            with ExitStack() as st:
                gts = {}
                y1r = self.sbr(st, 3, [128, D], F32, "y1")
                y2r = self.sbr(st, 3, [128, D], F32, "y2")
                xr = self.sbr(st, 3, [128, D], F32, "xc")
                tr_ = self.sbr(st, 3, [128, D], F32, "tc")
                if l == 0:
                    gts[4] = self.gate_tile(st, l, 1, 4)
                for ti, (b, tile) in enumerate(tiles):
                    if b not in gts:
                        gts[b] = self.gate_tile(st, l, 1, b)
                    isctx = (l == 0 and tile < 2)
                    gt = gts[4] if isctx else gts[b]
                    y1, y2, x_, t_ = y1r.next(), y2r.next(), xr.next(), tr_.next()
                    self._gather(y1[:], self.ys[:, :], d_i[0][:, ti:ti + 1], [dik[0][ti], kys], [y1])
                    self._gather(y2[:], self.ys[:, :], d_i[1][:, ti:ti + 1], [dik[1][ti], kys], [y2])
                    self.dma("sp", x_[:], self.xres[b, tile * 128:(tile + 1) * 128, :], [self.kx[b][tile]], [x_])
                    self.ts("dve", t_[:], y1[:], w_a[0][:, ti:ti + 1], None, ALU.mult, None, [y1, w_a[0]], [t_])
                    self.stt(t_[:], y2[:], w_a[1][:, ti:ti + 1], t_[:], ALU.mult, ALU.add, [y2, w_a[1], t_], [t_])
                    self.tt("dve", t_[:], t_[:], gt[:], ALU.mult, [t_, gt], [t_])
                    self.tt("dve", t_[:], t_[:], x_[:], ALU.add, [t_, x_], [t_])
                    if l == 0:
                        self.dma("sp", self.xres[b, tile * 128:(tile + 1) * 128, :], t_[:], [t_], [self.kx[b][tile]], semkey=t_)
                        if ("xf0" in self.D_) and b == 0:
                            self.dma("sp", self.D_["xf0"][tile * 128:(tile + 1) * 128, :], t_[:], [t_], [self.kscr], semkey=t_)
                    else:
                        self.dma("sp", self.out[b, (tile - 2) * 128:(tile - 1) * 128, :], t_[:], [t_], [], semkey=t_)
                info["c"] = S.flush()
        return info

    def precast(self, l, b):
        I = self.I
        per = (NEXP + self.NB - 1) // self.NB
        if not hasattr(self, "pck"):
            self.pck = Rot(self.S.keys(4))
        for e in range(b * per, min(NEXP, (b + 1) * per)):
            rows = slice(e * 128, (e + 1) * 128)
            for dst, src, kk in ((self.wgb[l], "moe_w_gate", 8), (self.wub[l], "moe_w_up", 8), (self.wdb[l], "moe_w_down", 4)):
                self.dma("pool", dst[rows, :].rearrange("p (k f) -> p k f", k=kk),
                         I[src][l, e].rearrange("(k p) f -> p k f", p=128), [], [], semkey=self.pck.next())

    def _gather(self, out, src, idx_ap, R, W, bounds=None):
        def fn(e):
            if bounds is None:
                return e.indirect_dma_start(out=out, out_offset=None, in_=src,
                                            in_offset=bass.IndirectOffsetOnAxis(ap=idx_ap, axis=0))
            if "r" not in bounds:
                bounds["r"] = e.to_reg(bounds["v"])
            return e.indirect_dma_start(out=out, out_offset=None, in_=src,
                                        in_offset=bass.IndirectOffsetOnAxis(ap=idx_ap, axis=0),
                                        bounds_check=bounds["r"], oob_is_err=False)
        self.S.dma("pool", fn, R, W, nbytes=128 * self._n(out) * 2, indirect=True)

    def _scatter(self, dst, idx_ap, in_, R, W):
        nrow = dst.shape[0]
        self.S.dma("pool", lambda e: e.indirect_dma_start(out=dst, out_offset=bass.IndirectOffsetOnAxis(ap=idx_ap, axis=0),
                                                          in_=in_, in_offset=None),
                   R, W, semkey=R[0], nbytes=128 * 2048, indirect=True)

    def build(self):
        with ExitStack() as gst:
            gst.enter_context(self.nc.allow_non_contiguous_dma(reason="small strided parameter loads"))
            self.setup_consts(gst)
            info = {}
            if "mod" in self.stages:
                info["mod"] = self.stage_mod()
            for b in range(self.NB):
                if "hgrn" in self.stages:
                    info["hgrn%d" % b] = self.stage_hgrn(b)
            if "moe0" in self.stages:
                info["moe0"] = self.moe_sparse(0)
            for b in range(self.NB):
                if "mla" in self.stages:
                    info["mla%d" % b] = self.stage_mla(b)
            if "moe1" in self.stages:
                info["moe1"] = self.moe_sparse(1)
            self.info = info
        self.S.close()
        return self.nc


def host_consts():
    s = np.arange(128)
    same = (s[:, None] // 32) == (s[None, :] // 32)
    maskf = (same & (s[:, None] <= s[None, :])).astype(np.float32)
    maskb = (same & (s[:, None] >= s[None, :])).astype(np.float32)
    bm = ((s[:, None] // 32) == np.arange(4)[None, :]).astype(np.float32)[:, :, None].repeat(128, axis=2)
    t = np.arange(SEQ)
    row, col = t // 64, t % 64
    inv = (10000.0 ** (-np.arange(0, 16, 2, dtype=np.float32) / 16)).astype(np.float32)
    ang = np.stack([row, col], axis=-1).astype(np.float32)[..., None] * inv
    cos = np.cos(ang).astype(np.float32).reshape(SEQ, 16)
    sin = np.sin(ang).astype(np.float32).reshape(SEQ, 16)
    return {"k_maskf": maskf, "k_maskb": maskb, "k_bm": np.ascontiguousarray(bm), "k_bmc": np.ascontiguousarray(bm[:, :, 0]),
            "k_cos": cos, "k_sin": sin}


def make_in_maps(inputs, NB, ncores, used=None):
    sq = {"hg_w_in": "hg_w_in", "hg_lower_bounds": "hg_lb", "hg_out_norm_g": "hg_out_norm_g", "hg_w_out": "hg_w_out",
          "mla_w_in": "mla_w_in", "mla_q_norm_g": "mla_q_norm_g", "mla_kv_norm_g": "mla_kv_norm_g", "mla_w_qb": "mla_w_qb",
          "mla_w_kvb": "mla_w_kvb", "mla_q_qknorm_g": "mla_q_qknorm_g", "mla_k_qknorm_g": "mla_k_qknorm_g", "mla_w_out": "mla_w_out"}
    shared = {}
    for k, v in inputs.items():
        v = np.asarray(v, dtype=np.float32)
        if k in ("x", "c", "ctx"):
            continue
        if k == "hg_lower_bounds":
            shared["hg_lb"] = np.ascontiguousarray(v)
        elif k in sq:
            shared[sq[k]] = np.ascontiguousarray(v.reshape(v.shape[1:]))
        else:
            shared[k] = np.ascontiguousarray(v)
    shared.update(host_consts())
    maps = []
    for i in range(ncores):
        m = dict(shared)
        for k in ("x", "c", "ctx"):
            m[k] = np.ascontiguousarray(np.asarray(inputs[k], dtype=np.float32)[i * NB:(i + 1) * NB])
        if used is not None:
            m = {k: v for k, v in m.items() if k in used}
        maps.append(m)
    return maps


def kernel(**inputs):
    NB = 4
    kb = KB(NB=NB)
    nc = kb.build()
    maps = make_in_maps(inputs, NB, 8, used=set(kb.I.keys()))
    res = run_bass_kernel_spmd(nc, maps, core_ids=list(range(8)))
    return np.concatenate([r["out"] for r in res.results], axis=0).astype(np.float32)
```

```python
import numpy as np
import os as _os
_F = lambda k: _os.environ.get(k, '1') == '1'
import concourse.bass as bass
import concourse.mybir as mybir
from concourse.bass_utils import run_bass_kernel_spmd
from contextlib import ExitStack

F32 = mybir.dt.float32
BF16 = mybir.dt.bfloat16
I32 = mybir.dt.int32
AF = mybir.ActivationFunctionType
ALU = mybir.AluOpType
AX = mybir.AxisListType

ENGS = ("pe", "act", "dve", "pool", "sp")

D = 1024
KC = 8
CTX = 256
SEQ = 2048
T = CTX + SEQ
NT = T // 128
EPS = 1e-6
NEXP = 32
FF = 512
BIG = 1.0e30


class Key:
    __slots__ = ("w", "r", "dsem", "dcnt", "excl")

    def __init__(self):
        self.w = None
        self.r = []
        self.dsem = None
        self.dcnt = 0
        self.excl = False


class Tl:
    __slots__ = ("t", "k")

    def __init__(self, t, k):
        self.t = t
        self.k = k

    def __getitem__(self, idx):
        return self.t[idx]


def _k(x):
    return x.k if isinstance(x, Tl) else x


class Rot:
    def __init__(self, items):
        self.items = items
        self.i = 0

    def next(self):
        it = self.items[self.i % len(self.items)]
        self.i += 1
        return it


class Sched:
    def __init__(self, nc, n_dma_sems=80):
        self.nc = nc
        self.stack = ExitStack()
        self.esem = {e: self.stack.enter_context(nc.semaphore("es_" + e)) for e in ENGS}
        self.ecnt = {e: 0 for e in ENGS}
        self.dpool = [[self.stack.enter_context(nc.semaphore("ds%d" % i)), 0] for i in range(n_dma_sems)]
        self.dfree = list(range(n_dma_sems))
        self.all_keys = []
        self.reorder = True
        self._reset_stage()

    def _reset_stage(self):
        self.recs = []
        self.dlast = {}

    def key(self):
        k = Key()
        self.all_keys.append(k)
        return k

    def keys(self, n):
        return [self.key() for _ in range(n)]

    def _deps(self, reads, writes):
        deps = set()
        for t in reads:
            if t.w is not None:
                deps.add(t.w)
        for t in writes:
            if t.w is not None:
                deps.add(t.w)
            deps.update(t.r)
        return deps

    def _add(self, eng, fn, reads, writes, cost, dma, lat):
        reads = [_k(x) for x in reads]
        writes = [_k(x) for x in writes]
        ex = [t for t in reads if t.excl and t not in writes]
        if ex:
            reads = [t for t in reads if not t.excl]
            writes = writes + ex
        deps = self._deps(reads, writes)
        i = len(self.recs)
        if dma is not None:
            prev = self.dlast.get(dma)
            if prev is not None:
                deps.add(prev)
            self.dlast[dma] = i
        self.recs.append({"eng": eng, "fn": fn, "deps": deps, "cost": cost, "dma": dma, "lat": lat, "inc": False})
        for t in reads:
            t.r.append(i)
        for t in writes:
            t.w = i
            t.r = []
        return i

    def op(self, eng, fn, reads=(), writes=(), cost=0.2):
        return self._add(eng, fn, reads, writes, cost, None, 0.0)

    def dma(self, eng, fn, reads=(), writes=(), semkey=None, nbytes=0, indirect=False):
        rk = [_k(x) for x in reads]
        wk = [_k(x) for x in writes]
        sk = _k(semkey) if semkey is not None else (wk[0] if wk else rk[0])
        if sk.dsem is None:
            sk.dsem = self.dfree.pop()
        i = self._add(eng, fn, rk, wk, 0.8 if indirect else 0.07, sk.dsem, 2.0 + nbytes / 150e3)
        return i

    def _schedule(self):
        recs = self.recs
        n = len(recs)
        users = [[] for _ in range(n)]
        ndep = [0] * n
        for i, r in enumerate(recs):
            ndep[i] = len(r["deps"])
            for d in r["deps"]:
                users[d].append(i)
        import heapq
        ready = {e: [] for e in ENGS}
        fin = [0.0] * n
        rt = [0.0] * n
        for i, r in enumerate(recs):
            if ndep[i] == 0:
                heapq.heappush(ready[r["eng"]], (0.0, i))
        free = {e: 0.0 for e in ENGS}
        order = {e: [] for e in ENGS}
        done = 0
        while done < n:
            best = None
            for e in ENGS:
                h = ready[e]
                if not h:
                    continue
                t0 = free[e]
                cand = None
                if h[0][0] <= t0:
                    tmp = []
                    while h and h[0][0] <= t0:
                        tmp.append(heapq.heappop(h))
                    ci = min(tmp, key=lambda x: x[1])
                    for x in tmp:
                        if x is not ci:
                            heapq.heappush(h, x)
                    cand = (t0, ci[1], ci)
                else:
                    x = h[0]
                    cand = (x[0], x[1], None)
                if best is None or (cand[0], cand[1]) < (best[0][0], best[0][1]):
                    if best is not None and best[0][2] is not None:
                        heapq.heappush(ready[best[1]], best[0][2])
                    best = (cand, e)
                elif cand[2] is not None:
                    heapq.heappush(h, cand[2])
            (start, i, popped), e = best
            if popped is None:
                heapq.heappop(ready[e])
            r = recs[i]
            free[e] = start + r["cost"]
            fin[i] = start + r["cost"] + r["lat"]
            order[e].append(i)
            done += 1
            for u in users[i]:
                ndep[u] -= 1
                ru = recs[u]
                if ru["eng"] == e:
                    lat = 0.0 if e in ("pe", "sp") else 0.22
                else:
                    lat = 0.3
                if fin[i] + lat > rt[u]:
                    rt[u] = fin[i] + lat
                if ndep[u] == 0:
                    heapq.heappush(ready[ru["eng"]], (rt[u], u))
        return order, max(fin) if n else 0.0

    def flush(self):
        nc = self.nc
        recs = self.recs
        if self.reorder:
            order, est = self._schedule()
        else:
            order = {e: [i for i, r in enumerate(recs) if r["eng"] == e] for e in ENGS}
            est = 0.0
        dval = {}
        dtot = {}
        for i, r in enumerate(recs):
            if r["dma"] is not None:
                c = self.dpool[r["dma"]][1] + 16
                self.dpool[r["dma"]][1] = c
                dval[i] = c
                dtot[r["dma"]] = c
        for i, r in enumerate(recs):
            for d in r["deps"]:
                rd = recs[d]
                if rd["dma"] is None and (rd["eng"] != r["eng"] or r["eng"] in ("act", "dve", "pool")):
                    rd["inc"] = True
        eval_ = {}
        for e in ENGS:
            c = self.ecnt[e]
            for i in order[e]:
                if recs[i]["inc"]:
                    c += 1
                eval_[i] = c
            self.ecnt[e] = c
        engobj = {"pe": "tensor", "act": "scalar", "dve": "vector", "pool": "gpsimd", "sp": "sync"}
        esem, dpool = self.esem, self.dpool

        def mk(e):
            def body(eng):
                seen = {}
                for i in order[e]:
                    r = recs[i]
                    waits = {}
                    for d in r["deps"]:
                        rd = recs[d]
                        if rd["dma"] is not None:
                            k, v = ("d", rd["dma"]), dval[d]
                        elif rd["eng"] != e or e in ("act", "dve", "pool"):
                            k, v = ("e", rd["eng"]), eval_[d]
                        else:
                            continue
                        if seen.get(k, -1) >= v:
                            continue
                        if waits.get(k, -1) < v:
                            waits[k] = v
                    for k, v in waits.items():
                        seen[k] = v
                        if k[0] == "e":
                            eng.wait_ge(esem[k[1]], v)
                        else:
                            eng.wait_ge(dpool[k[1]][0], v)
                    ins = r["fn"](eng)
                    if r["dma"] is not None:
                        ins.then_inc(dpool[r["dma"]][0], 16)
                    elif r["inc"]:
                        ins.then_inc(esem[e], 1)
                if e == "sp":
                    for idx, v in dtot.items():
                        if seen.get(("d", idx), -1) < v:
                            eng.wait_ge(dpool[idx][0], v)
            return body

        with nc.Block() as block:
            for e in ENGS:
                if order[e] or (e == "sp" and dtot):
                    getattr(block, engobj[e])(mk(e))
        for k in self.all_keys:
            if k.dsem is not None:
                self.dfree.append(k.dsem)
                k.dsem = None
            k.w = None
            k.r = []
        n = {e: len(order[e]) for e in ENGS}
        n["est_us"] = round(est, 1)
        self._reset_stage()
        return n

    def close(self):
        self.stack.close()


class KB:
    def __init__(self, NB=4, stages=("mod", "hgrn", "moe0", "mla", "moe1"), dbg=()):
        self.NB = NB
        self.stages = stages
        self.dbg = dbg
        nc = bass.Bass("TRN2", target_bir_lowering=False)
        self.nc = nc
        self.S = Sched(nc)
        self.uid = 0
        shapes = {
            "x": (NB, SEQ, D), "c": (NB, D), "ctx": (NB, CTX, D), "c_ctx": (D,),
            "ada_w": (2, D, 6 * D), "ada_b": (2, 6 * D), "norm_mix_g": (2, D), "norm_ffn_g": (2, D),
            "hg_w_in": (D, 5 * D), "hg_lb": (2, 3, D), "hg_out_norm_g": (128,), "hg_w_out": (D, D),
            "mla_w_in": (D, 416), "mla_q_norm_g": (256,), "mla_kv_norm_g": (128,), "mla_w_qb": (256, 1536),
            "mla_w_kvb": (128, 2048), "mla_q_qknorm_g": (96,), "mla_k_qknorm_g": (96,), "mla_w_out": (D, D),
            "moe_w_group": (2, D, 4), "moe_w_expert": (2, D, 32), "moe_w_gate": (2, NEXP, D, FF),
            "moe_w_up": (2, NEXP, D, FF), "moe_w_down": (2, NEXP, FF, D),
            "k_maskf": (128, 128), "k_maskb": (128, 128), "k_bm": (128, 4, 128), "k_bmc": (128, 4), "k_cos": (SEQ, 16), "k_sin": (SEQ, 16),
        }

        class LazyIn(dict):
            def __missing__(d_, name):
                ap = nc.dram_tensor(name, list(shapes[name]), F32, kind="ExternalInput").ap()
                d_[name] = ap
                return ap

        I = LazyIn()
        self.I = I
        self.out = nc.dram_tensor("out", [NB, SEQ, D], F32, kind="ExternalOutput").ap()
        self.xres = nc.dram_tensor("xres", [NB, T, D], F32).ap()
        self.mod = nc.dram_tensor("modv", [2, 5, 6 * D], F32).ap()
        self.cT_d = nc.dram_tensor("cT_d", [128, 3, T], BF16).ap()
        self.krr_d = nc.dram_tensor("krr_d", [128, NT, 32], F32).ap()
        self.sskr_d = nc.dram_tensor("sskr_d", [128, NT], F32).ap()
        NTLmax = NB * NT
        self.NBLKmax = 2 * NTLmax + NEXP
        self.h2d = nc.dram_tensor("h2d", [NTLmax * 128, D], BF16).ap()
        self.xs = nc.dram_tensor("xs", [self.NBLKmax * 128, D], BF16).ap()
        self.ys = nc.dram_tensor("ys", [self.NBLKmax * 128, D], F32).ap()
        self.wgb = [nc.dram_tensor("wgb%d" % l_, [NEXP * 128, 4096], BF16).ap() for l_ in range(2)]
        self.wub = [nc.dram_tensor("wub%d" % l_, [NEXP * 128, 4096], BF16).ap() for l_ in range(2)]
        self.wdb = [nc.dram_tensor("wdb%d" % l_, [NEXP * 128, 4096], BF16).ap() for l_ in range(2)]
        self.kwb = [self.S.key() for _ in range(2)]
        self.kx = [self.S.keys(NT) for _ in range(NB)]
        self.kmod = self.S.key()
        self.kscr = self.S.key()
        self.D_ = {}
        for name, shape in dbg:
            self.D_[name] = nc.dram_tensor("dbg_" + name, list(shape), F32, kind="ExternalOutput").ap()

    def sb(self, st, shape, dt, nm="t"):
        self.uid += 1
        t = st.enter_context(self.nc.sbuf_tensor("%s_%d" % (nm, self.uid), list(shape), dt))
        return Tl(t, self.S.key())

    def sbr(self, st, n, shape, dt, nm="r"):
        return Rot([self.sb(st, shape, dt, nm) for _ in range(n)])

    def psb(self, st, nm="ps"):
        self.uid += 1
        t = st.enter_context(self.nc.psum_tensor("%s_%d" % (nm, self.uid), [128, 512], F32))
        k = self.S.key()
        k.excl = True
        return Tl(t, k)

    def psb2(self, st, nm="ps2"):
        self.uid += 1
        t = st.enter_context(self.nc.psum_tensor("%s_%d" % (nm, self.uid), [128, 1024], F32))
        k = self.S.key()
        k.excl = True
        return Tl(t, k)

    @staticmethod
    def _n(ap):
        n = 1
        for d in ap.shape[1:]:
            n *= d
        return n

    def mm(self, out, lhsT, rhs, start, stop, R, W):
        c = max(64, self._n(out)) / 2400.0 + 0.02
        if rhs.dtype == F32:
            c *= 4
        self.S.op("pe", lambda e: e.matmul(out, lhsT=lhsT, rhs=rhs, start=start, stop=stop), R, W, cost=c)

    def tr(self, out, in_, ident, R, W):
        self.S.op("pe", lambda e: e.transpose(out=out, in_=in_, identity=ident), R, W, cost=0.09)

    def act(self, out, in_, func, R, W, bias=None, scale=None, accum_out=None):
        kw = {}
        if bias is not None:
            kw["bias"] = bias
        if scale is not None:
            kw["scale"] = scale
        if accum_out is not None:
            kw["accum_out"] = accum_out
        self.S.op("act", lambda e: e.activation(out=out, in_=in_, func=func, **kw), R, W, cost=0.22 + self._n(out) / 1200.0)

    def ts(self, eng, out, in0, s1, s2, op0, op1, R, W):
        if op1 is None:
            self.S.op(eng, lambda e: e.tensor_scalar(out=out, in0=in0, scalar1=s1, scalar2=None, op0=op0), R, W, cost=self._c(eng, out))
        else:
            self.S.op(eng, lambda e: e.tensor_scalar(out=out, in0=in0, scalar1=s1, scalar2=s2, op0=op0, op1=op1), R, W, cost=self._c(eng, out))

    def tt(self, eng, out, in0, in1, op, R, W):
        self.S.op(eng, lambda e: e.tensor_tensor(out=out, in0=in0, in1=in1, op=op), R, W, cost=self._c(eng, out, 1.5))

    def stt(self, out, in0, scalar, in1, op0, op1, R, W):
        self.S.op("dve", lambda e: e.scalar_tensor_tensor(out=out, in0=in0, scalar=scalar, in1=in1, op0=op0, op1=op1), R, W,
                  cost=self._c("dve", out, 1.5))

    def cp(self, eng, out, in_, R, W):
        if eng == "act":
            self.S.op("act", lambda e: e.activation(out=out, in_=in_, func=AF.Copy), R, W, cost=0.22 + self._n(out) / 1200.0)
        else:
            self.S.op(eng, lambda e: e.tensor_copy(out=out, in_=in_), R, W, cost=self._c(eng, out))

    def red(self, out, in_, op, R, W, negate=None):
        self.S.op("dve", lambda e: e.tensor_reduce(out=out, in_=in_, axis=AX.X, op=op, negate=negate), R, W, cost=self._c("dve", in_))

    def recip(self, out, in_, R, W):
        self.S.op("dve", lambda e: e.reciprocal(out=out, in_=in_), R, W, cost=self._c("dve", out, 8.0))

    def memset(self, eng, ap, val, W):
        self.S.op(eng, lambda e: e.memset(ap, val), (), W, cost=self._c(eng, ap))

    def _c(self, eng, ap, mult=1.0):
        n = self._n(ap)
        if eng == "pool":
            return 0.25 + n * mult / 500.0
        return 0.1 + n * mult / 960.0

    def dma(self, q, out, in_, R, W, semkey=None):
        nb = out.shape[0] * self._n(out) * 4
        self.S.dma(q, lambda e: e.dma_start(out=out, in_=in_), R, W, semkey=semkey, nbytes=nb)

    def sumsq(self, junk, in_, acc, R, W):
        self.act(junk, in_, AF.Square, R, W, accum_out=acc)

    def setup_consts(self, st):
        nc, S = self.nc, self.S
        self.identf = self.sb(st, [128, 128], F32, "identf")
        self.identb = self.sb(st, [128, 128], BF16, "identb")
        self.onesb = self.sb(st, [128, 128], BF16, "onesb")
        identf = self.identf
        self.memset("pool", identf[:], 0.0, [identf])
        S.op("pool", lambda e: e.affine_select(out=identf[:], in_=identf[:], pattern=[[-1, 128]], compare_op=ALU.not_equal,
                                               fill=1.0, base=0, channel_multiplier=1), [identf], [identf])
        self.cp("dve", self.identb[:], identf[:], [identf], [self.identb])
        self.memset("dve", self.onesb[:], 1.0, [self.onesb])
        self.epsc = self.sb(st, [128, 1], F32, "epsc")
        self.memset("dve", self.epsc[:], EPS, [self.epsc])
        self.onec = self.sb(st, [128, 1], F32, "onec")
        self.memset("dve", self.onec[:], 1.0, [self.onec])

    def stage_mod(self):
        I, NB = self.I, self.NB
        with ExitStack() as st:
            cT = self.sb(st, [128, KC, 5], F32, "cT")
            cs = self.sb(st, [128, KC, 5], F32, "cs")
            self.memset("dve", cT[:], 0.0, [cT])
            for r in range(NB):
                self.dma("sp", cT[:, :, r], I["c"][r, :].rearrange("(c p) -> p c", p=128), [], [cT])
            self.dma("sp", cT[:, :, 4], I["c_ctx"].rearrange("(c p) -> p c", p=128), [], [cT])
            self.act(cs[:], cT[:], AF.Silu, [cT], [cs])
            wrot = self.sbr(st, 3, [128, KC, 512], F32, "adaw")
            ps = Rot([self.psb(st) for _ in range(2)])
            for l in range(2):
                bt = self.sb(st, [5, 6 * D], F32, "adab")
                ms = self.sb(st, [5, 6 * D], F32, "modsb")
                self.dma("sp", bt[:], I["ada_b"][l, :].partition_broadcast(5), [], [bt])
                for n in range(12):
                    w = wrot.next()
                    self.dma("sp", w[:], I["ada_w"][l, :, n * 512:(n + 1) * 512].rearrange("(c p) n -> p c n", p=128), [], [w])
                    p = ps.next()
                    for k in range(KC):
                        self.mm(p[0:5, :], cs[:, k, :], w[:, k, :], k == 0, k == KC - 1, [cs, w], [p])
                    self.tt("dve", ms[:, n * 512:(n + 1) * 512], p[0:5, :], bt[:, n * 512:(n + 1) * 512], ALU.add, [p, bt], [ms])
                self.dma("sp", self.mod[l], ms[:], [ms], [self.kmod], semkey=ms)
            return self.S.flush()

    def mod_cols(self, st, l, m, r):
        I = self.I
        g = I["norm_mix_g"] if m == 0 else I["norm_ffn_g"]
        gc = self.sb(st, [128, KC], F32, "gc")
        sc = self.sb(st, [128, KC], F32, "sc")
        sh = self.sb(st, [128, KC], F32, "sh")
        A = self.sb(st, [128, KC], F32, "A")
        self.dma("sp", gc[:], g[l, :].rearrange("(c p) -> p c", p=128), [], [gc])
        self.dma("sp", sc[:], self.mod[l, r, (3 * m + 1) * D:(3 * m + 2) * D].rearrange("(c p) -> p c", p=128), [self.kmod], [sc])
        self.dma("sp", sh[:], self.mod[l, r, (3 * m) * D:(3 * m + 1) * D].rearrange("(c p) -> p c", p=128), [self.kmod], [sh])
        self.stt(A[:], sc[:], 1.0, gc[:], ALU.add, ALU.mult, [sc, gc], [A])
        return A, sh

    def gate_tile(self, st, l, m, r):
        gt = self.sb(st, [128, D], F32, "gt")
        self.dma("sp", gt[:], self.mod[l, r, (3 * m + 2) * D:(3 * m + 3) * D].partition_broadcast(128), [self.kmod], [gt])
        return gt

    def norm_res(self, st, pbanks, junk=None, nxn=1, nhtf=1, nxt=2):
        R = {}
        R["xt"] = self.sbr(st, nxt, [128, D], F32, "xt")
        R["xn"] = self.sbr(st, nxn, [128, D], F32, "xn")
        R["junk"] = junk if junk is not None else self.sb(st, [128, D], BF16, "junk")
        R["ss"] = self.sbr(st, 3, [128, 1], F32, "ss")
        R["sd"] = self.sbr(st, 3, [128, 1], F32, "sd")
        R["rs"] = self.sbr(st, 3, [128, 1], F32, "rs")
        R["hTf"] = self.sbr(st, nhtf, [128, KC, 128], F32, "hTf")
        R["pb"] = pbanks
        return R

    def norm_tile(self, R, src_ap, src_keys, A, Bc, dst_ap, dst_keys):
        xt = R["xt"].next()
        xn = R["xn"].next()
        ss = R["ss"].next()
        sd = R["sd"].next()
        rs = R["rs"].next()
        hTf = R["hTf"].next()
        junk = R["junk"]
        pa, pb = R["pb"]
        self.dma("sp", xt[:], src_ap, src_keys, [xt])
        self.sumsq(junk[:, 0:D], xt[:], ss[:], [xt], [junk, ss])
        self.act(sd[:], ss[:], AF.Ln, [ss, self.epsc], [sd], bias=self.epsc[:, 0:1], scale=1.0 / D)
        self.act(rs[:], sd[:], AF.Exp, [sd], [rs], scale=-0.5)
        self.act(xn[:], xt[:], AF.Copy, [xt, rs], [xn], scale=rs[:, 0:1])
        for k in range(KC):
            p = pa if k < 4 else pb
            self.tr(p[:, (k % 4) * 128:(k % 4 + 1) * 128], xn[:, k * 128:(k + 1) * 128], self.identf[:], [xn, self.identf], [p])
        for k in range(KC):
            p = pa if k < 4 else pb
            src = p[:, (k % 4) * 128:(k % 4 + 1) * 128]
            if k < 4:
                self.ts("dve", hTf[:, k, :], src, A[:, k:k + 1], Bc[:, k:k + 1], ALU.mult, ALU.add, [p, A, Bc], [hTf])
            else:
                self.act(hTf[:, k, :], src, AF.Identity, [p, A, Bc], [hTf], bias=Bc[:, k:k + 1], scale=A[:, k:k + 1])
        if dst_ap is not None:
            self.cp("dve", dst_ap, hTf[:], [hTf], dst_keys)
        self.last_xn = xn
        return hTf

    def src_l0(self, b, tile):
        if tile < 2:
            return self.I["ctx"][b, tile * 128:(tile + 1) * 128, :]
        return self.I["x"][b, (tile - 2) * 128:(tile - 1) * 128, :]

    def stage_hgrn(self, b):
        I, S = self.I, self.S
        l = 0
        with ExitStack() as st:
            PB = [self.psb(st) for _ in range(8)]
            if "moe0" in self.stages:
                self.precast(0, b)
            hT = self.sb(st, [128, KC, T], BF16, "hT")
            hTk = S.keys(NT)
            ogT = self.sb(st, [128, KC, T], BF16, "ogT")
            ogk = S.keys(KC)
            maskf = self.sb(st, [128, 128], F32, "maskf")
            maskb = self.sb(st, [128, 128], F32, "maskb")
            bmc = self.sb(st, [128, 4], F32, "bmc")
            if not _F("HG_A"):
                bm = self.sb(st, [128, 4, 128], BF16, "bm")
                self.dma("pool", bm[:], I["k_bm"], [], [bm])
                Vbd = self.sbr(st, 2, [128, 4, 128], BF16, "Vbd")
            self.dma("sp", maskf[:], I["k_maskf"], [], [maskf])
            self.dma("sp", maskb[:], I["k_maskb"], [], [maskb])
            self.dma("sp", bmc[:], I["k_bmc"], [], [bmc])
            m01 = self.sb(st, [128, T], BF16, "m01")
            self.memset("dve", m01[:], 1.0, [m01])
            self.memset("dve", m01[:, 0:T:32], 0.0, [m01])
            lbr = self.sb(st, [128, 2, 3, KC], F32, "lbr")
            with self.nc.allow_non_contiguous_dma(reason="tiny"):
                for d_ in range(2):
                    for j in range(3):
                        self.dma("sp", lbr[:, d_, j, :], I["hg_lb"][d_, j, :].rearrange("(h p) -> p h", p=128), [], [lbr])
            lbe = self.sb(st, [128, 2, 3, KC], F32, "lbe")
            self.act(lbe[:], lbr[:], AF.Exp, [lbr], [lbe])
            lbs = self.sb(st, [128, 2, KC], F32, "lbs")
            self.tt("dve", lbs[:], lbe[:, :, 0, :], lbe[:, :, 1, :], ALU.add, [lbe], [lbs])
            self.tt("dve", lbs[:], lbs[:], lbe[:, :, 2, :], ALU.add, [lbe, lbs], [lbs])
            lbi = self.sb(st, [128, 2, KC], F32, "lbi")
            self.recip(lbi[:], lbs[:], [lbs], [lbi])
            lb = self.sb(st, [128, 2, KC], F32, "lb")
            oml = self.sb(st, [128, 2, KC], F32, "oml")
            self.tt("dve", lb[:], lbe[:, :, 0, :], lbi[:], ALU.mult, [lbe, lbi], [lb])
            self.ts("dve", oml[:], lb[:], -1.0, 1.0, ALU.mult, ALU.add, [lb], [oml])
            ogc = self.sb(st, [128, 1], F32, "ogc")
            self.dma("sp", ogc[:], I["hg_out_norm_g"].rearrange("(p o) -> p o", o=1), [], [ogc])
            A_l, B_l = self.mod_cols(st, l, 0, b)
            A_c, B_c = self.mod_cols(st, l, 0, 4)
            gt_l = self.gate_tile(st, l, 0, b)
            gt_c = self.gate_tile(st, l, 0, 4)
            qdec = self.sb(st, [128, T], BF16, "qdec")
            NR = self.norm_res(st, (PB[0], PB[1]), junk=qdec)
            for tile in range(NT):
                A, Bc = (A_c, B_c) if tile < 2 else (A_l, B_l)
                self.norm_tile(NR, self.src_l0(b, tile), [], A, Bc, hT[:, :, tile * 128:(tile + 1) * 128], [hTk[tile]])
            wh = self.sbr(st, 1, [128, KC, 5, 128], BF16, "wh")
            Vh = self.sb(st, [128, NT, 128], BF16, "Vh")
            qs = self.sb(st, [128, T], BF16, "qs")
            sgate = self.sb(st, [128, T], BF16, "sgate")
            A1 = self.sb(st, [128, T], F32, "A1")
            A2 = self.sb(st, [128, T], F32, "A2")
            A3 = self.sb(st, [128, T], F32, "A3")
            kinc = self.sb(st, [128, T], BF16, "kinc")
            dec = self.sb(st, [128, T // 32], F32, "dec")
            tot = self.sb(st, [128, T // 32], F32, "tot")
            oacc = self.sb(st, [128, T], F32, "oacc")
            oak = S.keys(NT)
            sTm = self.sbr(st, 2, [128, 128], BF16, "sTm")
            kTs = self.sbr(st, 2, [128, 4, 128], BF16, "kTs")
            KVs = self.sbr(st, 2, [128, 4, 128], F32, "KVs")
            Sst = self.sb(st, [128, 8, 128], F32, "Sst")
            Sstk = S.keys(8)
            Sb = self.sb(st, [128, 8, 128], BF16, "Sb")
            Sbk = S.keys(2)
            pproj = Rot([PB[0], PB[1]])
            psT = Rot([PB[2], PB[3]])
            pkT = PB[4]
            pkTk = [PB[4].k, PB[4].k]
            pKV = PB[5]
            poT = Rot([PB[6], PB[7]])
            blocks = [(i * 512, 512) for i in range(4)] + [(2048, 256)]

            def proj(whh, sec, blk):
                t0, n = blk
                p = pproj.next()
                tiles = range(t0 // 128, (t0 + n) // 128)
                for k in range(KC):
                    self.mm(p[:, 0:n], whh[:, k, sec, :], hT[:, k, t0:t0 + n], k == 0, k == KC - 1,
                            [whh] + [hTk[t] for t in tiles], [p])
                return p

            for h in range(KC):
                whh = wh.next()
                for sec in range(5):
                    self.dma("pool", whh[:, :, sec, :],
                             I["hg_w_in"][:, sec * D + h * 128: sec * D + (h + 1) * 128].rearrange("(c p) e -> p c e", p=128), [], [whh])
                for tile in range(NT):
                    p = pproj.next()
                    for k in range(KC):
                        self.mm(p[:, 0:128], hT[:, k, tile * 128:(tile + 1) * 128], whh[:, k, 3, :], k == 0, k == KC - 1,
                                [whh, hTk[tile]], [p])
                    self.cp("act", Vh[:, tile, :], p[:, 0:128], [p], [Vh])
                for blk in blocks:
                    t0, n = blk
                    p = proj(whh, 0, blk)
                    self.act(qs[:, t0:t0 + n], p[:, 0:n], AF.Silu, [p], [qs])
                    p = proj(whh, 4, blk)
                    self.act(sgate[:, t0:t0 + n], p[:, 0:n], AF.Silu, [p], [sgate])
                for dr in range(2):
                    for blk in blocks:
                        t0, n = blk
                        p = proj(whh, 1 + dr, blk)
                        self.act(A1[:, t0:t0 + n], p[:, 0:n], AF.Sigmoid, [p], [A1])
                    self.ts("dve", A1[:], A1[:], oml[:, dr, h:h + 1], lb[:, dr, h:h + 1], ALU.mult, ALU.add, [A1, oml, lb], [A1])
                    self.act(A2[:], A1[:], AF.Ln, [A1], [A2])
                    if _F("HG_B"):
                        self.act(A1[:], A1[:], AF.Identity, [A1, self.onec], [A1], bias=self.onec[:, 0:1], scale=-1.0)
                    else:
                        self.ts("dve", A1[:], A1[:], -1.0, 1.0, ALU.mult, ALU.add, [A1], [A1])
                    S.op("dve", lambda e: e.tensor_tensor_scan(out=A3[:], data0=m01[:], data1=A2[:], initial=0.0,
                                                                op0=ALU.mult, op1=ALU.add), [m01, A2], [A3], cost=0.1 + 2 * T / 960.0)
                    a3v = A3[:].rearrange("p (j i) -> p j i", i=32)
                    self.cp("dve", tot[:], a3v[:, :, 31], [A3], [tot])
                    self.act(dec[:], tot[:], AF.Exp, [tot], [dec])
                    if dr == 0:
                        barr, free = A3, A2
                    else:
                        a2v = A2[:].rearrange("p (j i) -> p j i", i=32)
                        self.tt("dve", A2[:], A2[:], A3[:], ALU.subtract, [A2, A3], [A2])
                        self.tt("dve", a2v, a2v, tot[:].unsqueeze(2).broadcast_to([128, T // 32, 32]), ALU.add, [A2, tot], [A2])
                        barr, free = A2, A3
                    self.act(free[:], barr[:], AF.Exp, [barr], [free], scale=-1.0)
                    self.act(barr[:], barr[:], AF.Exp, [barr], [barr])
                    self.tt("dve", qdec[:], qs[:], barr[:], ALU.mult, [qs, barr], [qdec])
                    self.tt("dve", kinc[:], A1[:], free[:], ALU.mult, [A1, free], [kinc])
                    order = list(range(NT)) if dr == 0 else [1, 0] + list(range(NT - 1, 1, -1))
                    mask = maskf if dr == 0 else maskb
                    self.memset("dve", Sst[:, 0, :], 0.0, [Sstk[0]])
                    for i, tile in enumerate(order):
                        base = 4 * (i % 2)
                        ts_ = slice(tile * 128, (tile + 1) * 128)
                        ps_ = psT.next()
                        self.mm(ps_[:, 0:128], kinc[:, ts_], qdec[:, ts_], True, True, [kinc, qdec], [ps_])
                        sm = sTm.next()
                        self.tt("dve", sm[:], ps_[:, 0:128], mask[:], ALU.mult, [ps_, mask], [sm])
                        pk_i = i % 2
                        pkv = pkT[:, pk_i * 64:(pk_i + 1) * 64].bitcast(BF16)
                        self.tr(pkv, kinc[:, ts_], self.identb[:], [kinc, self.identb], [pkTk[pk_i]])
                        kt = kTs.next()
                        if _F("HG_A"):
                            for j in range(4):
                                self.act(kt[:, j, :], pkv, AF.Copy, [pkTk[pk_i], bmc], [kt], scale=bmc[:, j:j + 1])
                            for j in range(4):
                                self.mm(pKV[:, j * 128:(j + 1) * 128], kt[:, j, :], Vh[:, tile, :], True, True, [kt, Vh], [pKV])
                        else:
                            self.cp("act", kt[:, 0, :], pkv, [pkTk[pk_i]], [kt])
                            vb = Vbd.next()
                            self.tt("dve", vb[:], Vh[:, tile, :].unsqueeze(1).broadcast_to([128, 4, 128]), bm[:], ALU.mult, [Vh, bm], [vb])
                            self.mm(pKV[:, :], kt[:, 0, :], vb[:].rearrange("p j v -> p (j v)"), True, True, [kt, vb], [pKV])
                        kv = KVs.next()
                        self.tt("dve", kv[:], pKV[:, :].rearrange("p (j v) -> p j v", j=4),
                                dec[:, tile * 4:(tile + 1) * 4].unsqueeze(2).broadcast_to([128, 4, 128]), ALU.mult, [pKV, dec], [kv])
                        corder = [0, 1, 2, 3] if dr == 0 else [3, 2, 1, 0]
                        for jj, c in enumerate(corder):
                            s_in = base + jj
                            s_out = (base + jj + 1) % 8
                            self.stt(Sst[:, s_out, :], Sst[:, s_in, :], dec[:, tile * 4 + c: tile * 4 + c + 1], kv[:, c, :],
                                     ALU.mult, ALU.add, [Sstk[s_in], dec, kv], [Sstk[s_out]])
                        self.cp("pool" if not _F("HG_E") else "act", Sb[:, base:base + 4, :], Sst[:, base:base + 4, :],
                                [Sstk[base + q_] for q_ in range(4)], [Sbk[i % 2]])
                        po = poT.next()
                        self.mm(po[:, 0:128], Vh[:, tile, :], sm[:], True, False, [Vh, sm], [po])
                        for jj, c in enumerate(corder):
                            self.mm(po[:, c * 32:(c + 1) * 32], Sb[:, base + jj, :], qdec[:, tile * 128 + c * 32: tile * 128 + (c + 1) * 32],
                                    False, jj == 3, [Sbk[i % 2], qdec], [po])
                        if dr == 0:
                            self.cp("act", oacc[:, ts_], po[:, 0:128], [po], [oak[tile]])
                        else:
                            self.tt("dve", oacc[:, ts_], oacc[:, ts_], po[:, 0:128], ALU.add, [po, oak[tile]], [oak[tile]])
                if _F("HG_D"):
                    self.act(qdec[:], oacc[:], AF.Square, oak, [qdec])
                else:
                    self.tt("dve", qdec[:], oacc[:], oacc[:], ALU.mult, oak, [qdec])
                for blk in blocks:
                    t0, n = blk
                    p = pproj.next()
                    self.mm(p[:, 0:n], self.onesb[:], qdec[:, t0:t0 + n], True, True, [self.onesb, qdec], [p])
                    if _F("HG_C"):
                        self.act(A2[:, t0:t0 + n], p[:, 0:n], AF.Ln, [p, self.epsc], [A2], bias=self.epsc[:, 0:1], scale=1.0 / 128)
                    else:
                        self.act(A2[:, t0:t0 + n], p[:, 0:n], AF.Sqrt, [p, self.epsc], [A2], bias=self.epsc[:, 0:1], scale=1.0 / 128)
                if _F("HG_C"):
                    self.act(A3[:], A2[:], AF.Exp, [A2], [A3], scale=-0.5)
                else:
                    self.recip(A3[:], A2[:], [A2], [A3])
                self.tt("dve", A3[:], A3[:], oacc[:], ALU.mult, [A3] + oak, [A3])
                self.stt(ogT[:, h, :], A3[:], ogc[:, 0:1], sgate[:], ALU.mult, ALU.mult, [A3, ogc, sgate], [ogk[h]])
            wo = self.sb(st, [128, KC, D], BF16, "wo")
            self.dma("pool", wo[:], I["hg_w_out"].rearrange("(c p) n -> p c n", p=128), [], [wo])
            xt2 = NR["xt"]
            tmp = NR["xn"]
            for tile in range(NT):
                gt = gt_c if tile < 2 else gt_l
                x_ = xt2.next()
                self.dma("sp", x_[:], self.src_l0(b, tile), [], [x_])
                t_ = tmp.next()
                for half in range(2):
                    p = pproj.next()
                    hs = slice(half * 512, (half + 1) * 512)
                    for k in range(KC):
                        self.mm(p[:, :], ogT[:, k, tile * 128:(tile + 1) * 128], wo[:, k, hs], k == 0, k == KC - 1, [ogk[k], wo], [p])
                    self.tt("dve", t_[:, hs], p[:, :], gt[:, hs], ALU.mult, [p, gt], [t_])
                self.tt("dve", t_[:], t_[:], x_[:], ALU.add, [t_, x_], [t_])
                self.dma("sp", self.xres[b, tile * 128:(tile + 1) * 128, :], t_[:], [t_], [self.kx[b][tile]], semkey=t_)
                if ("xm0" in self.D_) and b == 0:
                    self.dma("sp", self.D_["xm0"][tile * 128:(tile + 1) * 128, :], t_[:], [t_], [self.kscr], semkey=t_)
            return S.flush()

    ROUTE_TMPS = (("lg", 36), ("gmax", 1), ("ngmax", 1), ("ge", 4), ("gsum", 1), ("pg", 1), ("gone", 4), ("pen", 4),
                  ("em", 32), ("m1", 1), ("oh1", 32), ("em2", 32), ("m2", 1), ("oh2", 32), ("dm", 1), ("e2", 1),
                  ("den", 1), ("rden", 1), ("w1", 1), ("w2", 1), ("tmpw", 32))

    def route_tile(self, sm, p):
        t = {nm: r.next() for nm, r in sm.items()}
        lg = t["lg"]
        self.cp("act", lg[:], p[:, 0:36], [p], [lg])
        self.red(t["gmax"][:], lg[:, 0:4], ALU.max, [lg], [t["gmax"]])
        self.ts("dve", t["ngmax"][:], t["gmax"][:], -1.0, None, ALU.mult, None, [t["gmax"]], [t["ngmax"]])
        self.act(t["ge"][:], lg[:, 0:4], AF.Exp, [lg, t["ngmax"]], [t["ge"], t["gsum"]], bias=t["ngmax"][:, 0:1], accum_out=t["gsum"][:])
        self.recip(t["pg"][:], t["gsum"][:], [t["gsum"]], [t["pg"]])
        self.ts("dve", t["gone"][:], lg[:, 0:4], t["gmax"][:, 0:1], None, ALU.is_ge, None, [lg, t["gmax"]], [t["gone"]])
        self.ts("dve", t["pen"][:], t["gone"][:], BIG, -BIG, ALU.mult, ALU.add, [t["gone"]], [t["pen"]])
        self.tt("dve", t["em"][:].rearrange("p (g j) -> p g j", g=4), lg[:, 4:36].rearrange("p (g j) -> p g j", g=4),
                t["pen"][:].unsqueeze(2).broadcast_to([128, 4, 8]), ALU.add, [lg, t["pen"]], [t["em"]])
        self.red(t["m1"][:], t["em"][:], ALU.max, [t["em"]], [t["m1"]])
        self.ts("dve", t["oh1"][:], t["em"][:], t["m1"][:, 0:1], None, ALU.is_ge, None, [t["em"], t["m1"]], [t["oh1"]])
        self.stt(t["em2"][:], t["oh1"][:], -BIG, t["em"][:], ALU.mult, ALU.add, [t["oh1"], t["em"]], [t["em2"]])
        self.red(t["m2"][:], t["em2"][:], ALU.max, [t["em2"]], [t["m2"]])
        self.ts("dve", t["oh2"][:], t["em2"][:], t["m2"][:, 0:1], None, ALU.is_ge, None, [t["em2"], t["m2"]], [t["oh2"]])
        self.tt("dve", t["dm"][:], t["m2"][:], t["m1"][:], ALU.subtract, [t["m2"], t["m1"]], [t["dm"]])
        self.act(t["e2"][:], t["dm"][:], AF.Exp, [t["dm"]], [t["e2"]])
        self.ts("dve", t["den"][:], t["e2"][:], 1.0, None, ALU.add, None, [t["e2"]], [t["den"]])
        self.recip(t["rden"][:], t["den"][:], [t["den"]], [t["rden"]])
        self.tt("dve", t["w1"][:], t["pg"][:], t["rden"][:], ALU.mult, [t["pg"], t["rden"]], [t["w1"]])
        self.tt("dve", t["w2"][:], t["w1"][:], t["e2"][:], ALU.mult, [t["w1"], t["e2"]], [t["w2"]])
        return t

    def stage_moe(self, l, b, half):
        I, S = self.I, self.S
        if l == 0:
            tiles = list(range(0, 9)) if half == 0 else list(range(9, 18))
        else:
            tiles = list(range(2, 10)) if half == 0 else list(range(10, 18))
        ntl = len(tiles)
        NTOK = ntl * 128
        with ExitStack() as st:
            PB = [self.psb(st) for _ in range(8)]
            hT = self.sb(st, [128, KC, NTOK], BF16, "hT")
            hTk = S.keys(ntl)
            acc = self.sb(st, [128, ntl, D], F32, "acc")
            acck = S.keys(ntl)
            Wt = self.sb(st, [128, ntl, NEXP], F32, "Wt")
            Wtk = S.keys(ntl)
            wr = self.sb(st, [128, KC, 36], F32, "wr")
            self.dma("sp", wr[:, :, 0:4], I["moe_w_group"][l].rearrange("(c p) g -> p c g", p=128), [], [wr])
            self.dma("sp", wr[:, :, 4:36], I["moe_w_expert"][l].rearrange("(c p) g -> p c g", p=128), [], [wr])
            A_l, B_l = self.mod_cols(st, l, 1, b)
            gt_l = self.gate_tile(st, l, 1, b)
            if l == 0 and half == 0:
                A_c, B_c = self.mod_cols(st, l, 1, 4)
                gt_c = self.gate_tile(st, l, 1, 4)
            NR = self.norm_res(st, (PB[0], PB[1]), nhtf=2)
            sm = {}
            for nm, w in (("lg", 36), ("gmax", 1), ("ngmax", 1), ("ge", 4), ("gsum", 1), ("pg", 1), ("gone", 4), ("pen", 4),
                          ("em", 32), ("m1", 1), ("oh1", 32), ("em2", 32), ("m2", 1), ("oh2", 32), ("dm", 1), ("e2", 1),
                          ("den", 1), ("rden", 1), ("w1", 1), ("w2", 1), ("tmpw", 32)):
                sm[nm] = self.sbr(st, 2, [128, w], F32, nm)
            for li, tile in enumerate(tiles):
                isctx = (l == 0 and tile < 2)
                A, Bc = (A_c, B_c) if isctx else (A_l, B_l)
                hTf = self.norm_tile(NR, self.xres[b, tile * 128:(tile + 1) * 128, :], [self.kx[b][tile]], A, Bc,
                                     hT[:, :, li * 128:(li + 1) * 128], [hTk[li]])
                p = PB[2 + li % 2]
                for k in range(KC):
                    self.mm(p[:, 0:36], hTf[:, k, :], wr[:, k, :], k == 0, k == KC - 1, [hTf, wr], [p])
                t = self.route_tile(sm, p)
                self.ts("dve", t["tmpw"][:], t["oh1"][:], t["w1"][:, 0:1], None, ALU.mult, None, [t["oh1"], t["w1"]], [t["tmpw"]])
                self.stt(Wt[:, li, :], t["oh2"][:], t["w2"][:, 0:1], t["tmpw"][:], ALU.mult, ALU.add, [t["oh2"], t["w2"], t["tmpw"]], [Wtk[li]])
            wg = self.sbr(st, 2, [128, KC, FF], BF16, "wg")
            wu = self.sbr(st, 2, [128, KC, FF], BF16, "wu")
            wd = self.sbr(st, 2, [128, 4, D], BF16, "wd")
            sg = self.sbr(st, 2, [128, 512], BF16, "sg")
            actT = self.sbr(st, 2, [128, 4, 512], BF16, "actT")
            pgu = Rot([(PB[0], PB[1]), (PB[2], PB[3])])
            pyr = Rot([(PB[4], PB[5]), (PB[6], PB[7])])
            blocks = []
            t0 = 0
            while t0 < NTOK:
                n = min(512, NTOK - t0)
                blocks.append((t0, n))
                t0 += n
            for e in range(NEXP):
                g_, u_, d_ = wg.next(), wu.next(), wd.next()
                self.dma("pool", g_[:], I["moe_w_gate"][l, e].rearrange("(c p) f -> p c f", p=128), [], [g_])
                self.dma("pool", u_[:], I["moe_w_up"][l, e].rearrange("(c p) f -> p c f", p=128), [], [u_])
                self.dma("pool", d_[:], I["moe_w_down"][l, e].rearrange("(c p) f -> p c f", p=128), [], [d_])
                for (t0, n) in blocks:
                    at = actT.next()
                    hk = [hTk[t] for t in range(t0 // 128, (t0 + n) // 128)]
                    for f in range(4):
                        pg_, pu_ = pgu.next()
                        fs = slice(f * 128, (f + 1) * 128)
                        for k in range(KC):
                            self.mm(pg_[:, 0:n], g_[:, k, fs], hT[:, k, t0:t0 + n], k == 0, k == KC - 1, [g_] + hk, [pg_])
                        for k in range(KC):
                            self.mm(pu_[:, 0:n], u_[:, k, fs], hT[:, k, t0:t0 + n], k == 0, k == KC - 1, [u_] + hk, [pu_])
                        s_ = sg.next()
                        self.act(s_[:, 0:n], pg_[:, 0:n], AF.Silu, [pg_], [s_])
                        self.tt("dve", at[:, f, 0:n], s_[:, 0:n], pu_[:, 0:n], ALU.mult, [s_, pu_], [at])
                    for tt_ in range(n // 128):
                        li = t0 // 128 + tt_
                        pa, pb = pyr.next()
                        for hf, p in ((0, pa), (1, pb)):
                            hs = slice(hf * 512, (hf + 1) * 512)
                            for f in range(4):
                                self.mm(p[:, :], at[:, f, tt_ * 128:(tt_ + 1) * 128], d_[:, f, hs], f == 0, f == 3, [at, d_], [p])
                            if e == 0:
                                self.ts("dve", acc[:, li, hs], p[:, :], Wt[:, li, e:e + 1], None, ALU.mult, None, [p, Wtk[li]], [acck[li]])
                            else:
                                self.stt(acc[:, li, hs], p[:, :], Wt[:, li, e:e + 1], acc[:, li, hs], ALU.mult, ALU.add,
                                         [p, Wtk[li], acck[li]], [acck[li]])
            for li, tile in enumerate(tiles):
                isctx = (l == 0 and tile < 2)
                gt = gt_c if isctx else gt_l
                x_ = NR["xt"].next()
                self.dma("sp", x_[:], self.xres[b, tile * 128:(tile + 1) * 128, :], [self.kx[b][tile]], [x_])
                t_ = NR["xn"].next()
                self.tt("dve", t_[:], acc[:, li, :], gt[:], ALU.mult, [acck[li], gt], [t_])
                self.tt("dve", t_[:], t_[:], x_[:], ALU.add, [t_, x_], [t_])
                if l == 0:
                    self.dma("sp", self.xres[b, tile * 128:(tile + 1) * 128, :], t_[:], [t_], [self.kx[b][tile]], semkey=t_)
                    if ("xf0" in self.D_) and b == 0:
                        self.dma("sp", self.D_["xf0"][tile * 128:(tile + 1) * 128, :], t_[:], [t_], [self.kscr], semkey=t_)
                else:
                    self.dma("sp", self.out[b, (tile - 2) * 128:(tile - 1) * 128, :], t_[:], [t_], [], semkey=t_)
            return S.flush()

    def rope(self, xin, xout, cos, sin, H, tm, R, W):
        x1, x2 = xin[:, :, :, 0, :], xin[:, :, :, 1, :]
        cb = cos.unsqueeze(1).broadcast_to([128, H, 2, 8])
        sb_ = sin.unsqueeze(1).broadcast_to([128, H, 2, 8])
        t1, t2 = tm
        v1 = t1[:, 0:H * 16].rearrange("p (h a f) -> p h a f", h=H, a=2)
        v2 = t2[:, 0:H * 16].rearrange("p (h a f) -> p h a f", h=H, a=2)
        self.tt("dve", v1, x1, cb, ALU.mult, R, [t1])
        self.tt("dve", v2, x2, sb_, ALU.mult, R, [t2])
        self.tt("dve", xout[:, :, :, 0, :], v1, v2, ALU.subtract, [t1, t2], W)
        self.tt("dve", v1, x2, cb, ALU.mult, R, [t1])
        self.tt("dve", v2, x1, sb_, ALU.mult, R, [t2])
        self.tt("dve", xout[:, :, :, 1, :], v1, v2, ALU.add, [t1, t2], W)

    def stage_mla(self, b):
        I, S = self.I, self.S
        l = 1
        NQT = SEQ // 128
        with ExitStack() as st:
            PB = [self.psb(st) for _ in range(8)]
            if "moe1" in self.stages:
                self.precast(1, b)
            A_l, B_l = self.mod_cols(st, l, 0, b)
            A_c, B_c = self.mod_cols(st, l, 0, 4)
            gt_l = self.gate_tile(st, l, 0, b)
            NR = self.norm_res(st, (PB[0], PB[1]), nxn=2, nhtf=2)
            win = self.sb(st, [128, KC, 416], BF16, "win")
            self.dma("pool", win[:], I["mla_w_in"].rearrange("(c p) n -> p c n", p=128), [], [win])
            cT = self.sb(st, [128, 3, T], BF16, "cT")
            cTk = S.keys(NT)
            krr = self.sb(st, [128, NT, 32], F32, "krr")
            krk = S.keys(NT)
            sskr = self.sb(st, [128, NT], F32, "sskr")
            ssk = S.keys(NT)
            gk = self.sb(st, [128, 96], F32, "gk")
            gq = self.sb(st, [128, 96], F32, "gq")
            self.dma("sp", gk[:], I["mla_k_qknorm_g"].partition_broadcast(128), [], [gk])
            self.dma("sp", gq[:], I["mla_q_qknorm_g"].partition_broadcast(128), [], [gq])
            self.ts("dve", gq[:], gq[:], float(96 ** -0.5), None, ALU.mult, None, [gq], [gq])
            qng = self.sb(st, [128, 2], F32, "qng")
            kvg = self.sb(st, [128, 1], F32, "kvg")
            self.dma("sp", qng[:], I["mla_q_norm_g"].rearrange("(k p) -> p k", p=128), [], [qng])
            self.dma("sp", kvg[:], I["mla_kv_norm_g"].rearrange("(p o) -> p o", o=1), [], [kvg])
            hTt = self.sbr(st, 2, [128, KC, 128], BF16, "hTt")
            csr = self.sbr(st, 2, [128, 416], F32, "cs")
            cnr = self.sbr(st, 2, [128, 384], BF16, "cn")
            junk2 = self.sb(st, [128, 256], BF16, "junk2")
            s1 = {nm: self.sbr(st, 2, [128, 1], F32, nm) for nm in ("ssq", "sskv", "sdq", "sdkv", "rsq", "rskv")}
            kr1 = self.sbr(st, 2, [128, 32], F32, "kr1")
            cosr = self.sbr(st, 2, [128, 16], F32, "cos")
            sinr = self.sbr(st, 2, [128, 16], F32, "sin")
            rt = (self.sb(st, [128, 64], F32, "rt1"), self.sb(st, [128, 64], F32, "rt2"))
            cost = {}
            for tile in range(NT):
                A, Bc = (A_c, B_c) if tile < 2 else (A_l, B_l)
                hb = hTt.next()
                self.norm_tile(NR, self.xres[b, tile * 128:(tile + 1) * 128, :], [self.kx[b][tile]], A, Bc, hb[:], [hb])
                p = PB[2 + tile % 2]
                for k in range(KC):
                    self.mm(p[:, 0:416], hb[:, k, :], win[:, k, :], k == 0, k == KC - 1, [hb, win], [p])
                cs = csr.next()
                self.cp("dve", cs[:], p[:, 0:416], [p], [cs])
                t = {nm: r.next() for nm, r in s1.items()}
                self.act(junk2[:, 0:256], cs[:, 0:256], AF.Square, [cs], [junk2, t["ssq"]], accum_out=t["ssq"][:])
                self.act(junk2[:, 0:128], cs[:, 256:384], AF.Square, [cs], [junk2, t["sskv"]], accum_out=t["sskv"][:])
                self.act(junk2[:, 0:32], cs[:, 384:416], AF.Square, [cs], [junk2, ssk[tile]], accum_out=sskr[:, tile:tile + 1])
                self.act(t["sdq"][:], t["ssq"][:], AF.Sqrt, [t["ssq"], self.epsc], [t["sdq"]], bias=self.epsc[:, 0:1], scale=1.0 / 256)
                self.act(t["sdkv"][:], t["sskv"][:], AF.Sqrt, [t["sskv"], self.epsc], [t["sdkv"]], bias=self.epsc[:, 0:1], scale=1.0 / 128)
                self.recip(t["rsq"][:], t["sdq"][:], [t["sdq"]], [t["rsq"]])
                self.recip(t["rskv"][:], t["sdkv"][:], [t["sdkv"]], [t["rskv"]])
                cn = cnr.next()
                self.act(cn[:, 0:256], cs[:, 0:256], AF.Copy, [cs, t["rsq"]], [cn], scale=t["rsq"][:, 0:1])
                self.act(cn[:, 256:384], cs[:, 256:384], AF.Copy, [cs, t["rskv"]], [cn], scale=t["rskv"][:, 0:1])
                pT = PB[4 + tile % 2]
                pv = pT[:, 0:192].bitcast(BF16).rearrange("p (j t) -> p j t", j=3)
                for j in range(3):
                    self.tr(pv[:, j, :], cn[:, j * 128:(j + 1) * 128], self.identb[:], [cn, self.identb], [pT])
                self.cp("dve", cT[:, :, tile * 128:(tile + 1) * 128], pv, [pT], [cTk[tile]])
                k1 = kr1.next()
                self.tt("dve", k1[:], cs[:, 384:416], gk[:, 64:96], ALU.mult, [cs, gk], [k1])
                if tile < 2:
                    self.cp("dve", krr[:, tile, :], k1[:], [k1], [krk[tile]])
                else:
                    co, si = cosr.next(), sinr.next()
                    self.dma("sp", co[:], I["k_cos"][(tile - 2) * 128:(tile - 1) * 128, :], [], [co])
                    self.dma("sp", si[:], I["k_sin"][(tile - 2) * 128:(tile - 1) * 128, :], [], [si])
                    self.rope(k1[:].rearrange("p (h a g f) -> p h a g f", h=1, a=2, g=2),
                              krr[:, tile, :].rearrange("p (h a g f) -> p h a g f", h=1, a=2, g=2),
                              co[:].rearrange("p (a f) -> p a f", a=2), si[:].rearrange("p (a f) -> p a f", a=2),
                              1, rt, [k1, co, si], [krk[tile]])
            HG = 2
            NG = 16 // HG
            oat = self.sb(st, [128, NQT, D], BF16, "oat")
            oak = S.keys(NQT)
            QTs = [self.sb(st, [128, HG, SEQ], BF16, "QT") for _ in range(2)]
            QTks = [S.keys(NQT) for _ in range(2)]
            KTs = [self.sb(st, [128, HG, T], BF16, "KT") for _ in range(2)]
            KTks = [S.keys(NT) for _ in range(2)]
            Vxs = [self.sb(st, [128, NT, HG, 65], BF16, "Vx") for _ in range(2)]
            Vxks = [S.keys(NT) for _ in range(2)]
            for s_ in range(2):
                self.memset("dve", Vxs[s_][:], 1.0, Vxks[s_])
            wqfr = self.sbr(st, 2, [128, 2, HG * 96], F32, "wqf")
            wqbr = self.sbr(st, 2, [128, 2, HG * 96], BF16, "wqb")
            wkfr = self.sbr(st, 2, [128, HG * 128], F32, "wkf")
            wkbr = self.sbr(st, 2, [128, HG * 128], BF16, "wkb")
            kvfr = self.sbr(st, 2, [128, HG, 128], F32, "kvf")
            sqk = self.sb(st, [128, HG, 96], F32, "sqk")
            tmpk = self.sb(st, [128, HG, 96], F32, "tmpk")
            s4 = {nm: self.sbr(st, 2, [128, HG], F32, nm) for nm in ("ssn", "ss", "sd", "rs", "ssq4", "sd4", "rs4")}
            kbr = self.sbr(st, 2, [128, HG, 96], BF16, "kb")
            qfr = self.sbr(st, 2, [128, HG, 96], F32, "qf")
            qnr = self.sbr(st, 2, [128, HG, 96], F32, "qn")
            qbr = self.sbr(st, 2, [128, HG, 96], BF16, "qb")
            ptr_ = self.sbr(st, 3, [128, 512], BF16, "pt")
            recr = self.sbr(st, 4, [128, 1], F32, "rec")
            pA, pB_ = PB[2], PB[3]

            def rstd(out, ss, tmp, n):
                self.act(tmp[:], ss[:], AF.Ln, [ss, self.epsc], [tmp], bias=self.epsc[:, 0:1], scale=1.0 / n)
                self.act(out[:], tmp[:], AF.Exp, [tmp], [out], scale=-0.5)

            for hg in range(NG):
                s_ = hg % 2
                QT, QTk, KT, KTk, Vx, Vxk = QTs[s_], QTks[s_], KTs[s_], KTks[s_], Vxs[s_], Vxks[s_]
                wqf, wqb, wkf, wkb = wqfr.next(), wqbr.next(), wkfr.next(), wkbr.next()
                self.dma("sp", wqf[:], I["mla_w_qb"][:, hg * HG * 96:(hg + 1) * HG * 96].rearrange("(k p) n -> p k n", p=128), [], [wqf])
                self.tt("dve", wqb[:], wqf[:], qng[:].unsqueeze(2).broadcast_to([128, 2, HG * 96]), ALU.mult, [wqf, qng], [wqb])
                self.dma("sp", wkf[:], I["mla_w_kvb"][:, hg * HG * 128:(hg + 1) * HG * 128], [], [wkf])
                self.ts("dve", wkb[:], wkf[:], kvg[:, 0:1], None, ALU.mult, None, [wkf, kvg], [wkb])
                for tile in range(NT):
                    ts_ = slice(tile * 128, (tile + 1) * 128)
                    self.mm(pA[:, 0:HG * 128], cT[:, 2, ts_], wkb[:], True, True, [cTk[tile], wkb], [pA])
                    kvf = kvfr.next()
                    self.cp("dve", kvf[:], pA[:, 0:HG * 128].rearrange("p (h e) -> p h e", h=HG), [pA], [kvf])
                    t = {nm: r.next() for nm, r in s4.items()}
                    self.tt("dve", sqk[:, :, 0:64], kvf[:, :, 0:64], kvf[:, :, 0:64], ALU.mult, [kvf], [sqk])
                    self.red(t["ssn"][:], sqk[:, :, 0:64], ALU.add, [sqk], [t["ssn"]])
                    self.ts("dve", t["ss"][:], t["ssn"][:], sskr[:, tile:tile + 1], None, ALU.add, None, [t["ssn"], ssk[tile]], [t["ss"]])
                    rstd(t["rs"], t["ss"], t["sd"], 96)
                    kb = kbr.next()
                    self.tt("dve", tmpk[:, :, 0:64], kvf[:, :, 0:64], t["rs"][:].unsqueeze(2).broadcast_to([128, HG, 64]), ALU.mult,
                            [kvf, t["rs"]], [tmpk])
                    self.tt("dve", kb[:, :, 0:64], tmpk[:, :, 0:64], gk[:, 0:64].unsqueeze(1).broadcast_to([128, HG, 64]), ALU.mult,
                            [tmpk, gk], [kb])
                    self.tt("dve", kb[:, :, 64:96], krr[:, tile, :].unsqueeze(1).broadcast_to([128, HG, 32]),
                            t["rs"][:].unsqueeze(2).broadcast_to([128, HG, 32]), ALU.mult, [krk[tile], t["rs"]], [kb])
                    pkv = pB_[:, 0:HG * 64].bitcast(BF16).rearrange("p (h t) -> p h t", h=HG)
                    for h in range(HG):
                        self.tr(pkv[0:96, h, :], kb[:, h, :], self.identb[:], [kb, self.identb], [pB_])
                    self.cp("dve", KT[0:96, :, ts_], pkv[0:96, :, :], [pB_], [KTk[tile]])
                    self.cp("dve", Vx[:, tile, :, 0:64], kvf[:, :, 64:128], [kvf], [Vxk[tile]])
                    if tile >= 2:
                        qt_ = tile - 2
                        for k in range(2):
                            self.mm(pA[:, 0:HG * 96], cT[:, k, ts_], wqb[:, k, :], k == 0, k == 1, [cTk[tile], wqb], [pA])
                        qf = qfr.next()
                        self.cp("dve", qf[:], pA[:, 0:HG * 96].rearrange("p (h e) -> p h e", h=HG), [pA], [qf])
                        self.tt("dve", sqk[:], qf[:], qf[:], ALU.mult, [qf], [sqk])
                        self.red(t["ssq4"][:], sqk[:], ALU.add, [sqk], [t["ssq4"]])
                        rstd(t["rs4"], t["ssq4"], t["sd4"], 96)
                        qn = qnr.next()
                        self.tt("dve", qn[:], qf[:], t["rs4"][:].unsqueeze(2).broadcast_to([128, HG, 96]), ALU.mult, [qf, t["rs4"]], [qn])
                        self.tt("dve", qn[:], qn[:], gq[:].unsqueeze(1).broadcast_to([128, HG, 96]), ALU.mult, [qn, gq], [qn])
                        qb = qbr.next()
                        self.cp("dve", qb[:, :, 0:64], qn[:, :, 0:64], [qn], [qb])
                        co, si = cosr.next(), sinr.next()
                        self.dma("sp", co[:], I["k_cos"][qt_ * 128:(qt_ + 1) * 128, :], [], [co])
                        self.dma("sp", si[:], I["k_sin"][qt_ * 128:(qt_ + 1) * 128, :], [], [si])
                        self.rope(qn[:, :, 64:96].rearrange("p h (a g f) -> p h a g f", a=2, g=2),
                                  qb[:, :, 64:96].rearrange("p h (a g f) -> p h a g f", a=2, g=2),
                                  co[:].rearrange("p (a f) -> p a f", a=2), si[:].rearrange("p (a f) -> p a f", a=2),
                                  HG, rt, [qn, co, si], [qb])
                        pqv = pB_[:, 0:HG * 64].bitcast(BF16).rearrange("p (h t) -> p h t", h=HG)
                        for h in range(HG):
                            self.tr(pqv[0:96, h, :], qb[:, h, :], self.identb[:], [qb, self.identb], [pB_])
                        self.cp("dve", QT[0:96, :, qt_ * 128:(qt_ + 1) * 128], pqv[0:96, :, :], [pB_], [QTk[qt_]])
                for h in range(HG):
                    hh = hg * HG + h
                    for qb_ in range(SEQ // 512):
                        po = PB[4:8]
                        qk = [QTk[qb_ * 4 + i] for i in range(4)]
                        for kt in range(NT):
                            ps_ = PB[kt % 2]
                            self.mm(ps_[:, :], KT[0:96, h, kt * 128:(kt + 1) * 128], QT[0:96, h, qb_ * 512:(qb_ + 1) * 512], True, True,
                                    [KTk[kt]] + qk, [ps_])
                            pt = ptr_.next()
                            self.act(pt[:], ps_[:, :], AF.Exp, [ps_], [pt])
                            for q4 in range(4):
                                self.mm(po[q4][:, 0:65], pt[:, q4 * 128:(q4 + 1) * 128], Vx[:, kt, h, :], kt == 0, kt == NT - 1,
                                        [pt, Vxk[kt]], [po[q4]])
                        for q4 in range(4):
                            rec = recr.next()
                            self.recip(rec[:], po[q4][:, 64:65], [po[q4]], [rec])
                            self.ts("dve", oat[:, qb_ * 4 + q4, hh * 64:(hh + 1) * 64], po[q4][:, 0:64], rec[:, 0:1], None, ALU.mult, None,
                                    [po[q4], rec], [oak[qb_ * 4 + q4]])
            wo = self.sb(st, [128, KC, D], BF16, "wo")
            self.dma("pool", wo[:], I["mla_w_out"].rearrange("(c p) n -> p c n", p=128), [], [wo])
            oTr = self.sbr(st, 2, [128, KC, 128], BF16, "oT")
            for qt_ in range(NQT):
                tile = qt_ + 2
                pT = PB[qt_ % 2]
                pv = pT[:, :].bitcast(BF16).rearrange("p (k t) -> p k t", k=KC)
                for k in range(KC):
                    self.tr(pv[:, k, :], oat[:, qt_, k * 128:(k + 1) * 128], self.identb[:], [oak[qt_], self.identb], [pT])
                oT = oTr.next()
                self.cp("act", oT[:], pv, [pT], [oT])
                x_ = NR["xt"].next()
                self.dma("sp", x_[:], self.xres[b, tile * 128:(tile + 1) * 128, :], [self.kx[b][tile]], [x_])
                t_ = NR["xn"].next()
                for hf in range(2):
                    p = PB[2 + hf]
                    hs = slice(hf * 512, (hf + 1) * 512)
                    for k in range(KC):
                        self.mm(p[:, :], oT[:, k, :], wo[:, k, hs], k == 0, k == KC - 1, [oT, wo], [p])
                    self.tt("dve", t_[:, hs], p[:, :], gt_l[:, hs], ALU.mult, [p, gt_l], [t_])
                self.tt("dve", t_[:], t_[:], x_[:], ALU.add, [t_, x_], [t_])
                self.dma("sp", self.xres[b, tile * 128:(tile + 1) * 128, :], t_[:], [t_], [self.kx[b][tile]], semkey=t_)
                if ("xm1" in self.D_) and b == 0:
                    self.dma("sp", self.D_["xm1"][qt_ * 128:(qt_ + 1) * 128, :], t_[:], [t_], [self.kscr], semkey=t_)
            return S.flush()

    def moe_sparse(self, l):
        I, S, NB = self.I, self.S, self.NB
        tiles = [(b, t) for b in range(NB) for t in (range(NT) if l == 0 else range(2, NT))]
        NTL = len(tiles)
        NBLK = 2 * NTL + NEXP
        info = {}
        with ExitStack() as pst:
            d_i = [self.sb(pst, [128, NTL], I32, "d%di" % k) for k in range(2)]
            w_a = [self.sb(pst, [128, NTL], F32, "w%da" % k) for k in range(2)]
            idxw = self.sb(pst, [128, NBLK], I32, "idxw")
            kh2 = S.keys(NTL)
            dik = [S.keys(NTL) for _ in range(2)]
            kxs, kys = S.key(), S.key()
            kwb = self.kwb[l]
            with ExitStack() as st:
                PB = [self.psb(st) for _ in range(8)]
                Ltri = self.sb(st, [128, 128], BF16, "Ltri")
                onesf = self.sb(st, [128, 128], BF16, "onesf")
                self.memset("dve", onesf[:], 1.0, [onesf])
                self.memset("pool", Ltri[:], 1.0, [Ltri])
                S.op("pool", lambda e_: e_.affine_select(out=Ltri[:], in_=Ltri[:], pattern=[[1, 128]], compare_op=ALU.is_gt,
                                                         fill=0.0, base=0, channel_multiplier=-1), [Ltri], [Ltri])
                jvi = self.sb(st, [128, NBLK], I32, "jvi")
                jv = self.sb(st, [128, NBLK], F32, "jv")
                S.op("pool", lambda e_: e_.iota(jvi[:], pattern=[[128, NBLK]], base=0, channel_multiplier=0), [], [jvi])
                self.cp("dve", jv[:], jvi[:], [jvi], [jv])
                pii = self.sb(st, [128, 1], I32, "pii")
                pif = self.sb(st, [128, 1], F32, "pif")
                S.op("pool", lambda e_: e_.iota(pii[:], pattern=[[0, 1]], base=0, channel_multiplier=1), [], [pii])
                self.cp("dve", pif[:], pii[:], [pii], [pif])
                ones32 = self.sb(st, [128, NEXP], F32, "ones32")
                self.memset("dve", ones32[:], 1.0, [ones32])
                wr = self.sb(st, [128, KC, 36], F32, "wr")
                self.dma("sp", wr[:, :, 0:4], I["moe_w_group"][l].rearrange("(c p) g -> p c g", p=128), [], [wr])
                self.dma("sp", wr[:, :, 4:36], I["moe_w_expert"][l].rearrange("(c p) g -> p c g", p=128), [], [wr])
                grow = self.sb(st, [128, D], F32, "grow")
                self.dma("sp", grow[:], I["norm_ffn_g"][l, :].partition_broadcast(128), [], [grow])

                def rows_for(r):
                    Ar = self.sb(st, [128, D], F32, "Arow")
                    Br = self.sb(st, [128, D], F32, "Brow")
                    return Ar, Br

                def load_rows(Ar, Br, r):
                    self.dma("sp", Ar[:], self.mod[l, r, 4 * D:5 * D].partition_broadcast(128), [self.kmod], [Ar])
                    self.dma("sp", Br[:], self.mod[l, r, 3 * D:4 * D].partition_broadcast(128), [self.kmod], [Br])
                    self.stt(Ar[:], Ar[:], 1.0, grow[:], ALU.add, ALU.mult, [Ar, grow], [Ar])

                Ar_l, Br_l = rows_for(0)
                if l == 0:
                    Ar_c, Br_c = rows_for(4)
                    load_rows(Ar_c, Br_c, 4)
                    A_c, B_c = self.mod_cols(st, l, 1, 4)
                NR = self.norm_res(st, (PB[0], PB[1]), nhtf=3, nxn=3, nxt=3)
                sm = {nm: self.sbr(st, 3, [128, w], F32, nm) for nm, w in self.ROUTE_TMPS}
                lTr = self.sbr(st, 2, [36, 128], F32, "lT")
                OHs = self.sb(st, [128, NEXP], BF16, "OHs")
                self.memset("dve", OHs[:], 0.0, [OHs])
                Rall = self.sb(st, [128, NTL, NEXP], F32, "Rall")
                Rk = S.keys(NTL)
                oha = [self.sb(st, [128, NTL, NEXP], F32, "oh%da" % k) for k in range(2)]
                ohk = [S.keys(NTL) for _ in range(2)]
                t32r = self.sbr(st, 2, [128, D], F32, "t32")
                h2r = self.sbr(st, 3, [128, D], BF16, "h2b")
                G = 4
                gt_ = {}
                for nm, w in (("lg", 36), ("t4", 4), ("ge", 4), ("gone", 4), ("pen", 4), ("em", 32), ("oh1", 32), ("em2", 32), ("oh2", 32)):
                    gt_[nm] = self.sbr(st, 2, [128, G, w], F32, "g" + nm)
                for nm in ("gmax", "gsum", "pg", "m1", "m2", "dm", "e2", "den", "rden", "w1", "w2"):
                    gt_[nm] = self.sbr(st, 2, [128, G], F32, "g" + nm)
                OHg = self.sbr(st, 2, [128, G, NEXP], BF16, "OHg")
                ohsum = self.sbr(st, 2, [128, NEXP], F32, "ohsum")
                cur_b = None
                cols = {}
                groups = [list(range(i, min(i + G, NTL))) for i in range(0, NTL, G)]
                for gi, grp in enumerate(groups):
                    g = len(grp)
                    ti0 = grp[0]
                    p = PB[2 + gi % 2]
                    for i, ti in enumerate(grp):
                        b, tile = tiles[ti]
                        if b != cur_b:
                            cur_b = b
                            load_rows(Ar_l, Br_l, b)
                            cols[b] = self.mod_cols(st, l, 1, b)
                        isctx = (l == 0 and tile < 2)
                        A, Bc = (A_c, B_c) if isctx else cols[b]
                        Ar, Br = (Ar_c, Br_c) if isctx else (Ar_l, Br_l)
                        hTf = self.norm_tile(NR, self.xres[b, tile * 128:(tile + 1) * 128, :], [self.kx[b][tile]], A, Bc, None, [])
                        xn = self.last_xn
                        t32 = t32r.next()
                        self.tt("dve", t32[:], xn[:], Ar[:], ALU.mult, [xn, Ar], [t32])
                        h2b = h2r.next()
                        self.tt("dve", h2b[:], t32[:], Br[:], ALU.add, [t32, Br], [h2b])
                        self.dma("sp", self.h2d[ti * 128:(ti + 1) * 128, :], h2b[:], [h2b], [kh2[ti]], semkey=h2b)
                        pl = PB[6 + ti % 2]
                        for k in range(KC):
                            self.mm(pl[0:36, 0:128], wr[:, k, :], hTf[:, k, :], k == 0, k == KC - 1, [hTf, wr], [pl])
                        lT = lTr.next()
                        self.cp("act", lT[:], pl[0:36, 0:128], [pl], [lT])
                        self.tr(p[:, i * 36:(i + 1) * 36], lT[:], self.identf[0:36, 0:36], [lT, self.identf], [p])
                    t = {nm: r.next() for nm, r in gt_.items()}
                    v3 = lambda x, w: x[:, 0:g, 0:w]
                    lg = t["lg"]
                    self.cp("act", lg[:, 0:g, :], p[:, 0:g * 36].rearrange("p (g e) -> p g e", g=g), [p], [lg])
                    lgg, lge = lg[:, 0:g, 0:4], lg[:, 0:g, 4:36]
                    bc = lambda x, w: x[:, 0:g].unsqueeze(2).broadcast_to([128, g, w])
                    self.red(t["gmax"][:, 0:g], lgg, ALU.max, [lg], [t["gmax"]])
                    self.tt("dve", v3(t["t4"], 4), lgg, bc(t["gmax"], 4), ALU.subtract, [lg, t["gmax"]], [t["t4"]])
                    self.act(v3(t["ge"], 4), v3(t["t4"], 4), AF.Exp, [t["t4"]], [t["ge"]])
                    self.red(t["gsum"][:, 0:g], v3(t["ge"], 4), ALU.add, [t["ge"]], [t["gsum"]])
                    self.recip(t["pg"][:, 0:g], t["gsum"][:, 0:g], [t["gsum"]], [t["pg"]])
                    self.tt("dve", v3(t["gone"], 4), lgg, bc(t["gmax"], 4), ALU.is_ge, [lg, t["gmax"]], [t["gone"]])
                    self.ts("dve", v3(t["pen"], 4), v3(t["gone"], 4), BIG, -BIG, ALU.mult, ALU.add, [t["gone"]], [t["pen"]])
                    self.tt("dve", v3(t["em"], 32).rearrange("p g (a j) -> p g a j", a=4), lge.rearrange("p g (a j) -> p g a j", a=4),
                            v3(t["pen"], 4).unsqueeze(3).broadcast_to([128, g, 4, 8]), ALU.add, [lg, t["pen"]], [t["em"]])
                    self.red(t["m1"][:, 0:g], v3(t["em"], 32), ALU.max, [t["em"]], [t["m1"]])
                    self.tt("dve", v3(t["oh1"], 32), v3(t["em"], 32), bc(t["m1"], 32), ALU.is_ge, [t["em"], t["m1"]], [t["oh1"]])
                    self.stt(t["em2"][:, 0:g, :].rearrange("p g e -> p (g e)"), t["oh1"][:, 0:g, :].rearrange("p g e -> p (g e)"), -BIG,
                             t["em"][:, 0:g, :].rearrange("p g e -> p (g e)"), ALU.mult, ALU.add, [t["oh1"], t["em"]], [t["em2"]])
                    self.red(t["m2"][:, 0:g], v3(t["em2"], 32), ALU.max, [t["em2"]], [t["m2"]])
                    self.tt("dve", v3(t["oh2"], 32), v3(t["em2"], 32), bc(t["m2"], 32), ALU.is_ge, [t["em2"], t["m2"]], [t["oh2"]])
                    self.tt("dve", t["dm"][:, 0:g], t["m2"][:, 0:g], t["m1"][:, 0:g], ALU.subtract, [t["m2"], t["m1"]], [t["dm"]])
                    self.act(t["e2"][:, 0:g], t["dm"][:, 0:g], AF.Exp, [t["dm"]], [t["e2"]])
                    self.ts("dve", t["den"][:, 0:g], t["e2"][:, 0:g], 1.0, None, ALU.add, None, [t["e2"]], [t["den"]])
                    self.recip(t["rden"][:, 0:g], t["den"][:, 0:g], [t["den"]], [t["rden"]])
                    self.tt("dve", t["w1"][:, 0:g], t["pg"][:, 0:g], t["rden"][:, 0:g], ALU.mult, [t["pg"], t["rden"]], [t["w1"]])
                    self.tt("dve", t["w2"][:, 0:g], t["w1"][:, 0:g], t["e2"][:, 0:g], ALU.mult, [t["w1"], t["e2"]], [t["w2"]])
                    gk = lambda ks: [ks[ti] for ti in grp]
                    self.cp("dve", oha[0][:, ti0:ti0 + g, :], v3(t["oh1"], 32), [t["oh1"]], gk(ohk[0]))
                    self.cp("dve", oha[1][:, ti0:ti0 + g, :], v3(t["oh2"], 32), [t["oh2"]], gk(ohk[1]))
                    self.cp("dve", w_a[0][:, ti0:ti0 + g], t["w1"][:, 0:g], [t["w1"]], [w_a[0]])
                    self.cp("dve", w_a[1][:, ti0:ti0 + g], t["w2"][:, 0:g], [t["w2"]], [w_a[1]])
                    oh = OHg.next()
                    self.tt("dve", oh[:, 0:g, :], v3(t["oh1"], 32), v3(t["oh2"], 32), ALU.add, [t["oh1"], t["oh2"]], [oh])
                    pr = PB[4 + gi % 2]
                    for i in range(g):
                        cs_ = slice(i * NEXP, (i + 1) * NEXP)
                        self.mm(pr[:, cs_], Ltri[:], oh[:, i, :], True, False, [Ltri, oh], [pr])
                        for i2 in range(i):
                            self.mm(pr[:, cs_], onesf[:], oh[:, i2, :], False, False, [onesf, oh], [pr])
                        self.mm(pr[:, cs_], onesf[:], OHs[:], False, True, [onesf, OHs], [pr])
                    self.cp("act", Rall[:, ti0:ti0 + g, :], pr[:, 0:g * NEXP].rearrange("p (g e) -> p g e", g=g), [pr], gk(Rk))
                    osum = ohsum.next()
                    self.red(osum[:], oh[:, 0:g, :].rearrange("p g e -> p e g"), ALU.add, [oh], [osum])
                    self.tt("dve", OHs[:], OHs[:], osum[:], ALU.add, [OHs, osum], [OHs])
                pc = PB[5]
                self.mm(pc[:, 0:NEXP], onesf[:], OHs[:], True, True, [onesf, OHs], [pc])
                cntf = self.sb(st, [128, NEXP], F32, "cntf")
                padf = self.sb(st, [128, NEXP], F32, "padf")
                pend = self.sb(st, [128, NEXP], F32, "pend")
                pstart = self.sb(st, [128, NEXP], F32, "pstart")
                cmpb = self.sb(st, [128, NBLK * NEXP], BF16, "cmpb")
                self.cp("dve", cntf[:], pc[:, 0:NEXP], [pc], [cntf])
                cv = cmpb[:].rearrange("p (e j) -> p e j", e=NEXP)
                self.tt("dve", cv, jv[:].unsqueeze(1).broadcast_to([128, NEXP, NBLK]),
                        cntf[:].unsqueeze(2).broadcast_to([128, NEXP, NBLK]), ALU.is_lt, [jv, cntf], [cmpb])
                self.red(padf[:], cv, ALU.add, [cmpb], [padf])
                self.ts("dve", padf[:], padf[:], 128.0, None, ALU.mult, None, [padf], [padf])
                S.op("dve", lambda e_: e_.tensor_tensor_scan(out=pend[:], data0=ones32[:], data1=padf[:], initial=0.0,
                                                             op0=ALU.mult, op1=ALU.add), [ones32, padf], [pend])
                self.tt("dve", pstart[:], pend[:], padf[:], ALU.subtract, [pend, padf], [pstart])
                bef = self.sb(st, [128, NBLK], F32, "bef")
                cv2 = cmpb[:].rearrange("p (j e) -> p j e", e=NEXP)
                self.tt("dve", cv2, pend[:].unsqueeze(1).broadcast_to([128, NBLK, NEXP]),
                        jv[:].unsqueeze(2).broadcast_to([128, NBLK, NEXP]), ALU.is_le, [pend, jv], [cmpb])
                self.red(bef[:], cv2, ALU.add, [cmpb], [bef])
                self.ts("dve", bef[:], bef[:], float(NEXP - 1), None, ALU.min, None, [bef], [bef])
                same2 = self.sb(st, [128, NBLK], F32, "same2")
                self.memset("dve", same2[:], 0.0, [same2])
                self.tt("dve", same2[:, 2:NBLK], bef[:, 2:NBLK], bef[:, 0:NBLK - 2], ALU.is_equal, [bef], [same2])
                self.ts("dve", bef[:], bef[:], 128.0, pif[:, 0:1], ALU.mult, ALU.add, [bef, pif], [bef])
                self.stt(bef[:], same2[:], 1.0e6, bef[:], ALU.mult, ALU.add, [same2, bef], [bef])
                self.cp("dve", idxw[:], bef[:], [bef], [idxw])
                sck = S.keys(4)
                dtmp = self.sbr(st, 3, [128, NEXP], F32, "dtmp")
                dtm2 = self.sbr(st, 4, [128, NEXP], F32, "dtm2")
                dfl = self.sbr(st, 4, [128, 1], F32, "dfl")
                for ti in range(NTL):
                    h2b = h2r.next()
                    self.dma("sp", h2b[:], self.h2d[ti * 128:(ti + 1) * 128, :], [kh2[ti]], [h2b])
                    d1 = dtmp.next()
                    self.tt("dve", d1[:], Rall[:, ti, :], pstart[:], ALU.add, [Rk[ti], pstart], [d1])
                    for k in range(2):
                        d2, df = dtm2.next(), dfl.next()
                        self.tt("dve", d2[:], d1[:], oha[k][:, ti, :], ALU.mult, [d1, ohk[k][ti]], [d2])
                        self.red(df[:], d2[:], ALU.add, [d2], [df])
                        self.cp("dve", d_i[k][:, ti:ti + 1], df[:], [df], [dik[k][ti]])
                        idx_ap = d_i[k][:, ti:ti + 1]
                        self._scatter(self.xs[:, :], idx_ap, h2b[:], [h2b, dik[k][ti]], [sck[(2 * ti + k) % 4]])
                info["rs"] = S.flush()
            with ExitStack() as st:
                PB = [self.psb(st) for _ in range(8)]
                xbr = self.sbr(st, 2, [128, D], BF16, "xb")
                xTr = self.sbr(st, 2, [128, KC, 128], BF16, "xT")
                wgr = self.sbr(st, 2, [128, 4096], BF16, "wgs")
                wur = self.sbr(st, 2, [128, 4096], BF16, "wus")
                wdr = self.sbr(st, 2, [128, 4096], BF16, "wds")
                sgr = self.sbr(st, 2, [128, 512], BF16, "sgs")
                acr = self.sbr(st, 2, [128, 4, 128], BF16, "acs")
                ysr = self.sbr(st, 2, [128, D], F32, "ysb")
                pgu = Rot([(PB[2], PB[3]), (PB[4], PB[5])])
                breg = {"v": NEXP * 128 - 1}
                for j in range(NBLK):
                    xb = xbr.next()
                    self.dma("sp", xb[:], self.xs[j * 128:(j + 1) * 128, :], [kxs], [xb])
                    wg, wu, wd = wgr.next(), wur.next(), wdr.next()
                    ia = idxw[:, j:j + 1]
                    self._gather(wg[:], self.wgb[l][:, :], ia, [idxw, kwb], [wg], bounds=breg)
                    self._gather(wu[:], self.wub[l][:, :], ia, [idxw, kwb], [wu], bounds=breg)
                    self._gather(wd[:], self.wdb[l][:, :], ia, [idxw, kwb], [wd], bounds=breg)
                    pT = PB[j % 2]
                    pv = pT[:, :].bitcast(BF16).rearrange("p (k t) -> p k t", k=KC)
                    for k in range(KC):
                        self.tr(pv[:, k, :], xb[:, k * 128:(k + 1) * 128], self.identb[:], [xb, self.identb], [pT])
                    xT = xTr.next()
                    self.cp("act", xT[:], pv, [pT], [xT])
                    pg_, pu_ = pgu.next()
                    wgv = wg[:].rearrange("p (k f) -> p k f", k=KC)
                    wuv = wu[:].rearrange("p (k f) -> p k f", k=KC)
                    wdv = wd[:].rearrange("p (k f) -> p k f", k=4)
                    for fc in range(4):
                        fs = slice(fc * 128, (fc + 1) * 128)
                        for k in range(KC):
                            self.mm(pg_[:, fs], wgv[:, k, fs], xT[:, k, :], k == 0, k == KC - 1, [wg, xT], [pg_])
                    for fc in range(4):
                        fs = slice(fc * 128, (fc + 1) * 128)
                        for k in range(KC):
                            self.mm(pu_[:, fs], wuv[:, k, fs], xT[:, k, :], k == 0, k == KC - 1, [wu, xT], [pu_])
                    sg = sgr.next()
                    self.act(sg[:], pg_[:, :], AF.Silu, [pg_], [sg])
                    ac = acr.next()
                    self.tt("dve", ac[:].rearrange("p k t -> p (k t)"), sg[:], pu_[:, :], ALU.mult, [sg, pu_], [ac])
                    ysb = ysr.next()
                    for hf, p in ((0, PB[6]), (1, PB[7])):
                        hs = slice(hf * 512, (hf + 1) * 512)
                        for k in range(4):
                            self.mm(p[:, :], ac[:, k, :], wdv[:, k, hs], k == 0, k == 3, [ac, wd], [p])
                        if hf == 0:
                            self.cp("act", ysb[:, hs], p[:, :], [p], [ysb])
                        else:
                            self.cp("dve", ysb[:, hs], p[:, :], [p], [ysb])
                    self.dma("sp", self.ys[j * 128:(j + 1) * 128, :], ysb[:], [ysb], [], semkey=ysb)
                info["e"] = S.flush()
            with ExitStack() as st:
                gts = {}
                y1r = self.sbr(st, 3, [128, D], F32, "y1")
                y2r = self.sbr(st, 3, [128, D], F32, "y2")
                xr = self.sbr(st, 3, [128, D], F32, "xc")
                tr_ = self.sbr(st, 3, [128, D], F32, "tc")
                if l == 0:
                    gts[4] = self.gate_tile(st, l, 1, 4)
                for ti, (b, tile) in enumerate(tiles):
                    if b not in gts:
                        gts[b] = self.gate_tile(st, l, 1, b)
                    isctx = (l == 0 and tile < 2)
                    gt = gts[4] if isctx else gts[b]
                    y1, y2, x_, t_ = y1r.next(), y2r.next(), xr.next(), tr_.next()
                    self._gather(y1[:], self.ys[:, :], d_i[0][:, ti:ti + 1], [dik[0][ti], kys], [y1])
                    self._gather(y2[:], self.ys[:, :], d_i[1][:, ti:ti + 1], [dik[1][ti], kys], [y2])
                    self.dma("sp", x_[:], self.xres[b, tile * 128:(tile + 1) * 128, :], [self.kx[b][tile]], [x_])
                    self.act(t_[:], y1[:], AF.Copy, [y1, w_a[0]], [t_], scale=w_a[0][:, ti:ti + 1])
                    self.stt(t_[:], y2[:], w_a[1][:, ti:ti + 1], t_[:], ALU.mult, ALU.add, [y2, w_a[1], t_], [t_])
                    self.tt("dve", t_[:], t_[:], gt[:], ALU.mult, [t_, gt], [t_])
                    self.tt("dve", t_[:], t_[:], x_[:], ALU.add, [t_, x_], [t_])
                    if l == 0:
                        self.dma("sp", self.xres[b, tile * 128:(tile + 1) * 128, :], t_[:], [t_], [self.kx[b][tile]], semkey=t_)
                        if ("xf0" in self.D_) and b == 0:
                            self.dma("sp", self.D_["xf0"][tile * 128:(tile + 1) * 128, :], t_[:], [t_], [self.kscr], semkey=t_)
                    else:
                        self.dma("sp", self.out[b, (tile - 2) * 128:(tile - 1) * 128, :], t_[:], [t_], [], semkey=t_)
                info["c"] = S.flush()
        return info

    def precast(self, l, b):
        I = self.I
        per = (NEXP + self.NB - 1) // self.NB
        if not hasattr(self, "pck"):
            self.pck = Rot(self.S.keys(4))
        for e in range(b * per, min(NEXP, (b + 1) * per)):
            rows = slice(e * 128, (e + 1) * 128)
            for dst, src, kk in ((self.wgb[l], "moe_w_gate", 8), (self.wub[l], "moe_w_up", 8), (self.wdb[l], "moe_w_down", 4)):
                self.dma("pool", dst[rows, :].rearrange("p (k f) -> p k f", k=kk),
                         I[src][l, e].rearrange("(k p) f -> p k f", p=128), [], [], semkey=self.pck.next())

    def _gather(self, out, src, idx_ap, R, W, bounds=None):
        def fn(e):
            if bounds is None:
                return e.indirect_dma_start(out=out, out_offset=None, in_=src,
                                            in_offset=bass.IndirectOffsetOnAxis(ap=idx_ap, axis=0))
            if "r" not in bounds:
                bounds["r"] = e.to_reg(bounds["v"])
            return e.indirect_dma_start(out=out, out_offset=None, in_=src,
                                        in_offset=bass.IndirectOffsetOnAxis(ap=idx_ap, axis=0),
                                        bounds_check=bounds["r"], oob_is_err=False)
        self.S.dma("pool", fn, R, W, nbytes=128 * self._n(out) * 2, indirect=True)

    def _scatter(self, dst, idx_ap, in_, R, W):
        nrow = dst.shape[0]
        self.S.dma("pool", lambda e: e.indirect_dma_start(out=dst, out_offset=bass.IndirectOffsetOnAxis(ap=idx_ap, axis=0),
                                                          in_=in_, in_offset=None),
                   R, W, semkey=R[0], nbytes=128 * 2048, indirect=True)

    def build(self):
        with ExitStack() as gst:
            gst.enter_context(self.nc.allow_non_contiguous_dma(reason="small strided parameter loads"))
            self.setup_consts(gst)
            info = {}
            if "mod" in self.stages:
                info["mod"] = self.stage_mod()
            for b in range(self.NB):
                if "hgrn" in self.stages:
                    info["hgrn%d" % b] = self.stage_hgrn(b)
            if "moe0" in self.stages:
                info["moe0"] = self.moe_sparse(0)
            for b in range(self.NB):
                if "mla" in self.stages:
                    info["mla%d" % b] = self.stage_mla(b)
            if "moe1" in self.stages:
                info["moe1"] = self.moe_sparse(1)
            self.info = info
        self.S.close()
        return self.nc


def host_consts():
    s = np.arange(128)
    same = (s[:, None] // 32) == (s[None, :] // 32)
    maskf = (same & (s[:, None] <= s[None, :])).astype(np.float32)
    maskb = (same & (s[:, None] >= s[None, :])).astype(np.float32)
    bm = ((s[:, None] // 32) == np.arange(4)[None, :]).astype(np.float32)[:, :, None].repeat(128, axis=2)
    t = np.arange(SEQ)
    row, col = t // 64, t % 64
    inv = (10000.0 ** (-np.arange(0, 16, 2, dtype=np.float32) / 16)).astype(np.float32)
    ang = np.stack([row, col], axis=-1).astype(np.float32)[..., None] * inv
    cos = np.cos(ang).astype(np.float32).reshape(SEQ, 16)
    sin = np.sin(ang).astype(np.float32).reshape(SEQ, 16)
    return {"k_maskf": maskf, "k_maskb": maskb, "k_bm": np.ascontiguousarray(bm), "k_bmc": np.ascontiguousarray(bm[:, :, 0]),
            "k_cos": cos, "k_sin": sin}


def make_in_maps(inputs, NB, ncores, used=None):
    sq = {"hg_w_in": "hg_w_in", "hg_lower_bounds": "hg_lb", "hg_out_norm_g": "hg_out_norm_g", "hg_w_out": "hg_w_out",
          "mla_w_in": "mla_w_in", "mla_q_norm_g": "mla_q_norm_g", "mla_kv_norm_g": "mla_kv_norm_g", "mla_w_qb": "mla_w_qb",
          "mla_w_kvb": "mla_w_kvb", "mla_q_qknorm_g": "mla_q_qknorm_g", "mla_k_qknorm_g": "mla_k_qknorm_g", "mla_w_out": "mla_w_out"}
    shared = {}
    for k, v in inputs.items():
        v = np.asarray(v, dtype=np.float32)
        if k in ("x", "c", "ctx"):
            continue
        if k == "hg_lower_bounds":
            shared["hg_lb"] = np.ascontiguousarray(v)
        elif k in sq:
            shared[sq[k]] = np.ascontiguousarray(v.reshape(v.shape[1:]))
        else:
            shared[k] = np.ascontiguousarray(v)
    shared.update(host_consts())
    maps = []
    for i in range(ncores):
        m = dict(shared)
        for k in ("x", "c", "ctx"):
            m[k] = np.ascontiguousarray(np.asarray(inputs[k], dtype=np.float32)[i * NB:(i + 1) * NB])
        if used is not None:
            m = {k: v for k, v in m.items() if k in used}
        maps.append(m)
    return maps


def kernel(**inputs):
    NB = 4
    kb = KB(NB=NB)
    nc = kb.build()
    maps = make_in_maps(inputs, NB, 8, used=set(kb.I.keys()))
    res = run_bass_kernel_spmd(nc, maps, core_ids=list(range(8)))
    return np.concatenate([r["out"] for r in res.results], axis=0).astype(np.float32)
```
